# Optimizing a Trainium2 kernel written in Bass

```python
import jax
import jax.numpy as jnp
from jax import lax
import numpy as np

D_MODEL = 1024
BATCH = 32
SEQ = 2048
DEPTH = 1

HEAD_DIM = 64
NSA_HEADS = 8
NSA_GROUPS = 2
NSA_CMP_LEN = 32
NSA_CMP_STRIDE = 16
NSA_CMP_HIDDEN = 128
NSA_SLC_LEN = 64
NSA_SLC_TOPN = 16
NSA_WINDOW = 512
DSA_HEADS = 8
IDX_HEADS = 8
IDX_DIM = 64
DSA_TOPK_MAX = 256
D_FF = ((8 * D_MODEL + 3 * 256 - 1) // (3 * 256)) * 256
N_ADA = 6
Q_BLOCK = 128
SLC_Q_BLOCK = 16
RMS_EPS = 1e-6
FORCE_SCORE = 1e9

IN_SIZES = (
    NSA_HEADS * HEAD_DIM,
    NSA_GROUPS * HEAD_DIM,
    NSA_GROUPS * HEAD_DIM,
    NSA_GROUPS * HEAD_DIM,
    NSA_GROUPS * HEAD_DIM,
    NSA_GROUPS * HEAD_DIM,
    NSA_GROUPS * HEAD_DIM,
    NSA_HEADS * 3,
    DSA_HEADS * HEAD_DIM,
    HEAD_DIM,
    HEAD_DIM,
    IDX_HEADS * IDX_DIM,
    IDX_DIM,
    IDX_HEADS,
    D_MODEL,
    D_MODEL,
)
D_IN = sum(IN_SIZES)

kernel_name = 'hybrid_nsa_dsa_gated_block'


def rms_norm(x, g):
    xf = x.astype(jnp.float32)
    y = xf * lax.rsqrt(jnp.mean(xf * xf, axis=-1, keepdims=True) + RMS_EPS)
    return (y * g.astype(jnp.float32)).astype(x.dtype)


def masked_softmax(s, mask):
    s = jnp.where(mask, s.astype(jnp.float32), -jnp.inf)
    m = jnp.max(s, axis=-1, keepdims=True)
    m = jnp.where(jnp.isfinite(m), m, 0.0)
    e = jnp.where(mask, jnp.exp(s - m), 0.0)
    return e / jnp.maximum(jnp.sum(e, axis=-1, keepdims=True), 1e-30)


def alibi_slopes(n_heads):
    return jnp.exp2(-8.0 * jnp.arange(1, n_heads + 1, dtype=jnp.float32) / n_heads)


def block_map(fn, n_rows, block):
    out = lax.map(fn, jnp.arange(n_rows // block))
    out = jnp.moveaxis(out, 0, 1)
    return out.reshape((out.shape[0], n_rows) + out.shape[3:])


def nsa_compress(t, pe, w1, w2):
    b, s, g, hd = t.shape
    n_cmp = (s - NSA_CMP_LEN) // NSA_CMP_STRIDE + 1
    idx = (jnp.arange(n_cmp) * NSA_CMP_STRIDE)[:, None] + jnp.arange(NSA_CMP_LEN)[None, :]
    blk = t[:, idx] + pe[:, None, :].astype(t.dtype)
    blk = jnp.moveaxis(blk, 3, 2).reshape(b, n_cmp, g, NSA_CMP_LEN * hd)
    return jax.nn.silu(blk @ w1) @ w2


def nsa_mixer(q, k_cmp, v_cmp, k_slc, v_slc, k_win, v_win, gates,
              g_q, g_kc, g_ks, g_kw, pe_ck, pe_cv, w_ck1, w_ck2, w_cv1, w_cv2):
    b, s, h, hd = q.shape
    g = k_cmp.shape[2]
    r = h // g
    f32 = jnp.float32
    slopes = alibi_slopes(h).reshape(g, r, 1, 1)
    qg = (rms_norm(q, g_q) * hd ** -0.5).reshape(b, s, g, r, hd)
    pos = jnp.arange(s)

    kc = rms_norm(nsa_compress(k_cmp, pe_ck, w_ck1, w_ck2), g_kc)
    vc = nsa_compress(v_cmp, pe_cv, w_cv1, w_cv2)
    n_cmp = kc.shape[1]
    cmp_start = jnp.arange(n_cmp) * NSA_CMP_STRIDE
    dist_c = (pos[:, None] - (cmp_start + NSA_CMP_LEN - 1)[None, :]).astype(f32)
    s_c = jnp.einsum('bsgrd,bngd->bgrsn', qg, kc).astype(f32) - slopes * dist_c
    p_c = masked_softmax(s_c, dist_c >= 0)
    o_cmp = jnp.einsum('bgrsn,bngd->bsgrd', p_c.astype(vc.dtype), vc)

    n_slc = s // NSA_SLC_LEN
    j = jnp.arange(n_slc)
    overlap = ((cmp_start[:, None] < (j[None, :] + 1) * NSA_SLC_LEN) &
               (cmp_start[:, None] + NSA_CMP_LEN > j[None, :] * NSA_SLC_LEN)).astype(f32)
    imp = jnp.einsum('bgrsn,nj->bgsj', p_c, overlap)
    cur = (pos // NSA_SLC_LEN)[:, None]
    forced = (j[None, :] == 0) | (j[None, :] == cur) | (j[None, :] == cur - 1)
    visible = j[None, :] * NSA_SLC_LEN <= pos[:, None]
    imp = jnp.where(forced, FORCE_SCORE, jnp.where(visible, imp, -FORCE_SCORE))
    n_top = min(NSA_SLC_TOPN, n_slc)
    _, sel = lax.top_k(imp, n_top)

    ks_blk = rms_norm(k_slc, g_ks).reshape(b, n_slc, NSA_SLC_LEN, g, hd).transpose(0, 3, 1, 2, 4)
    vs_blk = v_slc.reshape(b, n_slc, NSA_SLC_LEN, g, hd).transpose(0, 3, 1, 2, 4)
    take_blocks = jax.vmap(jax.vmap(lambda blocks, ix: blocks[ix]))
    n_keys = n_top * NSA_SLC_LEN

    def slc_block(i):
        q0 = i * SLC_Q_BLOCK
        qc = lax.dynamic_slice_in_dim(qg, q0, SLC_Q_BLOCK, axis=1)
        ic = lax.dynamic_slice_in_dim(sel, q0, SLC_Q_BLOCK, axis=2)
        kg = take_blocks(ks_blk, ic).reshape(b, g, SLC_Q_BLOCK, n_keys, hd)
        vg = take_blocks(vs_blk, ic).reshape(b, g, SLC_Q_BLOCK, n_keys, hd)
        kpos = (ic[..., None] * NSA_SLC_LEN + jnp.arange(NSA_SLC_LEN)).reshape(b, g, SLC_Q_BLOCK, n_keys)
        tq = q0 + jnp.arange(SLC_Q_BLOCK)
        dist = (tq[None, None, :, None] - kpos).astype(f32)[:, :, None]
        sc = jnp.einsum('bqgrd,bgqkd->bgrqk', qc, kg).astype(f32) - slopes * dist
        p = masked_softmax(sc, dist >= 0)
        return jnp.einsum('bgrqk,bgqkd->bqgrd', p.astype(vg.dtype), vg)

    o_slc = block_map(slc_block, s, SLC_Q_BLOCK)

    pad = ((0, 0), (NSA_WINDOW, 0), (0, 0), (0, 0))
    kw_p = jnp.pad(rms_norm(k_win, g_kw), pad)
    vw_p = jnp.pad(v_win, pad)
    span = NSA_WINDOW + Q_BLOCK

    def win_block(i):
        q0 = i * Q_BLOCK
        qc = lax.dynamic_slice_in_dim(qg, q0, Q_BLOCK, axis=1)
        kb = lax.dynamic_slice_in_dim(kw_p, q0, span, axis=1)
        vb = lax.dynamic_slice_in_dim(vw_p, q0, span, axis=1)
        tq = q0 + jnp.arange(Q_BLOCK)
        kpos = q0 - NSA_WINDOW + jnp.arange(span)
        dist_i = tq[:, None] - kpos[None, :]
        mask = (dist_i >= 0) & (dist_i < NSA_WINDOW) & (kpos[None, :] >= 0)
        sc = jnp.einsum('bqgrd,bkgd->bgrqk', qc, kb).astype(f32) - slopes * dist_i.astype(f32)
        p = masked_softmax(sc, mask)
        return jnp.einsum('bgrqk,bkgd->bqgrd', p.astype(vb.dtype), vb)

    o_win = block_map(win_block, s, Q_BLOCK)

    gt = jax.nn.sigmoid(gates.astype(f32)).astype(q.dtype).reshape(b, s, g, r, 3)
    o = gt[..., 0:1] * o_cmp + gt[..., 1:2] * o_slc + gt[..., 2:3] * o_win
    return o.reshape(b, s, h * hd)


def dsa_mixer(q, k, v, iq, ik, iw, g_q, g_k):
    b, s, h, hd = q.shape
    f32 = jnp.float32
    topk = min(DSA_TOPK_MAX, s // 4)
    slopes = alibi_slopes(h)[:, None, None]
    qn = rms_norm(q, g_q) * hd ** -0.5
    kn = rms_norm(k, g_k)
    iq = iq * IDX_DIM ** -0.5
    iw = iw * IDX_HEADS ** -0.5
    pos = jnp.arange(s)
    take_keys = jax.vmap(lambda t, ix: t[ix])

    def dsa_block(i):
        q0 = i * Q_BLOCK
        tq = q0 + jnp.arange(Q_BLOCK)
        iqc = lax.dynamic_slice_in_dim(iq, q0, Q_BLOCK, axis=1)
        iwc = lax.dynamic_slice_in_dim(iw, q0, Q_BLOCK, axis=1)
        logits = jax.nn.relu(jnp.einsum('bqhd,bsd->bqhs', iqc, ik))
        score = jnp.einsum('bqh,bqhs->bqs', iwc, logits).astype(f32)
        score = jnp.where(pos[None, None, :] <= tq[None, :, None], score, -jnp.inf)
        _, sel = lax.top_k(score, topk)
        kg = take_keys(kn, sel)
        vg = take_keys(v, sel)
        dist = (tq[None, :, None] - sel).astype(f32)[:, None]
        qc = lax.dynamic_slice_in_dim(qn, q0, Q_BLOCK, axis=1)
        sc = jnp.einsum('bqhd,bqkd->bhqk', qc, kg).astype(f32) - slopes * dist
        p = masked_softmax(sc, dist >= 0)
        return jnp.einsum('bhqk,bqkd->bqhd', p.astype(vg.dtype), vg)

    o = block_map(dsa_block, s, Q_BLOCK)
    return o.reshape(b, s, h * hd)


def hybrid_layer(x, c, w_ada, b_ada, g_norm1, g_norm2, w_in, g_q_a, g_kc_a, g_ks_a, g_kw_a,
                 pe_ck, pe_cv, w_ck1, w_ck2, w_cv1, w_cv2, g_q_b, g_k_b, w_o_a, w_o_b, w_out,
                 w_ff_gate, w_ff_up, w_ff_down):
    b, s, _ = x.shape
    mod = (jax.nn.silu(c) @ w_ada + b_ada)[:, None, :]
    shift1, scale1, gate1, shift2, scale2, gate2 = jnp.split(mod, N_ADA, axis=-1)

    h = rms_norm(x, g_norm1) * (1.0 + scale1) + shift1
    proj = h @ w_in
    (q_a, kc_a, vc_a, ks_a, vs_a, kw_a, vw_a, gate_nsa,
     q_b, k_b, v_b, iq_b, ik_b, iw_b, gate_a, gate_b) = jnp.split(
        proj, np.cumsum(IN_SIZES)[:-1].tolist(), axis=-1)

    def heads(t, n):
        return t.reshape(b, s, n, HEAD_DIM)

    o_a = nsa_mixer(heads(q_a, NSA_HEADS), heads(kc_a, NSA_GROUPS), heads(vc_a, NSA_GROUPS),
                    heads(ks_a, NSA_GROUPS), heads(vs_a, NSA_GROUPS),
                    heads(kw_a, NSA_GROUPS), heads(vw_a, NSA_GROUPS),
                    gate_nsa.reshape(b, s, NSA_HEADS, 3),
                    g_q_a, g_kc_a, g_ks_a, g_kw_a, pe_ck, pe_cv, w_ck1, w_ck2, w_cv1, w_cv2)
    o_b = dsa_mixer(heads(q_b, DSA_HEADS), k_b, v_b, iq_b.reshape(b, s, IDX_HEADS, IDX_DIM),
                    ik_b, iw_b, g_q_b, g_k_b)
    y = jax.nn.sigmoid(gate_a) * (o_a @ w_o_a) + jax.nn.sigmoid(gate_b) * (o_b @ w_o_b)
    x = x + gate1 * (y @ w_out)

    h2 = rms_norm(x, g_norm2) * (1.0 + scale2) + shift2
    ff = (jax.nn.silu(h2 @ w_ff_gate) * (h2 @ w_ff_up)) @ w_ff_down
    return x + gate2 * ff


def setup_inputs(seed: int = 0) -> dict:
    key = jax.random.key(seed)
    ks = jax.random.split(key, 32)
    f32 = jnp.float32
    hd = HEAD_DIM
    L = DEPTH

    def nrm(k, shape, scale):
        return jax.random.normal(k, shape, f32) * scale

    def gain(k, n):
        return 1.0 + 0.1 * jax.random.normal(k, (L, n), f32)

    cmp_in = NSA_CMP_LEN * hd
    return {
        'x': nrm(ks[0], (BATCH, SEQ, D_MODEL), 1.0),
        'c': nrm(ks[1], (BATCH, D_MODEL), 1.0),
        'w_ada': nrm(ks[2], (L, D_MODEL, N_ADA * D_MODEL), 0.5 * D_MODEL ** -0.5),
        'b_ada': nrm(ks[3], (L, N_ADA * D_MODEL), 0.01),
        'g_norm1': gain(ks[4], D_MODEL),
        'g_norm2': gain(ks[5], D_MODEL),
        'w_in': nrm(ks[6], (L, D_MODEL, D_IN), D_MODEL ** -0.5),
        'g_q_a': gain(ks[7], hd),
        'g_kc_a': gain(ks[8], hd),
        'g_ks_a': gain(ks[9], hd),
        'g_kw_a': gain(ks[10], hd),
        'pe_ck': nrm(ks[11], (L, NSA_CMP_LEN, hd), 0.1),
        'pe_cv': nrm(ks[12], (L, NSA_CMP_LEN, hd), 0.1),
        'w_ck1': nrm(ks[13], (L, cmp_in, NSA_CMP_HIDDEN), cmp_in ** -0.5),
        'w_ck2': nrm(ks[14], (L, NSA_CMP_HIDDEN, hd), NSA_CMP_HIDDEN ** -0.5),
        'w_cv1': nrm(ks[15], (L, cmp_in, NSA_CMP_HIDDEN), cmp_in ** -0.5),
        'w_cv2': nrm(ks[16], (L, NSA_CMP_HIDDEN, hd), NSA_CMP_HIDDEN ** -0.5),
        'g_q_b': gain(ks[17], hd),
        'g_k_b': gain(ks[18], hd),
        'w_o_a': nrm(ks[19], (L, NSA_HEADS * hd, D_MODEL), (NSA_HEADS * hd) ** -0.5),
        'w_o_b': nrm(ks[20], (L, DSA_HEADS * hd, D_MODEL), (DSA_HEADS * hd) ** -0.5),
        'w_out': nrm(ks[21], (L, D_MODEL, D_MODEL), D_MODEL ** -0.5),
        'w_ff_gate': nrm(ks[22], (L, D_MODEL, D_FF), D_MODEL ** -0.5),
        'w_ff_up': nrm(ks[23], (L, D_MODEL, D_FF), D_MODEL ** -0.5),
        'w_ff_down': nrm(ks[24], (L, D_FF, D_MODEL), D_FF ** -0.5),
    }


def reference(x, c, w_ada, b_ada, g_norm1, g_norm2, w_in, g_q_a, g_kc_a, g_ks_a, g_kw_a,
              pe_ck, pe_cv, w_ck1, w_ck2, w_cv1, w_cv2, g_q_b, g_k_b, w_o_a, w_o_b, w_out,
              w_ff_gate, w_ff_up, w_ff_down):
    for l in range(DEPTH):
        x = hybrid_layer(x, c, w_ada[l], b_ada[l], g_norm1[l], g_norm2[l], w_in[l],
                         g_q_a[l], g_kc_a[l], g_ks_a[l], g_kw_a[l], pe_ck[l], pe_cv[l],
                         w_ck1[l], w_ck2[l], w_cv1[l], w_cv2[l], g_q_b[l], g_k_b[l],
                         w_o_a[l], w_o_b[l], w_out[l], w_ff_gate[l], w_ff_up[l], w_ff_down[l])
    return x
```

```python
import contextlib
import numpy as np
import concourse.bass as bass
import concourse.mybir as mybir
from concourse.bass_utils import run_bass_kernel_spmd

F32 = mybir.dt.float32
BF16 = mybir.dt.bfloat16
AF = mybir.ActivationFunctionType
ALU = mybir.AluOpType
AX = mybir.AxisListType

S_TOK = 2048
D = 1024
NT = 16
DIN = 4576
DFF = 2816
EPS = 1e-6
NBIS = 22
NEG = -30000.0


class Buf:
    __slots__ = ("name", "w", "r")

    def __init__(self, name=""):
        self.name = name
        self.w = None
        self.r = {}


class Sched:
    ENGS = ("pe", "act", "dve", "pool", "sp")
    NDMA = 12

    def __init__(self, nc):
        self.nc = nc
        self.streams = {e: [] for e in self.ENGS}
        self.count = {e: 0 for e in self.ENGS}
        self.waited = {e: {} for e in self.ENGS}
        self.dma_uses = [0] * self.NDMA
        self.dma_rr = 0
        self.out_events = []

    def _deps(self, eng, reads, writes):
        deps = {}

        def add(ev):
            if ev is None:
                return
            k, v = ev
            if deps.get(k, 0) < v:
                deps[k] = v
        for b in reads:
            add(b.w)
        for b in writes:
            if b.w is not None and b.w[0] != eng:
                add(b.w)
            for k, v in b.r.items():
                if k != eng:
                    add((k, v))
        waits = []
        for k, v in deps.items():
            if k == "pe" and eng == "pe":
                continue
            if self.waited[eng].get(k, 0) < v:
                self.waited[eng][k] = v
                waits.append((k, v))
        return waits

    def _commit(self, ev, reads, writes):
        k, v = ev
        for b in writes:
            b.w = ev
            b.r = {}
        for b in reads:
            if b.r.get(k, 0) < v:
                b.r[k] = v

    def op(self, eng, fn, reads=(), writes=()):
        waits = self._deps(eng, reads, writes)
        self.count[eng] += 1
        ev = (eng, self.count[eng])
        self.streams[eng].append((fn, waits, ev))
        self._commit(ev, reads, writes)
        return ev

    def dma(self, out, in_, reads=(), writes=(), q="sp", is_output=False, **kw):
        i = self.dma_rr
        self.dma_rr = (self.dma_rr + 1) % self.NDMA
        waits = self._deps(q, reads, writes)
        key = "dma%d" % i
        prev = self.dma_uses[i] * 16
        if prev and self.waited[q].get(key, 0) < prev:
            self.waited[q][key] = prev
            waits.append((key, prev))
        self.dma_uses[i] += 1
        ev = (key, self.dma_uses[i] * 16)
        fn = lambda e, out=out, in_=in_, kw=kw: e.dma_start(out=out, in_=in_, **kw)
        self.streams[q].append((fn, waits, ev))
        self._commit(ev, reads, writes)
        if is_output:
            self.out_events.append(ev)
        return ev

    def barrier(self):
        allv = [(e, self.count[e]) for e in self.ENGS if self.count[e]]
        allv += [("dma%d" % i, self.dma_uses[i] * 16) for i in range(self.NDMA) if self.dma_uses[i]]
        for e in self.ENGS:
            waits = []
            for k, v in allv:
                if k == e:
                    continue
                if self.waited[e].get(k, 0) < v:
                    self.waited[e][k] = v
                    waits.append((k, v))
            if waits:
                self.streams[e].append((None, waits, None))

    def emit(self):
        nc = self.nc
        with contextlib.ExitStack() as st:
            sems = {}
            for e in self.ENGS:
                sems[e] = st.enter_context(nc.semaphore("s_" + e))
            for i in range(self.NDMA):
                sems["dma%d" % i] = st.enter_context(nc.semaphore("s_dma%d" % i))
            final = {}
            for k, v in self.out_events:
                final[k] = max(final.get(k, 0), v)
            block = st.enter_context(nc.Block())

            def run(engname, e):
                for fn, waits, ev in self.streams[engname]:
                    for k, v in waits:
                        e.wait_ge(sems[k], v)
                    if fn is None:
                        continue
                    ins = fn(e)
                    k, v = ev
                    ins.then_inc(sems[k], 16 if k.startswith("dma") else 1)
                if engname == "sp":
                    for k, v in final.items():
                        e.wait_ge(sems[k], v)

            @block.tensor
            def _(e):
                run("pe", e)

            @block.scalar
            def _(e):
                run("act", e)

            @block.vector
            def _(e):
                run("dve", e)

            @block.gpsimd
            def _(e):
                run("pool", e)

            @block.sync
            def _(e):
                run("sp", e)


class Arena:
    def __init__(self, ap16):
        self.ap = ap16
        self.off = 0

    def reset(self):
        self.off = 0

    def take(self, ncols, dt=BF16):
        if dt == F32:
            self.off = (self.off + 1) // 2 * 2
            n16 = ncols * 2
        else:
            n16 = ncols
        assert self.off + n16 <= self.ap.shape[1], (self.off, n16, self.ap.shape)
        v = self.ap[:, self.off:self.off + n16]
        self.off += n16
        self.off = (self.off + 1) // 2 * 2
        return v.bitcast(F32) if dt == F32 else v


_REGS = {}


def _I(name, *args, **kw):
    if name == "affine_select":
        def thunk(e):
            a = list(args)
            key = (id(e), float(a[4]))
            if key not in _REGS:
                _REGS[key] = e.to_reg(float(a[4]))
            a[4] = _REGS[key]
            return e.affine_select(*a, **kw)
        return thunk
    return lambda e: getattr(e, name)(*args, **kw)


def build_nc(nseq=4, dbg=False):
    nc = bass.Bass("TRN2", target_bir_lowering=False)
    _REGS.clear()
    S = Sched(nc)

    def din(name, shape):
        return nc.dram_tensor(name, shape, F32, kind="ExternalInput").ap()
    x_d = din("x", [nseq, S_TOK, D])
    cT_d = din("cT", [128, 8, 4])
    wada_d = din("w_ada", [D, 6 * D])
    bada_d = din("b_ada", [1, 6 * D])
    gn_d = din("gn", [128, 16])
    win_d = din("w_in", [D, DIN])
    gv_d = din("gvec", [1, 6 * 64])
    peT_d = din("peT", [64, 64])
    wck1_d = din("w_ck1", [2048, 128])
    wck2_d = din("w_ck2", [128, 64])
    wcv1_d = din("w_cv1", [2048, 128])
    wcv2_d = din("w_cv2", [128, 64])
    woa_d = din("w_o_a", [512, D])
    wob_d = din("w_o_b", [512, D])
    wout_d = din("w_out", [D, D])
    wfg_d = din("w_ff_gate", [D, DFF])
    wfu_d = din("w_ff_up", [D, DFF])
    wfd_d = din("w_ff_down", [DFF, D])
    out_d = nc.dram_tensor("out", [nseq, S_TOK, D], F32, kind="ExternalOutput").ap()
    modrow_d = nc.dram_tensor("modrow", [4, 6 * D], F32, kind="Internal").ap()
    oT_d = nc.dram_tensor("oT_scr", [2, 4, 128, S_TOK], BF16, kind="ExternalOutput" if dbg else "Internal").ap()
    if dbg:
        hT_dbg = nc.dram_tensor("hT_dbg", [128, 8, S_TOK], BF16, kind="ExternalOutput").ap()
        x1_dbg = nc.dram_tensor("x1_dbg", [S_TOK, D], F32, kind="ExternalOutput").ap()
        mod_dbg = nc.dram_tensor("mod_dbg", [4, 6 * D], F32, kind="ExternalOutput").ap()

    st = contextlib.ExitStack()

    def sb(name, shape, dt=F32):
        return st.enter_context(nc.sbuf_tensor(name, shape, dt))

    with st:
        PS = [st.enter_context(nc.psum_tensor("ps%d" % i, [128, 512], F32)) for i in range(8)]
        PB = [Buf("ps%d" % i) for i in range(8)]

        def psb(i):
            return PS[i][:].bitcast(BF16)

        ident_b = sb("ident_b", [128, 128], BF16)
        ident_f = sb("ident_f", [128, 128], F32)
        Cm = sb("Cm", [128, 128], BF16)
        Wm = sb("Wm", [128, 128], BF16)
        cmask = sb("cmask", [128, S_TOK], BF16)
        ET = sb("ET", [32, S_TOK], BF16)
        selA = sb("selA", [128, NT, 32], F32)
        selB = sb("selB", [128, NT, 32], F32)
        aqc = sb("aqc", [128, NT, 8, 4], BF16)
        akc = sb("akc", [128, NT, 4], BF16)
        gbc = sb("gbc", [128, 6 * 64], F32)
        gains = sb("gains", [128, 4, 64], F32)
        gnc = sb("gnc", [128, 16], F32)
        modT = sb("modT", [128, 32, 4], F32)
        cb2 = sb("cb2", [128, 2], F32)
        pow2 = sb("pow2", [128, NBIS + 1], F32)
        hT = sb("hT", [128, 8, S_TOK], BF16)
        Vc = sb("Vc", [128, 2, 97], BF16)
        kc_aug = sb("kc_aug", [128, 2, 68], BF16)
        KcT = sb("KcT", [128, 2, 128], BF16)
        U_t = sb("U", [128, 65400], BF16)
        U = Arena(U_t[:])
        bC = Buf("consts")
        bOT = [Buf(), Buf()]
        bOut = Buf()

        hTb = [Buf("hT%d" % c) for c in range(4)]

        def pool(fn, reads=(), writes=()):
            return S.op("pool", fn, reads, writes)

        def dve(fn, reads=(), writes=()):
            return S.op("dve", fn, reads, writes)

        def act(fn, reads=(), writes=()):
            return S.op("act", fn, reads, writes)

        def pe(fn, reads=(), writes=()):
            return S.op("pe", fn, reads, writes)

        pool(_I("memset", ident_b[:], 1.0), writes=[bC])
        pool(_I("affine_select", ident_b[:], ident_b[:], [[1, 128]], ALU.is_equal, 0.0, base=0, channel_multiplier=-1), writes=[bC])
        pool(_I("memset", ident_f[:], 1.0), writes=[bC])
        pool(_I("affine_select", ident_f[:], ident_f[:], [[1, 128]], ALU.is_equal, 0.0, base=0, channel_multiplier=-1), writes=[bC])
        pool(_I("memset", Cm[:], 1.0), writes=[bC])
        pool(_I("affine_select", Cm[:], Cm[:], [[1, 128]], ALU.is_ge, 0.0, base=0, channel_multiplier=-1), writes=[bC])
        pool(_I("memset", Wm[:], 1.0), writes=[bC])
        pool(_I("affine_select", Wm[:], Wm[:], [[-1, 128]], ALU.is_gt, 0.0, base=0, channel_multiplier=1), writes=[bC])
        pool(_I("memset", cmask[:], 1.0), writes=[bC])
        pool(_I("affine_select", cmask[:], cmask[:], [[1, S_TOK]], ALU.is_ge, 0.0, base=-31, channel_multiplier=-16), writes=[bC])
        pool(_I("memset", ET[:], 1.0), writes=[bC])
        pool(_I("affine_select", ET[:], ET[:], [[1, S_TOK]], ALU.is_ge, 0.0, base=0, channel_multiplier=-64), writes=[bC])
        pool(_I("affine_select", ET[:], ET[:], [[-1, S_TOK]], ALU.is_ge, 0.0, base=63, channel_multiplier=64), writes=[bC])
        for g in range(2):
            pool(_I("memset", Vc[:, g, 64:97], 1.0), writes=[bC])
            pool(_I("affine_select", Vc[:, g, 65:97], Vc[:, g, 65:97], [[-64, 32]], ALU.is_ge, 0.0, base=31, channel_multiplier=16), writes=[bC])
            pool(_I("affine_select", Vc[:, g, 65:97], Vc[:, g, 65:97], [[64, 32]], ALU.is_ge, 0.0, base=63, channel_multiplier=-16), writes=[bC])
        Dt = U.take(NT * 32, F32).rearrange("p (t j) -> p t j", j=32)
        jt = U.take(NT * 32, F32).rearrange("p (t j) -> p t j", j=32)
        f0 = U.take(NT * 32, F32).rearrange("p (t j) -> p t j", j=32)
        for lo_, base in ((0, 0), (64, -1)):
            pool(_I("iota", Dt[lo_:lo_ + 64], [[-2, NT], [1, 32]], base=base, channel_multiplier=0, allow_small_or_imprecise_dtypes=True), writes=[bC])
        pool(_I("iota", jt[:], [[0, NT], [1, 32]], base=0, channel_multiplier=0, allow_small_or_imprecise_dtypes=True), writes=[bC])
        dve(_I("tensor_single_scalar", f0[:], jt[:], 0.0, ALU.is_equal), reads=[bC], writes=[bC])
        dve(_I("tensor_single_scalar", jt[:], Dt[:], 0.0, ALU.is_equal), reads=[bC], writes=[bC])
        dve(_I("tensor_max", f0[:], f0[:], jt[:]), reads=[bC], writes=[bC])
        dve(_I("tensor_single_scalar", jt[:], Dt[:], -1.0, ALU.is_equal), reads=[bC], writes=[bC])
        dve(_I("tensor_max", f0[:], f0[:], jt[:]), reads=[bC], writes=[bC])
        dve(_I("tensor_single_scalar", jt[:], Dt[:], 0.0, ALU.is_le), reads=[bC], writes=[bC])
        dve(_I("tensor_sub", selA[:], jt[:], f0[:]), reads=[bC], writes=[bC])
        dve(_I("tensor_add", selB[:], jt[:], f0[:]), reads=[bC], writes=[bC])
        dve(_I("tensor_scalar", selB[:], selB[:], -1.0, 1e9, ALU.add, ALU.mult), reads=[bC], writes=[bC])
        hi_t = sb("hi_t", [128, NT], F32)
        lo_t = sb("lo_t", [128, 1], F32)
        for lo_, base in ((0, 0), (64, 64)):
            pool(_I("iota", hi_t[lo_:lo_ + 64], [[128, NT]], base=base, channel_multiplier=0, allow_small_or_imprecise_dtypes=True), writes=[bC])
            pool(_I("iota", lo_t[lo_:lo_ + 64], [[0, 1]], base=0, channel_multiplier=1, allow_small_or_imprecise_dtypes=True), writes=[bC])
        for h in range(8):
            sl = 2.0 ** -(h + 1)
            dve(_I("memset", aqc[:, :, h, 0:2], sl), reads=[bC], writes=[bC])
            dve(_I("tensor_scalar", aqc[:, :, h, 2], hi_t[:], -sl, None, ALU.mult), reads=[bC], writes=[bC])
            dve(_I("tensor_scalar", aqc[:, :, h, 3], lo_t[:].to_broadcast([128, NT]), -sl, None, ALU.mult), reads=[bC], writes=[bC])
        dve(_I("memset", akc[:, :, 2:4], 1.0), reads=[bC], writes=[bC])
        dve(_I("tensor_copy", akc[:, :, 0], hi_t[:]), reads=[bC], writes=[bC])
        dve(_I("tensor_copy", akc[:, :, 1], lo_t[:].to_broadcast([128, NT])), reads=[bC], writes=[bC])
        pn = sb("pn", [128, 1], F32)
        pool(_I("iota", pn[:], [[0, 1]], base=0, channel_multiplier=16, allow_small_or_imprecise_dtypes=True), writes=[bC])
        for g in range(2):
            dve(_I("tensor_copy", kc_aug[:, g, 64:65], pn[:]), reads=[bC], writes=[bC])
            dve(_I("memset", kc_aug[:, g, 65:66], 31.0), reads=[bC], writes=[bC])
            dve(_I("memset", kc_aug[:, g, 66:68], 1.0), reads=[bC], writes=[bC])
        for j in range(NBIS + 1):
            dve(_I("memset", pow2[:, j:j + 1], 2.0 ** -j), reads=[bC], writes=[bC])
        S.dma(gbc[:], gv_d.partition_broadcast(128), writes=[bC])
        S.dma(gnc[:], gn_d, writes=[bC])

        def gsl(i):
            return gbc[:, i * 64:(i + 1) * 64]
        for idx, (gk, gq) in enumerate(((2, 0), (3, 0), (1, 0), (5, 4))):
            dve(_I("scalar_tensor_tensor", gains[:, idx, :], gsl(gk), 0.125, gsl(gq), ALU.mult, ALU.mult), reads=[bC], writes=[bC])

        S.barrier()
        U.reset()
        scT = U.take(32, F32).rearrange("p (k b) -> p k b", b=4)
        wchunk = U.take(8 * 512, F32).rearrange("p (k n) -> p k n", k=8)
        bchunk = U.take(512, F32)
        mchunk = U.take(512, F32)
        bsc, bw, bbc, bm = Buf(), Buf(), Buf(), Buf()
        S.dma(scT, cT_d, writes=[bsc])
        act(_I("activation", scT, scT, AF.Silu), reads=[bsc], writes=[bsc])
        wada_v = wada_d.rearrange("(k p) n -> p k n", p=128)
        LNV = {0: 0, 1: 1, 3: 2, 4: 3}
        for c in range(12):
            S.dma(wchunk, wada_v[:, :, c * 512:(c + 1) * 512], writes=[bw])
            S.dma(bchunk[0:4, :], bada_d[:, c * 512:(c + 1) * 512].partition_broadcast(4), writes=[bbc])
            for k in range(8):
                pe(_I("matmul", PS[0][0:4, :], lhsT=scT[:, k, :], rhs=wchunk[:, k, :], start=(k == 0), stop=(k == 7)), reads=[bsc, bw], writes=[PB[0]])
            dve(_I("tensor_tensor", mchunk[0:4, :], PS[0][0:4, :], bchunk[0:4, :], ALU.add), reads=[PB[0], bbc], writes=[bm])
            S.dma(modrow_d[:, c * 512:(c + 1) * 512], mchunk[0:4, :], reads=[bm], writes=[bC])
            if dbg:
                S.dma(mod_dbg[:, c * 512:(c + 1) * 512], mchunk[0:4, :], reads=[bm], writes=[Buf()], is_output=True)
            vec, half = c // 2, c % 2
            if vec in LNV:
                for i in range(4):
                    col = (LNV[vec] * 8 + half * 4 + i) * 4
                    pe(_I("transpose", PS[1][:, col:col + 4], mchunk[0:4, i * 128:(i + 1) * 128], ident_f[0:4, 0:4]), reads=[bm, bC], writes=[PB[1]])
        dve(_I("tensor_copy", modT[:].rearrange("p a b -> p (a b)"), PS[1][:, 0:128]), reads=[PB[1]], writes=[bC])
        for which, gi in ((1, 0), (3, 1)):
            dve(_I("scalar_tensor_tensor",
                modT[:, which * 8:(which + 1) * 8, :], modT[:, which * 8:(which + 1) * 8, :], 1.0,
                gnc[:, gi * 8:(gi + 1) * 8].unsqueeze(2).to_broadcast([128, 8, 4]), ALU.add, ALU.mult), reads=[bC], writes=[bC])
        S.barrier()

        xt = [sb("xt%d" % i, [128, D], F32) for i in range(2)]
        xtb = [Buf() for _ in range(2)]
        xn = sb("xn", [128, D], BF16)
        xnb = Buf()
        sq = [sb("sq%d" % i, [128, 512], F32) for i in range(2)]
        sqb = [Buf(), Buf()]
        st16 = sb("st16", [128, 16], F32)
        stb = Buf()
        PT = [sb("PT%d" % i, [128, 512], BF16) for i in range(4)]
        PTb = [Buf() for _ in range(4)]
        sn = sb("sn", [128, 16], F32)
        snb = Buf()
        tmpo = sb("tmpo", [128, 4, 64], F32)
        tmpb = Buf()
        oacc = sb("oacc", [128, 8, 64], F32)
        oab = Buf()
        obf = sb("obf", [128, 512], BF16)
        obfb = Buf()
        oTs = sb("oTs", [128, 4, 128], BF16)
        oTsb = Buf()
        sm = sb("sm", [128, 64], F32)
        smb = Buf()
        state = {"pt": 0, "sb": 0}
        vstate = {}

        def layernorm(src_tile, src_buf, tl, which, b, psbank):
            act(_I("activation", sq[0][:, :].bitcast(BF16), src_tile, AF.Square, accum_out=st16[:, 0:1]), reads=[src_buf], writes=[sqb[0], stb])
            act(_I("activation", st16[:, 1:2], st16[:, 0:1], AF.Sqrt, bias=EPS, scale=1.0 / D), reads=[stb], writes=[stb])
            dve(_I("reciprocal", st16[:, 2:3], st16[:, 1:2]), reads=[stb], writes=[stb])
            dve(_I("tensor_scalar", xn[:], src_tile, st16[:, 2:3], None, ALU.mult), reads=[src_buf, stb], writes=[xnb])
            for j in range(8):
                bk = psbank + j // 4
                o0 = ((j % 4) * 2 + tl) * 128
                pe(_I("transpose", psb(bk)[:, o0:o0 + 128], xn[:, j * 128:(j + 1) * 128], ident_b[:]), reads=[xnb, bC], writes=[PB[bk]])

        def ln_evac(c2, which, b, psbank, hbuf):
            for j in range(8):
                bk = psbank + j // 4
                src = psb(bk)[:, (j % 4) * 256:(j % 4) * 256 + 256]
                Gc = modT[:, (2 * which + 1) * 8 + j, b:b + 1]
                Sc = modT[:, (2 * which) * 8 + j, b:b + 1]
                dve(_I("tensor_scalar", hT[:, j, c2 * 256:(c2 + 1) * 256], src, Gc, Sc, ALU.mult, ALU.add),
                    reads=[PB[bk], bC], writes=[hbuf])

        def load_w(dst, src, buf, q="pool"):
            S.dma(dst, src, writes=[buf], q=q)

        win_v = win_d.rearrange("(k p) n -> p k n", p=128)

        def rms_heads(psbank, nh, dst_stats):
            i = state["sb"] = (state["sb"] + 1) % 2
            act(_I("activation", sq[i][:, 0:nh * 64], PS[psbank][:, 0:nh * 64], AF.Square), reads=[PB[psbank]], writes=[sqb[i]])
            dve(_I("tensor_reduce", dst_stats, sq[i][:, 0:nh * 64].rearrange("p (h d) -> p h d", d=64), AX.X, ALU.add), reads=[sqb[i]], writes=[stb])

        def rstd_from(stats):
            act(_I("activation", stats, stats, AF.Sqrt, bias=EPS, scale=1.0 / 64), reads=[stb], writes=[stb])
            dve(_I("reciprocal", stats, stats), reads=[stb], writes=[stb])

        def attention(QTh, qb, KTk, kb, kind, t, tiles, maskmm, pvb, slot, full_mask=None):
            qs = slice(t * 128, (t + 1) * 128)
            ntl = len(tiles)
            po = PS[pvb][:, slot * 65:(slot + 1) * 65]
            for g0 in range(0, ntl, 4):
                grp = tiles[g0:g0 + 4]
                bk = 4 + (state["pt"] % 2)
                pi = state["pt"] % len(PT)
                state["pt"] += 1
                for i, (j, mt) in enumerate(grp):
                    ks = slice(j * 128, (j + 1) * 128)
                    pe(_I("matmul", PS[bk][:, i * 128:(i + 1) * 128], lhsT=KTk[0:68, ks], rhs=QTh[0:68, qs], start=True, stop=(maskmm is None)),
                       reads=[kb, qb], writes=[PB[bk]])
                    if maskmm is not None:
                        MTg, mb = maskmm
                        pe(_I("matmul", PS[bk][:, i * 128:(i + 1) * 128], lhsT=ET[0:32, ks], rhs=MTg[0:32, qs], start=False, stop=True),
                           reads=[mb, bC], writes=[PB[bk]])
                n = len(grp) * 128
                act(_I("activation", PT[pi][:, 0:n], PS[bk][:, 0:n], AF.Exp), reads=[PB[bk]], writes=[PTb[pi]])
                if full_mask is not None:
                    mT, mTb = full_mask
                    j0 = grp[0][0]
                    pool(_I("tensor_tensor", PT[pi][:, 0:n], PT[pi][:, 0:n], mT[:, j0:j0 + len(grp), :].rearrange("p j c -> p (j c)"), ALU.mult),
                         reads=[mTb], writes=[PTb[pi]])
                for i, (j, mt) in enumerate(grp):
                    if mt is not None:
                        mk = Cm if mt == "C" else Wm
                        pool(_I("tensor_tensor", PT[pi][:, i * 128:(i + 1) * 128], PT[pi][:, i * 128:(i + 1) * 128], mk[:], ALU.mult),
                             reads=[bC], writes=[PTb[pi]])
                for i, (j, mt) in enumerate(grp):
                    gi = g0 + i
                    pe(_I("matmul", po, lhsT=PT[pi][:, i * 128:(i + 1) * 128], rhs=vstate["V"][:, j, kind, :], start=(gi == 0), stop=(gi == ntl - 1)),
                       reads=[PTb[pi], vstate["bV"]], writes=[PB[pvb]])

        def next_pv():
            state["pv"] = state.get("pv", 0) + 1
            return 6 if state["pv"] % 2 == 0 else 2

        def norm4(pvb, g, gate_view, gate_bufs, first):
            o4 = PS[pvb][:, 0:260].rearrange("p (h c) -> p h c", h=4)
            dve(_I("tensor_scalar", sn[:, 0:4], o4[:, :, 64], 1e-30, None, ALU.max), reads=[PB[pvb]], writes=[snb])
            dve(_I("reciprocal", sn[:, 4:8], sn[:, 0:4]), reads=[snb], writes=[snb])
            if gate_view is not None:
                dve(_I("tensor_tensor", sn[:, 4:8], sn[:, 4:8], gate_view, ALU.mult), reads=[snb] + gate_bufs, writes=[snb])
            wb = sn[:, 4:8].unsqueeze(2).to_broadcast([128, 4, 64])
            if first:
                dve(_I("tensor_tensor", oacc[:, 4 * g:4 * g + 4, :], o4[:, :, 0:64], wb, ALU.mult), reads=[PB[pvb], snb], writes=[oab])
            else:
                dve(_I("tensor_tensor", tmpo[:], o4[:, :, 0:64], wb, ALU.mult), reads=[PB[pvb], snb], writes=[tmpb])
                pool(_I("tensor_tensor", oacc[:, 4 * g:4 * g + 4, :], oacc[:, 4 * g:4 * g + 4, :], tmpo[:], ALU.add), reads=[tmpb, oab], writes=[oab])

        def flush_o(t, mix):
            dve(_I("tensor_copy", obf[:], oacc[:].rearrange("p h d -> p (h d)")), reads=[oab], writes=[obfb])
            for j in range(4):
                pe(_I("transpose", psb(3)[:, j * 128:(j + 1) * 128], obf[:, j * 128:(j + 1) * 128], ident_b[:]), reads=[obfb, bC], writes=[PB[3]])
            act(_I("activation", oTs[:].rearrange("p j c -> p (j c)"), psb(3)[:, 0:512], AF.Copy), reads=[PB[3]], writes=[oTsb])
            S.dma(oT_d[mix, :, :, t * 128:(t + 1) * 128].rearrange("j p c -> p j c"), oTs[:], reads=[oTsb], writes=[bOT[mix]])

        for s in range(nseq):
            b = s
            S.barrier()

            for c2 in range(8):
                for tl in range(2):
                    t = c2 * 2 + tl
                    i = t % 2
                    S.dma(xt[i][:], x_d[s, t * 128:(t + 1) * 128, :], writes=[xtb[i]])
                    layernorm(xt[i][:], xtb[i], tl, 0, b, 0)
                ln_evac(c2, 0, b, 0, hTb[c2 // 2])

            if dbg and s == 0:
                S.dma(hT_dbg, hT[:], reads=hTb, writes=[Buf()], is_output=True)
            U.reset()
            V_all = U.take(NT * 5 * 65).rearrange("p (t k c) -> p t k c", t=NT, k=5)
            bV = Buf()
            pool(_I("memset", V_all[:, :, :, 64:65], 1.0), writes=[bV])
            vstate["V"] = V_all
            vstate["bV"] = bV
            Wn = U.take(8 * 1304).rearrange("p (k n) -> p k n", k=8)
            QT = U.take(8 * S_TOK).rearrange("p (h n) -> p h n", h=8)
            KT = U.take(5 * S_TOK).rearrange("p (h n) -> p h n", h=5)
            q_aug = U.take(8 * 68).rearrange("p (h d) -> p h d", h=8)
            k_aug = U.take(4 * 68).rearrange("p (h d) -> p h d", h=4)
            kcT = U.take(S_TOK)
            vcT = U.take(S_TOK)
            MT = U.take(2 * S_TOK).rearrange("p (g n) -> p g n", g=2)
            sg = U.take(NT * 24, F32).rearrange("p (t c) -> p t c", c=24)
            bWn, bq_aug, bk_aug, bkc, bsg = Buf(), Buf(), Buf(), Buf(), Buf()
            QTb = [Buf() for _ in range(8)]
            KTb = [Buf() for _ in range(5)]
            MTb = [Buf(), Buf()]
            for k in range(8):
                load_w(Wn[:, k, :], win_v[:, k, 0:1304], bWn)
            for c in range(4):
                for tl in range(4):
                    t = c * 4 + tl
                    ts = slice(t * 128, (t + 1) * 128)
                    for k in range(8):
                        pe(_I("matmul", PS[0][:, :], lhsT=hT[:, k, ts], rhs=Wn[:, k, 0:512], start=(k == 0), stop=(k == 7)), reads=[hTb[c], bWn], writes=[PB[0]])
                    for k in range(8):
                        pe(_I("matmul", PS[1][:, :], lhsT=hT[:, k, ts], rhs=Wn[:, k, 768:1280], start=(k == 0), stop=(k == 7)), reads=[hTb[c], bWn], writes=[PB[1]])
                    for k in range(8):
                        pe(_I("matmul", PS[2][:, 0:24], lhsT=hT[:, k, ts], rhs=Wn[:, k, 1280:1304], start=(k == 0), stop=(k == 7)), reads=[hTb[c], bWn], writes=[PB[2]])
                    rms_heads(0, 8, st16[:, 0:8])
                    rms_heads(1, 8, st16[:, 8:16])
                    rstd_from(st16[:, 0:16])
                    dve(_I("tensor_tensor", q_aug[:, :, 0:64], PS[0][:, :].rearrange("p (h d) -> p h d", d=64), st16[:, 0:8].unsqueeze(2).to_broadcast([128, 8, 64]), ALU.mult),
                        reads=[PB[0], stb], writes=[bq_aug])
                    dve(_I("tensor_copy", q_aug[:, :, 64:68], aqc[:, t, :, :]), reads=[bC], writes=[bq_aug])
                    for (c0, s0, kk, gi) in ((0, 8, 0, 0), (256, 12, 2, 1)):
                        dve(_I("tensor_tensor", sq[0][:, 0:128].rearrange("p (h d) -> p h d", d=64), PS[1][:, c0:c0 + 128].rearrange("p (h d) -> p h d", d=64),
                                                                   st16[:, s0:s0 + 2].unsqueeze(2).to_broadcast([128, 2, 64]), ALU.mult), reads=[PB[1], stb], writes=[sqb[0]])
                        dve(_I("tensor_tensor", k_aug[:, kk:kk + 2, 0:64], sq[0][:, 0:128].rearrange("p (h d) -> p h d", d=64),
                                                                   gains[:, gi, :].unsqueeze(1).to_broadcast([128, 2, 64]), ALU.mult), reads=[sqb[0], bC], writes=[bk_aug])
                    dve(_I("tensor_copy", k_aug[:, :, 64:68], akc[:, t, :].unsqueeze(1).to_broadcast([128, 4, 4])), reads=[bC], writes=[bk_aug])
                    act(_I("activation", V_all[:, t, 0:2, 0:64], PS[1][:, 128:256].rearrange("p (h d) -> p h d", d=64), AF.Copy), reads=[PB[1]], writes=[bV])
                    act(_I("activation", V_all[:, t, 2:4, 0:64], PS[1][:, 384:512].rearrange("p (h d) -> p h d", d=64), AF.Copy), reads=[PB[1]], writes=[bV])
                    act(_I("activation", sg[:, t, :], PS[2][:, 0:24], AF.Sigmoid), reads=[PB[2]], writes=[bsg])
                    for h in range(8):
                        pe(_I("transpose", psb(3)[0:68, h * 128:(h + 1) * 128], q_aug[:, h, :], ident_b[:]), reads=[bq_aug, bC], writes=[PB[3]])
                    for kk in range(4):
                        pe(_I("transpose", psb(7)[0:68, kk * 128:(kk + 1) * 128], k_aug[:, kk, :], ident_b[:]), reads=[bk_aug, bC], writes=[PB[7]])
                    act(_I("activation", QT[0:68, :, ts], psb(3)[0:68, :].rearrange("p (h c) -> p h c", h=8), AF.Copy), reads=[PB[3]], writes=QTb)
                    dve(_I("tensor_copy", KT[0:68, 0:4, ts], psb(7)[0:68, 0:512].rearrange("p (h c) -> p h c", h=4)), reads=[PB[7]], writes=KTb[0:4])
                cs = slice(c * 512, (c + 1) * 512)
                for (c0, dst) in ((512, kcT), (640, vcT)):
                    for k in range(8):
                        pe(_I("matmul", PS[0][:, :], lhsT=Wn[:, k, c0:c0 + 128], rhs=hT[:, k, cs], start=(k == 0), stop=(k == 7)), reads=[hTb[c], bWn], writes=[PB[0]])
                    act(_I("activation", dst[:, cs], PS[0][:, :], AF.Copy), reads=[PB[0]], writes=[bkc])

            S.barrier()
            W1 = Wn.rearrange("p k n -> p (k n)")[:, 0:2 * 32 * 128].rearrange("p (a l n) -> p a l n", a=2, l=32)
            W2 = U.take(2 * 64).rearrange("p (a n) -> p a n", a=2)
            peT = U.take(64)
            HT = U.take(128)
            bW1, bH = Buf(), Buf()
            for a, (w1d, w2d) in enumerate(((wck1_d, wck2_d), (wcv1_d, wcv2_d))):
                for half in range(2):
                    load_w(W1[half * 64:half * 64 + 64, a, :, :], w1d.rearrange("(l d) n -> d l n", d=64), bW1)
                load_w(W2[:, a, :], w2d, bW1)
            load_w(peT[0:64, :], peT_d, bW1)
            if s == 0:
                for a in range(2):
                    for l in range(32):
                        pe(_I("matmul", PS[2][:, a:a + 1], lhsT=W1[0:64, a, l, :], rhs=peT[0:64, a * 32 + l:a * 32 + l + 1], start=(l == 0), stop=(l == 31)),
                           reads=[bW1], writes=[PB[2]])
                dve(_I("tensor_copy", cb2[:], PS[2][:, 0:2]), reads=[PB[2]], writes=[bC])
            for a, srcT in enumerate((kcT, vcT)):
                for g in range(2):
                    base = g * 64
                    v3 = srcT[base:base + 64, :].rearrange("p (n s) -> p n s", s=16)
                    for l in range(32):
                        rhs = v3[:, (l // 16):(l // 16) + 127, l % 16]
                        pe(_I("matmul", PS[0][:, 0:127], lhsT=W1[base:base + 64, a, l, :], rhs=rhs, start=(l == 0), stop=(l == 31)),
                           reads=[bW1, bkc], writes=[PB[0]])
                    act(_I("activation", HT[:, 0:127], PS[0][:, 0:127], AF.Silu, bias=cb2[:, a:a + 1]), reads=[PB[0], bC], writes=[bH])
                    pe(_I("matmul", PS[1][0:127, 0:64], lhsT=HT[:, 0:127], rhs=W2[:, a, :], start=True, stop=True), reads=[bH, bW1], writes=[PB[1]])
                    if a == 0:
                        act(_I("activation", sq[0][0:127, 0:64], PS[1][0:127, 0:64], AF.Square, accum_out=st16[0:127, 0:1]), reads=[PB[1]], writes=[sqb[0], stb])
                        rstd_from(st16[0:127, 0:1])
                        dve(_I("tensor_scalar", sq[0][0:127, 0:64], PS[1][0:127, 0:64], st16[0:127, 0:1], None, ALU.mult), reads=[PB[1], stb], writes=[sqb[0]])
                        dve(_I("tensor_tensor", kc_aug[0:127, g, 0:64], sq[0][0:127, 0:64], gains[0:127, 2, :], ALU.mult), reads=[sqb[0], bC], writes=[bC])
                        pe(_I("transpose", psb(3)[0:68, 0:127], kc_aug[0:127, g, :], ident_b[0:127, 0:127]), reads=[bC], writes=[PB[3]])
                        dve(_I("tensor_copy", KcT[0:68, g, 0:127], psb(3)[0:68, 0:127]), reads=[PB[3]], writes=[bC])
                    else:
                        act(_I("activation", Vc[0:127, g, 0:64], PS[1][0:127, 0:64], AF.Copy), reads=[PB[1]], writes=[bC])

            imp = sb("imp_%d" % s, [128, 4, 32], F32) if s == 0 else imp
            impb = Buf()
            for t in range(NT):
                qs = slice(t * 128, (t + 1) * 128)
                for g in range(2):
                    for hh in range(4):
                        h = 4 * g + hh
                        pe(_I("matmul", PS[4][0:127, hh * 128:(hh + 1) * 128], lhsT=KcT[0:68, g, 0:127], rhs=QT[0:68, h, qs], start=True, stop=True),
                           reads=[bC, QTb[h]], writes=[PB[4]])
                    dve(_I("tensor_scalar", sq[1][0:127, :], PS[4][0:127, :], 60.0, None, ALU.min), reads=[PB[4]], writes=[sqb[1]])
                    act(_I("activation", PT[0][0:127, :], sq[1][0:127, :], AF.Exp), reads=[sqb[1]], writes=[PTb[0]])
                    dve(_I("tensor_tensor", PT[0][0:127, :].rearrange("p (h c) -> p h c", h=4), PT[0][0:127, :].rearrange("p (h c) -> p h c", h=4),
                                                  cmask[0:127, qs].unsqueeze(1).to_broadcast([127, 4, 128]), ALU.mult), reads=[bC], writes=[PTb[0]])
                    for hh in range(4):
                        pe(_I("matmul", PS[7][:, hh * 97:(hh + 1) * 97], lhsT=PT[0][0:127, hh * 128:(hh + 1) * 128], rhs=Vc[0:127, g, :], start=True, stop=True),
                           reads=[PTb[0], bC], writes=[PB[7]])
                    o4 = PS[7][:, 0:388].rearrange("p (h c) -> p h c", h=4)
                    dve(_I("tensor_scalar", sm[:, 0:4], o4[:, :, 64], 1e-30, None, ALU.max), reads=[PB[7]], writes=[smb])
                    dve(_I("reciprocal", sm[:, 4:8], sm[:, 0:4]), reads=[smb], writes=[smb])
                    dve(_I("tensor_tensor", sm[:, 8:12], sm[:, 4:8], sg[:, t, :].rearrange("p (h r) -> p h r", r=3)[:, 4 * g:4 * g + 4, 0], ALU.mult), reads=[smb, bsg], writes=[smb])
                    dve(_I("tensor_tensor", oacc[:, 4 * g:4 * g + 4, :], o4[:, :, 0:64], sm[:, 8:12].unsqueeze(2).to_broadcast([128, 4, 64]), ALU.mult),
                        reads=[PB[7], smb], writes=[oab])
                    dve(_I("tensor_tensor", imp[:], o4[:, :, 65:97], sm[:, 4:8].unsqueeze(2).to_broadcast([128, 4, 32]), ALU.mult), reads=[PB[7], smb], writes=[impb])
                    dve(_I("tensor_reduce", sm[:, 16:48], imp[:].rearrange("p h j -> p j h"), AX.X, ALU.add), reads=[impb], writes=[smb])
                    dve(_I("tensor_tensor", sm[:, 16:48], sm[:, 16:48], selA[:, t, :], ALU.mult), reads=[smb, bC], writes=[smb])
                    dve(_I("tensor_tensor", sm[:, 16:48], sm[:, 16:48], selB[:, t, :], ALU.add), reads=[smb, bC], writes=[smb])
                    dve(_I("max", out=sm[:, 48:56], in_=sm[:, 16:48]), reads=[smb], writes=[smb])
                    dve(_I("match_replace", out=imp[:, 0, :], in_to_replace=sm[:, 48:56], in_values=sm[:, 16:48], imm_value=-3e38), reads=[smb], writes=[impb])
                    dve(_I("max", out=sm[:, 56:64], in_=imp[:, 0, :]), reads=[impb], writes=[smb])
                    dve(_I("tensor_scalar", sm[:, 16:48], sm[:, 16:48], sm[:, 63:64], None, ALU.is_ge), reads=[smb], writes=[smb])
                    dve(_I("tensor_scalar", obf[:, 0:32], sm[:, 16:48], -1.0, -NEG, ALU.add, ALU.mult), reads=[smb], writes=[obfb])
                    pe(_I("transpose", psb(3)[0:32, 0:128], obf[:, 0:32], ident_b[:]), reads=[obfb, bC], writes=[PB[3]])
                    act(_I("activation", MT[0:32, g, qs], psb(3)[0:32, 0:128], AF.Copy), reads=[PB[3]], writes=[MTb[g]])
                sg3 = sg[:, t, :].rearrange("p (h r) -> p h r", r=3)
                for g in range(2):
                    pvb = next_pv()
                    tiles = [(j, "C" if j == t else None) for j in range(t + 1)]
                    for hh in range(4):
                        h = 4 * g + hh
                        attention(QT[:, h, :], QTb[h], KT[:, g, :], KTb[g], g, t, tiles, (MT[:, g, :], MTb[g]), pvb, hh)
                    norm4(pvb, g, sg3[:, 4 * g:4 * g + 4, 1], [bsg], False)
                    pvb = next_pv()
                    tiles = [(j, "C" if j == t else ("W" if j == t - 4 else None)) for j in range(max(0, t - 4), t + 1)]
                    for hh in range(4):
                        h = 4 * g + hh
                        attention(QT[:, h, :], QTb[h], KT[:, 2 + g, :], KTb[2 + g], 2 + g, t, tiles, None, pvb, hh)
                    norm4(pvb, g, sg3[:, 4 * g:4 * g + 4, 2], [bsg], False)
                flush_o(t, 0)

            S.barrier()
            U.reset()
            V_all = U.take(NT * 5 * 65).rearrange("p (t k c) -> p t k c", t=NT, k=5)
            bV = Buf()
            pool(_I("memset", V_all[:, :, :, 64:65], 1.0), writes=[bV])
            vstate["V"] = V_all
            vstate["bV"] = bV
            Wd = U.take(8 * 1352).rearrange("p (k n) -> p k n", k=8)
            QT = U.take(8 * S_TOK).rearrange("p (h n) -> p h n", h=8)
            KT = U.take(5 * S_TOK).rearrange("p (h n) -> p h n", h=5)
            q_aug = U.take(8 * 68).rearrange("p (h d) -> p h d", h=8)
            k_aug = U.take(4 * 68).rearrange("p (h d) -> p h d", h=4)
            iqT = U.take(4 * S_TOK).rearrange("p (m n) -> p m n", m=4)
            ikT = U.take(S_TOK)
            iw = U.take(NT * 8, F32).rearrange("p (t c) -> p t c", c=8)
            sc = U.take(S_TOK, F32)
            rl = U.take(512, F32)
            maskq = U.take(S_TOK)
            maskT = U.take(S_TOK).rearrange("p (j c) -> p j c", c=128)
            junk = U.take(S_TOK)
            bWd, biq, bik, biw, bsc, brl, bmq, bmT, bjk = (Buf() for _ in range(9))
            QTb = [Buf() for _ in range(8)]
            KTb = [Buf() for _ in range(5)]
            for k in range(8):
                load_w(Wd[:, k, 0:1224], win_v[:, k, 1304:2528], bWd)
                load_w(Wd[:, k, 1224:1288], win_v[:, k, 2456:2520], bWd)
                load_w(Wd[:, k, 1288:1352], win_v[:, k, 2456:2520], bWd)
            for c in range(4):
                cs = slice(c * 512, (c + 1) * 512)
                for tl in range(4):
                    t = c * 4 + tl
                    ts = slice(t * 128, (t + 1) * 128)
                    for k in range(8):
                        pe(_I("matmul", PS[0][:, :], lhsT=hT[:, k, ts], rhs=Wd[:, k, 0:512], start=(k == 0), stop=(k == 7)), reads=[hTb[c], bWd], writes=[PB[0]])
                    for k in range(8):
                        pe(_I("matmul", PS[1][:, 0:128], lhsT=hT[:, k, ts], rhs=Wd[:, k, 512:640], start=(k == 0), stop=(k == 7)), reads=[hTb[c], bWd], writes=[PB[1]])
                    for k in range(8):
                        pe(_I("matmul", PS[2][:, 0:8], lhsT=hT[:, k, ts], rhs=Wd[:, k, 1216:1224], start=(k == 0), stop=(k == 7)), reads=[hTb[c], bWd], writes=[PB[2]])
                    rms_heads(0, 8, st16[:, 0:8])
                    rms_heads(1, 1, st16[:, 8:9])
                    rstd_from(st16[:, 0:9])
                    dve(_I("tensor_tensor", q_aug[:, :, 0:64], PS[0][:, :].rearrange("p (h d) -> p h d", d=64), st16[:, 0:8].unsqueeze(2).to_broadcast([128, 8, 64]), ALU.mult),
                        reads=[PB[0], stb], writes=[bq_aug])
                    dve(_I("tensor_copy", q_aug[:, :, 64:68], aqc[:, t, :, :]), reads=[bC], writes=[bq_aug])
                    dve(_I("tensor_scalar", sq[0][:, 0:64], PS[1][:, 0:64], st16[:, 8:9], None, ALU.mult), reads=[PB[1], stb], writes=[sqb[0]])
                    dve(_I("tensor_tensor", k_aug[:, 0, 0:64], sq[0][:, 0:64], gains[:, 3, :], ALU.mult), reads=[sqb[0], bC], writes=[bk_aug])
                    dve(_I("tensor_copy", k_aug[:, 0, 64:68], akc[:, t, :]), reads=[bC], writes=[bk_aug])
                    act(_I("activation", V_all[:, t, 4, 0:64], PS[1][:, 64:128], AF.Copy), reads=[PB[1]], writes=[bV])
                    act(_I("activation", iw[:, t, :], PS[2][:, 0:8], AF.Copy, scale=8.0 ** -0.5), reads=[PB[2]], writes=[biw])
                    for h in range(8):
                        pe(_I("transpose", psb(3)[0:68, h * 128:(h + 1) * 128], q_aug[:, h, :], ident_b[:]), reads=[bq_aug, bC], writes=[PB[3]])
                    pe(_I("transpose", psb(7)[0:68, 0:128], k_aug[:, 0, :], ident_b[:]), reads=[bk_aug, bC], writes=[PB[7]])
                    act(_I("activation", QT[0:68, :, ts], psb(3)[0:68, :].rearrange("p (h c) -> p h c", h=8), AF.Copy), reads=[PB[3]], writes=QTb)
                    dve(_I("tensor_copy", KT[0:68, 4, ts], psb(7)[0:68, 0:128]), reads=[PB[7]], writes=[KTb[4]])
                for m in range(4):
                    for k in range(8):
                        pe(_I("matmul", PS[0][:, :], lhsT=Wd[:, k, 640 + m * 128:640 + (m + 1) * 128], rhs=hT[:, k, cs], start=(k == 0), stop=(k == 7)), reads=[hTb[c], bWd], writes=[PB[0]])
                    act(_I("activation", iqT[:, m, cs], PS[0][:, :], AF.Copy, scale=0.125), reads=[PB[0]], writes=[biq])
                for k in range(8):
                    pe(_I("matmul", PS[1][:, :], lhsT=Wd[:, k, 1224:1352], rhs=hT[:, k, cs], start=(k == 0), stop=(k == 7)), reads=[hTb[c], bWd], writes=[PB[1]])
                act(_I("activation", ikT[:, cs], PS[1][:, :], AF.Copy), reads=[PB[1]], writes=[bik])

            for t in range(NT):
                qs = slice(t * 128, (t + 1) * 128)
                nk = (t + 1) * 128
                nch = (nk + 511) // 512
                for h in range(8):
                    base = (h % 2) * 64
                    for cc in range(nch):
                        w = min(512, nk - cc * 512)
                        cs = slice(cc * 512, cc * 512 + w)
                        bk = cc % 2
                        pe(_I("matmul", PS[bk][:, 0:w], lhsT=iqT[base:base + 64, h // 2, qs], rhs=ikT[base:base + 64, cs], start=True, stop=True),
                           reads=[biq, bik], writes=[PB[bk]])
                        if h == 0:
                            act(_I("activation", rl[:, 0:w], PS[bk][:, 0:w], AF.Relu), reads=[PB[bk]], writes=[brl])
                            dve(_I("tensor_scalar", sc[:, cs], rl[:, 0:w], iw[:, t, 0:1], None, ALU.mult), reads=[brl, biw], writes=[bsc])
                        else:
                            act(_I("activation", rl[:, 0:w], PS[bk][:, 0:w], AF.Relu), reads=[PB[bk]], writes=[brl])
                            dve(_I("scalar_tensor_tensor", sc[:, cs], rl[:, 0:w], iw[:, t, h:h + 1], sc[:, cs], ALU.mult, ALU.add), reads=[brl, biw, bsc], writes=[bsc])
                dve(_I("tensor_reduce", sm[:, 0:1], sc[:, 0:nk], AX.X, ALU.max, apply_absolute_value=True), reads=[bsc], writes=[smb])
                pool(_I("affine_select", sc[:, t * 128:(t + 1) * 128], sc[:, t * 128:(t + 1) * 128], [[-1, 128]], ALU.is_ge, -3e38, base=0, channel_multiplier=1), reads=[bsc, smb], writes=[bsc])
                dve(_I("tensor_scalar", sm[:, 8:8 + NBIS + 1], pow2[:], sm[:, 0:1], None, ALU.mult), reads=[smb, bC], writes=[smb])
                dve(_I("memset", sm[:, 1:2], 0.0), reads=[smb], writes=[smb])
                for j in range(NBIS):
                    dve(_I("tensor_scalar", junk[:, 0:nk], sc[:, 0:nk], sm[:, 1:2], None, ALU.is_ge, ALU.add, accum_out=sm[:, 2:3]), reads=[bsc, smb], writes=[bjk, smb])
                    dve(_I("tensor_scalar", sm[:, 3:4], sm[:, 2:3], 255.5, -0.5, ALU.is_ge, ALU.add), reads=[smb], writes=[smb])
                    dve(_I("scalar_tensor_tensor", sm[:, 1:2], sm[:, 3:4], sm[:, 8 + j:9 + j], sm[:, 1:2], ALU.mult, ALU.add), reads=[smb], writes=[smb])
                dve(_I("tensor_tensor", sm[:, 1:2], sm[:, 1:2], sm[:, 8 + NBIS:9 + NBIS], ALU.subtract), reads=[smb], writes=[smb])
                dve(_I("tensor_scalar", maskq[:, 0:nk], sc[:, 0:nk], sm[:, 1:2], None, ALU.is_ge), reads=[bsc, smb], writes=[bmq])
                for j in range(t + 1):
                    bk = 3 if (j // 8) % 2 == 0 else 7
                    pe(_I("transpose", psb(bk)[:, (j % 8) * 128:(j % 8 + 1) * 128], maskq[:, j * 128:(j + 1) * 128], ident_b[:]), reads=[bmq, bC], writes=[PB[bk]])
                    if j % 8 == 7 or j == t:
                        j0 = (j // 8) * 8
                        n = j - j0 + 1
                        act(_I("activation", maskT[:, j0:j0 + n, :].rearrange("p j c -> p (j c)"), psb(bk)[:, 0:n * 128], AF.Copy), reads=[PB[bk]], writes=[bmT])
                tiles = [(j, None) for j in range(t + 1)]
                for g2 in range(2):
                    pvb = next_pv()
                    for hh in range(4):
                        h = 4 * g2 + hh
                        attention(QT[:, h, :], QTb[h], KT[:, 4, :], KTb[4], 4, t, tiles, None, pvb, hh, full_mask=(maskT, bmT))
                    norm4(pvb, g2, None, [], True)
                flush_o(t, 1)

            for hf in range(2):
                S.barrier()
                U.reset()
                Wg = U.take(8 * 2048).rearrange("p (k n) -> p k n", k=8)
                Woa = U.take(4 * D).rearrange("p (k n) -> p k n", k=4)
                Wob = U.take(4 * D).rearrange("p (k n) -> p k n", k=4)
                Wout = U.take(8 * D).rearrange("p (k n) -> p k n", k=8)
                Wff_region = (Wg, Woa, Wob, Wout)
                xacc = U.take(8 * D, F32).rearrange("p (t n) -> p t n", t=8)
                yT = U.take(8 * 512).rearrange("p (k n) -> p k n", k=8)
                oaT = U.take(4 * 512).rearrange("p (k n) -> p k n", k=4)
                obT = U.take(4 * 512).rearrange("p (k n) -> p k n", k=4)
                sga = U.take(512)
                sgb = U.take(512)
                t1 = U.take(512, F32)
                t2 = U.take(512, F32)
                aT = yT
                g1bc = U.take(D, F32)
                g2bc = U.take(D, F32)
                bG = Buf()
                S.dma(g1bc, modrow_d[b:b + 1, 2 * D:3 * D].partition_broadcast(128), writes=[bG])
                S.dma(g2bc, modrow_d[b:b + 1, 5 * D:6 * D].partition_broadcast(128), writes=[bG])
                bW, bya, byT, boa, bob, bsga, bsgb, bt1, bt2, baT = (Buf() for _ in range(10))
                xab = [Buf() for _ in range(8)]
                for k in range(8):
                    load_w(Wg[:, k, :], win_v[:, k, 2528:4576], bW)
                    load_w(Wout[:, k, :], wout_d.rearrange("(k p) n -> p k n", p=128)[:, k, :], bW)
                for k in range(4):
                    load_w(Woa[:, k, :], woa_d.rearrange("(k p) n -> p k n", p=128)[:, k, :], bW)
                    load_w(Wob[:, k, :], wob_d.rearrange("(k p) n -> p k n", p=128)[:, k, :], bW)
                for cl in range(2):
                    c = hf * 2 + cl
                    cs = slice(c * 512, (c + 1) * 512)
                    S.dma(oaT, oT_d[0, :, :, cs].rearrange("j p c -> p j c"), reads=[bOT[0]], writes=[boa])
                    S.dma(obT, oT_d[1, :, :, cs].rearrange("j p c -> p j c"), reads=[bOT[1]], writes=[bob])
                    for f in range(8):
                        fs = slice(f * 128, (f + 1) * 128)
                        for k in range(8):
                            pe(_I("matmul", PS[0][:, :], lhsT=Wg[:, k, fs], rhs=hT[:, k, cs], start=(k == 0), stop=(k == 7)), reads=[bW, hTb[c]], writes=[PB[0]])
                        act(_I("activation", sga, PS[0][:, :], AF.Sigmoid), reads=[PB[0]], writes=[bsga])
                        for k in range(8):
                            pe(_I("matmul", PS[1][:, :], lhsT=Wg[:, k, 1024 + f * 128:1024 + (f + 1) * 128], rhs=hT[:, k, cs], start=(k == 0), stop=(k == 7)), reads=[bW, hTb[c]], writes=[PB[1]])
                        act(_I("activation", sgb, PS[1][:, :], AF.Sigmoid), reads=[PB[1]], writes=[bsgb])
                        for k in range(4):
                            pe(_I("matmul", PS[2][:, :], lhsT=Woa[:, k, fs], rhs=oaT[:, k, :], start=(k == 0), stop=(k == 3)), reads=[bW, boa], writes=[PB[2]])
                        for k in range(4):
                            pe(_I("matmul", PS[4][:, :], lhsT=Wob[:, k, fs], rhs=obT[:, k, :], start=(k == 0), stop=(k == 3)), reads=[bW, bob], writes=[PB[4]])
                        dve(_I("tensor_tensor", t1, PS[2][:, :], sga, ALU.mult), reads=[PB[2], bsga], writes=[bt1])
                        dve(_I("tensor_tensor", t2, PS[4][:, :], sgb, ALU.mult), reads=[PB[4], bsgb], writes=[bt2])
                        dve(_I("tensor_tensor", yT[:, f, :], t1, t2, ALU.add), reads=[bt1, bt2], writes=[byT])
                    for tl in range(4):
                        t = c * 4 + tl
                        tt = t - hf * 8
                        i = t % 2
                        S.dma(xt[i][:], x_d[s, t * 128:(t + 1) * 128, :], writes=[xtb[i]])
                        for h2 in range(2):
                            ns = slice(h2 * 512, (h2 + 1) * 512)
                            bk = 5 + h2
                            for k in range(8):
                                pe(_I("matmul", PS[bk][:, :], lhsT=yT[:, k, tl * 128:(tl + 1) * 128], rhs=Wout[:, k, ns], start=(k == 0), stop=(k == 7)), reads=[byT, bW], writes=[PB[bk]])
                            dve(_I("tensor_tensor", t1, PS[bk][:, :], g1bc[:, ns], ALU.mult), reads=[PB[bk], bG], writes=[bt1])
                            dve(_I("tensor_tensor", xacc[:, tt, ns], t1, xt[i][:, ns], ALU.add), reads=[bt1, xtb[i]], writes=[xab[tt]])
                        if dbg and s == 0:
                            S.dma(x1_dbg[t * 128:(t + 1) * 128, :], xacc[:, tt, :], reads=[xab[tt]], writes=[Buf()], is_output=True)
                        layernorm(xacc[:, tt, :], xab[tt], tl % 2, 1, b, 0)
                        if tl % 2 == 1:
                            ln_evac(t // 2, 1, b, 0, hTb[c])
                S.barrier()
                for (f0_, nf) in ((0, 8), (8, 8), (16, 6)):
                    Wfg = Wg.rearrange("p k n -> p (k n)")[:, 0:8 * 1024].rearrange("p (k n) -> p k n", k=8)
                    Wfu = Wg.rearrange("p k n -> p (k n)")[:, 8 * 1024:16 * 1024].rearrange("p (k n) -> p k n", k=8)
                    Wfd = Wout.rearrange("p k n -> p (k n)")[:, 0:8 * D].rearrange("p (k n) -> p k n", k=8)
                    nfc = nf * 128
                    for k in range(8):
                        load_w(Wfg[:, k, 0:nfc], wfg_d.rearrange("(k p) n -> p k n", p=128)[:, k, f0_ * 128:f0_ * 128 + nfc], bW)
                        load_w(Wfu[:, k, 0:nfc], wfu_d.rearrange("(k p) n -> p k n", p=128)[:, k, f0_ * 128:f0_ * 128 + nfc], bW)
                    for k in range(nf):
                        load_w(Wfd[:, k, :], wfd_d[(f0_ + k) * 128:(f0_ + k + 1) * 128, :], bW)
                    for cl in range(2):
                        c = hf * 2 + cl
                        cs = slice(c * 512, (c + 1) * 512)
                        for f in range(nf):
                            fs = slice(f * 128, (f + 1) * 128)
                            for k in range(8):
                                pe(_I("matmul", PS[0][:, :], lhsT=Wfg[:, k, fs], rhs=hT[:, k, cs], start=(k == 0), stop=(k == 7)), reads=[bW, hTb[c]], writes=[PB[0]])
                            for k in range(8):
                                pe(_I("matmul", PS[1][:, :], lhsT=Wfu[:, k, fs], rhs=hT[:, k, cs], start=(k == 0), stop=(k == 7)), reads=[bW, hTb[c]], writes=[PB[1]])
                            act(_I("activation", t1, PS[0][:, :], AF.Silu), reads=[PB[0]], writes=[bt1])
                            dve(_I("tensor_tensor", aT[:, f, :], t1, PS[1][:, :], ALU.mult), reads=[bt1, PB[1]], writes=[baT])
                        for tl in range(4):
                            tt = cl * 4 + tl
                            for h2 in range(2):
                                ns = slice(h2 * 512, (h2 + 1) * 512)
                                bk = 5 + h2
                                for f in range(nf):
                                    pe(_I("matmul", PS[bk][:, :], lhsT=aT[:, f, tl * 128:(tl + 1) * 128], rhs=Wfd[:, f, ns], start=(f == 0), stop=(f == nf - 1)), reads=[baT, bW], writes=[PB[bk]])
                                dve(_I("tensor_tensor", t2, PS[bk][:, :], g2bc[:, ns], ALU.mult), reads=[PB[bk], bG], writes=[bt2])
                                dve(_I("tensor_tensor", xacc[:, tt, ns], xacc[:, tt, ns], t2, ALU.add), reads=[bt2, xab[tt]], writes=[xab[tt]])
                for tt in range(8):
                    t = hf * 8 + tt
                    S.dma(out_d[s, t * 128:(t + 1) * 128, :], xacc[:, tt, :], reads=[xab[tt]], writes=[Buf()], is_output=True)
        S.emit()
    return nc


def _prep_common(inp):
    f = lambda a: np.ascontiguousarray(np.asarray(a, dtype=np.float32))
    gn = np.concatenate([inp["g_norm1"][0].reshape(8, 128).T, inp["g_norm2"][0].reshape(8, 128).T], axis=1)
    gvec = np.concatenate([inp[k][0] for k in ("g_q_a", "g_kc_a", "g_ks_a", "g_kw_a", "g_q_b", "g_k_b")])[None, :]
    peT = np.concatenate([inp["pe_ck"][0].T, inp["pe_cv"][0].T], axis=1)
    return {
        "w_ada": f(inp["w_ada"][0]), "b_ada": f(inp["b_ada"]), "gn": f(gn), "w_in": f(inp["w_in"][0]),
        "gvec": f(gvec), "peT": f(peT), "w_ck1": f(inp["w_ck1"][0]), "w_ck2": f(inp["w_ck2"][0]),
        "w_cv1": f(inp["w_cv1"][0]), "w_cv2": f(inp["w_cv2"][0]), "w_o_a": f(inp["w_o_a"][0]),
        "w_o_b": f(inp["w_o_b"][0]), "w_out": f(inp["w_out"][0]), "w_ff_gate": f(inp["w_ff_gate"][0]),
        "w_ff_up": f(inp["w_ff_up"][0]), "w_ff_down": f(inp["w_ff_down"][0]),
    }


def _core_map(common, x, c, i, nseq):
    m = dict(common)
    m["x"] = np.ascontiguousarray(x[i * nseq:(i + 1) * nseq])
    cc = np.zeros((4, D), np.float32)
    cc[:nseq] = c[i * nseq:(i + 1) * nseq]
    m["cT"] = np.ascontiguousarray(cc.T.reshape(8, 128, 4).transpose(1, 0, 2))
    return m


def kernel(**inputs):
    x = np.asarray(inputs["x"], dtype=np.float32)
    c = np.asarray(inputs["c"], dtype=np.float32)
    n = 8
    nseq = x.shape[0] // n
    nc = build_nc(nseq)
    common = _prep_common(inputs)
    in_maps = [_core_map(common, x, c, i, nseq) for i in range(n)]
    res = run_bass_kernel_spmd(nc, in_maps, core_ids=list(range(n)))
    return np.concatenate([r["out"] for r in res.results], axis=0).astype(np.float32)
```

```python
import contextlib
import numpy as np
import concourse.bass as bass
import concourse.mybir as mybir
from concourse.bass_utils import run_bass_kernel_spmd

F32 = mybir.dt.float32
BF16 = mybir.dt.bfloat16
AF = mybir.ActivationFunctionType
ALU = mybir.AluOpType
AX = mybir.AxisListType

S_TOK = 2048
D = 1024
NT = 16
DIN = 4576
DFF = 2816
EPS = 1e-6
NBIS = 22
NEG = -30000.0


class Buf:
    __slots__ = ("name", "w", "r")

    def __init__(self, name=""):
        self.name = name
        self.w = None
        self.r = {}


class Sched:
    ENGS = ("pe", "act", "dve", "pool", "sp")
    NDMA = 24

    def __init__(self, nc):
        self.nc = nc
        self.streams = {e: [] for e in self.ENGS}
        self.count = {e: 0 for e in self.ENGS}
        self.waited = {e: {} for e in self.ENGS}
        self.dma_uses = [0] * self.NDMA
        self.dma_rr = 0
        self.out_events = []

    def _deps(self, eng, reads, writes):
        deps = {}

        def add(ev):
            if ev is None:
                return
            k, v = ev
            if deps.get(k, 0) < v:
                deps[k] = v
        for b in reads:
            add(b.w)
        for b in writes:
            if b.w is not None and b.w[0] != eng:
                add(b.w)
            for k, v in b.r.items():
                if k != eng:
                    add((k, v))
        waits = []
        for k, v in deps.items():
            if k == "pe" and eng == "pe":
                continue
            if self.waited[eng].get(k, 0) < v:
                self.waited[eng][k] = v
                waits.append((k, v))
        return waits

    def _commit(self, ev, reads, writes):
        k, v = ev
        for b in writes:
            b.w = ev
            b.r = {}
        for b in reads:
            if b.r.get(k, 0) < v:
                b.r[k] = v

    def op(self, eng, fn, reads=(), writes=()):
        waits = self._deps(eng, reads, writes)
        self.count[eng] += 1
        ev = (eng, self.count[eng])
        self.streams[eng].append((fn, waits, ev))
        self._commit(ev, reads, writes)
        return ev

    def dma(self, out, in_, reads=(), writes=(), q="sp", is_output=False, **kw):
        i = self.dma_rr
        self.dma_rr = (self.dma_rr + 1) % self.NDMA
        waits = self._deps(q, reads, writes)
        key = "dma%d" % i
        prev = self.dma_uses[i] * 16
        if prev and self.waited[q].get(key, 0) < prev:
            self.waited[q][key] = prev
            waits.append((key, prev))
        self.dma_uses[i] += 1
        ev = (key, self.dma_uses[i] * 16)
        fn = lambda e, out=out, in_=in_, kw=kw: e.dma_start(out=out, in_=in_, **kw)
        self.streams[q].append((fn, waits, ev))
        self._commit(ev, reads, writes)
        if is_output:
            self.out_events.append(ev)
        return ev

    def barrier(self):
        allv = [(e, self.count[e]) for e in self.ENGS if self.count[e]]
        allv += [("dma%d" % i, self.dma_uses[i] * 16) for i in range(self.NDMA) if self.dma_uses[i]]
        for e in self.ENGS:
            waits = []
            for k, v in allv:
                if k == e:
                    continue
                if self.waited[e].get(k, 0) < v:
                    self.waited[e][k] = v
                    waits.append((k, v))
            if waits:
                self.streams[e].append((None, waits, None))

    def emit(self):
        nc = self.nc
        with contextlib.ExitStack() as st:
            sems = {}
            for e in self.ENGS:
                sems[e] = st.enter_context(nc.semaphore("s_" + e))
            for i in range(self.NDMA):
                sems["dma%d" % i] = st.enter_context(nc.semaphore("s_dma%d" % i))
            final = {}
            for k, v in self.out_events:
                final[k] = max(final.get(k, 0), v)
            block = st.enter_context(nc.Block())

            def run(engname, e):
                for fn, waits, ev in self.streams[engname]:
                    for k, v in waits:
                        e.wait_ge(sems[k], v)
                    if fn is None:
                        continue
                    ins = fn(e)
                    k, v = ev
                    ins.then_inc(sems[k], 16 if k.startswith("dma") else 1)
                if engname == "sp":
                    for k, v in final.items():
                        e.wait_ge(sems[k], v)

            @block.tensor
            def _(e):
                run("pe", e)

            @block.scalar
            def _(e):
                run("act", e)

            @block.vector
            def _(e):
                run("dve", e)

            @block.gpsimd
            def _(e):
                run("pool", e)

            @block.sync
            def _(e):
                run("sp", e)


class Arena:
    def __init__(self, ap16):
        self.ap = ap16
        self.off = 0

    def reset(self):
        self.off = 0

    def take(self, ncols, dt=BF16):
        if dt == F32:
            self.off = (self.off + 1) // 2 * 2
            n16 = ncols * 2
        else:
            n16 = ncols
        assert self.off + n16 <= self.ap.shape[1], (self.off, n16, self.ap.shape)
        v = self.ap[:, self.off:self.off + n16]
        self.off += n16
        self.off = (self.off + 1) // 2 * 2
        return v.bitcast(F32) if dt == F32 else v


_REGS = {}


def _I(name, *args, **kw):
    if name == "affine_select":
        def thunk(e):
            a = list(args)
            key = (id(e), float(a[4]))
            if key not in _REGS:
                _REGS[key] = e.to_reg(float(a[4]))
            a[4] = _REGS[key]
            return e.affine_select(*a, **kw)
        return thunk
    return lambda e: getattr(e, name)(*args, **kw)


def build_nc(nseq=4, dbg=False):
    nc = bass.Bass("TRN2", target_bir_lowering=False)
    _REGS.clear()
    S = Sched(nc)

    def din(name, shape):
        return nc.dram_tensor(name, shape, F32, kind="ExternalInput").ap()
    x_d = din("x", [nseq, S_TOK, D])
    cT_d = din("cT", [128, 8, 4])
    wada_d = din("w_ada", [D, 6 * D])
    bada_d = din("b_ada", [1, 6 * D])
    gn_d = din("gn", [128, 16])
    win_d = din("w_in", [D, DIN])
    gv_d = din("gvec", [1, 6 * 64])
    peT_d = din("peT", [64, 64])
    wck1_d = din("w_ck1", [2048, 128])
    wck2_d = din("w_ck2", [128, 64])
    wcv1_d = din("w_cv1", [2048, 128])
    wcv2_d = din("w_cv2", [128, 64])
    woa_d = din("w_o_a", [512, D])
    wob_d = din("w_o_b", [512, D])
    wout_d = din("w_out", [D, D])
    wfg_d = din("w_ff_gate", [D, DFF])
    wfu_d = din("w_ff_up", [D, DFF])
    wfd_d = din("w_ff_down", [DFF, D])
    out_d = nc.dram_tensor("out", [nseq, S_TOK, D], F32, kind="ExternalOutput").ap()
    modrow_d = nc.dram_tensor("modrow", [4, 6 * D], F32, kind="Internal").ap()
    oT_d = nc.dram_tensor("oT_scr", [2, 4, 128, S_TOK], BF16, kind="ExternalOutput" if dbg else "Internal").ap()
    if dbg:
        hT_dbg = nc.dram_tensor("hT_dbg", [128, 8, S_TOK], BF16, kind="ExternalOutput").ap()
        x1_dbg = nc.dram_tensor("x1_dbg", [S_TOK, D], F32, kind="ExternalOutput").ap()
        mod_dbg = nc.dram_tensor("mod_dbg", [4, 6 * D], F32, kind="ExternalOutput").ap()

    st = contextlib.ExitStack()

    def sb(name, shape, dt=F32):
        return st.enter_context(nc.sbuf_tensor(name, shape, dt))

    with st:
        PS = [st.enter_context(nc.psum_tensor("ps%d" % i, [128, 512], F32)) for i in range(8)]
        PB = [Buf("ps%d" % i) for i in range(8)]

        def psb(i):
            return PS[i][:].bitcast(BF16)

        ident_b = sb("ident_b", [128, 128], BF16)
        ident_f = sb("ident_f", [128, 128], F32)
        Cm = sb("Cm", [128, 128], BF16)
        Wm = sb("Wm", [128, 128], BF16)
        cmask = sb("cmask", [128, S_TOK], BF16)
        ET = sb("ET", [32, S_TOK], BF16)
        selA = sb("selA", [128, NT, 32], F32)
        selB = sb("selB", [128, NT, 32], F32)
        aqc = sb("aqc", [128, NT, 8, 4], BF16)
        akc = sb("akc", [128, NT, 4], BF16)
        gbc = sb("gbc", [128, 6 * 64], F32)
        gains = sb("gains", [128, 4, 64], F32)
        gnc = sb("gnc", [128, 16], F32)
        modT = sb("modT", [128, 32, 4], F32)
        cb2 = sb("cb2", [128, 2], F32)
        pow2 = sb("pow2", [128, NBIS + 1], F32)
        hT = sb("hT", [128, 8, S_TOK], BF16)
        Vc = sb("Vc", [128, 2, 97], BF16)
        kc_aug = sb("kc_aug", [128, 2, 68], BF16)
        KcT = sb("KcT", [128, 2, 128], BF16)
        U_t = sb("U", [128, 65400], BF16)
        U = Arena(U_t[:])
        bC = Buf("consts")
        bOT = [Buf(), Buf()]
        bOut = Buf()

        hTb = [Buf("hT%d" % c) for c in range(4)]

        def pool(fn, reads=(), writes=()):
            return S.op("pool", fn, reads, writes)

        def dve(fn, reads=(), writes=()):
            return S.op("dve", fn, reads, writes)

        def act(fn, reads=(), writes=()):
            return S.op("act", fn, reads, writes)

        def pe(fn, reads=(), writes=()):
            return S.op("pe", fn, reads, writes)

        pool(_I("memset", ident_b[:], 1.0), writes=[bC])
        pool(_I("affine_select", ident_b[:], ident_b[:], [[1, 128]], ALU.is_equal, 0.0, base=0, channel_multiplier=-1), writes=[bC])
        pool(_I("memset", ident_f[:], 1.0), writes=[bC])
        pool(_I("affine_select", ident_f[:], ident_f[:], [[1, 128]], ALU.is_equal, 0.0, base=0, channel_multiplier=-1), writes=[bC])
        pool(_I("memset", Cm[:], 1.0), writes=[bC])
        pool(_I("affine_select", Cm[:], Cm[:], [[1, 128]], ALU.is_ge, 0.0, base=0, channel_multiplier=-1), writes=[bC])
        pool(_I("memset", Wm[:], 1.0), writes=[bC])
        pool(_I("affine_select", Wm[:], Wm[:], [[-1, 128]], ALU.is_gt, 0.0, base=0, channel_multiplier=1), writes=[bC])
        pool(_I("memset", cmask[:], 1.0), writes=[bC])
        pool(_I("affine_select", cmask[:], cmask[:], [[1, S_TOK]], ALU.is_ge, 0.0, base=-31, channel_multiplier=-16), writes=[bC])
        pool(_I("memset", ET[:], 1.0), writes=[bC])
        pool(_I("affine_select", ET[:], ET[:], [[1, S_TOK]], ALU.is_ge, 0.0, base=0, channel_multiplier=-64), writes=[bC])
        pool(_I("affine_select", ET[:], ET[:], [[-1, S_TOK]], ALU.is_ge, 0.0, base=63, channel_multiplier=64), writes=[bC])
        for g in range(2):
            pool(_I("memset", Vc[:, g, 64:97], 1.0), writes=[bC])
            pool(_I("affine_select", Vc[:, g, 65:97], Vc[:, g, 65:97], [[-64, 32]], ALU.is_ge, 0.0, base=31, channel_multiplier=16), writes=[bC])
            pool(_I("affine_select", Vc[:, g, 65:97], Vc[:, g, 65:97], [[64, 32]], ALU.is_ge, 0.0, base=63, channel_multiplier=-16), writes=[bC])
        Dt = U.take(NT * 32, F32).rearrange("p (t j) -> p t j", j=32)
        jt = U.take(NT * 32, F32).rearrange("p (t j) -> p t j", j=32)
        f0 = U.take(NT * 32, F32).rearrange("p (t j) -> p t j", j=32)
        for lo_, base in ((0, 0), (64, -1)):
            pool(_I("iota", Dt[lo_:lo_ + 64], [[-2, NT], [1, 32]], base=base, channel_multiplier=0, allow_small_or_imprecise_dtypes=True), writes=[bC])
        pool(_I("iota", jt[:], [[0, NT], [1, 32]], base=0, channel_multiplier=0, allow_small_or_imprecise_dtypes=True), writes=[bC])
        dve(_I("tensor_single_scalar", f0[:], jt[:], 0.0, ALU.is_equal), reads=[bC], writes=[bC])
        dve(_I("tensor_single_scalar", jt[:], Dt[:], 0.0, ALU.is_equal), reads=[bC], writes=[bC])
        dve(_I("tensor_max", f0[:], f0[:], jt[:]), reads=[bC], writes=[bC])
        dve(_I("tensor_single_scalar", jt[:], Dt[:], -1.0, ALU.is_equal), reads=[bC], writes=[bC])
        dve(_I("tensor_max", f0[:], f0[:], jt[:]), reads=[bC], writes=[bC])
        dve(_I("tensor_single_scalar", jt[:], Dt[:], 0.0, ALU.is_le), reads=[bC], writes=[bC])
        dve(_I("tensor_sub", selA[:], jt[:], f0[:]), reads=[bC], writes=[bC])
        dve(_I("tensor_add", selB[:], jt[:], f0[:]), reads=[bC], writes=[bC])
        dve(_I("tensor_scalar", selB[:], selB[:], -1.0, 1e9, ALU.add, ALU.mult), reads=[bC], writes=[bC])
        hi_t = sb("hi_t", [128, NT], F32)
        lo_t = sb("lo_t", [128, 1], F32)
        for lo_, base in ((0, 0), (64, 64)):
            pool(_I("iota", hi_t[lo_:lo_ + 64], [[128, NT]], base=base, channel_multiplier=0, allow_small_or_imprecise_dtypes=True), writes=[bC])
            pool(_I("iota", lo_t[lo_:lo_ + 64], [[0, 1]], base=0, channel_multiplier=1, allow_small_or_imprecise_dtypes=True), writes=[bC])
        for h in range(8):
            sl = 2.0 ** -(h + 1)
            dve(_I("memset", aqc[:, :, h, 0:2], sl), reads=[bC], writes=[bC])
            dve(_I("tensor_scalar", aqc[:, :, h, 2], hi_t[:], -sl, None, ALU.mult), reads=[bC], writes=[bC])
            dve(_I("tensor_scalar", aqc[:, :, h, 3], lo_t[:].to_broadcast([128, NT]), -sl, None, ALU.mult), reads=[bC], writes=[bC])
        dve(_I("memset", akc[:, :, 2:4], 1.0), reads=[bC], writes=[bC])
        dve(_I("tensor_copy", akc[:, :, 0], hi_t[:]), reads=[bC], writes=[bC])
        dve(_I("tensor_copy", akc[:, :, 1], lo_t[:].to_broadcast([128, NT])), reads=[bC], writes=[bC])
        pn = sb("pn", [128, 1], F32)
        pool(_I("iota", pn[:], [[0, 1]], base=0, channel_multiplier=16, allow_small_or_imprecise_dtypes=True), writes=[bC])
        for g in range(2):
            dve(_I("tensor_copy", kc_aug[:, g, 64:65], pn[:]), reads=[bC], writes=[bC])
            dve(_I("memset", kc_aug[:, g, 65:66], 31.0), reads=[bC], writes=[bC])
            dve(_I("memset", kc_aug[:, g, 66:68], 1.0), reads=[bC], writes=[bC])
        for j in range(NBIS + 1):
            dve(_I("memset", pow2[:, j:j + 1], 2.0 ** -j), reads=[bC], writes=[bC])
        S.dma(gbc[:], gv_d.partition_broadcast(128), writes=[bC])
        S.dma(gnc[:], gn_d, writes=[bC])

        def gsl(i):
            return gbc[:, i * 64:(i + 1) * 64]
        for idx, (gk, gq) in enumerate(((2, 0), (3, 0), (1, 0), (5, 4))):
            dve(_I("scalar_tensor_tensor", gains[:, idx, :], gsl(gk), 0.125, gsl(gq), ALU.mult, ALU.mult), reads=[bC], writes=[bC])

        S.barrier()
        U.reset()
        scT = U.take(32, F32).rearrange("p (k b) -> p k b", b=4)
        wchunk = U.take(8 * 512, F32).rearrange("p (k n) -> p k n", k=8)
        bchunk = U.take(512, F32)
        mchunk = U.take(512, F32)
        bsc, bw, bbc, bm = Buf(), Buf(), Buf(), Buf()
        S.dma(scT, cT_d, writes=[bsc])
        act(_I("activation", scT, scT, AF.Silu), reads=[bsc], writes=[bsc])
        wada_v = wada_d.rearrange("(k p) n -> p k n", p=128)
        LNV = {0: 0, 1: 1, 3: 2, 4: 3}
        for c in range(12):
            S.dma(wchunk, wada_v[:, :, c * 512:(c + 1) * 512], writes=[bw])
            S.dma(bchunk[0:4, :], bada_d[:, c * 512:(c + 1) * 512].partition_broadcast(4), writes=[bbc])
            for k in range(8):
                pe(_I("matmul", PS[0][0:4, :], lhsT=scT[:, k, :], rhs=wchunk[:, k, :], start=(k == 0), stop=(k == 7)), reads=[bsc, bw], writes=[PB[0]])
            dve(_I("tensor_tensor", mchunk[0:4, :], PS[0][0:4, :], bchunk[0:4, :], ALU.add), reads=[PB[0], bbc], writes=[bm])
            S.dma(modrow_d[:, c * 512:(c + 1) * 512], mchunk[0:4, :], reads=[bm], writes=[bC])
            if dbg:
                S.dma(mod_dbg[:, c * 512:(c + 1) * 512], mchunk[0:4, :], reads=[bm], writes=[Buf()], is_output=True)
            vec, half = c // 2, c % 2
            if vec in LNV:
                for i in range(4):
                    col = (LNV[vec] * 8 + half * 4 + i) * 4
                    pe(_I("transpose", PS[1][:, col:col + 4], mchunk[0:4, i * 128:(i + 1) * 128], ident_f[0:4, 0:4]), reads=[bm, bC], writes=[PB[1]])
        dve(_I("tensor_copy", modT[:].rearrange("p a b -> p (a b)"), PS[1][:, 0:128]), reads=[PB[1]], writes=[bC])
        for which, gi in ((1, 0), (3, 1)):
            dve(_I("scalar_tensor_tensor",
                modT[:, which * 8:(which + 1) * 8, :], modT[:, which * 8:(which + 1) * 8, :], 1.0,
                gnc[:, gi * 8:(gi + 1) * 8].unsqueeze(2).to_broadcast([128, 8, 4]), ALU.add, ALU.mult), reads=[bC], writes=[bC])
        S.barrier()

        xt = [sb("xt%d" % i, [128, D], F32) for i in range(2)]
        xtb = [Buf() for _ in range(2)]
        xn = sb("xn", [128, D], BF16)
        xnb = Buf()
        sq = [sb("sq%d" % i, [128, 512], F32) for i in range(2)]
        sqb = [Buf(), Buf()]
        st16 = sb("st16", [128, 16], F32)
        stb = Buf()
        PT = [sb("PT%d" % i, [128, 512], BF16) for i in range(6)]
        PTb = [Buf() for _ in range(6)]
        sn = sb("sn", [128, 16], F32)
        snb = Buf()
        tmpo = sb("tmpo", [128, 4, 64], F32)
        tmpb = Buf()
        oacc = sb("oacc", [128, 8, 64], F32)
        oab = Buf()
        obf = sb("obf", [128, 512], BF16)
        obfb = Buf()
        oTs = sb("oTs", [128, 4, 128], BF16)
        oTsb = Buf()
        sm = sb("sm", [128, 64], F32)
        smb = Buf()
        state = {"pt": 0, "sb": 0}
        vstate = {}

        def layernorm(src_tile, src_buf, tl, which, b, psbank):
            act(_I("activation", sq[0][:, :].bitcast(BF16), src_tile, AF.Square, accum_out=st16[:, 0:1]), reads=[src_buf], writes=[sqb[0], stb])
            act(_I("activation", st16[:, 1:2], st16[:, 0:1], AF.Sqrt, bias=EPS, scale=1.0 / D), reads=[stb], writes=[stb])
            dve(_I("reciprocal", st16[:, 2:3], st16[:, 1:2]), reads=[stb], writes=[stb])
            dve(_I("tensor_scalar", xn[:], src_tile, st16[:, 2:3], None, ALU.mult), reads=[src_buf, stb], writes=[xnb])
            for j in range(8):
                bk = psbank + j // 4
                o0 = ((j % 4) * 2 + tl) * 128
                pe(_I("transpose", psb(bk)[:, o0:o0 + 128], xn[:, j * 128:(j + 1) * 128], ident_b[:]), reads=[xnb, bC], writes=[PB[bk]])

        def ln_evac(c2, which, b, psbank, hbuf):
            for j in range(8):
                bk = psbank + j // 4
                src = psb(bk)[:, (j % 4) * 256:(j % 4) * 256 + 256]
                Gc = modT[:, (2 * which + 1) * 8 + j, b:b + 1]
                Sc = modT[:, (2 * which) * 8 + j, b:b + 1]
                dve(_I("tensor_scalar", hT[:, j, c2 * 256:(c2 + 1) * 256], src, Gc, Sc, ALU.mult, ALU.add),
                    reads=[PB[bk], bC], writes=[hbuf])

        def load_w(dst, src, buf, q="pool"):
            S.dma(dst, src, writes=[buf], q=q)

        win_v = win_d.rearrange("(k p) n -> p k n", p=128)

        def rms_heads(psbank, nh, dst_stats):
            i = state["sb"] = (state["sb"] + 1) % 2
            act(_I("activation", sq[i][:, 0:nh * 64], PS[psbank][:, 0:nh * 64], AF.Square), reads=[PB[psbank]], writes=[sqb[i]])
            dve(_I("tensor_reduce", dst_stats, sq[i][:, 0:nh * 64].rearrange("p (h d) -> p h d", d=64), AX.X, ALU.add), reads=[sqb[i]], writes=[stb])

        def rstd_from(stats):
            act(_I("activation", stats, stats, AF.Sqrt, bias=EPS, scale=1.0 / 64), reads=[stb], writes=[stb])
            dve(_I("reciprocal", stats, stats), reads=[stb], writes=[stb])

        def make_units(QTh, qb, KTk, kb, kind, t, tiles, maskmm, pvb, slot, full_mask=None, banks=(4, 5)):
            ntl = len(tiles)
            return [dict(QTh=QTh, qb=qb, KTk=KTk, kb=kb, kind=kind, t=t, grp=tiles[g0:g0 + 4], g0=g0, ntl=ntl, maskmm=maskmm,
                         pvb=pvb, slot=slot, full_mask=full_mask, banks=banks) for g0 in range(0, ntl, 4)]

        def emit_A(u):
            qs = slice(u["t"] * 128, (u["t"] + 1) * 128)
            banks = u["banks"]
            bk = banks[state["pt"] % len(banks)]
            pi = state["pt"] % len(PT)
            state["pt"] += 1
            u["pi"] = pi
            grp = u["grp"]
            for i, (j, mt) in enumerate(grp):
                ks = slice(j * 128, (j + 1) * 128)
                pe(_I("matmul", PS[bk][:, i * 128:(i + 1) * 128], lhsT=u["KTk"][0:68, ks], rhs=u["QTh"][0:68, qs], start=True, stop=(u["maskmm"] is None)),
                   reads=[u["kb"], u["qb"]], writes=[PB[bk]])
                if u["maskmm"] is not None:
                    MTg, mb = u["maskmm"]
                    pe(_I("matmul", PS[bk][:, i * 128:(i + 1) * 128], lhsT=ET[0:32, ks], rhs=MTg[0:32, qs], start=False, stop=True),
                       reads=[mb, bC], writes=[PB[bk]])
            n = len(grp) * 128
            act(_I("activation", PT[pi][:, 0:n], PS[bk][:, 0:n], AF.Exp), reads=[PB[bk]], writes=[PTb[pi]])
            if u["full_mask"] is not None:
                mT, mTb = u["full_mask"]
                j0 = grp[0][0]
                pool(_I("tensor_tensor", PT[pi][:, 0:n], PT[pi][:, 0:n], mT[:, j0:j0 + len(grp), :].rearrange("p j c -> p (j c)"), ALU.mult),
                     reads=[mTb], writes=[PTb[pi]])
            for i, (j, mt) in enumerate(grp):
                if mt is not None:
                    mk = Cm if mt == "C" else Wm
                    pool(_I("tensor_tensor", PT[pi][:, i * 128:(i + 1) * 128], PT[pi][:, i * 128:(i + 1) * 128], mk[:], ALU.mult),
                         reads=[bC], writes=[PTb[pi]])

        def emit_B(u):
            pi = u["pi"]
            pvb = u["pvb"]
            po = PS[pvb][:, u["slot"] * 65:(u["slot"] + 1) * 65]
            for i, (j, mt) in enumerate(u["grp"]):
                gi = u["g0"] + i
                pe(_I("matmul", po, lhsT=PT[pi][:, i * 128:(i + 1) * 128], rhs=vstate["V"][:, j, u["kind"], :], start=(gi == 0), stop=(gi == u["ntl"] - 1)),
                   reads=[PTb[pi], vstate["bV"]], writes=[PB[pvb]])

        def run_units(units, L, between=None):
            n = len(units)
            for i in range(min(L, n)):
                emit_A(units[i])
            for i in range(n):
                if i + L < n:
                    emit_A(units[i + L])
                emit_B(units[i])
                if units[i].get("post") is not None:
                    units[i]["post"]()
                if between is not None:
                    between(i, n)

        def next_pv():
            state["pv"] = state.get("pv", 0) + 1
            return 6 if state["pv"] % 2 == 0 else 2

        def norm4(pvb, g, gate_view, gate_bufs, first):
            o4 = PS[pvb][:, 0:260].rearrange("p (h c) -> p h c", h=4)
            dve(_I("tensor_scalar", sn[:, 0:4], o4[:, :, 64], 1e-30, None, ALU.max), reads=[PB[pvb]], writes=[snb])
            dve(_I("reciprocal", sn[:, 4:8], sn[:, 0:4]), reads=[snb], writes=[snb])
            if gate_view is not None:
                dve(_I("tensor_tensor", sn[:, 4:8], sn[:, 4:8], gate_view, ALU.mult), reads=[snb] + gate_bufs, writes=[snb])
            wb = sn[:, 4:8].unsqueeze(2).to_broadcast([128, 4, 64])
            if first:
                dve(_I("tensor_tensor", oacc[:, 4 * g:4 * g + 4, :], o4[:, :, 0:64], wb, ALU.mult), reads=[PB[pvb], snb], writes=[oab])
            else:
                dve(_I("tensor_tensor", tmpo[:], o4[:, :, 0:64], wb, ALU.mult), reads=[PB[pvb], snb], writes=[tmpb])
                pool(_I("tensor_tensor", oacc[:, 4 * g:4 * g + 4, :], oacc[:, 4 * g:4 * g + 4, :], tmpo[:], ALU.add), reads=[tmpb, oab], writes=[oab])

        def flush_o(t, mix):
            dve(_I("tensor_copy", obf[:], oacc[:].rearrange("p h d -> p (h d)")), reads=[oab], writes=[obfb])
            for j in range(4):
                pe(_I("transpose", psb(3)[:, j * 128:(j + 1) * 128], obf[:, j * 128:(j + 1) * 128], ident_b[:]), reads=[obfb, bC], writes=[PB[3]])
            act(_I("activation", oTs[:].rearrange("p j c -> p (j c)"), psb(3)[:, 0:512], AF.Copy), reads=[PB[3]], writes=[oTsb])
            S.dma(oT_d[mix, :, :, t * 128:(t + 1) * 128].rearrange("j p c -> p j c"), oTs[:], reads=[oTsb], writes=[bOT[mix]])

        for s in range(nseq):
            b = s
            S.barrier()

            for c2 in range(8):
                for tl in range(2):
                    t = c2 * 2 + tl
                    i = t % 2
                    S.dma(xt[i][:], x_d[s, t * 128:(t + 1) * 128, :], writes=[xtb[i]])
                    layernorm(xt[i][:], xtb[i], tl, 0, b, 0)
                ln_evac(c2, 0, b, 0, hTb[c2 // 2])

            if dbg and s == 0:
                S.dma(hT_dbg, hT[:], reads=hTb, writes=[Buf()], is_output=True)
            U.reset()
            V_all = U.take(NT * 5 * 65).rearrange("p (t k c) -> p t k c", t=NT, k=5)
            bV = Buf()
            pool(_I("memset", V_all[:, :, :, 64:65], 1.0), writes=[bV])
            vstate["V"] = V_all
            vstate["bV"] = bV
            Wn = U.take(8 * 1304).rearrange("p (k n) -> p k n", k=8)
            QT = U.take(8 * S_TOK).rearrange("p (h n) -> p h n", h=8)
            KT = U.take(5 * S_TOK).rearrange("p (h n) -> p h n", h=5)
            q_aug = U.take(8 * 68).rearrange("p (h d) -> p h d", h=8)
            k_aug = U.take(4 * 68).rearrange("p (h d) -> p h d", h=4)
            kcT = U.take(S_TOK)
            vcT = U.take(S_TOK)
            MT = U.take(2 * S_TOK).rearrange("p (g n) -> p g n", g=2)
            sg = U.take(NT * 24, F32).rearrange("p (t c) -> p t c", c=24)
            bq_aug, bk_aug, bkc, bsg = Buf(), Buf(), Buf(), Buf()
            bWn = [Buf() for _ in range(8)]
            QTb = [Buf() for _ in range(8)]
            KTb = [Buf() for _ in range(5)]
            MTb = [Buf(), Buf()]
            for k in range(8):
                load_w(Wn[:, k, :], win_v[:, k, 0:1304], bWn[k])
            for c in range(4):
                for tl in range(4):
                    t = c * 4 + tl
                    ts = slice(t * 128, (t + 1) * 128)
                    for k in range(8):
                        pe(_I("matmul", PS[0][:, :], lhsT=hT[:, k, ts], rhs=Wn[:, k, 0:512], start=(k == 0), stop=(k == 7)), reads=[hTb[c], bWn[k]], writes=[PB[0]])
                    for k in range(8):
                        pe(_I("matmul", PS[1][:, :], lhsT=hT[:, k, ts], rhs=Wn[:, k, 768:1280], start=(k == 0), stop=(k == 7)), reads=[hTb[c], bWn[k]], writes=[PB[1]])
                    for k in range(8):
                        pe(_I("matmul", PS[2][:, 0:24], lhsT=hT[:, k, ts], rhs=Wn[:, k, 1280:1304], start=(k == 0), stop=(k == 7)), reads=[hTb[c], bWn[k]], writes=[PB[2]])
                    rms_heads(0, 8, st16[:, 0:8])
                    rms_heads(1, 8, st16[:, 8:16])
                    rstd_from(st16[:, 0:16])
                    dve(_I("tensor_tensor", q_aug[:, :, 0:64], PS[0][:, :].rearrange("p (h d) -> p h d", d=64), st16[:, 0:8].unsqueeze(2).to_broadcast([128, 8, 64]), ALU.mult),
                        reads=[PB[0], stb], writes=[bq_aug])
                    dve(_I("tensor_copy", q_aug[:, :, 64:68], aqc[:, t, :, :]), reads=[bC], writes=[bq_aug])
                    for (c0, s0, kk, gi) in ((0, 8, 0, 0), (256, 12, 2, 1)):
                        dve(_I("tensor_tensor", sq[0][:, 0:128].rearrange("p (h d) -> p h d", d=64), PS[1][:, c0:c0 + 128].rearrange("p (h d) -> p h d", d=64),
                                                                   st16[:, s0:s0 + 2].unsqueeze(2).to_broadcast([128, 2, 64]), ALU.mult), reads=[PB[1], stb], writes=[sqb[0]])
                        dve(_I("tensor_tensor", k_aug[:, kk:kk + 2, 0:64], sq[0][:, 0:128].rearrange("p (h d) -> p h d", d=64),
                                                                   gains[:, gi, :].unsqueeze(1).to_broadcast([128, 2, 64]), ALU.mult), reads=[sqb[0], bC], writes=[bk_aug])
                    dve(_I("tensor_copy", k_aug[:, :, 64:68], akc[:, t, :].unsqueeze(1).to_broadcast([128, 4, 4])), reads=[bC], writes=[bk_aug])
                    act(_I("activation", V_all[:, t, 0:2, 0:64], PS[1][:, 128:256].rearrange("p (h d) -> p h d", d=64), AF.Copy), reads=[PB[1]], writes=[bV])
                    act(_I("activation", V_all[:, t, 2:4, 0:64], PS[1][:, 384:512].rearrange("p (h d) -> p h d", d=64), AF.Copy), reads=[PB[1]], writes=[bV])
                    act(_I("activation", sg[:, t, :], PS[2][:, 0:24], AF.Sigmoid), reads=[PB[2]], writes=[bsg])
                    for h in range(8):
                        pe(_I("transpose", psb(3)[0:68, h * 128:(h + 1) * 128], q_aug[:, h, :], ident_b[:]), reads=[bq_aug, bC], writes=[PB[3]])
                    for kk in range(4):
                        pe(_I("transpose", psb(7)[0:68, kk * 128:(kk + 1) * 128], k_aug[:, kk, :], ident_b[:]), reads=[bk_aug, bC], writes=[PB[7]])
                    act(_I("activation", QT[0:68, :, ts], psb(3)[0:68, :].rearrange("p (h c) -> p h c", h=8), AF.Copy), reads=[PB[3]], writes=QTb)
                    dve(_I("tensor_copy", KT[0:68, 0:4, ts], psb(7)[0:68, 0:512].rearrange("p (h c) -> p h c", h=4)), reads=[PB[7]], writes=KTb[0:4])
                cs = slice(c * 512, (c + 1) * 512)
                for (c0, dst) in ((512, kcT), (640, vcT)):
                    for k in range(8):
                        pe(_I("matmul", PS[0][:, :], lhsT=Wn[:, k, c0:c0 + 128], rhs=hT[:, k, cs], start=(k == 0), stop=(k == 7)), reads=[hTb[c], bWn[k]], writes=[PB[0]])
                    act(_I("activation", dst[:, cs], PS[0][:, :], AF.Copy), reads=[PB[0]], writes=[bkc])

            S.barrier()
            W1 = Wn.rearrange("p k n -> p (k n)")[:, 0:2 * 32 * 128].rearrange("p (a l n) -> p a l n", a=2, l=32)
            W2 = U.take(2 * 64).rearrange("p (a n) -> p a n", a=2)
            peT = U.take(64)
            HT = U.take(128)
            bW1, bH = Buf(), Buf()
            for a, (w1d, w2d) in enumerate(((wck1_d, wck2_d), (wcv1_d, wcv2_d))):
                for half in range(2):
                    load_w(W1[half * 64:half * 64 + 64, a, :, :], w1d.rearrange("(l d) n -> d l n", d=64), bW1)
                load_w(W2[:, a, :], w2d, bW1)
            load_w(peT[0:64, :], peT_d, bW1)
            if s == 0:
                for a in range(2):
                    for l in range(32):
                        pe(_I("matmul", PS[2][:, a:a + 1], lhsT=W1[0:64, a, l, :], rhs=peT[0:64, a * 32 + l:a * 32 + l + 1], start=(l == 0), stop=(l == 31)),
                           reads=[bW1], writes=[PB[2]])
                dve(_I("tensor_copy", cb2[:], PS[2][:, 0:2]), reads=[PB[2]], writes=[bC])
            for a, srcT in enumerate((kcT, vcT)):
                for g in range(2):
                    base = g * 64
                    v3 = srcT[base:base + 64, :].rearrange("p (n s) -> p n s", s=16)
                    for l in range(32):
                        rhs = v3[:, (l // 16):(l // 16) + 127, l % 16]
                        pe(_I("matmul", PS[0][:, 0:127], lhsT=W1[base:base + 64, a, l, :], rhs=rhs, start=(l == 0), stop=(l == 31)),
                           reads=[bW1, bkc], writes=[PB[0]])
                    act(_I("activation", HT[:, 0:127], PS[0][:, 0:127], AF.Silu, bias=cb2[:, a:a + 1]), reads=[PB[0], bC], writes=[bH])
                    pe(_I("matmul", PS[1][0:127, 0:64], lhsT=HT[:, 0:127], rhs=W2[:, a, :], start=True, stop=True), reads=[bH, bW1], writes=[PB[1]])
                    if a == 0:
                        act(_I("activation", sq[0][0:127, 0:64], PS[1][0:127, 0:64], AF.Square, accum_out=st16[0:127, 0:1]), reads=[PB[1]], writes=[sqb[0], stb])
                        rstd_from(st16[0:127, 0:1])
                        dve(_I("tensor_scalar", sq[0][0:127, 0:64], PS[1][0:127, 0:64], st16[0:127, 0:1], None, ALU.mult), reads=[PB[1], stb], writes=[sqb[0]])
                        dve(_I("tensor_tensor", kc_aug[0:127, g, 0:64], sq[0][0:127, 0:64], gains[0:127, 2, :], ALU.mult), reads=[sqb[0], bC], writes=[bC])
                        pe(_I("transpose", psb(3)[0:68, 0:127], kc_aug[0:127, g, :], ident_b[0:127, 0:127]), reads=[bC], writes=[PB[3]])
                        dve(_I("tensor_copy", KcT[0:68, g, 0:127], psb(3)[0:68, 0:127]), reads=[PB[3]], writes=[bC])
                    else:
                        act(_I("activation", Vc[0:127, g, 0:64], PS[1][0:127, 0:64], AF.Copy), reads=[PB[1]], writes=[bC])

            imp = sb("imp_%d" % s, [128, 4, 32], F32) if s == 0 else imp
            impb = Buf()
            for t in range(NT):
                qs = slice(t * 128, (t + 1) * 128)
                for g in range(2):
                    for hh in range(4):
                        h = 4 * g + hh
                        pe(_I("matmul", PS[4][0:127, hh * 128:(hh + 1) * 128], lhsT=KcT[0:68, g, 0:127], rhs=QT[0:68, h, qs], start=True, stop=True),
                           reads=[bC, QTb[h]], writes=[PB[4]])
                    dve(_I("tensor_scalar", sq[1][0:127, :], PS[4][0:127, :], 60.0, None, ALU.min), reads=[PB[4]], writes=[sqb[1]])
                    act(_I("activation", PT[0][0:127, :], sq[1][0:127, :], AF.Exp), reads=[sqb[1]], writes=[PTb[0]])
                    dve(_I("tensor_tensor", PT[0][0:127, :].rearrange("p (h c) -> p h c", h=4), PT[0][0:127, :].rearrange("p (h c) -> p h c", h=4),
                                                  cmask[0:127, qs].unsqueeze(1).to_broadcast([127, 4, 128]), ALU.mult), reads=[bC], writes=[PTb[0]])
                    for hh in range(4):
                        pe(_I("matmul", PS[7][:, hh * 97:(hh + 1) * 97], lhsT=PT[0][0:127, hh * 128:(hh + 1) * 128], rhs=Vc[0:127, g, :], start=True, stop=True),
                           reads=[PTb[0], bC], writes=[PB[7]])
                    o4 = PS[7][:, 0:388].rearrange("p (h c) -> p h c", h=4)
                    dve(_I("tensor_scalar", sm[:, 0:4], o4[:, :, 64], 1e-30, None, ALU.max), reads=[PB[7]], writes=[smb])
                    dve(_I("reciprocal", sm[:, 4:8], sm[:, 0:4]), reads=[smb], writes=[smb])
                    dve(_I("tensor_tensor", sm[:, 8:12], sm[:, 4:8], sg[:, t, :].rearrange("p (h r) -> p h r", r=3)[:, 4 * g:4 * g + 4, 0], ALU.mult), reads=[smb, bsg], writes=[smb])
                    dve(_I("tensor_tensor", oacc[:, 4 * g:4 * g + 4, :], o4[:, :, 0:64], sm[:, 8:12].unsqueeze(2).to_broadcast([128, 4, 64]), ALU.mult),
                        reads=[PB[7], smb], writes=[oab])
                    dve(_I("tensor_tensor", imp[:], o4[:, :, 65:97], sm[:, 4:8].unsqueeze(2).to_broadcast([128, 4, 32]), ALU.mult), reads=[PB[7], smb], writes=[impb])
                    dve(_I("tensor_reduce", sm[:, 16:48], imp[:].rearrange("p h j -> p j h"), AX.X, ALU.add), reads=[impb], writes=[smb])
                    dve(_I("tensor_tensor", sm[:, 16:48], sm[:, 16:48], selA[:, t, :], ALU.mult), reads=[smb, bC], writes=[smb])
                    dve(_I("tensor_tensor", sm[:, 16:48], sm[:, 16:48], selB[:, t, :], ALU.add), reads=[smb, bC], writes=[smb])
                    dve(_I("max", out=sm[:, 48:56], in_=sm[:, 16:48]), reads=[smb], writes=[smb])
                    dve(_I("match_replace", out=imp[:, 0, :], in_to_replace=sm[:, 48:56], in_values=sm[:, 16:48], imm_value=-3e38), reads=[smb], writes=[impb])
                    dve(_I("max", out=sm[:, 56:64], in_=imp[:, 0, :]), reads=[impb], writes=[smb])
                    dve(_I("tensor_scalar", sm[:, 16:48], sm[:, 16:48], sm[:, 63:64], None, ALU.is_ge), reads=[smb], writes=[smb])
                    dve(_I("tensor_scalar", obf[:, 0:32], sm[:, 16:48], -1.0, -NEG, ALU.add, ALU.mult), reads=[smb], writes=[obfb])
                    pe(_I("transpose", psb(3)[0:32, 0:128], obf[:, 0:32], ident_b[:]), reads=[obfb, bC], writes=[PB[3]])
                    act(_I("activation", MT[0:32, g, qs], psb(3)[0:32, 0:128], AF.Copy), reads=[PB[3]], writes=[MTb[g]])
                sg3 = sg[:, t, :].rearrange("p (h r) -> p h r", r=3)
                units = []
                for g in range(2):
                    pvb = next_pv()
                    tiles = [(j, "C" if j == t else None) for j in range(t + 1)]
                    for hh in range(4):
                        h = 4 * g + hh
                        units += make_units(QT[:, h, :], QTb[h], KT[:, g, :], KTb[g], g, t, tiles, (MT[:, g, :], MTb[g]), pvb, hh, banks=(4, 5, 0, 1))
                    units[-1]["post"] = (lambda pvb=pvb, g=g, gv=sg3[:, 4 * g:4 * g + 4, 1]: norm4(pvb, g, gv, [bsg], False))
                    pvb = next_pv()
                    tiles = [(j, "C" if j == t else ("W" if j == t - 4 else None)) for j in range(max(0, t - 4), t + 1)]
                    for hh in range(4):
                        h = 4 * g + hh
                        units += make_units(QT[:, h, :], QTb[h], KT[:, 2 + g, :], KTb[2 + g], 2 + g, t, tiles, None, pvb, hh, banks=(4, 5, 0, 1))
                    units[-1]["post"] = (lambda pvb=pvb, g=g, gv=sg3[:, 4 * g:4 * g + 4, 2]: norm4(pvb, g, gv, [bsg], False))
                run_units(units, 2)
                flush_o(t, 0)

            S.barrier()
            U.reset()
            V_all = U.take(NT * 5 * 65).rearrange("p (t k c) -> p t k c", t=NT, k=5)
            bV = Buf()
            pool(_I("memset", V_all[:, :, :, 64:65], 1.0), writes=[bV])
            vstate["V"] = V_all
            vstate["bV"] = bV
            Wd = U.take(8 * 1352).rearrange("p (k n) -> p k n", k=8)
            QT = U.take(8 * S_TOK).rearrange("p (h n) -> p h n", h=8)
            KT = U.take(5 * S_TOK).rearrange("p (h n) -> p h n", h=5)
            q_aug = U.take(8 * 68).rearrange("p (h d) -> p h d", h=8)
            k_aug = U.take(4 * 68).rearrange("p (h d) -> p h d", h=4)
            iqT = U.take(4 * S_TOK).rearrange("p (m n) -> p m n", m=4)
            ikT = U.take(S_TOK)
            iw = U.take(NT * 8, F32).rearrange("p (t c) -> p t c", c=8)
            sc = U.take(S_TOK, F32)
            rl = U.take(512, F32)
            maskq = U.take(S_TOK)
            maskT = U.take(S_TOK).rearrange("p (j c) -> p j c", c=128)
            junk = U.take(S_TOK)
            biq, bik, biw, bsc, brl, bmq, bmT, bjk = (Buf() for _ in range(8))
            bWd = [Buf() for _ in range(8)]
            QTb = [Buf() for _ in range(8)]
            KTb = [Buf() for _ in range(5)]
            for k in range(8):
                load_w(Wd[:, k, 0:1224], win_v[:, k, 1304:2528], bWd[k])
                load_w(Wd[:, k, 1224:1288], win_v[:, k, 2456:2520], bWd[k])
                load_w(Wd[:, k, 1288:1352], win_v[:, k, 2456:2520], bWd[k])
            for c in range(4):
                cs = slice(c * 512, (c + 1) * 512)
                for tl in range(4):
                    t = c * 4 + tl
                    ts = slice(t * 128, (t + 1) * 128)
                    for k in range(8):
                        pe(_I("matmul", PS[0][:, :], lhsT=hT[:, k, ts], rhs=Wd[:, k, 0:512], start=(k == 0), stop=(k == 7)), reads=[hTb[c], bWd[k]], writes=[PB[0]])
                    for k in range(8):
                        pe(_I("matmul", PS[1][:, 0:128], lhsT=hT[:, k, ts], rhs=Wd[:, k, 512:640], start=(k == 0), stop=(k == 7)), reads=[hTb[c], bWd[k]], writes=[PB[1]])
                    for k in range(8):
                        pe(_I("matmul", PS[2][:, 0:8], lhsT=hT[:, k, ts], rhs=Wd[:, k, 1216:1224], start=(k == 0), stop=(k == 7)), reads=[hTb[c], bWd[k]], writes=[PB[2]])
                    rms_heads(0, 8, st16[:, 0:8])
                    rms_heads(1, 1, st16[:, 8:9])
                    rstd_from(st16[:, 0:9])
                    dve(_I("tensor_tensor", q_aug[:, :, 0:64], PS[0][:, :].rearrange("p (h d) -> p h d", d=64), st16[:, 0:8].unsqueeze(2).to_broadcast([128, 8, 64]), ALU.mult),
                        reads=[PB[0], stb], writes=[bq_aug])
                    dve(_I("tensor_copy", q_aug[:, :, 64:68], aqc[:, t, :, :]), reads=[bC], writes=[bq_aug])
                    dve(_I("tensor_scalar", sq[0][:, 0:64], PS[1][:, 0:64], st16[:, 8:9], None, ALU.mult), reads=[PB[1], stb], writes=[sqb[0]])
                    dve(_I("tensor_tensor", k_aug[:, 0, 0:64], sq[0][:, 0:64], gains[:, 3, :], ALU.mult), reads=[sqb[0], bC], writes=[bk_aug])
                    dve(_I("tensor_copy", k_aug[:, 0, 64:68], akc[:, t, :]), reads=[bC], writes=[bk_aug])
                    act(_I("activation", V_all[:, t, 4, 0:64], PS[1][:, 64:128], AF.Copy), reads=[PB[1]], writes=[bV])
                    act(_I("activation", iw[:, t, :], PS[2][:, 0:8], AF.Copy, scale=8.0 ** -0.5), reads=[PB[2]], writes=[biw])
                    for h in range(8):
                        pe(_I("transpose", psb(3)[0:68, h * 128:(h + 1) * 128], q_aug[:, h, :], ident_b[:]), reads=[bq_aug, bC], writes=[PB[3]])
                    pe(_I("transpose", psb(7)[0:68, 0:128], k_aug[:, 0, :], ident_b[:]), reads=[bk_aug, bC], writes=[PB[7]])
                    act(_I("activation", QT[0:68, :, ts], psb(3)[0:68, :].rearrange("p (h c) -> p h c", h=8), AF.Copy), reads=[PB[3]], writes=QTb)
                    dve(_I("tensor_copy", KT[0:68, 4, ts], psb(7)[0:68, 0:128]), reads=[PB[7]], writes=[KTb[4]])
                for m in range(4):
                    for k in range(8):
                        pe(_I("matmul", PS[0][:, :], lhsT=Wd[:, k, 640 + m * 128:640 + (m + 1) * 128], rhs=hT[:, k, cs], start=(k == 0), stop=(k == 7)), reads=[hTb[c], bWd[k]], writes=[PB[0]])
                    act(_I("activation", iqT[:, m, cs], PS[0][:, :], AF.Copy, scale=0.125), reads=[PB[0]], writes=[biq])
                for k in range(8):
                    pe(_I("matmul", PS[1][:, :], lhsT=Wd[:, k, 1224:1352], rhs=hT[:, k, cs], start=(k == 0), stop=(k == 7)), reads=[hTb[c], bWd[k]], writes=[PB[1]])
                act(_I("activation", ikT[:, cs], PS[1][:, :], AF.Copy), reads=[PB[1]], writes=[bik])

            for t in range(NT):
                qs = slice(t * 128, (t + 1) * 128)
                nk = (t + 1) * 128
                nch = (nk + 511) // 512
                for h in range(8):
                    base = (h % 2) * 64
                    for cc in range(nch):
                        w = min(512, nk - cc * 512)
                        cs = slice(cc * 512, cc * 512 + w)
                        bk = cc % 2
                        pe(_I("matmul", PS[bk][:, 0:w], lhsT=iqT[base:base + 64, h // 2, qs], rhs=ikT[base:base + 64, cs], start=True, stop=True),
                           reads=[biq, bik], writes=[PB[bk]])
                        if h == 0:
                            act(_I("activation", rl[:, 0:w], PS[bk][:, 0:w], AF.Relu), reads=[PB[bk]], writes=[brl])
                            dve(_I("tensor_scalar", sc[:, cs], rl[:, 0:w], iw[:, t, 0:1], None, ALU.mult), reads=[brl, biw], writes=[bsc])
                        else:
                            act(_I("activation", rl[:, 0:w], PS[bk][:, 0:w], AF.Relu), reads=[PB[bk]], writes=[brl])
                            dve(_I("scalar_tensor_tensor", sc[:, cs], rl[:, 0:w], iw[:, t, h:h + 1], sc[:, cs], ALU.mult, ALU.add), reads=[brl, biw, bsc], writes=[bsc])
                dve(_I("tensor_reduce", sm[:, 0:1], sc[:, 0:nk], AX.X, ALU.max, apply_absolute_value=True), reads=[bsc], writes=[smb])
                pool(_I("affine_select", sc[:, t * 128:(t + 1) * 128], sc[:, t * 128:(t + 1) * 128], [[-1, 128]], ALU.is_ge, -3e38, base=0, channel_multiplier=1), reads=[bsc, smb], writes=[bsc])
                dve(_I("tensor_scalar", sm[:, 8:8 + NBIS + 1], pow2[:], sm[:, 0:1], None, ALU.mult), reads=[smb, bC], writes=[smb])
                dve(_I("memset", sm[:, 1:2], 0.0), reads=[smb], writes=[smb])
                for j in range(NBIS):
                    dve(_I("tensor_scalar", junk[:, 0:nk], sc[:, 0:nk], sm[:, 1:2], None, ALU.is_ge, ALU.add, accum_out=sm[:, 2:3]), reads=[bsc, smb], writes=[bjk, smb])
                    dve(_I("tensor_scalar", sm[:, 3:4], sm[:, 2:3], 255.5, -0.5, ALU.is_ge, ALU.add), reads=[smb], writes=[smb])
                    dve(_I("scalar_tensor_tensor", sm[:, 1:2], sm[:, 3:4], sm[:, 8 + j:9 + j], sm[:, 1:2], ALU.mult, ALU.add), reads=[smb], writes=[smb])
                dve(_I("tensor_tensor", sm[:, 1:2], sm[:, 1:2], sm[:, 8 + NBIS:9 + NBIS], ALU.subtract), reads=[smb], writes=[smb])
                dve(_I("tensor_scalar", maskq[:, 0:nk], sc[:, 0:nk], sm[:, 1:2], None, ALU.is_ge), reads=[bsc, smb], writes=[bmq])
                for j in range(t + 1):
                    bk = 3 if (j // 8) % 2 == 0 else 7
                    pe(_I("transpose", psb(bk)[:, (j % 8) * 128:(j % 8 + 1) * 128], maskq[:, j * 128:(j + 1) * 128], ident_b[:]), reads=[bmq, bC], writes=[PB[bk]])
                    if j % 8 == 7 or j == t:
                        j0 = (j // 8) * 8
                        n = j - j0 + 1
                        act(_I("activation", maskT[:, j0:j0 + n, :].rearrange("p j c -> p (j c)"), psb(bk)[:, 0:n * 128], AF.Copy), reads=[PB[bk]], writes=[bmT])
                tiles = [(j, None) for j in range(t + 1)]
                units = []
                for g2 in range(2):
                    pvb = next_pv()
                    for hh in range(4):
                        h = 4 * g2 + hh
                        units += make_units(QT[:, h, :], QTb[h], KT[:, 4, :], KTb[4], 4, t, tiles, None, pvb, hh, full_mask=(maskT, bmT))
                    units[-1]["post"] = (lambda pvb=pvb, g2=g2: norm4(pvb, g2, None, [], True))
                run_units(units, 1)
                flush_o(t, 1)

            for hf in range(2):
                S.barrier()
                U.reset()
                Wg = U.take(8 * 2048).rearrange("p (k n) -> p k n", k=8)
                Woa = U.take(4 * D).rearrange("p (k n) -> p k n", k=4)
                Wob = U.take(4 * D).rearrange("p (k n) -> p k n", k=4)
                Wout = U.take(8 * D).rearrange("p (k n) -> p k n", k=8)
                Wff_region = (Wg, Woa, Wob, Wout)
                xacc = U.take(8 * D, F32).rearrange("p (t n) -> p t n", t=8)
                yT = U.take(8 * 512).rearrange("p (k n) -> p k n", k=8)
                oaT = U.take(4 * 512).rearrange("p (k n) -> p k n", k=4)
                obT = U.take(4 * 512).rearrange("p (k n) -> p k n", k=4)
                sga = U.take(512)
                sgb = U.take(512)
                t1 = U.take(512, F32)
                t2 = U.take(512, F32)
                aT = yT
                g1bc = U.take(D, F32)
                g2bc = U.take(D, F32)
                bG = Buf()
                S.dma(g1bc, modrow_d[b:b + 1, 2 * D:3 * D].partition_broadcast(128), writes=[bG])
                S.dma(g2bc, modrow_d[b:b + 1, 5 * D:6 * D].partition_broadcast(128), writes=[bG])
                bya, byT, boa, bob, bsga, bsgb, bt1, bt2, baT = (Buf() for _ in range(9))
                bWg = [Buf() for _ in range(8)]
                bWo = [Buf() for _ in range(8)]
                bWa = [Buf() for _ in range(4)]
                bWb = [Buf() for _ in range(4)]
                xab = [Buf() for _ in range(8)]
                for k in range(8):
                    load_w(Wg[:, k, :], win_v[:, k, 2528:4576], bWg[k])
                    load_w(Wout[:, k, :], wout_d.rearrange("(k p) n -> p k n", p=128)[:, k, :], bWo[k])
                for k in range(4):
                    load_w(Woa[:, k, :], woa_d.rearrange("(k p) n -> p k n", p=128)[:, k, :], bWa[k])
                    load_w(Wob[:, k, :], wob_d.rearrange("(k p) n -> p k n", p=128)[:, k, :], bWb[k])
                for cl in range(2):
                    c = hf * 2 + cl
                    cs = slice(c * 512, (c + 1) * 512)
                    S.dma(oaT, oT_d[0, :, :, cs].rearrange("j p c -> p j c"), reads=[bOT[0]], writes=[boa])
                    S.dma(obT, oT_d[1, :, :, cs].rearrange("j p c -> p j c"), reads=[bOT[1]], writes=[bob])
                    for f in range(8):
                        fs = slice(f * 128, (f + 1) * 128)
                        for k in range(8):
                            pe(_I("matmul", PS[0][:, :], lhsT=Wg[:, k, fs], rhs=hT[:, k, cs], start=(k == 0), stop=(k == 7)), reads=[bWg[k], hTb[c]], writes=[PB[0]])
                        act(_I("activation", sga, PS[0][:, :], AF.Sigmoid), reads=[PB[0]], writes=[bsga])
                        for k in range(8):
                            pe(_I("matmul", PS[1][:, :], lhsT=Wg[:, k, 1024 + f * 128:1024 + (f + 1) * 128], rhs=hT[:, k, cs], start=(k == 0), stop=(k == 7)), reads=[bWg[k], hTb[c]], writes=[PB[1]])
                        act(_I("activation", sgb, PS[1][:, :], AF.Sigmoid), reads=[PB[1]], writes=[bsgb])
                        for k in range(4):
                            pe(_I("matmul", PS[2][:, :], lhsT=Woa[:, k, fs], rhs=oaT[:, k, :], start=(k == 0), stop=(k == 3)), reads=[bWa[k], boa], writes=[PB[2]])
                        for k in range(4):
                            pe(_I("matmul", PS[4][:, :], lhsT=Wob[:, k, fs], rhs=obT[:, k, :], start=(k == 0), stop=(k == 3)), reads=[bWb[k], bob], writes=[PB[4]])
                        dve(_I("tensor_tensor", t1, PS[2][:, :], sga, ALU.mult), reads=[PB[2], bsga], writes=[bt1])
                        dve(_I("tensor_tensor", t2, PS[4][:, :], sgb, ALU.mult), reads=[PB[4], bsgb], writes=[bt2])
                        dve(_I("tensor_tensor", yT[:, f, :], t1, t2, ALU.add), reads=[bt1, bt2], writes=[byT])
                    for tl in range(4):
                        t = c * 4 + tl
                        tt = t - hf * 8
                        i = t % 2
                        S.dma(xt[i][:], x_d[s, t * 128:(t + 1) * 128, :], writes=[xtb[i]])
                        for h2 in range(2):
                            ns = slice(h2 * 512, (h2 + 1) * 512)
                            bk = 5 + h2
                            for k in range(8):
                                pe(_I("matmul", PS[bk][:, :], lhsT=yT[:, k, tl * 128:(tl + 1) * 128], rhs=Wout[:, k, ns], start=(k == 0), stop=(k == 7)), reads=[byT, bWo[k]], writes=[PB[bk]])
                            dve(_I("tensor_tensor", t1, PS[bk][:, :], g1bc[:, ns], ALU.mult), reads=[PB[bk], bG], writes=[bt1])
                            dve(_I("tensor_tensor", xacc[:, tt, ns], t1, xt[i][:, ns], ALU.add), reads=[bt1, xtb[i]], writes=[xab[tt]])
                        if dbg and s == 0:
                            S.dma(x1_dbg[t * 128:(t + 1) * 128, :], xacc[:, tt, :], reads=[xab[tt]], writes=[Buf()], is_output=True)
                        layernorm(xacc[:, tt, :], xab[tt], tl % 2, 1, b, 0)
                        if tl % 2 == 1:
                            ln_evac(t // 2, 1, b, 0, hTb[c])
                S.barrier()
                for (f0_, nf) in ((0, 8), (8, 8), (16, 6)):
                    Wfg = Wg.rearrange("p k n -> p (k n)")[:, 0:8 * 1024].rearrange("p (k n) -> p k n", k=8)
                    Wfu = Wg.rearrange("p k n -> p (k n)")[:, 8 * 1024:16 * 1024].rearrange("p (k n) -> p k n", k=8)
                    Wfd = Wout.rearrange("p k n -> p (k n)")[:, 0:8 * D].rearrange("p (k n) -> p k n", k=8)
                    nfc = nf * 128
                    if f0_ == 0:
                        bFg = [Buf() for _ in range(8)]
                        bFu = [Buf() for _ in range(8)]
                        bFd = [Buf() for _ in range(8)]
                    for k in range(8):
                        load_w(Wfg[:, k, 0:nfc], wfg_d.rearrange("(k p) n -> p k n", p=128)[:, k, f0_ * 128:f0_ * 128 + nfc], bFg[k])
                        load_w(Wfu[:, k, 0:nfc], wfu_d.rearrange("(k p) n -> p k n", p=128)[:, k, f0_ * 128:f0_ * 128 + nfc], bFu[k])
                    for k in range(nf):
                        load_w(Wfd[:, k, :], wfd_d[(f0_ + k) * 128:(f0_ + k + 1) * 128, :], bFd[k])
                    for cl in range(2):
                        c = hf * 2 + cl
                        cs = slice(c * 512, (c + 1) * 512)
                        for f in range(nf):
                            fs = slice(f * 128, (f + 1) * 128)
                            for k in range(8):
                                pe(_I("matmul", PS[0][:, :], lhsT=Wfg[:, k, fs], rhs=hT[:, k, cs], start=(k == 0), stop=(k == 7)), reads=[bFg[k], hTb[c]], writes=[PB[0]])
                            for k in range(8):
                                pe(_I("matmul", PS[1][:, :], lhsT=Wfu[:, k, fs], rhs=hT[:, k, cs], start=(k == 0), stop=(k == 7)), reads=[bFu[k], hTb[c]], writes=[PB[1]])
                            act(_I("activation", t1, PS[0][:, :], AF.Silu), reads=[PB[0]], writes=[bt1])
                            dve(_I("tensor_tensor", aT[:, f, :], t1, PS[1][:, :], ALU.mult), reads=[bt1, PB[1]], writes=[baT])
                        for tl in range(4):
                            tt = cl * 4 + tl
                            for h2 in range(2):
                                ns = slice(h2 * 512, (h2 + 1) * 512)
                                bk = 5 + h2
                                for f in range(nf):
                                    pe(_I("matmul", PS[bk][:, :], lhsT=aT[:, f, tl * 128:(tl + 1) * 128], rhs=Wfd[:, f, ns], start=(f == 0), stop=(f == nf - 1)), reads=[baT, bFd[f]], writes=[PB[bk]])
                                dve(_I("tensor_tensor", t2, PS[bk][:, :], g2bc[:, ns], ALU.mult), reads=[PB[bk], bG], writes=[bt2])
                                dve(_I("tensor_tensor", xacc[:, tt, ns], xacc[:, tt, ns], t2, ALU.add), reads=[bt2, xab[tt]], writes=[xab[tt]])
                for tt in range(8):
                    t = hf * 8 + tt
                    S.dma(out_d[s, t * 128:(t + 1) * 128, :], xacc[:, tt, :], reads=[xab[tt]], writes=[Buf()], is_output=True)
        S.emit()
    return nc


def _prep_common(inp):
    f = lambda a: np.ascontiguousarray(np.asarray(a, dtype=np.float32))
    gn = np.concatenate([inp["g_norm1"][0].reshape(8, 128).T, inp["g_norm2"][0].reshape(8, 128).T], axis=1)
    gvec = np.concatenate([inp[k][0] for k in ("g_q_a", "g_kc_a", "g_ks_a", "g_kw_a", "g_q_b", "g_k_b")])[None, :]
    peT = np.concatenate([inp["pe_ck"][0].T, inp["pe_cv"][0].T], axis=1)
    return {
        "w_ada": f(inp["w_ada"][0]), "b_ada": f(inp["b_ada"]), "gn": f(gn), "w_in": f(inp["w_in"][0]),
        "gvec": f(gvec), "peT": f(peT), "w_ck1": f(inp["w_ck1"][0]), "w_ck2": f(inp["w_ck2"][0]),
        "w_cv1": f(inp["w_cv1"][0]), "w_cv2": f(inp["w_cv2"][0]), "w_o_a": f(inp["w_o_a"][0]),
        "w_o_b": f(inp["w_o_b"][0]), "w_out": f(inp["w_out"][0]), "w_ff_gate": f(inp["w_ff_gate"][0]),
        "w_ff_up": f(inp["w_ff_up"][0]), "w_ff_down": f(inp["w_ff_down"][0]),
    }


def _core_map(common, x, c, i, nseq):
    m = dict(common)
    m["x"] = np.ascontiguousarray(x[i * nseq:(i + 1) * nseq])
    cc = np.zeros((4, D), np.float32)
    cc[:nseq] = c[i * nseq:(i + 1) * nseq]
    m["cT"] = np.ascontiguousarray(cc.T.reshape(8, 128, 4).transpose(1, 0, 2))
    return m


def kernel(**inputs):
    x = np.asarray(inputs["x"], dtype=np.float32)
    c = np.asarray(inputs["c"], dtype=np.float32)
    n = 8
    nseq = x.shape[0] // n
    nc = build_nc(nseq)
    common = _prep_common(inputs)
    in_maps = [_core_map(common, x, c, i, nseq) for i in range(n)]
    res = run_bass_kernel_spmd(nc, in_maps, core_ids=list(range(n)))
    return np.concatenate([r["out"] for r in res.results], axis=0).astype(np.float32)
```

```python
import contextlib
import numpy as np
import concourse.bass as bass
import concourse.mybir as mybir
from concourse.bass_utils import run_bass_kernel_spmd

F32 = mybir.dt.float32
BF16 = mybir.dt.bfloat16
AF = mybir.ActivationFunctionType
ALU = mybir.AluOpType
AX = mybir.AxisListType

S_TOK = 2048
D = 1024
NT = 16
DIN = 4576
DFF = 2816
EPS = 1e-6
NBIS = 22
NEG = -30000.0


class Buf:
    __slots__ = ("name", "w", "r")

    def __init__(self, name=""):
        self.name = name
        self.w = None
        self.r = {}


class Sched:
    ENGS = ("pe", "act", "dve", "pool", "sp")
    NDMA = 24

    def __init__(self, nc):
        self.nc = nc
        self.streams = {e: [] for e in self.ENGS}
        self.count = {e: 0 for e in self.ENGS}
        self.waited = {e: {} for e in self.ENGS}
        self.dma_uses = [0] * self.NDMA
        self.dma_rr = 0
        self.out_events = []

    def _deps(self, eng, reads, writes):
        deps = {}

        def add(ev):
            if ev is None:
                return
            k, v = ev
            if deps.get(k, 0) < v:
                deps[k] = v
        for b in reads:
            add(b.w)
        for b in writes:
            if b.w is not None and b.w[0] != eng:
                add(b.w)
            for k, v in b.r.items():
                if k != eng:
                    add((k, v))
        waits = []
        for k, v in deps.items():
            if k == "pe" and eng == "pe":
                continue
            if self.waited[eng].get(k, 0) < v:
                self.waited[eng][k] = v
                waits.append((k, v))
        return waits

    def _commit(self, ev, reads, writes):
        k, v = ev
        for b in writes:
            b.w = ev
            b.r = {}
        for b in reads:
            if b.r.get(k, 0) < v:
                b.r[k] = v

    def op(self, eng, fn, reads=(), writes=()):
        waits = self._deps(eng, reads, writes)
        self.count[eng] += 1
        ev = (eng, self.count[eng])
        self.streams[eng].append((fn, waits, ev))
        self._commit(ev, reads, writes)
        return ev

    def dma(self, out, in_, reads=(), writes=(), q="sp", is_output=False, **kw):
        i = self.dma_rr
        self.dma_rr = (self.dma_rr + 1) % self.NDMA
        waits = self._deps(q, reads, writes)
        key = "dma%d" % i
        prev = self.dma_uses[i] * 16
        if prev and self.waited[q].get(key, 0) < prev:
            self.waited[q][key] = prev
            waits.append((key, prev))
        self.dma_uses[i] += 1
        ev = (key, self.dma_uses[i] * 16)
        fn = lambda e, out=out, in_=in_, kw=kw: e.dma_start(out=out, in_=in_, **kw)
        self.streams[q].append((fn, waits, ev))
        self._commit(ev, reads, writes)
        if is_output:
            self.out_events.append(ev)
        return ev

    def barrier(self):
        allv = [(e, self.count[e]) for e in self.ENGS if self.count[e]]
        allv += [("dma%d" % i, self.dma_uses[i] * 16) for i in range(self.NDMA) if self.dma_uses[i]]
        for e in self.ENGS:
            waits = []
            for k, v in allv:
                if k == e:
                    continue
                if self.waited[e].get(k, 0) < v:
                    self.waited[e][k] = v
                    waits.append((k, v))
            if waits:
                self.streams[e].append((None, waits, None))

    def emit(self):
        nc = self.nc
        with contextlib.ExitStack() as st:
            sems = {}
            for e in self.ENGS:
                sems[e] = st.enter_context(nc.semaphore("s_" + e))
            for i in range(self.NDMA):
                sems["dma%d" % i] = st.enter_context(nc.semaphore("s_dma%d" % i))
            final = {}
            for k, v in self.out_events:
                final[k] = max(final.get(k, 0), v)
            block = st.enter_context(nc.Block())

            def run(engname, e):
                for fn, waits, ev in self.streams[engname]:
                    for k, v in waits:
                        e.wait_ge(sems[k], v)
                    if fn is None:
                        continue
                    ins = fn(e)
                    k, v = ev
                    ins.then_inc(sems[k], 16 if k.startswith("dma") else 1)
                if engname == "sp":
                    for k, v in final.items():
                        e.wait_ge(sems[k], v)

            @block.tensor
            def _(e):
                run("pe", e)

            @block.scalar
            def _(e):
                run("act", e)

            @block.vector
            def _(e):
                run("dve", e)

            @block.gpsimd
            def _(e):
                run("pool", e)

            @block.sync
            def _(e):
                run("sp", e)


class Arena:
    def __init__(self, ap16):
        self.ap = ap16
        self.off = 0

    def reset(self):
        self.off = 0

    def take(self, ncols, dt=BF16):
        if dt == F32:
            self.off = (self.off + 1) // 2 * 2
            n16 = ncols * 2
        else:
            n16 = ncols
        assert self.off + n16 <= self.ap.shape[1], (self.off, n16, self.ap.shape)
        v = self.ap[:, self.off:self.off + n16]
        self.off += n16
        self.off = (self.off + 1) // 2 * 2
        return v.bitcast(F32) if dt == F32 else v


_REGS = {}


def _I(name, *args, **kw):
    if name == "affine_select":
        def thunk(e):
            a = list(args)
            key = (id(e), float(a[4]))
            if key not in _REGS:
                _REGS[key] = e.to_reg(float(a[4]))
            a[4] = _REGS[key]
            return e.affine_select(*a, **kw)
        return thunk
    return lambda e: getattr(e, name)(*args, **kw)


def build_nc(nseq=4, dbg=False):
    nc = bass.Bass("TRN2", target_bir_lowering=False)
    _REGS.clear()
    S = Sched(nc)

    def din(name, shape):
        return nc.dram_tensor(name, shape, F32, kind="ExternalInput").ap()
    x_d = din("x", [nseq, S_TOK, D])
    cT_d = din("cT", [128, 8, 4])
    wada_d = din("w_ada", [D, 6 * D])
    bada_d = din("b_ada", [1, 6 * D])
    gn_d = din("gn", [128, 16])
    win_d = din("w_in", [D, DIN])
    gv_d = din("gvec", [1, 6 * 64])
    peT_d = din("peT", [64, 64])
    wck1_d = din("w_ck1", [2048, 128])
    wck2_d = din("w_ck2", [128, 64])
    wcv1_d = din("w_cv1", [2048, 128])
    wcv2_d = din("w_cv2", [128, 64])
    woa_d = din("w_o_a", [512, D])
    wob_d = din("w_o_b", [512, D])
    wout_d = din("w_out", [D, D])
    wfg_d = din("w_ff_gate", [D, DFF])
    wfu_d = din("w_ff_up", [D, DFF])
    wfd_d = din("w_ff_down", [DFF, D])
    out_d = nc.dram_tensor("out", [nseq, S_TOK, D], F32, kind="ExternalOutput").ap()
    modrow_d = nc.dram_tensor("modrow", [4, 6 * D], F32, kind="Internal").ap()
    oT_d = nc.dram_tensor("oT_scr", [2, 4, 128, S_TOK], BF16, kind="ExternalOutput" if dbg else "Internal").ap()
    if dbg:
        hT_dbg = nc.dram_tensor("hT_dbg", [128, 8, S_TOK], BF16, kind="ExternalOutput").ap()
        x1_dbg = nc.dram_tensor("x1_dbg", [S_TOK, D], F32, kind="ExternalOutput").ap()
        mod_dbg = nc.dram_tensor("mod_dbg", [4, 6 * D], F32, kind="ExternalOutput").ap()

    st = contextlib.ExitStack()

    def sb(name, shape, dt=F32):
        return st.enter_context(nc.sbuf_tensor(name, shape, dt))

    with st:
        PS = [st.enter_context(nc.psum_tensor("ps%d" % i, [128, 512], F32)) for i in range(8)]
        PB = [Buf("ps%d" % i) for i in range(8)]

        def psb(i):
            return PS[i][:].bitcast(BF16)

        ident_b = sb("ident_b", [128, 128], BF16)
        ident_f = sb("ident_f", [128, 128], F32)
        Cm = sb("Cm", [128, 128], BF16)
        Wm = sb("Wm", [128, 128], BF16)
        cmask = sb("cmask", [128, S_TOK], BF16)
        ET = sb("ET", [32, S_TOK], BF16)
        selA = sb("selA", [128, NT, 32], F32)
        selB = sb("selB", [128, NT, 32], F32)
        aqc = sb("aqc", [128, NT, 8, 4], BF16)
        akc = sb("akc", [128, NT, 4], BF16)
        gbc = sb("gbc", [128, 6 * 64], F32)
        gains = sb("gains", [128, 4, 64], F32)
        gnc = sb("gnc", [128, 16], F32)
        modT = sb("modT", [128, 32, 4], F32)
        cb2 = sb("cb2", [128, 2], F32)
        pow2 = sb("pow2", [128, NBIS + 1], F32)
        hT = sb("hT", [128, 8, S_TOK], BF16)
        Vc = sb("Vc", [128, 2, 97], BF16)
        kc_aug = sb("kc_aug", [128, 2, 68], BF16)
        KcT = sb("KcT", [128, 2, 128], BF16)
        U_t = sb("U", [128, 65400], BF16)
        U = Arena(U_t[:])
        bC = Buf("consts")
        bOT = [Buf(), Buf()]
        bOut = Buf()

        hTb = [Buf("hT%d" % c) for c in range(4)]

        def pool(fn, reads=(), writes=()):
            return S.op("pool", fn, reads, writes)

        def dve(fn, reads=(), writes=()):
            return S.op("dve", fn, reads, writes)

        def act(fn, reads=(), writes=()):
            return S.op("act", fn, reads, writes)

        def pe(fn, reads=(), writes=()):
            return S.op("pe", fn, reads, writes)

        pool(_I("memset", ident_b[:], 1.0), writes=[bC])
        pool(_I("affine_select", ident_b[:], ident_b[:], [[1, 128]], ALU.is_equal, 0.0, base=0, channel_multiplier=-1), writes=[bC])
        pool(_I("memset", ident_f[:], 1.0), writes=[bC])
        pool(_I("affine_select", ident_f[:], ident_f[:], [[1, 128]], ALU.is_equal, 0.0, base=0, channel_multiplier=-1), writes=[bC])
        pool(_I("memset", Cm[:], 1.0), writes=[bC])
        pool(_I("affine_select", Cm[:], Cm[:], [[1, 128]], ALU.is_ge, 0.0, base=0, channel_multiplier=-1), writes=[bC])
        pool(_I("memset", Wm[:], 1.0), writes=[bC])
        pool(_I("affine_select", Wm[:], Wm[:], [[-1, 128]], ALU.is_gt, 0.0, base=0, channel_multiplier=1), writes=[bC])
        pool(_I("memset", cmask[:], 1.0), writes=[bC])
        pool(_I("affine_select", cmask[:], cmask[:], [[1, S_TOK]], ALU.is_ge, 0.0, base=-31, channel_multiplier=-16), writes=[bC])
        pool(_I("memset", ET[:], 1.0), writes=[bC])
        pool(_I("affine_select", ET[:], ET[:], [[1, S_TOK]], ALU.is_ge, 0.0, base=0, channel_multiplier=-64), writes=[bC])
        pool(_I("affine_select", ET[:], ET[:], [[-1, S_TOK]], ALU.is_ge, 0.0, base=63, channel_multiplier=64), writes=[bC])
        for g in range(2):
            pool(_I("memset", Vc[:, g, 64:97], 1.0), writes=[bC])
            pool(_I("affine_select", Vc[:, g, 65:97], Vc[:, g, 65:97], [[-64, 32]], ALU.is_ge, 0.0, base=31, channel_multiplier=16), writes=[bC])
            pool(_I("affine_select", Vc[:, g, 65:97], Vc[:, g, 65:97], [[64, 32]], ALU.is_ge, 0.0, base=63, channel_multiplier=-16), writes=[bC])
        Dt = U.take(NT * 32, F32).rearrange("p (t j) -> p t j", j=32)
        jt = U.take(NT * 32, F32).rearrange("p (t j) -> p t j", j=32)
        f0 = U.take(NT * 32, F32).rearrange("p (t j) -> p t j", j=32)
        for lo_, base in ((0, 0), (64, -1)):
            pool(_I("iota", Dt[lo_:lo_ + 64], [[-2, NT], [1, 32]], base=base, channel_multiplier=0, allow_small_or_imprecise_dtypes=True), writes=[bC])
        pool(_I("iota", jt[:], [[0, NT], [1, 32]], base=0, channel_multiplier=0, allow_small_or_imprecise_dtypes=True), writes=[bC])
        dve(_I("tensor_single_scalar", f0[:], jt[:], 0.0, ALU.is_equal), reads=[bC], writes=[bC])
        dve(_I("tensor_single_scalar", jt[:], Dt[:], 0.0, ALU.is_equal), reads=[bC], writes=[bC])
        dve(_I("tensor_max", f0[:], f0[:], jt[:]), reads=[bC], writes=[bC])
        dve(_I("tensor_single_scalar", jt[:], Dt[:], -1.0, ALU.is_equal), reads=[bC], writes=[bC])
        dve(_I("tensor_max", f0[:], f0[:], jt[:]), reads=[bC], writes=[bC])
        dve(_I("tensor_single_scalar", jt[:], Dt[:], 0.0, ALU.is_le), reads=[bC], writes=[bC])
        dve(_I("tensor_sub", selA[:], jt[:], f0[:]), reads=[bC], writes=[bC])
        dve(_I("tensor_add", selB[:], jt[:], f0[:]), reads=[bC], writes=[bC])
        dve(_I("tensor_scalar", selB[:], selB[:], -1.0, 1e9, ALU.add, ALU.mult), reads=[bC], writes=[bC])
        hi_t = sb("hi_t", [128, NT], F32)
        lo_t = sb("lo_t", [128, 1], F32)
        for lo_, base in ((0, 0), (64, 64)):
            pool(_I("iota", hi_t[lo_:lo_ + 64], [[128, NT]], base=base, channel_multiplier=0, allow_small_or_imprecise_dtypes=True), writes=[bC])
            pool(_I("iota", lo_t[lo_:lo_ + 64], [[0, 1]], base=0, channel_multiplier=1, allow_small_or_imprecise_dtypes=True), writes=[bC])
        for h in range(8):
            sl = 2.0 ** -(h + 1)
            dve(_I("memset", aqc[:, :, h, 0:2], sl), reads=[bC], writes=[bC])
            dve(_I("tensor_scalar", aqc[:, :, h, 2], hi_t[:], -sl, None, ALU.mult), reads=[bC], writes=[bC])
            dve(_I("tensor_scalar", aqc[:, :, h, 3], lo_t[:].to_broadcast([128, NT]), -sl, None, ALU.mult), reads=[bC], writes=[bC])
        dve(_I("memset", akc[:, :, 2:4], 1.0), reads=[bC], writes=[bC])
        dve(_I("tensor_copy", akc[:, :, 0], hi_t[:]), reads=[bC], writes=[bC])
        dve(_I("tensor_copy", akc[:, :, 1], lo_t[:].to_broadcast([128, NT])), reads=[bC], writes=[bC])
        pn = sb("pn", [128, 1], F32)
        pool(_I("iota", pn[:], [[0, 1]], base=0, channel_multiplier=16, allow_small_or_imprecise_dtypes=True), writes=[bC])
        for g in range(2):
            dve(_I("tensor_copy", kc_aug[:, g, 64:65], pn[:]), reads=[bC], writes=[bC])
            dve(_I("memset", kc_aug[:, g, 65:66], 31.0), reads=[bC], writes=[bC])
            dve(_I("memset", kc_aug[:, g, 66:68], 1.0), reads=[bC], writes=[bC])
        for j in range(NBIS + 1):
            dve(_I("memset", pow2[:, j:j + 1], 2.0 ** -j), reads=[bC], writes=[bC])
        S.dma(gbc[:], gv_d.partition_broadcast(128), writes=[bC])
        S.dma(gnc[:], gn_d, writes=[bC])

        def gsl(i):
            return gbc[:, i * 64:(i + 1) * 64]
        for idx, (gk, gq) in enumerate(((2, 0), (3, 0), (1, 0), (5, 4))):
            dve(_I("scalar_tensor_tensor", gains[:, idx, :], gsl(gk), 0.125, gsl(gq), ALU.mult, ALU.mult), reads=[bC], writes=[bC])

        S.barrier()
        U.reset()
        scT = U.take(32, F32).rearrange("p (k b) -> p k b", b=4)
        wchunk = U.take(8 * 512, F32).rearrange("p (k n) -> p k n", k=8)
        bchunk = U.take(512, F32)
        mchunk = U.take(512, F32)
        bsc, bw, bbc, bm = Buf(), Buf(), Buf(), Buf()
        S.dma(scT, cT_d, writes=[bsc])
        act(_I("activation", scT, scT, AF.Silu), reads=[bsc], writes=[bsc])
        wada_v = wada_d.rearrange("(k p) n -> p k n", p=128)
        LNV = {0: 0, 1: 1, 3: 2, 4: 3}
        for c in range(12):
            S.dma(wchunk, wada_v[:, :, c * 512:(c + 1) * 512], writes=[bw])
            S.dma(bchunk[0:4, :], bada_d[:, c * 512:(c + 1) * 512].partition_broadcast(4), writes=[bbc])
            for k in range(8):
                pe(_I("matmul", PS[0][0:4, :], lhsT=scT[:, k, :], rhs=wchunk[:, k, :], start=(k == 0), stop=(k == 7)), reads=[bsc, bw], writes=[PB[0]])
            dve(_I("tensor_tensor", mchunk[0:4, :], PS[0][0:4, :], bchunk[0:4, :], ALU.add), reads=[PB[0], bbc], writes=[bm])
            S.dma(modrow_d[:, c * 512:(c + 1) * 512], mchunk[0:4, :], reads=[bm], writes=[bC])
            if dbg:
                S.dma(mod_dbg[:, c * 512:(c + 1) * 512], mchunk[0:4, :], reads=[bm], writes=[Buf()], is_output=True)
            vec, half = c // 2, c % 2
            if vec in LNV:
                for i in range(4):
                    col = (LNV[vec] * 8 + half * 4 + i) * 4
                    pe(_I("transpose", PS[1][:, col:col + 4], mchunk[0:4, i * 128:(i + 1) * 128], ident_f[0:4, 0:4]), reads=[bm, bC], writes=[PB[1]])
        dve(_I("tensor_copy", modT[:].rearrange("p a b -> p (a b)"), PS[1][:, 0:128]), reads=[PB[1]], writes=[bC])
        for which, gi in ((1, 0), (3, 1)):
            dve(_I("scalar_tensor_tensor",
                modT[:, which * 8:(which + 1) * 8, :], modT[:, which * 8:(which + 1) * 8, :], 1.0,
                gnc[:, gi * 8:(gi + 1) * 8].unsqueeze(2).to_broadcast([128, 8, 4]), ALU.add, ALU.mult), reads=[bC], writes=[bC])
        S.barrier()

        xt = [sb("xt%d" % i, [128, D], F32) for i in range(2)]
        xtb = [Buf() for _ in range(2)]
        xn = sb("xn", [128, D], BF16)
        xnb = Buf()
        sq = [sb("sq%d" % i, [128, 512], F32) for i in range(2)]
        sqb = [Buf(), Buf()]
        st16 = sb("st16", [128, 16], F32)
        stb = Buf()
        PT = [sb("PT%d" % i, [128, 512], BF16) for i in range(6)]
        PTb = [Buf() for _ in range(6)]
        sn = sb("sn", [128, 16], F32)
        snb = Buf()
        tmpo = sb("tmpo", [128, 4, 64], F32)
        tmpb = Buf()
        oacc = sb("oacc", [128, 8, 64], F32)
        oab = Buf()
        obf = sb("obf", [128, 512], BF16)
        obfb = Buf()
        oTs = sb("oTs", [128, 4, 128], BF16)
        oTsb = Buf()
        sm = sb("sm", [128, 64], F32)
        smb = Buf()
        state = {"pt": 0, "sb": 0}
        vstate = {}

        def layernorm(src_tile, src_buf, tl, which, b, psbank):
            act(_I("activation", sq[0][:, :].bitcast(BF16), src_tile, AF.Square, accum_out=st16[:, 0:1]), reads=[src_buf], writes=[sqb[0], stb])
            act(_I("activation", st16[:, 1:2], st16[:, 0:1], AF.Sqrt, bias=EPS, scale=1.0 / D), reads=[stb], writes=[stb])
            dve(_I("reciprocal", st16[:, 2:3], st16[:, 1:2]), reads=[stb], writes=[stb])
            dve(_I("tensor_scalar", xn[:], src_tile, st16[:, 2:3], None, ALU.mult), reads=[src_buf, stb], writes=[xnb])
            for j in range(8):
                bk = psbank + j // 4
                o0 = ((j % 4) * 2 + tl) * 128
                pe(_I("transpose", psb(bk)[:, o0:o0 + 128], xn[:, j * 128:(j + 1) * 128], ident_b[:]), reads=[xnb, bC], writes=[PB[bk]])

        def ln_evac(c2, which, b, psbank, hbuf):
            for j in range(8):
                bk = psbank + j // 4
                src = psb(bk)[:, (j % 4) * 256:(j % 4) * 256 + 256]
                Gc = modT[:, (2 * which + 1) * 8 + j, b:b + 1]
                Sc = modT[:, (2 * which) * 8 + j, b:b + 1]
                dve(_I("tensor_scalar", hT[:, j, c2 * 256:(c2 + 1) * 256], src, Gc, Sc, ALU.mult, ALU.add),
                    reads=[PB[bk], bC], writes=[hbuf])

        def load_w(dst, src, buf, q="pool"):
            S.dma(dst, src, writes=[buf], q=q)

        win_v = win_d.rearrange("(k p) n -> p k n", p=128)

        def rms_heads(psbank, nh, dst_stats):
            i = state["sb"] = (state["sb"] + 1) % 2
            act(_I("activation", sq[i][:, 0:nh * 64], PS[psbank][:, 0:nh * 64], AF.Square), reads=[PB[psbank]], writes=[sqb[i]])
            dve(_I("tensor_reduce", dst_stats, sq[i][:, 0:nh * 64].rearrange("p (h d) -> p h d", d=64), AX.X, ALU.add), reads=[sqb[i]], writes=[stb])

        def rstd_from(stats):
            act(_I("activation", stats, stats, AF.Sqrt, bias=EPS, scale=1.0 / 64), reads=[stb], writes=[stb])
            dve(_I("reciprocal", stats, stats), reads=[stb], writes=[stb])

        def make_units(QTh, qb, KTk, kb, kind, t, tiles, maskmm, pvb, slot, full_mask=None, banks=(4, 5)):
            ntl = len(tiles)
            return [dict(QTh=QTh, qb=qb, KTk=KTk, kb=kb, kind=kind, t=t, grp=tiles[g0:g0 + 4], g0=g0, ntl=ntl, maskmm=maskmm,
                         pvb=pvb, slot=slot, full_mask=full_mask, banks=banks) for g0 in range(0, ntl, 4)]

        def emit_A(u):
            qs = slice(u["t"] * 128, (u["t"] + 1) * 128)
            banks = u["banks"]
            bk = banks[state["pt"] % len(banks)]
            pi = state["pt"] % len(PT)
            state["pt"] += 1
            u["pi"] = pi
            grp = u["grp"]
            for i, (j, mt) in enumerate(grp):
                ks = slice(j * 128, (j + 1) * 128)
                pe(_I("matmul", PS[bk][:, i * 128:(i + 1) * 128], lhsT=u["KTk"][0:68, ks], rhs=u["QTh"][0:68, qs], start=True, stop=(u["maskmm"] is None)),
                   reads=[u["kb"], u["qb"]], writes=[PB[bk]])
                if u["maskmm"] is not None:
                    MTg, mb = u["maskmm"]
                    pe(_I("matmul", PS[bk][:, i * 128:(i + 1) * 128], lhsT=ET[0:32, ks], rhs=MTg[0:32, qs], start=False, stop=True),
                       reads=[mb, bC], writes=[PB[bk]])
            n = len(grp) * 128
            act(_I("activation", PT[pi][:, 0:n], PS[bk][:, 0:n], AF.Exp), reads=[PB[bk]], writes=[PTb[pi]])
            if u["full_mask"] is not None:
                mT, mTb = u["full_mask"]
                j0 = grp[0][0]
                pool(_I("tensor_tensor", PT[pi][:, 0:n], PT[pi][:, 0:n], mT[:, j0:j0 + len(grp), :].rearrange("p j c -> p (j c)"), ALU.mult),
                     reads=[mTb], writes=[PTb[pi]])
            for i, (j, mt) in enumerate(grp):
                if mt is not None:
                    mk = Cm if mt == "C" else Wm
                    pool(_I("tensor_tensor", PT[pi][:, i * 128:(i + 1) * 128], PT[pi][:, i * 128:(i + 1) * 128], mk[:], ALU.mult),
                         reads=[bC], writes=[PTb[pi]])

        def emit_B(u):
            pi = u["pi"]
            pvb = u["pvb"]
            po = PS[pvb][:, u["slot"] * 65:(u["slot"] + 1) * 65]
            for i, (j, mt) in enumerate(u["grp"]):
                gi = u["g0"] + i
                pe(_I("matmul", po, lhsT=PT[pi][:, i * 128:(i + 1) * 128], rhs=vstate["V"][:, j, u["kind"], :], start=(gi == 0), stop=(gi == u["ntl"] - 1)),
                   reads=[PTb[pi], vstate["bV"]], writes=[PB[pvb]])

        def run_units(units, L, between=None):
            n = len(units)
            for i in range(min(L, n)):
                emit_A(units[i])
            for i in range(n):
                if i + L < n:
                    emit_A(units[i + L])
                emit_B(units[i])
                if units[i].get("post") is not None:
                    units[i]["post"]()
                if between is not None:
                    between(i, n)

        def next_pv():
            state["pv"] = state.get("pv", 0) + 1
            return 6 if state["pv"] % 2 == 0 else 2

        def norm4(pvb, g, gate_view, gate_bufs, first):
            o4 = PS[pvb][:, 0:260].rearrange("p (h c) -> p h c", h=4)
            dve(_I("tensor_scalar", sn[:, 0:4], o4[:, :, 64], 1e-30, None, ALU.max), reads=[PB[pvb]], writes=[snb])
            dve(_I("reciprocal", sn[:, 4:8], sn[:, 0:4]), reads=[snb], writes=[snb])
            if gate_view is not None:
                dve(_I("tensor_tensor", sn[:, 4:8], sn[:, 4:8], gate_view, ALU.mult), reads=[snb] + gate_bufs, writes=[snb])
            wb = sn[:, 4:8].unsqueeze(2).to_broadcast([128, 4, 64])
            if first:
                dve(_I("tensor_tensor", oacc[:, 4 * g:4 * g + 4, :], o4[:, :, 0:64], wb, ALU.mult), reads=[PB[pvb], snb], writes=[oab])
            else:
                dve(_I("tensor_tensor", tmpo[:], o4[:, :, 0:64], wb, ALU.mult), reads=[PB[pvb], snb], writes=[tmpb])
                pool(_I("tensor_tensor", oacc[:, 4 * g:4 * g + 4, :], oacc[:, 4 * g:4 * g + 4, :], tmpo[:], ALU.add), reads=[tmpb, oab], writes=[oab])

        def flush_o(t, mix):
            dve(_I("tensor_copy", obf[:], oacc[:].rearrange("p h d -> p (h d)")), reads=[oab], writes=[obfb])
            for j in range(4):
                pe(_I("transpose", psb(3)[:, j * 128:(j + 1) * 128], obf[:, j * 128:(j + 1) * 128], ident_b[:]), reads=[obfb, bC], writes=[PB[3]])
            act(_I("activation", oTs[:].rearrange("p j c -> p (j c)"), psb(3)[:, 0:512], AF.Copy), reads=[PB[3]], writes=[oTsb])
            S.dma(oT_d[mix, :, :, t * 128:(t + 1) * 128].rearrange("j p c -> p j c"), oTs[:], reads=[oTsb], writes=[bOT[mix]])

        for s in range(nseq):
            b = s
            S.barrier()

            for c2 in range(8):
                for tl in range(2):
                    t = c2 * 2 + tl
                    i = t % 2
                    S.dma(xt[i][:], x_d[s, t * 128:(t + 1) * 128, :], writes=[xtb[i]])
                    layernorm(xt[i][:], xtb[i], tl, 0, b, 0)
                ln_evac(c2, 0, b, 0, hTb[c2 // 2])

            if dbg and s == 0:
                S.dma(hT_dbg, hT[:], reads=hTb, writes=[Buf()], is_output=True)
            U.reset()
            V_all = U.take(NT * 5 * 65).rearrange("p (t k c) -> p t k c", t=NT, k=5)
            bV = Buf()
            pool(_I("memset", V_all[:, :, :, 64:65], 1.0), writes=[bV])
            vstate["V"] = V_all
            vstate["bV"] = bV
            Wn = U.take(8 * 1304).rearrange("p (k n) -> p k n", k=8)
            QT = U.take(8 * S_TOK).rearrange("p (h n) -> p h n", h=8)
            KT = U.take(5 * S_TOK).rearrange("p (h n) -> p h n", h=5)
            q_aug = U.take(8 * 68).rearrange("p (h d) -> p h d", h=8)
            k_aug = U.take(4 * 68).rearrange("p (h d) -> p h d", h=4)
            kcT = U.take(S_TOK)
            vcT = U.take(S_TOK)
            MT = U.take(2 * S_TOK).rearrange("p (g n) -> p g n", g=2)
            sg = U.take(NT * 24, F32).rearrange("p (t c) -> p t c", c=24)
            bq_aug, bk_aug, bkc, bsg = Buf(), Buf(), Buf(), Buf()
            bWn = [Buf() for _ in range(8)]
            QTb = [Buf() for _ in range(8)]
            KTb = [Buf() for _ in range(5)]
            MTb = [Buf(), Buf()]
            for k in range(8):
                load_w(Wn[:, k, :], win_v[:, k, 0:1304], bWn[k])
            for c in range(4):
                for tl in range(4):
                    t = c * 4 + tl
                    ts = slice(t * 128, (t + 1) * 128)
                    for k in range(8):
                        pe(_I("matmul", PS[0][:, :], lhsT=hT[:, k, ts], rhs=Wn[:, k, 0:512], start=(k == 0), stop=(k == 7)), reads=[hTb[c], bWn[k]], writes=[PB[0]])
                    for k in range(8):
                        pe(_I("matmul", PS[1][:, :], lhsT=hT[:, k, ts], rhs=Wn[:, k, 768:1280], start=(k == 0), stop=(k == 7)), reads=[hTb[c], bWn[k]], writes=[PB[1]])
                    for k in range(8):
                        pe(_I("matmul", PS[2][:, 0:24], lhsT=hT[:, k, ts], rhs=Wn[:, k, 1280:1304], start=(k == 0), stop=(k == 7)), reads=[hTb[c], bWn[k]], writes=[PB[2]])
                    rms_heads(0, 8, st16[:, 0:8])
                    rms_heads(1, 8, st16[:, 8:16])
                    rstd_from(st16[:, 0:16])
                    dve(_I("tensor_tensor", q_aug[:, :, 0:64], PS[0][:, :].rearrange("p (h d) -> p h d", d=64), st16[:, 0:8].unsqueeze(2).to_broadcast([128, 8, 64]), ALU.mult),
                        reads=[PB[0], stb], writes=[bq_aug])
                    dve(_I("tensor_copy", q_aug[:, :, 64:68], aqc[:, t, :, :]), reads=[bC], writes=[bq_aug])
                    for (c0, s0, kk, gi) in ((0, 8, 0, 0), (256, 12, 2, 1)):
                        dve(_I("tensor_tensor", sq[0][:, 0:128].rearrange("p (h d) -> p h d", d=64), PS[1][:, c0:c0 + 128].rearrange("p (h d) -> p h d", d=64),
                                                                   st16[:, s0:s0 + 2].unsqueeze(2).to_broadcast([128, 2, 64]), ALU.mult), reads=[PB[1], stb], writes=[sqb[0]])
                        dve(_I("tensor_tensor", k_aug[:, kk:kk + 2, 0:64], sq[0][:, 0:128].rearrange("p (h d) -> p h d", d=64),
                                                                   gains[:, gi, :].unsqueeze(1).to_broadcast([128, 2, 64]), ALU.mult), reads=[sqb[0], bC], writes=[bk_aug])
                    dve(_I("tensor_copy", k_aug[:, :, 64:68], akc[:, t, :].unsqueeze(1).to_broadcast([128, 4, 4])), reads=[bC], writes=[bk_aug])
                    act(_I("activation", V_all[:, t, 0:2, 0:64], PS[1][:, 128:256].rearrange("p (h d) -> p h d", d=64), AF.Copy), reads=[PB[1]], writes=[bV])
                    act(_I("activation", V_all[:, t, 2:4, 0:64], PS[1][:, 384:512].rearrange("p (h d) -> p h d", d=64), AF.Copy), reads=[PB[1]], writes=[bV])
                    act(_I("activation", sg[:, t, :], PS[2][:, 0:24], AF.Sigmoid), reads=[PB[2]], writes=[bsg])
                    for h in range(8):
                        pe(_I("transpose", psb(3)[0:68, h * 128:(h + 1) * 128], q_aug[:, h, :], ident_b[:]), reads=[bq_aug, bC], writes=[PB[3]])
                    for kk in range(4):
                        pe(_I("transpose", psb(7)[0:68, kk * 128:(kk + 1) * 128], k_aug[:, kk, :], ident_b[:]), reads=[bk_aug, bC], writes=[PB[7]])
                    act(_I("activation", QT[0:68, :, ts], psb(3)[0:68, :].rearrange("p (h c) -> p h c", h=8), AF.Copy), reads=[PB[3]], writes=QTb)
                    dve(_I("tensor_copy", KT[0:68, 0:4, ts], psb(7)[0:68, 0:512].rearrange("p (h c) -> p h c", h=4)), reads=[PB[7]], writes=KTb[0:4])
                cs = slice(c * 512, (c + 1) * 512)
                for (c0, dst) in ((512, kcT), (640, vcT)):
                    for k in range(8):
                        pe(_I("matmul", PS[0][:, :], lhsT=Wn[:, k, c0:c0 + 128], rhs=hT[:, k, cs], start=(k == 0), stop=(k == 7)), reads=[hTb[c], bWn[k]], writes=[PB[0]])
                    act(_I("activation", dst[:, cs], PS[0][:, :], AF.Copy), reads=[PB[0]], writes=[bkc])

            S.barrier()
            W1 = Wn.rearrange("p k n -> p (k n)")[:, 0:2 * 32 * 128].rearrange("p (a l n) -> p a l n", a=2, l=32)
            W2 = U.take(2 * 64).rearrange("p (a n) -> p a n", a=2)
            peT = U.take(64)
            HT = U.take(128)
            bW1, bH = Buf(), Buf()
            for a, (w1d, w2d) in enumerate(((wck1_d, wck2_d), (wcv1_d, wcv2_d))):
                for half in range(2):
                    load_w(W1[half * 64:half * 64 + 64, a, :, :], w1d.rearrange("(l d) n -> d l n", d=64), bW1)
                load_w(W2[:, a, :], w2d, bW1)
            load_w(peT[0:64, :], peT_d, bW1)
            if s == 0:
                for a in range(2):
                    for l in range(32):
                        pe(_I("matmul", PS[2][:, a:a + 1], lhsT=W1[0:64, a, l, :], rhs=peT[0:64, a * 32 + l:a * 32 + l + 1], start=(l == 0), stop=(l == 31)),
                           reads=[bW1], writes=[PB[2]])
                dve(_I("tensor_copy", cb2[:], PS[2][:, 0:2]), reads=[PB[2]], writes=[bC])
            for a, srcT in enumerate((kcT, vcT)):
                for g in range(2):
                    base = g * 64
                    v3 = srcT[base:base + 64, :].rearrange("p (n s) -> p n s", s=16)
                    for l in range(32):
                        rhs = v3[:, (l // 16):(l // 16) + 127, l % 16]
                        pe(_I("matmul", PS[0][:, 0:127], lhsT=W1[base:base + 64, a, l, :], rhs=rhs, start=(l == 0), stop=(l == 31)),
                           reads=[bW1, bkc], writes=[PB[0]])
                    act(_I("activation", HT[:, 0:127], PS[0][:, 0:127], AF.Silu, bias=cb2[:, a:a + 1]), reads=[PB[0], bC], writes=[bH])
                    pe(_I("matmul", PS[1][0:127, 0:64], lhsT=HT[:, 0:127], rhs=W2[:, a, :], start=True, stop=True), reads=[bH, bW1], writes=[PB[1]])
                    if a == 0:
                        act(_I("activation", sq[0][0:127, 0:64], PS[1][0:127, 0:64], AF.Square, accum_out=st16[0:127, 0:1]), reads=[PB[1]], writes=[sqb[0], stb])
                        rstd_from(st16[0:127, 0:1])
                        dve(_I("tensor_scalar", sq[0][0:127, 0:64], PS[1][0:127, 0:64], st16[0:127, 0:1], None, ALU.mult), reads=[PB[1], stb], writes=[sqb[0]])
                        dve(_I("tensor_tensor", kc_aug[0:127, g, 0:64], sq[0][0:127, 0:64], gains[0:127, 2, :], ALU.mult), reads=[sqb[0], bC], writes=[bC])
                        pe(_I("transpose", psb(3)[0:68, 0:127], kc_aug[0:127, g, :], ident_b[0:127, 0:127]), reads=[bC], writes=[PB[3]])
                        dve(_I("tensor_copy", KcT[0:68, g, 0:127], psb(3)[0:68, 0:127]), reads=[PB[3]], writes=[bC])
                    else:
                        act(_I("activation", Vc[0:127, g, 0:64], PS[1][0:127, 0:64], AF.Copy), reads=[PB[1]], writes=[bC])

            imp = sb("imp_%d" % s, [128, 4, 32], F32) if s == 0 else imp
            impb = Buf()
            for t in range(NT):
                qs = slice(t * 128, (t + 1) * 128)
                for g in range(2):
                    for hh in range(4):
                        h = 4 * g + hh
                        pe(_I("matmul", PS[4][0:127, hh * 128:(hh + 1) * 128], lhsT=KcT[0:68, g, 0:127], rhs=QT[0:68, h, qs], start=True, stop=True),
                           reads=[bC, QTb[h]], writes=[PB[4]])
                    dve(_I("tensor_scalar", sq[1][0:127, :], PS[4][0:127, :], 60.0, None, ALU.min), reads=[PB[4]], writes=[sqb[1]])
                    act(_I("activation", PT[0][0:127, :], sq[1][0:127, :], AF.Exp), reads=[sqb[1]], writes=[PTb[0]])
                    dve(_I("tensor_tensor", PT[0][0:127, :].rearrange("p (h c) -> p h c", h=4), PT[0][0:127, :].rearrange("p (h c) -> p h c", h=4),
                                                  cmask[0:127, qs].unsqueeze(1).to_broadcast([127, 4, 128]), ALU.mult), reads=[bC], writes=[PTb[0]])
                    for hh in range(4):
                        pe(_I("matmul", PS[7][:, hh * 97:(hh + 1) * 97], lhsT=PT[0][0:127, hh * 128:(hh + 1) * 128], rhs=Vc[0:127, g, :], start=True, stop=True),
                           reads=[PTb[0], bC], writes=[PB[7]])
                    o4 = PS[7][:, 0:388].rearrange("p (h c) -> p h c", h=4)
                    dve(_I("tensor_scalar", sm[:, 0:4], o4[:, :, 64], 1e-30, None, ALU.max), reads=[PB[7]], writes=[smb])
                    dve(_I("reciprocal", sm[:, 4:8], sm[:, 0:4]), reads=[smb], writes=[smb])
                    dve(_I("tensor_tensor", sm[:, 8:12], sm[:, 4:8], sg[:, t, :].rearrange("p (h r) -> p h r", r=3)[:, 4 * g:4 * g + 4, 0], ALU.mult), reads=[smb, bsg], writes=[smb])
                    dve(_I("tensor_tensor", oacc[:, 4 * g:4 * g + 4, :], o4[:, :, 0:64], sm[:, 8:12].unsqueeze(2).to_broadcast([128, 4, 64]), ALU.mult),
                        reads=[PB[7], smb], writes=[oab])
                    dve(_I("tensor_tensor", imp[:], o4[:, :, 65:97], sm[:, 4:8].unsqueeze(2).to_broadcast([128, 4, 32]), ALU.mult), reads=[PB[7], smb], writes=[impb])
                    dve(_I("tensor_reduce", sm[:, 16:48], imp[:].rearrange("p h j -> p j h"), AX.X, ALU.add), reads=[impb], writes=[smb])
                    dve(_I("tensor_tensor", sm[:, 16:48], sm[:, 16:48], selA[:, t, :], ALU.mult), reads=[smb, bC], writes=[smb])
                    dve(_I("tensor_tensor", sm[:, 16:48], sm[:, 16:48], selB[:, t, :], ALU.add), reads=[smb, bC], writes=[smb])
                    dve(_I("max", out=sm[:, 48:56], in_=sm[:, 16:48]), reads=[smb], writes=[smb])
                    dve(_I("match_replace", out=imp[:, 0, :], in_to_replace=sm[:, 48:56], in_values=sm[:, 16:48], imm_value=-3e38), reads=[smb], writes=[impb])
                    dve(_I("max", out=sm[:, 56:64], in_=imp[:, 0, :]), reads=[impb], writes=[smb])
                    dve(_I("tensor_scalar", sm[:, 16:48], sm[:, 16:48], sm[:, 63:64], None, ALU.is_ge), reads=[smb], writes=[smb])
                    dve(_I("tensor_scalar", obf[:, 0:32], sm[:, 16:48], -1.0, -NEG, ALU.add, ALU.mult), reads=[smb], writes=[obfb])
                    pe(_I("transpose", psb(3)[0:32, 0:128], obf[:, 0:32], ident_b[:]), reads=[obfb, bC], writes=[PB[3]])
                    act(_I("activation", MT[0:32, g, qs], psb(3)[0:32, 0:128], AF.Copy), reads=[PB[3]], writes=[MTb[g]])
                sg3 = sg[:, t, :].rearrange("p (h r) -> p h r", r=3)
                units = []
                for g in range(2):
                    pvb = next_pv()
                    tiles = [(j, "C" if j == t else None) for j in range(t + 1)]
                    for hh in range(4):
                        h = 4 * g + hh
                        units += make_units(QT[:, h, :], QTb[h], KT[:, g, :], KTb[g], g, t, tiles, (MT[:, g, :], MTb[g]), pvb, hh, banks=(4, 5, 0, 1))
                    units[-1]["post"] = (lambda pvb=pvb, g=g, gv=sg3[:, 4 * g:4 * g + 4, 1]: norm4(pvb, g, gv, [bsg], False))
                    pvb = next_pv()
                    tiles = [(j, "C" if j == t else ("W" if j == t - 4 else None)) for j in range(max(0, t - 4), t + 1)]
                    for hh in range(4):
                        h = 4 * g + hh
                        units += make_units(QT[:, h, :], QTb[h], KT[:, 2 + g, :], KTb[2 + g], 2 + g, t, tiles, None, pvb, hh, banks=(4, 5, 0, 1))
                    units[-1]["post"] = (lambda pvb=pvb, g=g, gv=sg3[:, 4 * g:4 * g + 4, 2]: norm4(pvb, g, gv, [bsg], False))
                run_units(units, 2)
                flush_o(t, 0)

            S.barrier()
            U.reset()
            V_all = U.take(NT * 5 * 65).rearrange("p (t k c) -> p t k c", t=NT, k=5)
            bV = Buf()
            pool(_I("memset", V_all[:, :, :, 64:65], 1.0), writes=[bV])
            vstate["V"] = V_all
            vstate["bV"] = bV
            Wd = U.take(8 * 1352).rearrange("p (k n) -> p k n", k=8)
            QT = U.take(8 * S_TOK).rearrange("p (h n) -> p h n", h=8)
            KT = U.take(5 * S_TOK).rearrange("p (h n) -> p h n", h=5)
            q_aug = U.take(8 * 68).rearrange("p (h d) -> p h d", h=8)
            k_aug = U.take(4 * 68).rearrange("p (h d) -> p h d", h=4)
            iqT = U.take(4 * S_TOK).rearrange("p (m n) -> p m n", m=4)
            ikT = U.take(S_TOK)
            iw = U.take(NT * 8, F32).rearrange("p (t c) -> p t c", c=8)
            sc = U.take(S_TOK, F32)
            rl = U.take(512, F32)
            maskq = U.take(S_TOK)
            maskT = U.take(S_TOK).rearrange("p (j c) -> p j c", c=128)
            junk = U.take(S_TOK)
            biq, bik, biw, bsc, brl, bmq, bmT, bjk = (Buf() for _ in range(8))
            bWd = [Buf() for _ in range(8)]
            QTb = [Buf() for _ in range(8)]
            KTb = [Buf() for _ in range(5)]
            for k in range(8):
                load_w(Wd[:, k, 0:1224], win_v[:, k, 1304:2528], bWd[k])
                load_w(Wd[:, k, 1224:1288], win_v[:, k, 2456:2520], bWd[k])
                load_w(Wd[:, k, 1288:1352], win_v[:, k, 2456:2520], bWd[k])
            for c in range(4):
                cs = slice(c * 512, (c + 1) * 512)
                for tl in range(4):
                    t = c * 4 + tl
                    ts = slice(t * 128, (t + 1) * 128)
                    for k in range(8):
                        pe(_I("matmul", PS[0][:, :], lhsT=hT[:, k, ts], rhs=Wd[:, k, 0:512], start=(k == 0), stop=(k == 7)), reads=[hTb[c], bWd[k]], writes=[PB[0]])
                    for k in range(8):
                        pe(_I("matmul", PS[1][:, 0:128], lhsT=hT[:, k, ts], rhs=Wd[:, k, 512:640], start=(k == 0), stop=(k == 7)), reads=[hTb[c], bWd[k]], writes=[PB[1]])
                    for k in range(8):
                        pe(_I("matmul", PS[2][:, 0:8], lhsT=hT[:, k, ts], rhs=Wd[:, k, 1216:1224], start=(k == 0), stop=(k == 7)), reads=[hTb[c], bWd[k]], writes=[PB[2]])
                    rms_heads(0, 8, st16[:, 0:8])
                    rms_heads(1, 1, st16[:, 8:9])
                    rstd_from(st16[:, 0:9])
                    dve(_I("tensor_tensor", q_aug[:, :, 0:64], PS[0][:, :].rearrange("p (h d) -> p h d", d=64), st16[:, 0:8].unsqueeze(2).to_broadcast([128, 8, 64]), ALU.mult),
                        reads=[PB[0], stb], writes=[bq_aug])
                    dve(_I("tensor_copy", q_aug[:, :, 64:68], aqc[:, t, :, :]), reads=[bC], writes=[bq_aug])
                    dve(_I("tensor_scalar", sq[0][:, 0:64], PS[1][:, 0:64], st16[:, 8:9], None, ALU.mult), reads=[PB[1], stb], writes=[sqb[0]])
                    dve(_I("tensor_tensor", k_aug[:, 0, 0:64], sq[0][:, 0:64], gains[:, 3, :], ALU.mult), reads=[sqb[0], bC], writes=[bk_aug])
                    dve(_I("tensor_copy", k_aug[:, 0, 64:68], akc[:, t, :]), reads=[bC], writes=[bk_aug])
                    act(_I("activation", V_all[:, t, 4, 0:64], PS[1][:, 64:128], AF.Copy), reads=[PB[1]], writes=[bV])
                    act(_I("activation", iw[:, t, :], PS[2][:, 0:8], AF.Copy, scale=8.0 ** -0.5), reads=[PB[2]], writes=[biw])
                    for h in range(8):
                        pe(_I("transpose", psb(3)[0:68, h * 128:(h + 1) * 128], q_aug[:, h, :], ident_b[:]), reads=[bq_aug, bC], writes=[PB[3]])
                    pe(_I("transpose", psb(7)[0:68, 0:128], k_aug[:, 0, :], ident_b[:]), reads=[bk_aug, bC], writes=[PB[7]])
                    act(_I("activation", QT[0:68, :, ts], psb(3)[0:68, :].rearrange("p (h c) -> p h c", h=8), AF.Copy), reads=[PB[3]], writes=QTb)
                    dve(_I("tensor_copy", KT[0:68, 4, ts], psb(7)[0:68, 0:128]), reads=[PB[7]], writes=[KTb[4]])
                for m in range(4):
                    for k in range(8):
                        pe(_I("matmul", PS[0][:, :], lhsT=Wd[:, k, 640 + m * 128:640 + (m + 1) * 128], rhs=hT[:, k, cs], start=(k == 0), stop=(k == 7)), reads=[hTb[c], bWd[k]], writes=[PB[0]])
                    act(_I("activation", iqT[:, m, cs], PS[0][:, :], AF.Copy, scale=0.125), reads=[PB[0]], writes=[biq])
                for k in range(8):
                    pe(_I("matmul", PS[1][:, :], lhsT=Wd[:, k, 1224:1352], rhs=hT[:, k, cs], start=(k == 0), stop=(k == 7)), reads=[hTb[c], bWd[k]], writes=[PB[1]])
                act(_I("activation", ikT[:, cs], PS[1][:, :], AF.Copy), reads=[PB[1]], writes=[bik])

            S.barrier()
            Wd_flat = Wd.rearrange("p k n -> p (k n)")
            scs = [sc, Wd_flat[:, 0:4096].bitcast(F32)]
            maskqs = [maskq, Wd_flat[:, 4096:6144]]
            maskTs = [maskT, Wd_flat[:, 6144:8192].rearrange("p (j c) -> p j c", c=128)]
            bscs, bmqs, bmTs = [Buf(), Buf()], [Buf(), Buf()], [Buf(), Buf()]
            smx = [sb("smx%d_%d" % (s, i), [128, 40], F32) for i in range(2)] if s == 0 else smx
            smxb = [Buf(), Buf()]

            def indexer(t):
                p = t % 2
                sc_, bsc_, sm_, smb_ = scs[p], bscs[p], smx[p], smxb[p]
                qs = slice(t * 128, (t + 1) * 128)
                nk = (t + 1) * 128
                nch = (nk + 511) // 512
                for h in range(8):
                    base = (h % 2) * 64
                    for cc in range(nch):
                        w = min(512, nk - cc * 512)
                        cs = slice(cc * 512, cc * 512 + w)
                        bk = cc % 2
                        pe(_I("matmul", PS[bk][:, 0:w], lhsT=iqT[base:base + 64, h // 2, qs], rhs=ikT[base:base + 64, cs], start=True, stop=True),
                           reads=[biq, bik], writes=[PB[bk]])
                        act(_I("activation", rl[:, 0:w], PS[bk][:, 0:w], AF.Relu), reads=[PB[bk]], writes=[brl])
                        if h == 0:
                            dve(_I("tensor_scalar", sc_[:, cs], rl[:, 0:w], iw[:, t, 0:1], None, ALU.mult), reads=[brl, biw], writes=[bsc_])
                        else:
                            dve(_I("scalar_tensor_tensor", sc_[:, cs], rl[:, 0:w], iw[:, t, h:h + 1], sc_[:, cs], ALU.mult, ALU.add), reads=[brl, biw, bsc_], writes=[bsc_])
                dve(_I("tensor_reduce", sm_[:, 0:1], sc_[:, 0:nk], AX.X, ALU.max, apply_absolute_value=True), reads=[bsc_], writes=[smb_])
                pool(_I("affine_select", sc_[:, t * 128:(t + 1) * 128], sc_[:, t * 128:(t + 1) * 128], [[-1, 128]], ALU.is_ge, -3e38, base=0, channel_multiplier=1), reads=[bsc_, smb_], writes=[bsc_])
                dve(_I("tensor_scalar", sm_[:, 8:8 + NBIS + 1], pow2[:], sm_[:, 0:1], None, ALU.mult), reads=[smb_, bC], writes=[smb_])
                dve(_I("memset", sm_[:, 1:2], 0.0), reads=[smb_], writes=[smb_])

            def bisect_iter(t, j):
                p = t % 2
                sc_, bsc_, sm_, smb_ = scs[p], bscs[p], smx[p], smxb[p]
                nk = (t + 1) * 128
                dve(_I("tensor_scalar", maskqs[p][:, 0:nk], sc_[:, 0:nk], sm_[:, 1:2], None, ALU.is_ge, ALU.add, accum_out=sm_[:, 2:3]), reads=[bsc_, smb_], writes=[bmqs[p], smb_])
                dve(_I("tensor_scalar", sm_[:, 3:4], sm_[:, 2:3], 255.5, -0.5, ALU.is_ge, ALU.add), reads=[smb_], writes=[smb_])
                dve(_I("scalar_tensor_tensor", sm_[:, 1:2], sm_[:, 3:4], sm_[:, 8 + j:9 + j], sm_[:, 1:2], ALU.mult, ALU.add), reads=[smb_], writes=[smb_])

            def finish_mask(t):
                p = t % 2
                sc_, bsc_, sm_, smb_ = scs[p], bscs[p], smx[p], smxb[p]
                nk = (t + 1) * 128
                dve(_I("tensor_tensor", sm_[:, 1:2], sm_[:, 1:2], sm_[:, 8 + NBIS:9 + NBIS], ALU.subtract), reads=[smb_], writes=[smb_])
                dve(_I("tensor_scalar", maskqs[p][:, 0:nk], sc_[:, 0:nk], sm_[:, 1:2], None, ALU.is_ge), reads=[bsc_, smb_], writes=[bmqs[p]])
                for j in range(t + 1):
                    bk = 3 if (j // 8) % 2 == 0 else 7
                    pe(_I("transpose", psb(bk)[:, (j % 8) * 128:(j % 8 + 1) * 128], maskqs[p][:, j * 128:(j + 1) * 128], ident_b[:]), reads=[bmqs[p], bC], writes=[PB[bk]])
                    if j % 8 == 7 or j == t:
                        j0 = (j // 8) * 8
                        n = j - j0 + 1
                        act(_I("activation", maskTs[p][:, j0:j0 + n, :].rearrange("p j c -> p (j c)"), psb(bk)[:, 0:n * 128], AF.Copy), reads=[PB[bk]], writes=[bmTs[p]])

            indexer(0)
            for j in range(NBIS):
                bisect_iter(0, j)
            finish_mask(0)
            for t in range(NT):
                if t + 1 < NT:
                    indexer(t + 1)
                tiles = [(j, None) for j in range(t + 1)]
                units = []
                for g2 in range(2):
                    pvb = next_pv()
                    for hh in range(4):
                        h = 4 * g2 + hh
                        units += make_units(QT[:, h, :], QTb[h], KT[:, 4, :], KTb[4], 4, t, tiles, None, pvb, hh, full_mask=(maskTs[t % 2], bmTs[t % 2]))
                    units[-1]["post"] = (lambda pvb=pvb, g2=g2: norm4(pvb, g2, None, [], True))
                done = [0]

                def between(i, n, t=t, done=done):
                    if t + 1 >= NT:
                        return
                    target = ((i + 1) * NBIS) // n
                    while done[0] < target:
                        bisect_iter(t + 1, done[0])
                        done[0] += 1
                run_units(units, 1, between)
                if t + 1 < NT:
                    while done[0] < NBIS:
                        bisect_iter(t + 1, done[0])
                        done[0] += 1
                    finish_mask(t + 1)
                flush_o(t, 1)

            for hf in range(2):
                S.barrier()
                U.reset()
                Wg = U.take(8 * 2048).rearrange("p (k n) -> p k n", k=8)
                Woa = U.take(4 * D).rearrange("p (k n) -> p k n", k=4)
                Wob = U.take(4 * D).rearrange("p (k n) -> p k n", k=4)
                Wout = U.take(8 * D).rearrange("p (k n) -> p k n", k=8)
                Wff_region = (Wg, Woa, Wob, Wout)
                xacc = U.take(8 * D, F32).rearrange("p (t n) -> p t n", t=8)
                yT = U.take(8 * 512).rearrange("p (k n) -> p k n", k=8)
                oaT = U.take(4 * 512).rearrange("p (k n) -> p k n", k=4)
                obT = U.take(4 * 512).rearrange("p (k n) -> p k n", k=4)
                sga = U.take(512)
                sgb = U.take(512)
                t1 = U.take(512, F32)
                t2 = U.take(512, F32)
                aT = yT
                g1bc = U.take(D, F32)
                g2bc = U.take(D, F32)
                bG = Buf()
                S.dma(g1bc, modrow_d[b:b + 1, 2 * D:3 * D].partition_broadcast(128), writes=[bG])
                S.dma(g2bc, modrow_d[b:b + 1, 5 * D:6 * D].partition_broadcast(128), writes=[bG])
                bya, byT, boa, bob, bsga, bsgb, bt1, bt2, baT = (Buf() for _ in range(9))
                bWg = [Buf() for _ in range(8)]
                bWo = [Buf() for _ in range(8)]
                bWa = [Buf() for _ in range(4)]
                bWb = [Buf() for _ in range(4)]
                xab = [Buf() for _ in range(8)]
                for k in range(8):
                    load_w(Wg[:, k, :], win_v[:, k, 2528:4576], bWg[k])
                    load_w(Wout[:, k, :], wout_d.rearrange("(k p) n -> p k n", p=128)[:, k, :], bWo[k])
                for k in range(4):
                    load_w(Woa[:, k, :], woa_d.rearrange("(k p) n -> p k n", p=128)[:, k, :], bWa[k])
                    load_w(Wob[:, k, :], wob_d.rearrange("(k p) n -> p k n", p=128)[:, k, :], bWb[k])
                for cl in range(2):
                    c = hf * 2 + cl
                    cs = slice(c * 512, (c + 1) * 512)
                    S.dma(oaT, oT_d[0, :, :, cs].rearrange("j p c -> p j c"), reads=[bOT[0]], writes=[boa])
                    S.dma(obT, oT_d[1, :, :, cs].rearrange("j p c -> p j c"), reads=[bOT[1]], writes=[bob])
                    for f in range(8):
                        fs = slice(f * 128, (f + 1) * 128)
                        for k in range(8):
                            pe(_I("matmul", PS[0][:, :], lhsT=Wg[:, k, fs], rhs=hT[:, k, cs], start=(k == 0), stop=(k == 7)), reads=[bWg[k], hTb[c]], writes=[PB[0]])
                        act(_I("activation", sga, PS[0][:, :], AF.Sigmoid), reads=[PB[0]], writes=[bsga])
                        for k in range(8):
                            pe(_I("matmul", PS[1][:, :], lhsT=Wg[:, k, 1024 + f * 128:1024 + (f + 1) * 128], rhs=hT[:, k, cs], start=(k == 0), stop=(k == 7)), reads=[bWg[k], hTb[c]], writes=[PB[1]])
                        act(_I("activation", sgb, PS[1][:, :], AF.Sigmoid), reads=[PB[1]], writes=[bsgb])
                        for k in range(4):
                            pe(_I("matmul", PS[2][:, :], lhsT=Woa[:, k, fs], rhs=oaT[:, k, :], start=(k == 0), stop=(k == 3)), reads=[bWa[k], boa], writes=[PB[2]])
                        for k in range(4):
                            pe(_I("matmul", PS[4][:, :], lhsT=Wob[:, k, fs], rhs=obT[:, k, :], start=(k == 0), stop=(k == 3)), reads=[bWb[k], bob], writes=[PB[4]])
                        dve(_I("tensor_tensor", t1, PS[2][:, :], sga, ALU.mult), reads=[PB[2], bsga], writes=[bt1])
                        dve(_I("tensor_tensor", t2, PS[4][:, :], sgb, ALU.mult), reads=[PB[4], bsgb], writes=[bt2])
                        dve(_I("tensor_tensor", yT[:, f, :], t1, t2, ALU.add), reads=[bt1, bt2], writes=[byT])
                    for tl in range(4):
                        t = c * 4 + tl
                        tt = t - hf * 8
                        i = t % 2
                        S.dma(xt[i][:], x_d[s, t * 128:(t + 1) * 128, :], writes=[xtb[i]])
                        for h2 in range(2):
                            ns = slice(h2 * 512, (h2 + 1) * 512)
                            bk = 5 + h2
                            for k in range(8):
                                pe(_I("matmul", PS[bk][:, :], lhsT=yT[:, k, tl * 128:(tl + 1) * 128], rhs=Wout[:, k, ns], start=(k == 0), stop=(k == 7)), reads=[byT, bWo[k]], writes=[PB[bk]])
                            dve(_I("tensor_tensor", t1, PS[bk][:, :], g1bc[:, ns], ALU.mult), reads=[PB[bk], bG], writes=[bt1])
                            dve(_I("tensor_tensor", xacc[:, tt, ns], t1, xt[i][:, ns], ALU.add), reads=[bt1, xtb[i]], writes=[xab[tt]])
                        if dbg and s == 0:
                            S.dma(x1_dbg[t * 128:(t + 1) * 128, :], xacc[:, tt, :], reads=[xab[tt]], writes=[Buf()], is_output=True)
                        layernorm(xacc[:, tt, :], xab[tt], tl % 2, 1, b, 0)
                        if tl % 2 == 1:
                            ln_evac(t // 2, 1, b, 0, hTb[c])
                S.barrier()
                for (f0_, nf) in ((0, 8), (8, 8), (16, 6)):
                    Wfg = Wg.rearrange("p k n -> p (k n)")[:, 0:8 * 1024].rearrange("p (k n) -> p k n", k=8)
                    Wfu = Wg.rearrange("p k n -> p (k n)")[:, 8 * 1024:16 * 1024].rearrange("p (k n) -> p k n", k=8)
                    Wfd = Wout.rearrange("p k n -> p (k n)")[:, 0:8 * D].rearrange("p (k n) -> p k n", k=8)
                    nfc = nf * 128
                    if f0_ == 0:
                        bFg = [Buf() for _ in range(8)]
                        bFu = [Buf() for _ in range(8)]
                        bFd = [Buf() for _ in range(8)]
                    for k in range(8):
                        load_w(Wfg[:, k, 0:nfc], wfg_d.rearrange("(k p) n -> p k n", p=128)[:, k, f0_ * 128:f0_ * 128 + nfc], bFg[k])
                        load_w(Wfu[:, k, 0:nfc], wfu_d.rearrange("(k p) n -> p k n", p=128)[:, k, f0_ * 128:f0_ * 128 + nfc], bFu[k])
                    for k in range(nf):
                        load_w(Wfd[:, k, :], wfd_d[(f0_ + k) * 128:(f0_ + k + 1) * 128, :], bFd[k])
                    for cl in range(2):
                        c = hf * 2 + cl
                        cs = slice(c * 512, (c + 1) * 512)
                        for f in range(nf):
                            fs = slice(f * 128, (f + 1) * 128)
                            for k in range(8):
                                pe(_I("matmul", PS[0][:, :], lhsT=Wfg[:, k, fs], rhs=hT[:, k, cs], start=(k == 0), stop=(k == 7)), reads=[bFg[k], hTb[c]], writes=[PB[0]])
                            for k in range(8):
                                pe(_I("matmul", PS[1][:, :], lhsT=Wfu[:, k, fs], rhs=hT[:, k, cs], start=(k == 0), stop=(k == 7)), reads=[bFu[k], hTb[c]], writes=[PB[1]])
                            act(_I("activation", t1, PS[0][:, :], AF.Silu), reads=[PB[0]], writes=[bt1])
                            dve(_I("tensor_tensor", aT[:, f, :], t1, PS[1][:, :], ALU.mult), reads=[bt1, PB[1]], writes=[baT])
                        for tl in range(4):
                            tt = cl * 4 + tl
                            for h2 in range(2):
                                ns = slice(h2 * 512, (h2 + 1) * 512)
                                bk = 5 + h2
                                for f in range(nf):
                                    pe(_I("matmul", PS[bk][:, :], lhsT=aT[:, f, tl * 128:(tl + 1) * 128], rhs=Wfd[:, f, ns], start=(f == 0), stop=(f == nf - 1)), reads=[baT, bFd[f]], writes=[PB[bk]])
                                dve(_I("tensor_tensor", t2, PS[bk][:, :], g2bc[:, ns], ALU.mult), reads=[PB[bk], bG], writes=[bt2])
                                dve(_I("tensor_tensor", xacc[:, tt, ns], xacc[:, tt, ns], t2, ALU.add), reads=[bt2, xab[tt]], writes=[xab[tt]])
                for tt in range(8):
                    t = hf * 8 + tt
                    S.dma(out_d[s, t * 128:(t + 1) * 128, :], xacc[:, tt, :], reads=[xab[tt]], writes=[Buf()], is_output=True)
        S.emit()
    return nc


def _prep_common(inp):
    f = lambda a: np.ascontiguousarray(np.asarray(a, dtype=np.float32))
    gn = np.concatenate([inp["g_norm1"][0].reshape(8, 128).T, inp["g_norm2"][0].reshape(8, 128).T], axis=1)
    gvec = np.concatenate([inp[k][0] for k in ("g_q_a", "g_kc_a", "g_ks_a", "g_kw_a", "g_q_b", "g_k_b")])[None, :]
    peT = np.concatenate([inp["pe_ck"][0].T, inp["pe_cv"][0].T], axis=1)
    return {
        "w_ada": f(inp["w_ada"][0]), "b_ada": f(inp["b_ada"]), "gn": f(gn), "w_in": f(inp["w_in"][0]),
        "gvec": f(gvec), "peT": f(peT), "w_ck1": f(inp["w_ck1"][0]), "w_ck2": f(inp["w_ck2"][0]),
        "w_cv1": f(inp["w_cv1"][0]), "w_cv2": f(inp["w_cv2"][0]), "w_o_a": f(inp["w_o_a"][0]),
        "w_o_b": f(inp["w_o_b"][0]), "w_out": f(inp["w_out"][0]), "w_ff_gate": f(inp["w_ff_gate"][0]),
        "w_ff_up": f(inp["w_ff_up"][0]), "w_ff_down": f(inp["w_ff_down"][0]),
    }


def _core_map(common, x, c, i, nseq):
    m = dict(common)
    m["x"] = np.ascontiguousarray(x[i * nseq:(i + 1) * nseq])
    cc = np.zeros((4, D), np.float32)
    cc[:nseq] = c[i * nseq:(i + 1) * nseq]
    m["cT"] = np.ascontiguousarray(cc.T.reshape(8, 128, 4).transpose(1, 0, 2))
    return m


def kernel(**inputs):
    x = np.asarray(inputs["x"], dtype=np.float32)
    c = np.asarray(inputs["c"], dtype=np.float32)
    n = 8
    nseq = x.shape[0] // n
    nc = build_nc(nseq)
    common = _prep_common(inputs)
    in_maps = [_core_map(common, x, c, i, nseq) for i in range(n)]
    res = run_bass_kernel_spmd(nc, in_maps, core_ids=list(range(n)))
    return np.concatenate([r["out"] for r in res.results], axis=0).astype(np.float32)
```

```python
import contextlib
import numpy as np
import concourse.bass as bass
import concourse.mybir as mybir
from concourse.bass_utils import run_bass_kernel_spmd

F32 = mybir.dt.float32
BF16 = mybir.dt.bfloat16
AF = mybir.ActivationFunctionType
ALU = mybir.AluOpType
AX = mybir.AxisListType

S_TOK = 2048
D = 1024
NT = 16
DIN = 4576
DFF = 2816
EPS = 1e-6
NBIS = 22
NEG = -30000.0


class Buf:
    __slots__ = ("name", "w", "r")

    def __init__(self, name=""):
        self.name = name
        self.w = None
        self.r = {}


class Sched:
    ENGS = ("pe", "act", "dve", "pool", "sp")
    NDMA = 24

    def __init__(self, nc):
        self.nc = nc
        self.streams = {e: [] for e in self.ENGS}
        self.count = {e: 0 for e in self.ENGS}
        self.waited = {e: {} for e in self.ENGS}
        self.dma_uses = [0] * self.NDMA
        self.dma_rr = 0
        self.out_events = []

    def _deps(self, eng, reads, writes):
        deps = {}

        def add(ev):
            if ev is None:
                return
            k, v = ev
            if deps.get(k, 0) < v:
                deps[k] = v
        for b in reads:
            add(b.w)
        for b in writes:
            if b.w is not None and b.w[0] != eng:
                add(b.w)
            for k, v in b.r.items():
                if k != eng:
                    add((k, v))
        waits = []
        for k, v in deps.items():
            if k == "pe" and eng == "pe":
                continue
            if self.waited[eng].get(k, 0) < v:
                self.waited[eng][k] = v
                waits.append((k, v))
        return waits

    def _commit(self, ev, reads, writes):
        k, v = ev
        for b in writes:
            b.w = ev
            b.r = {}
        for b in reads:
            if b.r.get(k, 0) < v:
                b.r[k] = v

    def op(self, eng, fn, reads=(), writes=()):
        waits = self._deps(eng, reads, writes)
        self.count[eng] += 1
        ev = (eng, self.count[eng])
        self.streams[eng].append((fn, waits, ev))
        self._commit(ev, reads, writes)
        return ev

    def dma(self, out, in_, reads=(), writes=(), q="sp", is_output=False, **kw):
        i = self.dma_rr
        self.dma_rr = (self.dma_rr + 1) % self.NDMA
        waits = self._deps(q, reads, writes)
        key = "dma%d" % i
        prev = self.dma_uses[i] * 16
        if prev and self.waited[q].get(key, 0) < prev:
            self.waited[q][key] = prev
            waits.append((key, prev))
        self.dma_uses[i] += 1
        ev = (key, self.dma_uses[i] * 16)
        fn = lambda e, out=out, in_=in_, kw=kw: e.dma_start(out=out, in_=in_, **kw)
        self.streams[q].append((fn, waits, ev))
        self._commit(ev, reads, writes)
        if is_output:
            self.out_events.append(ev)
        return ev

    def barrier(self):
        allv = [(e, self.count[e]) for e in self.ENGS if self.count[e]]
        allv += [("dma%d" % i, self.dma_uses[i] * 16) for i in range(self.NDMA) if self.dma_uses[i]]
        for e in self.ENGS:
            waits = []
            for k, v in allv:
                if k == e:
                    continue
                if self.waited[e].get(k, 0) < v:
                    self.waited[e][k] = v
                    waits.append((k, v))
            if waits:
                self.streams[e].append((None, waits, None))

    def emit(self):
        nc = self.nc
        with contextlib.ExitStack() as st:
            sems = {}
            for e in self.ENGS:
                sems[e] = st.enter_context(nc.semaphore("s_" + e))
            for i in range(self.NDMA):
                sems["dma%d" % i] = st.enter_context(nc.semaphore("s_dma%d" % i))
            final = {}
            for k, v in self.out_events:
                final[k] = max(final.get(k, 0), v)
            block = st.enter_context(nc.Block())

            def run(engname, e):
                for fn, waits, ev in self.streams[engname]:
                    for k, v in waits:
                        e.wait_ge(sems[k], v)
                    if fn is None:
                        continue
                    ins = fn(e)
                    k, v = ev
                    ins.then_inc(sems[k], 16 if k.startswith("dma") else 1)
                if engname == "sp":
                    for k, v in final.items():
                        e.wait_ge(sems[k], v)

            @block.tensor
            def _(e):
                run("pe", e)

            @block.scalar
            def _(e):
                run("act", e)

            @block.vector
            def _(e):
                run("dve", e)

            @block.gpsimd
            def _(e):
                run("pool", e)

            @block.sync
            def _(e):
                run("sp", e)


class Arena:
    def __init__(self, ap16):
        self.ap = ap16
        self.off = 0

    def reset(self):
        self.off = 0

    def take(self, ncols, dt=BF16):
        if dt == F32:
            self.off = (self.off + 1) // 2 * 2
            n16 = ncols * 2
        else:
            n16 = ncols
        assert self.off + n16 <= self.ap.shape[1], (self.off, n16, self.ap.shape)
        v = self.ap[:, self.off:self.off + n16]
        self.off += n16
        self.off = (self.off + 1) // 2 * 2
        return v.bitcast(F32) if dt == F32 else v


_REGS = {}


def _I(name, *args, **kw):
    if name == "affine_select":
        def thunk(e):
            a = list(args)
            key = (id(e), float(a[4]))
            if key not in _REGS:
                _REGS[key] = e.to_reg(float(a[4]))
            a[4] = _REGS[key]
            return e.affine_select(*a, **kw)
        return thunk
    return lambda e: getattr(e, name)(*args, **kw)


def build_nc(nseq=4, dbg=False):
    nc = bass.Bass("TRN2", target_bir_lowering=False)
    _REGS.clear()
    S = Sched(nc)

    def din(name, shape):
        return nc.dram_tensor(name, shape, F32, kind="ExternalInput").ap()
    x_d = din("x", [nseq, S_TOK, D])
    cT_d = din("cT", [128, 8, 4])
    wada_d = din("w_ada", [D, 6 * D])
    bada_d = din("b_ada", [1, 6 * D])
    gn_d = din("gn", [128, 16])
    win_d = din("w_in", [D, DIN])
    gv_d = din("gvec", [1, 6 * 64])
    peT_d = din("peT", [64, 64])
    wck1_d = din("w_ck1", [2048, 128])
    wck2_d = din("w_ck2", [128, 64])
    wcv1_d = din("w_cv1", [2048, 128])
    wcv2_d = din("w_cv2", [128, 64])
    woa_d = din("w_o_a", [512, D])
    wob_d = din("w_o_b", [512, D])
    wout_d = din("w_out", [D, D])
    wfg_d = din("w_ff_gate", [D, DFF])
    wfu_d = din("w_ff_up", [D, DFF])
    wfd_d = din("w_ff_down", [DFF, D])
    out_d = nc.dram_tensor("out", [nseq, S_TOK, D], F32, kind="ExternalOutput").ap()
    modrow_d = nc.dram_tensor("modrow", [4, 6 * D], F32, kind="Internal").ap()
    oT_d = nc.dram_tensor("oT_scr", [2, 4, 128, S_TOK], BF16, kind="ExternalOutput" if dbg else "Internal").ap()
    if dbg:
        hT_dbg = nc.dram_tensor("hT_dbg", [128, 8, S_TOK], BF16, kind="ExternalOutput").ap()
        x1_dbg = nc.dram_tensor("x1_dbg", [S_TOK, D], F32, kind="ExternalOutput").ap()
        mod_dbg = nc.dram_tensor("mod_dbg", [4, 6 * D], F32, kind="ExternalOutput").ap()

    st = contextlib.ExitStack()

    def sb(name, shape, dt=F32):
        return st.enter_context(nc.sbuf_tensor(name, shape, dt))

    with st:
        PS = [st.enter_context(nc.psum_tensor("ps%d" % i, [128, 512], F32)) for i in range(8)]
        PB = [Buf("ps%d" % i) for i in range(8)]

        def psb(i):
            return PS[i][:].bitcast(BF16)

        ident_b = sb("ident_b", [128, 128], BF16)
        ident_f = sb("ident_f", [128, 128], F32)
        Cm = sb("Cm", [128, 128], BF16)
        Wm = sb("Wm", [128, 128], BF16)
        cmask = sb("cmask", [128, S_TOK], BF16)
        ET = sb("ET", [32, S_TOK], BF16)
        selA = sb("selA", [128, NT, 32], F32)
        selB = sb("selB", [128, NT, 32], F32)
        aqc = sb("aqc", [128, NT, 8, 4], BF16)
        akc = sb("akc", [128, NT, 4], BF16)
        gbc = sb("gbc", [128, 6 * 64], F32)
        gains = sb("gains", [128, 4, 64], F32)
        gnc = sb("gnc", [128, 16], F32)
        modT = sb("modT", [128, 32, 4], F32)
        cb2 = sb("cb2", [128, 2], F32)
        pow2 = sb("pow2", [128, NBIS + 1], F32)
        hT = sb("hT", [128, 8, S_TOK], BF16)
        Vc = sb("Vc", [128, 2, 97], BF16)
        kc_aug = sb("kc_aug", [128, 2, 68], BF16)
        KcT = sb("KcT", [128, 2, 128], BF16)
        U_t = sb("U", [128, 65400], BF16)
        U = Arena(U_t[:])
        bC = Buf("consts")
        bOT = [Buf(), Buf()]
        bOut = Buf()

        hTb = [Buf("hT%d" % c) for c in range(4)]

        def pool(fn, reads=(), writes=()):
            return S.op("pool", fn, reads, writes)

        def dve(fn, reads=(), writes=()):
            return S.op("dve", fn, reads, writes)

        def act(fn, reads=(), writes=()):
            return S.op("act", fn, reads, writes)

        def pe(fn, reads=(), writes=()):
            return S.op("pe", fn, reads, writes)

        pool(_I("memset", ident_b[:], 1.0), writes=[bC])
        pool(_I("affine_select", ident_b[:], ident_b[:], [[1, 128]], ALU.is_equal, 0.0, base=0, channel_multiplier=-1), reads=[bC], writes=[bC])
        pool(_I("memset", ident_f[:], 1.0), writes=[bC])
        pool(_I("affine_select", ident_f[:], ident_f[:], [[1, 128]], ALU.is_equal, 0.0, base=0, channel_multiplier=-1), reads=[bC], writes=[bC])
        pool(_I("memset", Cm[:], 1.0), writes=[bC])
        pool(_I("affine_select", Cm[:], Cm[:], [[1, 128]], ALU.is_ge, 0.0, base=0, channel_multiplier=-1), reads=[bC], writes=[bC])
        pool(_I("memset", Wm[:], 1.0), writes=[bC])
        pool(_I("affine_select", Wm[:], Wm[:], [[-1, 128]], ALU.is_gt, 0.0, base=0, channel_multiplier=1), reads=[bC], writes=[bC])
        pool(_I("memset", cmask[:], 1.0), writes=[bC])
        pool(_I("affine_select", cmask[:], cmask[:], [[1, S_TOK]], ALU.is_ge, 0.0, base=-31, channel_multiplier=-16), reads=[bC], writes=[bC])
        pool(_I("memset", ET[:], 1.0), writes=[bC])
        pool(_I("affine_select", ET[:], ET[:], [[1, S_TOK]], ALU.is_ge, 0.0, base=0, channel_multiplier=-64), reads=[bC], writes=[bC])
        pool(_I("affine_select", ET[:], ET[:], [[-1, S_TOK]], ALU.is_ge, 0.0, base=63, channel_multiplier=64), reads=[bC], writes=[bC])
        for g in range(2):
            pool(_I("memset", Vc[:, g, 64:97], 1.0), writes=[bC])
            pool(_I("affine_select", Vc[:, g, 65:97], Vc[:, g, 65:97], [[-64, 32]], ALU.is_ge, 0.0, base=31, channel_multiplier=16), reads=[bC], writes=[bC])
            pool(_I("affine_select", Vc[:, g, 65:97], Vc[:, g, 65:97], [[64, 32]], ALU.is_ge, 0.0, base=63, channel_multiplier=-16), reads=[bC], writes=[bC])
        Dt = U.take(NT * 32, F32).rearrange("p (t j) -> p t j", j=32)
        jt = U.take(NT * 32, F32).rearrange("p (t j) -> p t j", j=32)
        f0 = U.take(NT * 32, F32).rearrange("p (t j) -> p t j", j=32)
        for lo_, base in ((0, 0), (64, -1)):
            pool(_I("iota", Dt[lo_:lo_ + 64], [[-2, NT], [1, 32]], base=base, channel_multiplier=0, allow_small_or_imprecise_dtypes=True), writes=[bC])
        pool(_I("iota", jt[:], [[0, NT], [1, 32]], base=0, channel_multiplier=0, allow_small_or_imprecise_dtypes=True), writes=[bC])
        dve(_I("tensor_single_scalar", f0[:], jt[:], 0.0, ALU.is_equal), reads=[bC], writes=[bC])
        dve(_I("tensor_single_scalar", jt[:], Dt[:], 0.0, ALU.is_equal), reads=[bC], writes=[bC])
        dve(_I("tensor_max", f0[:], f0[:], jt[:]), reads=[bC], writes=[bC])
        dve(_I("tensor_single_scalar", jt[:], Dt[:], -1.0, ALU.is_equal), reads=[bC], writes=[bC])
        dve(_I("tensor_max", f0[:], f0[:], jt[:]), reads=[bC], writes=[bC])
        dve(_I("tensor_single_scalar", jt[:], Dt[:], 0.0, ALU.is_le), reads=[bC], writes=[bC])
        dve(_I("tensor_sub", selA[:], jt[:], f0[:]), reads=[bC], writes=[bC])
        dve(_I("tensor_add", selB[:], jt[:], f0[:]), reads=[bC], writes=[bC])
        dve(_I("tensor_scalar", selB[:], selB[:], -1.0, 1e9, ALU.add, ALU.mult), reads=[bC], writes=[bC])
        hi_t = sb("hi_t", [128, NT], F32)
        lo_t = sb("lo_t", [128, 1], F32)
        for lo_, base in ((0, 0), (64, 64)):
            pool(_I("iota", hi_t[lo_:lo_ + 64], [[128, NT]], base=base, channel_multiplier=0, allow_small_or_imprecise_dtypes=True), writes=[bC])
            pool(_I("iota", lo_t[lo_:lo_ + 64], [[0, 1]], base=0, channel_multiplier=1, allow_small_or_imprecise_dtypes=True), writes=[bC])
        for h in range(8):
            sl = 2.0 ** -(h + 1)
            dve(_I("memset", aqc[:, :, h, 0:2], sl), reads=[bC], writes=[bC])
            dve(_I("tensor_scalar", aqc[:, :, h, 2], hi_t[:], -sl, None, ALU.mult), reads=[bC], writes=[bC])
            dve(_I("tensor_scalar", aqc[:, :, h, 3], lo_t[:].to_broadcast([128, NT]), -sl, None, ALU.mult), reads=[bC], writes=[bC])
        dve(_I("memset", akc[:, :, 2:4], 1.0), reads=[bC], writes=[bC])
        dve(_I("tensor_copy", akc[:, :, 0], hi_t[:]), reads=[bC], writes=[bC])
        dve(_I("tensor_copy", akc[:, :, 1], lo_t[:].to_broadcast([128, NT])), reads=[bC], writes=[bC])
        pn = sb("pn", [128, 1], F32)
        pool(_I("iota", pn[:], [[0, 1]], base=0, channel_multiplier=16, allow_small_or_imprecise_dtypes=True), writes=[bC])
        for g in range(2):
            dve(_I("tensor_copy", kc_aug[:, g, 64:65], pn[:]), reads=[bC], writes=[bC])
            dve(_I("memset", kc_aug[:, g, 65:66], 31.0), reads=[bC], writes=[bC])
            dve(_I("memset", kc_aug[:, g, 66:68], 1.0), reads=[bC], writes=[bC])
        for j in range(NBIS + 1):
            dve(_I("memset", pow2[:, j:j + 1], 2.0 ** -j), reads=[bC], writes=[bC])
        S.dma(gbc[:], gv_d.partition_broadcast(128), writes=[bC])
        S.dma(gnc[:], gn_d, writes=[bC])

        def gsl(i):
            return gbc[:, i * 64:(i + 1) * 64]
        for idx, (gk, gq) in enumerate(((2, 0), (3, 0), (1, 0), (5, 4))):
            dve(_I("scalar_tensor_tensor", gains[:, idx, :], gsl(gk), 0.125, gsl(gq), ALU.mult, ALU.mult), reads=[bC], writes=[bC])

        S.barrier()
        U.reset()
        scT = U.take(32, F32).rearrange("p (k b) -> p k b", b=4)
        wchunk = U.take(8 * 512, F32).rearrange("p (k n) -> p k n", k=8)
        bchunk = U.take(512, F32)
        mchunk = U.take(512, F32)
        bsc, bw, bbc, bm = Buf(), Buf(), Buf(), Buf()
        S.dma(scT, cT_d, writes=[bsc])
        act(_I("activation", scT, scT, AF.Silu), reads=[bsc], writes=[bsc])
        wada_v = wada_d.rearrange("(k p) n -> p k n", p=128)
        LNV = {0: 0, 1: 1, 3: 2, 4: 3}
        for c in range(12):
            S.dma(wchunk, wada_v[:, :, c * 512:(c + 1) * 512], writes=[bw])
            S.dma(bchunk[0:4, :], bada_d[:, c * 512:(c + 1) * 512].partition_broadcast(4), writes=[bbc])
            for k in range(8):
                pe(_I("matmul", PS[0][0:4, :], lhsT=scT[:, k, :], rhs=wchunk[:, k, :], start=(k == 0), stop=(k == 7)), reads=[bsc, bw], writes=[PB[0]])
            dve(_I("tensor_tensor", mchunk[0:4, :], PS[0][0:4, :], bchunk[0:4, :], ALU.add), reads=[PB[0], bbc], writes=[bm])
            S.dma(modrow_d[:, c * 512:(c + 1) * 512], mchunk[0:4, :], reads=[bm], writes=[bC])
            if dbg:
                S.dma(mod_dbg[:, c * 512:(c + 1) * 512], mchunk[0:4, :], reads=[bm], writes=[Buf()], is_output=True)
            vec, half = c // 2, c % 2
            if vec in LNV:
                for i in range(4):
                    col = (LNV[vec] * 8 + half * 4 + i) * 4
                    pe(_I("transpose", PS[1][:, col:col + 4], mchunk[0:4, i * 128:(i + 1) * 128], ident_f[0:4, 0:4]), reads=[bm, bC], writes=[PB[1]])
        dve(_I("tensor_copy", modT[:].rearrange("p a b -> p (a b)"), PS[1][:, 0:128]), reads=[PB[1]], writes=[bC])
        for which, gi in ((1, 0), (3, 1)):
            dve(_I("scalar_tensor_tensor",
                modT[:, which * 8:(which + 1) * 8, :], modT[:, which * 8:(which + 1) * 8, :], 1.0,
                gnc[:, gi * 8:(gi + 1) * 8].unsqueeze(2).to_broadcast([128, 8, 4]), ALU.add, ALU.mult), reads=[bC], writes=[bC])
        S.barrier()

        xt = [sb("xt%d" % i, [128, D], F32) for i in range(2)]
        xtb = [Buf() for _ in range(2)]
        xn = sb("xn", [128, D], BF16)
        xnb = Buf()
        sq = [sb("sq%d" % i, [128, 512], F32) for i in range(2)]
        sqb = [Buf(), Buf()]
        st16 = sb("st16", [128, 16], F32)
        stb = Buf()
        PT = [sb("PT%d" % i, [128, 512], BF16) for i in range(6)]
        PTb = [Buf() for _ in range(6)]
        sn = sb("sn", [128, 16], F32)
        snb = Buf()
        tmpo = sb("tmpo", [128, 4, 64], F32)
        tmpb = Buf()
        oacc = sb("oacc", [128, 8, 64], F32)
        oab = Buf()
        obf = sb("obf", [128, 512], BF16)
        obfb = Buf()
        oTs = sb("oTs", [128, 4, 128], BF16)
        oTsb = Buf()
        sm = sb("sm", [128, 64], F32)
        smb = Buf()
        state = {"pt": 0, "sb": 0}
        vstate = {}

        def layernorm(src_tile, src_buf, tl, which, b, psbank):
            act(_I("activation", sq[0][:, :].bitcast(BF16), src_tile, AF.Square, accum_out=st16[:, 0:1]), reads=[src_buf], writes=[sqb[0], stb])
            act(_I("activation", st16[:, 1:2], st16[:, 0:1], AF.Sqrt, bias=EPS, scale=1.0 / D), reads=[stb], writes=[stb])
            dve(_I("reciprocal", st16[:, 2:3], st16[:, 1:2]), reads=[stb], writes=[stb])
            dve(_I("tensor_scalar", xn[:], src_tile, st16[:, 2:3], None, ALU.mult), reads=[src_buf, stb], writes=[xnb])
            for j in range(8):
                bk = psbank + j // 4
                o0 = ((j % 4) * 2 + tl) * 128
                pe(_I("transpose", psb(bk)[:, o0:o0 + 128], xn[:, j * 128:(j + 1) * 128], ident_b[:]), reads=[xnb, bC], writes=[PB[bk]])

        def ln_evac(c2, which, b, psbank, hbuf):
            for j in range(8):
                bk = psbank + j // 4
                src = psb(bk)[:, (j % 4) * 256:(j % 4) * 256 + 256]
                Gc = modT[:, (2 * which + 1) * 8 + j, b:b + 1]
                Sc = modT[:, (2 * which) * 8 + j, b:b + 1]
                dve(_I("tensor_scalar", hT[:, j, c2 * 256:(c2 + 1) * 256], src, Gc, Sc, ALU.mult, ALU.add),
                    reads=[PB[bk], bC], writes=[hbuf])

        def load_w(dst, src, buf, q="pool"):
            S.dma(dst, src, writes=[buf], q=q)

        win_v = win_d.rearrange("(k p) n -> p k n", p=128)

        def rms_heads(psbank, nh, dst_stats):
            i = state["sb"] = (state["sb"] + 1) % 2
            act(_I("activation", sq[i][:, 0:nh * 64], PS[psbank][:, 0:nh * 64], AF.Square), reads=[PB[psbank]], writes=[sqb[i]])
            dve(_I("tensor_reduce", dst_stats, sq[i][:, 0:nh * 64].rearrange("p (h d) -> p h d", d=64), AX.X, ALU.add), reads=[sqb[i]], writes=[stb])

        def rstd_from(stats):
            act(_I("activation", stats, stats, AF.Sqrt, bias=EPS, scale=1.0 / 64), reads=[stb], writes=[stb])
            dve(_I("reciprocal", stats, stats), reads=[stb], writes=[stb])

        def make_units(QTh, qb, KTk, kb, kind, t, tiles, maskmm, pvb, slot, full_mask=None, banks=(4, 5)):
            ntl = len(tiles)
            return [dict(QTh=QTh, qb=qb, KTk=KTk, kb=kb, kind=kind, t=t, grp=tiles[g0:g0 + 4], g0=g0, ntl=ntl, maskmm=maskmm,
                         pvb=pvb, slot=slot, full_mask=full_mask, banks=banks) for g0 in range(0, ntl, 4)]

        def emit_A(u):
            qs = slice(u["t"] * 128, (u["t"] + 1) * 128)
            banks = u["banks"]
            bk = banks[state["pt"] % len(banks)]
            pi = state["pt"] % len(PT)
            state["pt"] += 1
            u["pi"] = pi
            grp = u["grp"]
            for i, (j, mt) in enumerate(grp):
                ks = slice(j * 128, (j + 1) * 128)
                pe(_I("matmul", PS[bk][:, i * 128:(i + 1) * 128], lhsT=u["KTk"][0:68, ks], rhs=u["QTh"][0:68, qs], start=True, stop=(u["maskmm"] is None)),
                   reads=[u["kb"], u["qb"]], writes=[PB[bk]])
                if u["maskmm"] is not None:
                    MTg, mb = u["maskmm"]
                    pe(_I("matmul", PS[bk][:, i * 128:(i + 1) * 128], lhsT=ET[0:32, ks], rhs=MTg[0:32, qs], start=False, stop=True),
                       reads=[mb, bC], writes=[PB[bk]])
            n = len(grp) * 128
            act(_I("activation", PT[pi][:, 0:n], PS[bk][:, 0:n], AF.Exp), reads=[PB[bk]], writes=[PTb[pi]])
            if u["full_mask"] is not None:
                mT, mTb = u["full_mask"]
                j0 = grp[0][0]
                pool(_I("tensor_tensor", PT[pi][:, 0:n], PT[pi][:, 0:n], mT[:, j0:j0 + len(grp), :].rearrange("p j c -> p (j c)"), ALU.mult),
                     reads=[mTb, PTb[pi]], writes=[PTb[pi]])
            for i, (j, mt) in enumerate(grp):
                if mt is not None:
                    mk = Cm if mt == "C" else Wm
                    pool(_I("tensor_tensor", PT[pi][:, i * 128:(i + 1) * 128], PT[pi][:, i * 128:(i + 1) * 128], mk[:], ALU.mult),
                         reads=[bC, PTb[pi]], writes=[PTb[pi]])

        def emit_B(u):
            pi = u["pi"]
            pvb = u["pvb"]
            po = PS[pvb][:, u["slot"] * 65:(u["slot"] + 1) * 65]
            for i, (j, mt) in enumerate(u["grp"]):
                gi = u["g0"] + i
                pe(_I("matmul", po, lhsT=PT[pi][:, i * 128:(i + 1) * 128], rhs=vstate["V"][:, j, u["kind"], :], start=(gi == 0), stop=(gi == u["ntl"] - 1)),
                   reads=[PTb[pi], vstate["bV"]], writes=[PB[pvb]])

        def run_units(units, L, between=None):
            n = len(units)
            for i in range(min(L, n)):
                emit_A(units[i])
            for i in range(n):
                if i + L < n:
                    emit_A(units[i + L])
                emit_B(units[i])
                if units[i].get("post") is not None:
                    units[i]["post"]()
                if between is not None:
                    between(i, n)

        def next_pv():
            state["pv"] = state.get("pv", 0) + 1
            return 6 if state["pv"] % 2 == 0 else 2

        def norm4(pvb, g, gate_view, gate_bufs, first):
            o4 = PS[pvb][:, 0:260].rearrange("p (h c) -> p h c", h=4)
            dve(_I("tensor_scalar", sn[:, 0:4], o4[:, :, 64], 1e-30, None, ALU.max), reads=[PB[pvb]], writes=[snb])
            dve(_I("reciprocal", sn[:, 4:8], sn[:, 0:4]), reads=[snb], writes=[snb])
            if gate_view is not None:
                dve(_I("tensor_tensor", sn[:, 4:8], sn[:, 4:8], gate_view, ALU.mult), reads=[snb] + gate_bufs, writes=[snb])
            wb = sn[:, 4:8].unsqueeze(2).to_broadcast([128, 4, 64])
            if first:
                dve(_I("tensor_tensor", oacc[:, 4 * g:4 * g + 4, :], o4[:, :, 0:64], wb, ALU.mult), reads=[PB[pvb], snb], writes=[oab])
            else:
                dve(_I("tensor_tensor", tmpo[:], o4[:, :, 0:64], wb, ALU.mult), reads=[PB[pvb], snb], writes=[tmpb])
                pool(_I("tensor_tensor", oacc[:, 4 * g:4 * g + 4, :], oacc[:, 4 * g:4 * g + 4, :], tmpo[:], ALU.add), reads=[tmpb, oab], writes=[oab])

        def flush_o(t, mix):
            dve(_I("tensor_copy", obf[:], oacc[:].rearrange("p h d -> p (h d)")), reads=[oab], writes=[obfb])
            for j in range(4):
                pe(_I("transpose", psb(3)[:, j * 128:(j + 1) * 128], obf[:, j * 128:(j + 1) * 128], ident_b[:]), reads=[obfb, bC], writes=[PB[3]])
            act(_I("activation", oTs[:].rearrange("p j c -> p (j c)"), psb(3)[:, 0:512], AF.Copy), reads=[PB[3]], writes=[oTsb])
            S.dma(oT_d[mix, :, :, t * 128:(t + 1) * 128].rearrange("j p c -> p j c"), oTs[:], reads=[oTsb], writes=[bOT[mix]])

        for s in range(nseq):
            b = s
            S.barrier()

            for c2 in range(8):
                for tl in range(2):
                    t = c2 * 2 + tl
                    i = t % 2
                    S.dma(xt[i][:], x_d[s, t * 128:(t + 1) * 128, :], writes=[xtb[i]])
                    layernorm(xt[i][:], xtb[i], tl, 0, b, 0)
                ln_evac(c2, 0, b, 0, hTb[c2 // 2])

            if dbg and s == 0:
                S.dma(hT_dbg, hT[:], reads=hTb, writes=[Buf()], is_output=True)
            U.reset()
            V_all = U.take(NT * 5 * 65).rearrange("p (t k c) -> p t k c", t=NT, k=5)
            bV = Buf()
            pool(_I("memset", V_all[:, :, :, 64:65], 1.0), writes=[bV])
            vstate["V"] = V_all
            vstate["bV"] = bV
            Wn = U.take(8 * 1304).rearrange("p (k n) -> p k n", k=8)
            QT = U.take(8 * S_TOK).rearrange("p (h n) -> p h n", h=8)
            KT = U.take(5 * S_TOK).rearrange("p (h n) -> p h n", h=5)
            q_aug = U.take(8 * 68).rearrange("p (h d) -> p h d", h=8)
            k_aug = U.take(4 * 68).rearrange("p (h d) -> p h d", h=4)
            kcT = U.take(S_TOK)
            vcT = U.take(S_TOK)
            MT = U.take(2 * S_TOK).rearrange("p (g n) -> p g n", g=2)
            sg = U.take(NT * 24, F32).rearrange("p (t c) -> p t c", c=24)
            bq_aug, bk_aug, bkc, bsg = Buf(), Buf(), Buf(), Buf()
            bWn = [Buf() for _ in range(8)]
            QTb = [Buf() for _ in range(8)]
            KTb = [Buf() for _ in range(5)]
            MTb = [Buf(), Buf()]
            for k in range(8):
                load_w(Wn[:, k, :], win_v[:, k, 0:1304], bWn[k])
            for c in range(4):
                for tl in range(4):
                    t = c * 4 + tl
                    ts = slice(t * 128, (t + 1) * 128)
                    for k in range(8):
                        pe(_I("matmul", PS[0][:, :], lhsT=hT[:, k, ts], rhs=Wn[:, k, 0:512], start=(k == 0), stop=(k == 7)), reads=[hTb[c], bWn[k]], writes=[PB[0]])
                    for k in range(8):
                        pe(_I("matmul", PS[1][:, :], lhsT=hT[:, k, ts], rhs=Wn[:, k, 768:1280], start=(k == 0), stop=(k == 7)), reads=[hTb[c], bWn[k]], writes=[PB[1]])
                    for k in range(8):
                        pe(_I("matmul", PS[2][:, 0:24], lhsT=hT[:, k, ts], rhs=Wn[:, k, 1280:1304], start=(k == 0), stop=(k == 7)), reads=[hTb[c], bWn[k]], writes=[PB[2]])
                    rms_heads(0, 8, st16[:, 0:8])
                    rms_heads(1, 8, st16[:, 8:16])
                    rstd_from(st16[:, 0:16])
                    dve(_I("tensor_tensor", q_aug[:, :, 0:64], PS[0][:, :].rearrange("p (h d) -> p h d", d=64), st16[:, 0:8].unsqueeze(2).to_broadcast([128, 8, 64]), ALU.mult),
                        reads=[PB[0], stb], writes=[bq_aug])
                    dve(_I("tensor_copy", q_aug[:, :, 64:68], aqc[:, t, :, :]), reads=[bC], writes=[bq_aug])
                    for (c0, s0, kk, gi) in ((0, 8, 0, 0), (256, 12, 2, 1)):
                        dve(_I("tensor_tensor", sq[0][:, 0:128].rearrange("p (h d) -> p h d", d=64), PS[1][:, c0:c0 + 128].rearrange("p (h d) -> p h d", d=64),
                                                                   st16[:, s0:s0 + 2].unsqueeze(2).to_broadcast([128, 2, 64]), ALU.mult), reads=[PB[1], stb], writes=[sqb[0]])
                        dve(_I("tensor_tensor", k_aug[:, kk:kk + 2, 0:64], sq[0][:, 0:128].rearrange("p (h d) -> p h d", d=64),
                                                                   gains[:, gi, :].unsqueeze(1).to_broadcast([128, 2, 64]), ALU.mult), reads=[sqb[0], bC], writes=[bk_aug])
                    dve(_I("tensor_copy", k_aug[:, :, 64:68], akc[:, t, :].unsqueeze(1).to_broadcast([128, 4, 4])), reads=[bC], writes=[bk_aug])
                    act(_I("activation", V_all[:, t, 0:2, 0:64], PS[1][:, 128:256].rearrange("p (h d) -> p h d", d=64), AF.Copy), reads=[PB[1]], writes=[bV])
                    act(_I("activation", V_all[:, t, 2:4, 0:64], PS[1][:, 384:512].rearrange("p (h d) -> p h d", d=64), AF.Copy), reads=[PB[1]], writes=[bV])
                    act(_I("activation", sg[:, t, :], PS[2][:, 0:24], AF.Sigmoid), reads=[PB[2]], writes=[bsg])
                    for h in range(8):
                        pe(_I("transpose", psb(3)[0:68, h * 128:(h + 1) * 128], q_aug[:, h, :], ident_b[:]), reads=[bq_aug, bC], writes=[PB[3]])
                    for kk in range(4):
                        pe(_I("transpose", psb(7)[0:68, kk * 128:(kk + 1) * 128], k_aug[:, kk, :], ident_b[:]), reads=[bk_aug, bC], writes=[PB[7]])
                    act(_I("activation", QT[0:68, :, ts], psb(3)[0:68, :].rearrange("p (h c) -> p h c", h=8), AF.Copy), reads=[PB[3]], writes=QTb)
                    dve(_I("tensor_copy", KT[0:68, 0:4, ts], psb(7)[0:68, 0:512].rearrange("p (h c) -> p h c", h=4)), reads=[PB[7]], writes=KTb[0:4])
                cs = slice(c * 512, (c + 1) * 512)
                for (c0, dst) in ((512, kcT), (640, vcT)):
                    for k in range(8):
                        pe(_I("matmul", PS[0][:, :], lhsT=Wn[:, k, c0:c0 + 128], rhs=hT[:, k, cs], start=(k == 0), stop=(k == 7)), reads=[hTb[c], bWn[k]], writes=[PB[0]])
                    act(_I("activation", dst[:, cs], PS[0][:, :], AF.Copy), reads=[PB[0]], writes=[bkc])

            S.barrier()
            W1 = Wn.rearrange("p k n -> p (k n)")[:, 0:2 * 32 * 128].rearrange("p (a l n) -> p a l n", a=2, l=32)
            W2 = U.take(2 * 64).rearrange("p (a n) -> p a n", a=2)
            peT = U.take(64)
            HT = U.take(128)
            bW1, bH = Buf(), Buf()
            for a, (w1d, w2d) in enumerate(((wck1_d, wck2_d), (wcv1_d, wcv2_d))):
                for half in range(2):
                    load_w(W1[half * 64:half * 64 + 64, a, :, :], w1d.rearrange("(l d) n -> d l n", d=64), bW1)
                load_w(W2[:, a, :], w2d, bW1)
            load_w(peT[0:64, :], peT_d, bW1)
            if s == 0:
                for a in range(2):
                    for l in range(32):
                        pe(_I("matmul", PS[2][:, a:a + 1], lhsT=W1[0:64, a, l, :], rhs=peT[0:64, a * 32 + l:a * 32 + l + 1], start=(l == 0), stop=(l == 31)),
                           reads=[bW1], writes=[PB[2]])
                dve(_I("tensor_copy", cb2[:], PS[2][:, 0:2]), reads=[PB[2]], writes=[bC])
            for a, srcT in enumerate((kcT, vcT)):
                for g in range(2):
                    base = g * 64
                    v3 = srcT[base:base + 64, :].rearrange("p (n s) -> p n s", s=16)
                    for l in range(32):
                        rhs = v3[:, (l // 16):(l // 16) + 127, l % 16]
                        pe(_I("matmul", PS[0][:, 0:127], lhsT=W1[base:base + 64, a, l, :], rhs=rhs, start=(l == 0), stop=(l == 31)),
                           reads=[bW1, bkc], writes=[PB[0]])
                    act(_I("activation", HT[:, 0:127], PS[0][:, 0:127], AF.Silu, bias=cb2[:, a:a + 1]), reads=[PB[0], bC], writes=[bH])
                    pe(_I("matmul", PS[1][0:127, 0:64], lhsT=HT[:, 0:127], rhs=W2[:, a, :], start=True, stop=True), reads=[bH, bW1], writes=[PB[1]])
                    if a == 0:
                        act(_I("activation", sq[0][0:127, 0:64], PS[1][0:127, 0:64], AF.Square, accum_out=st16[0:127, 0:1]), reads=[PB[1]], writes=[sqb[0], stb])
                        rstd_from(st16[0:127, 0:1])
                        dve(_I("tensor_scalar", sq[0][0:127, 0:64], PS[1][0:127, 0:64], st16[0:127, 0:1], None, ALU.mult), reads=[PB[1], stb], writes=[sqb[0]])
                        dve(_I("tensor_tensor", kc_aug[0:127, g, 0:64], sq[0][0:127, 0:64], gains[0:127, 2, :], ALU.mult), reads=[sqb[0], bC], writes=[bC])
                        pe(_I("transpose", psb(3)[0:68, 0:127], kc_aug[0:127, g, :], ident_b[0:127, 0:127]), reads=[bC], writes=[PB[3]])
                        dve(_I("tensor_copy", KcT[0:68, g, 0:127], psb(3)[0:68, 0:127]), reads=[PB[3]], writes=[bC])
                    else:
                        act(_I("activation", Vc[0:127, g, 0:64], PS[1][0:127, 0:64], AF.Copy), reads=[PB[1]], writes=[bC])

            imp = sb("imp_%d" % s, [128, 4, 32], F32) if s == 0 else imp
            impb = Buf()
            for t in range(NT):
                qs = slice(t * 128, (t + 1) * 128)
                for g in range(2):
                    for hh in range(4):
                        h = 4 * g + hh
                        pe(_I("matmul", PS[4][0:127, hh * 128:(hh + 1) * 128], lhsT=KcT[0:68, g, 0:127], rhs=QT[0:68, h, qs], start=True, stop=True),
                           reads=[bC, QTb[h]], writes=[PB[4]])
                    dve(_I("tensor_scalar", sq[1][0:127, :], PS[4][0:127, :], 60.0, None, ALU.min), reads=[PB[4]], writes=[sqb[1]])
                    act(_I("activation", PT[0][0:127, :], sq[1][0:127, :], AF.Exp), reads=[sqb[1]], writes=[PTb[0]])
                    dve(_I("tensor_tensor", PT[0][0:127, :].rearrange("p (h c) -> p h c", h=4), PT[0][0:127, :].rearrange("p (h c) -> p h c", h=4),
                                                  cmask[0:127, qs].unsqueeze(1).to_broadcast([127, 4, 128]), ALU.mult), reads=[bC, PTb[0]], writes=[PTb[0]])
                    for hh in range(4):
                        pe(_I("matmul", PS[7][:, hh * 97:(hh + 1) * 97], lhsT=PT[0][0:127, hh * 128:(hh + 1) * 128], rhs=Vc[0:127, g, :], start=True, stop=True),
                           reads=[PTb[0], bC], writes=[PB[7]])
                    o4 = PS[7][:, 0:388].rearrange("p (h c) -> p h c", h=4)
                    dve(_I("tensor_scalar", sm[:, 0:4], o4[:, :, 64], 1e-30, None, ALU.max), reads=[PB[7]], writes=[smb])
                    dve(_I("reciprocal", sm[:, 4:8], sm[:, 0:4]), reads=[smb], writes=[smb])
                    dve(_I("tensor_tensor", sm[:, 8:12], sm[:, 4:8], sg[:, t, :].rearrange("p (h r) -> p h r", r=3)[:, 4 * g:4 * g + 4, 0], ALU.mult), reads=[smb, bsg], writes=[smb])
                    dve(_I("tensor_tensor", oacc[:, 4 * g:4 * g + 4, :], o4[:, :, 0:64], sm[:, 8:12].unsqueeze(2).to_broadcast([128, 4, 64]), ALU.mult),
                        reads=[PB[7], smb], writes=[oab])
                    dve(_I("tensor_tensor", imp[:], o4[:, :, 65:97], sm[:, 4:8].unsqueeze(2).to_broadcast([128, 4, 32]), ALU.mult), reads=[PB[7], smb], writes=[impb])
                    dve(_I("tensor_reduce", sm[:, 16:48], imp[:].rearrange("p h j -> p j h"), AX.X, ALU.add), reads=[impb], writes=[smb])
                    dve(_I("tensor_tensor", sm[:, 16:48], sm[:, 16:48], selA[:, t, :], ALU.mult), reads=[smb, bC], writes=[smb])
                    dve(_I("tensor_tensor", sm[:, 16:48], sm[:, 16:48], selB[:, t, :], ALU.add), reads=[smb, bC], writes=[smb])
                    dve(_I("max", out=sm[:, 48:56], in_=sm[:, 16:48]), reads=[smb], writes=[smb])
                    dve(_I("match_replace", out=imp[:, 0, :], in_to_replace=sm[:, 48:56], in_values=sm[:, 16:48], imm_value=-3e38), reads=[smb], writes=[impb])
                    dve(_I("max", out=sm[:, 56:64], in_=imp[:, 0, :]), reads=[impb], writes=[smb])
                    dve(_I("tensor_scalar", sm[:, 16:48], sm[:, 16:48], sm[:, 63:64], None, ALU.is_ge), reads=[smb], writes=[smb])
                    dve(_I("tensor_scalar", obf[:, 0:32], sm[:, 16:48], -1.0, -NEG, ALU.add, ALU.mult), reads=[smb], writes=[obfb])
                    pe(_I("transpose", psb(3)[0:32, 0:128], obf[:, 0:32], ident_b[:]), reads=[obfb, bC], writes=[PB[3]])
                    act(_I("activation", MT[0:32, g, qs], psb(3)[0:32, 0:128], AF.Copy), reads=[PB[3]], writes=[MTb[g]])
                sg3 = sg[:, t, :].rearrange("p (h r) -> p h r", r=3)
                units = []
                for g in range(2):
                    pvb = next_pv()
                    tiles = [(j, "C" if j == t else None) for j in range(t + 1)]
                    for hh in range(4):
                        h = 4 * g + hh
                        units += make_units(QT[:, h, :], QTb[h], KT[:, g, :], KTb[g], g, t, tiles, (MT[:, g, :], MTb[g]), pvb, hh, banks=(4, 5, 0, 1))
                    units[-1]["post"] = (lambda pvb=pvb, g=g, gv=sg3[:, 4 * g:4 * g + 4, 1]: norm4(pvb, g, gv, [bsg], False))
                    pvb = next_pv()
                    tiles = [(j, "C" if j == t else ("W" if j == t - 4 else None)) for j in range(max(0, t - 4), t + 1)]
                    for hh in range(4):
                        h = 4 * g + hh
                        units += make_units(QT[:, h, :], QTb[h], KT[:, 2 + g, :], KTb[2 + g], 2 + g, t, tiles, None, pvb, hh, banks=(4, 5, 0, 1))
                    units[-1]["post"] = (lambda pvb=pvb, g=g, gv=sg3[:, 4 * g:4 * g + 4, 2]: norm4(pvb, g, gv, [bsg], False))
                run_units(units, 2)
                flush_o(t, 0)

            S.barrier()
            U.reset()
            V_all = U.take(NT * 5 * 65).rearrange("p (t k c) -> p t k c", t=NT, k=5)
            bV = Buf()
            pool(_I("memset", V_all[:, :, :, 64:65], 1.0), writes=[bV])
            vstate["V"] = V_all
            vstate["bV"] = bV
            Wd = U.take(8 * 1352).rearrange("p (k n) -> p k n", k=8)
            QT = U.take(8 * S_TOK).rearrange("p (h n) -> p h n", h=8)
            KT = U.take(5 * S_TOK).rearrange("p (h n) -> p h n", h=5)
            q_aug = U.take(8 * 68).rearrange("p (h d) -> p h d", h=8)
            k_aug = U.take(4 * 68).rearrange("p (h d) -> p h d", h=4)
            iqT = U.take(4 * S_TOK).rearrange("p (m n) -> p m n", m=4)
            ikT = U.take(S_TOK)
            iw = U.take(NT * 8, F32).rearrange("p (t c) -> p t c", c=8)
            sc = U.take(S_TOK, F32)
            rl = U.take(512, F32)
            maskq = U.take(S_TOK)
            maskT = U.take(S_TOK).rearrange("p (j c) -> p j c", c=128)
            junk = U.take(S_TOK)
            biq, bik, biw, bsc, brl, bmq, bmT, bjk = (Buf() for _ in range(8))
            bWd = [Buf() for _ in range(8)]
            QTb = [Buf() for _ in range(8)]
            KTb = [Buf() for _ in range(5)]
            for k in range(8):
                load_w(Wd[:, k, 0:1224], win_v[:, k, 1304:2528], bWd[k])
                load_w(Wd[:, k, 1224:1288], win_v[:, k, 2456:2520], bWd[k])
                load_w(Wd[:, k, 1288:1352], win_v[:, k, 2456:2520], bWd[k])
            for c in range(4):
                cs = slice(c * 512, (c + 1) * 512)
                for tl in range(4):
                    t = c * 4 + tl
                    ts = slice(t * 128, (t + 1) * 128)
                    for k in range(8):
                        pe(_I("matmul", PS[0][:, :], lhsT=hT[:, k, ts], rhs=Wd[:, k, 0:512], start=(k == 0), stop=(k == 7)), reads=[hTb[c], bWd[k]], writes=[PB[0]])
                    for k in range(8):
                        pe(_I("matmul", PS[1][:, 0:128], lhsT=hT[:, k, ts], rhs=Wd[:, k, 512:640], start=(k == 0), stop=(k == 7)), reads=[hTb[c], bWd[k]], writes=[PB[1]])
                    for k in range(8):
                        pe(_I("matmul", PS[2][:, 0:8], lhsT=hT[:, k, ts], rhs=Wd[:, k, 1216:1224], start=(k == 0), stop=(k == 7)), reads=[hTb[c], bWd[k]], writes=[PB[2]])
                    rms_heads(0, 8, st16[:, 0:8])
                    rms_heads(1, 1, st16[:, 8:9])
                    rstd_from(st16[:, 0:9])
                    dve(_I("tensor_tensor", q_aug[:, :, 0:64], PS[0][:, :].rearrange("p (h d) -> p h d", d=64), st16[:, 0:8].unsqueeze(2).to_broadcast([128, 8, 64]), ALU.mult),
                        reads=[PB[0], stb], writes=[bq_aug])
                    dve(_I("tensor_copy", q_aug[:, :, 64:68], aqc[:, t, :, :]), reads=[bC], writes=[bq_aug])
                    dve(_I("tensor_scalar", sq[0][:, 0:64], PS[1][:, 0:64], st16[:, 8:9], None, ALU.mult), reads=[PB[1], stb], writes=[sqb[0]])
                    dve(_I("tensor_tensor", k_aug[:, 0, 0:64], sq[0][:, 0:64], gains[:, 3, :], ALU.mult), reads=[sqb[0], bC], writes=[bk_aug])
                    dve(_I("tensor_copy", k_aug[:, 0, 64:68], akc[:, t, :]), reads=[bC], writes=[bk_aug])
                    act(_I("activation", V_all[:, t, 4, 0:64], PS[1][:, 64:128], AF.Copy), reads=[PB[1]], writes=[bV])
                    act(_I("activation", iw[:, t, :], PS[2][:, 0:8], AF.Copy, scale=8.0 ** -0.5), reads=[PB[2]], writes=[biw])
                    for h in range(8):
                        pe(_I("transpose", psb(3)[0:68, h * 128:(h + 1) * 128], q_aug[:, h, :], ident_b[:]), reads=[bq_aug, bC], writes=[PB[3]])
                    pe(_I("transpose", psb(7)[0:68, 0:128], k_aug[:, 0, :], ident_b[:]), reads=[bk_aug, bC], writes=[PB[7]])
                    act(_I("activation", QT[0:68, :, ts], psb(3)[0:68, :].rearrange("p (h c) -> p h c", h=8), AF.Copy), reads=[PB[3]], writes=QTb)
                    dve(_I("tensor_copy", KT[0:68, 4, ts], psb(7)[0:68, 0:128]), reads=[PB[7]], writes=[KTb[4]])
                for m in range(4):
                    for k in range(8):
                        pe(_I("matmul", PS[0][:, :], lhsT=Wd[:, k, 640 + m * 128:640 + (m + 1) * 128], rhs=hT[:, k, cs], start=(k == 0), stop=(k == 7)), reads=[hTb[c], bWd[k]], writes=[PB[0]])
                    act(_I("activation", iqT[:, m, cs], PS[0][:, :], AF.Copy, scale=0.125), reads=[PB[0]], writes=[biq])
                for k in range(8):
                    pe(_I("matmul", PS[1][:, :], lhsT=Wd[:, k, 1224:1352], rhs=hT[:, k, cs], start=(k == 0), stop=(k == 7)), reads=[hTb[c], bWd[k]], writes=[PB[1]])
                act(_I("activation", ikT[:, cs], PS[1][:, :], AF.Copy), reads=[PB[1]], writes=[bik])

            S.barrier()
            Wd_flat = Wd.rearrange("p k n -> p (k n)")
            scs = [sc, Wd_flat[:, 0:4096].bitcast(F32)]
            maskqs = [maskq, Wd_flat[:, 4096:6144]]
            maskTs = [maskT, Wd_flat[:, 6144:8192].rearrange("p (j c) -> p j c", c=128)]
            bscs, bmqs, bmTs = [Buf(), Buf()], [Buf(), Buf()], [Buf(), Buf()]
            smx = [sb("smx%d_%d" % (s, i), [128, 40], F32) for i in range(2)] if s == 0 else smx
            smxb = [Buf(), Buf()]

            def indexer(t):
                p = t % 2
                sc_, bsc_, sm_, smb_ = scs[p], bscs[p], smx[p], smxb[p]
                qs = slice(t * 128, (t + 1) * 128)
                nk = (t + 1) * 128
                nch = (nk + 511) // 512
                for h in range(8):
                    base = (h % 2) * 64
                    for cc in range(nch):
                        w = min(512, nk - cc * 512)
                        cs = slice(cc * 512, cc * 512 + w)
                        bk = cc % 2
                        pe(_I("matmul", PS[bk][:, 0:w], lhsT=iqT[base:base + 64, h // 2, qs], rhs=ikT[base:base + 64, cs], start=True, stop=True),
                           reads=[biq, bik], writes=[PB[bk]])
                        act(_I("activation", rl[:, 0:w], PS[bk][:, 0:w], AF.Relu), reads=[PB[bk]], writes=[brl])
                        if h == 0:
                            dve(_I("tensor_scalar", sc_[:, cs], rl[:, 0:w], iw[:, t, 0:1], None, ALU.mult), reads=[brl, biw], writes=[bsc_])
                        else:
                            dve(_I("scalar_tensor_tensor", sc_[:, cs], rl[:, 0:w], iw[:, t, h:h + 1], sc_[:, cs], ALU.mult, ALU.add), reads=[brl, biw, bsc_], writes=[bsc_])
                dve(_I("tensor_reduce", sm_[:, 0:1], sc_[:, 0:nk], AX.X, ALU.max, apply_absolute_value=True), reads=[bsc_], writes=[smb_])
                pool(_I("affine_select", sc_[:, t * 128:(t + 1) * 128], sc_[:, t * 128:(t + 1) * 128], [[-1, 128]], ALU.is_ge, -3e38, base=0, channel_multiplier=1), reads=[bsc_, smb_], writes=[bsc_])
                dve(_I("tensor_scalar", sm_[:, 8:8 + NBIS + 1], pow2[:], sm_[:, 0:1], None, ALU.mult), reads=[smb_, bC], writes=[smb_])
                dve(_I("memset", sm_[:, 1:2], 0.0), reads=[smb_], writes=[smb_])

            def bisect_iter(t, j):
                p = t % 2
                sc_, bsc_, sm_, smb_ = scs[p], bscs[p], smx[p], smxb[p]
                nk = (t + 1) * 128
                dve(_I("tensor_scalar", maskqs[p][:, 0:nk], sc_[:, 0:nk], sm_[:, 1:2], None, ALU.is_ge, ALU.add, accum_out=sm_[:, 2:3]), reads=[bsc_, smb_], writes=[bmqs[p], smb_])
                dve(_I("tensor_scalar", sm_[:, 3:4], sm_[:, 2:3], 255.5, -0.5, ALU.is_ge, ALU.add), reads=[smb_], writes=[smb_])
                dve(_I("scalar_tensor_tensor", sm_[:, 1:2], sm_[:, 3:4], sm_[:, 8 + j:9 + j], sm_[:, 1:2], ALU.mult, ALU.add), reads=[smb_], writes=[smb_])

            def finish_mask(t):
                p = t % 2
                sc_, bsc_, sm_, smb_ = scs[p], bscs[p], smx[p], smxb[p]
                nk = (t + 1) * 128
                dve(_I("tensor_tensor", sm_[:, 1:2], sm_[:, 1:2], sm_[:, 8 + NBIS:9 + NBIS], ALU.subtract), reads=[smb_], writes=[smb_])
                dve(_I("tensor_scalar", maskqs[p][:, 0:nk], sc_[:, 0:nk], sm_[:, 1:2], None, ALU.is_ge), reads=[bsc_, smb_], writes=[bmqs[p]])
                for j in range(t + 1):
                    bk = 3 if (j // 8) % 2 == 0 else 7
                    pe(_I("transpose", psb(bk)[:, (j % 8) * 128:(j % 8 + 1) * 128], maskqs[p][:, j * 128:(j + 1) * 128], ident_b[:]), reads=[bmqs[p], bC], writes=[PB[bk]])
                    if j % 8 == 7 or j == t:
                        j0 = (j // 8) * 8
                        n = j - j0 + 1
                        act(_I("activation", maskTs[p][:, j0:j0 + n, :].rearrange("p j c -> p (j c)"), psb(bk)[:, 0:n * 128], AF.Copy), reads=[PB[bk]], writes=[bmTs[p]])

            indexer(0)
            for j in range(NBIS):
                bisect_iter(0, j)
            finish_mask(0)
            for t in range(NT):
                if t + 1 < NT:
                    indexer(t + 1)
                tiles = [(j, None) for j in range(t + 1)]
                units = []
                for g2 in range(2):
                    pvb = next_pv()
                    for hh in range(4):
                        h = 4 * g2 + hh
                        units += make_units(QT[:, h, :], QTb[h], KT[:, 4, :], KTb[4], 4, t, tiles, None, pvb, hh, full_mask=(maskTs[t % 2], bmTs[t % 2]))
                    units[-1]["post"] = (lambda pvb=pvb, g2=g2: norm4(pvb, g2, None, [], True))
                done = [0]

                def between(i, n, t=t, done=done):
                    if t + 1 >= NT:
                        return
                    target = ((i + 1) * NBIS) // n
                    while done[0] < target:
                        bisect_iter(t + 1, done[0])
                        done[0] += 1
                run_units(units, 1, between)
                if t + 1 < NT:
                    while done[0] < NBIS:
                        bisect_iter(t + 1, done[0])
                        done[0] += 1
                    finish_mask(t + 1)
                flush_o(t, 1)

            for hf in range(2):
                S.barrier()
                U.reset()
                Wg = U.take(8 * 2048).rearrange("p (k n) -> p k n", k=8)
                Woa = U.take(4 * D).rearrange("p (k n) -> p k n", k=4)
                Wob = U.take(4 * D).rearrange("p (k n) -> p k n", k=4)
                Wout = U.take(8 * D).rearrange("p (k n) -> p k n", k=8)
                Wff_region = (Wg, Woa, Wob, Wout)
                xacc = U.take(8 * D, F32).rearrange("p (t n) -> p t n", t=8)
                yT = U.take(8 * 512).rearrange("p (k n) -> p k n", k=8)
                oaT = U.take(4 * 512).rearrange("p (k n) -> p k n", k=4)
                obT = U.take(4 * 512).rearrange("p (k n) -> p k n", k=4)
                sga = U.take(512)
                sgb = U.take(512)
                t1 = U.take(512, F32)
                t2 = U.take(512, F32)
                aT = yT
                g1bc = U.take(D, F32)
                g2bc = U.take(D, F32)
                bG = Buf()
                S.dma(g1bc, modrow_d[b:b + 1, 2 * D:3 * D].partition_broadcast(128), writes=[bG])
                S.dma(g2bc, modrow_d[b:b + 1, 5 * D:6 * D].partition_broadcast(128), writes=[bG])
                bya, byT, boa, bob, bsga, bsgb, bt1, bt2, baT = (Buf() for _ in range(9))
                bWg = [Buf() for _ in range(8)]
                bWo = [Buf() for _ in range(8)]
                bWa = [Buf() for _ in range(4)]
                bWb = [Buf() for _ in range(4)]
                xab = [Buf() for _ in range(8)]
                for k in range(8):
                    load_w(Wg[:, k, :], win_v[:, k, 2528:4576], bWg[k])
                    load_w(Wout[:, k, :], wout_d.rearrange("(k p) n -> p k n", p=128)[:, k, :], bWo[k])
                for k in range(4):
                    load_w(Woa[:, k, :], woa_d.rearrange("(k p) n -> p k n", p=128)[:, k, :], bWa[k])
                    load_w(Wob[:, k, :], wob_d.rearrange("(k p) n -> p k n", p=128)[:, k, :], bWb[k])
                for cl in range(2):
                    c = hf * 2 + cl
                    cs = slice(c * 512, (c + 1) * 512)
                    S.dma(oaT, oT_d[0, :, :, cs].rearrange("j p c -> p j c"), reads=[bOT[0]], writes=[boa])
                    S.dma(obT, oT_d[1, :, :, cs].rearrange("j p c -> p j c"), reads=[bOT[1]], writes=[bob])
                    for f in range(8):
                        fs = slice(f * 128, (f + 1) * 128)
                        for k in range(8):
                            pe(_I("matmul", PS[0][:, :], lhsT=Wg[:, k, fs], rhs=hT[:, k, cs], start=(k == 0), stop=(k == 7)), reads=[bWg[k], hTb[c]], writes=[PB[0]])
                        act(_I("activation", sga, PS[0][:, :], AF.Sigmoid), reads=[PB[0]], writes=[bsga])
                        for k in range(8):
                            pe(_I("matmul", PS[1][:, :], lhsT=Wg[:, k, 1024 + f * 128:1024 + (f + 1) * 128], rhs=hT[:, k, cs], start=(k == 0), stop=(k == 7)), reads=[bWg[k], hTb[c]], writes=[PB[1]])
                        act(_I("activation", sgb, PS[1][:, :], AF.Sigmoid), reads=[PB[1]], writes=[bsgb])
                        for k in range(4):
                            pe(_I("matmul", PS[2][:, :], lhsT=Woa[:, k, fs], rhs=oaT[:, k, :], start=(k == 0), stop=(k == 3)), reads=[bWa[k], boa], writes=[PB[2]])
                        for k in range(4):
                            pe(_I("matmul", PS[4][:, :], lhsT=Wob[:, k, fs], rhs=obT[:, k, :], start=(k == 0), stop=(k == 3)), reads=[bWb[k], bob], writes=[PB[4]])
                        dve(_I("tensor_tensor", t1, PS[2][:, :], sga, ALU.mult), reads=[PB[2], bsga], writes=[bt1])
                        dve(_I("tensor_tensor", t2, PS[4][:, :], sgb, ALU.mult), reads=[PB[4], bsgb], writes=[bt2])
                        dve(_I("tensor_tensor", yT[:, f, :], t1, t2, ALU.add), reads=[bt1, bt2], writes=[byT])
                    for tl in range(4):
                        t = c * 4 + tl
                        tt = t - hf * 8
                        i = t % 2
                        S.dma(xt[i][:], x_d[s, t * 128:(t + 1) * 128, :], writes=[xtb[i]])
                        for h2 in range(2):
                            ns = slice(h2 * 512, (h2 + 1) * 512)
                            bk = 5 + h2
                            for k in range(8):
                                pe(_I("matmul", PS[bk][:, :], lhsT=yT[:, k, tl * 128:(tl + 1) * 128], rhs=Wout[:, k, ns], start=(k == 0), stop=(k == 7)), reads=[byT, bWo[k]], writes=[PB[bk]])
                            dve(_I("tensor_tensor", t1, PS[bk][:, :], g1bc[:, ns], ALU.mult), reads=[PB[bk], bG], writes=[bt1])
                            dve(_I("tensor_tensor", xacc[:, tt, ns], t1, xt[i][:, ns], ALU.add), reads=[bt1, xtb[i]], writes=[xab[tt]])
                        if dbg and s == 0:
                            S.dma(x1_dbg[t * 128:(t + 1) * 128, :], xacc[:, tt, :], reads=[xab[tt]], writes=[Buf()], is_output=True)
                        layernorm(xacc[:, tt, :], xab[tt], tl % 2, 1, b, 0)
                        if tl % 2 == 1:
                            ln_evac(t // 2, 1, b, 0, hTb[c])
                S.barrier()
                for (f0_, nf) in ((0, 8), (8, 8), (16, 6)):
                    Wfg = Wg.rearrange("p k n -> p (k n)")[:, 0:8 * 1024].rearrange("p (k n) -> p k n", k=8)
                    Wfu = Wg.rearrange("p k n -> p (k n)")[:, 8 * 1024:16 * 1024].rearrange("p (k n) -> p k n", k=8)
                    Wfd = Wout.rearrange("p k n -> p (k n)")[:, 0:8 * D].rearrange("p (k n) -> p k n", k=8)
                    nfc = nf * 128
                    if f0_ == 0:
                        bFg = [Buf() for _ in range(8)]
                        bFu = [Buf() for _ in range(8)]
                        bFd = [Buf() for _ in range(8)]
                    for k in range(8):
                        load_w(Wfg[:, k, 0:nfc], wfg_d.rearrange("(k p) n -> p k n", p=128)[:, k, f0_ * 128:f0_ * 128 + nfc], bFg[k])
                        load_w(Wfu[:, k, 0:nfc], wfu_d.rearrange("(k p) n -> p k n", p=128)[:, k, f0_ * 128:f0_ * 128 + nfc], bFu[k])
                    for k in range(nf):
                        load_w(Wfd[:, k, :], wfd_d[(f0_ + k) * 128:(f0_ + k + 1) * 128, :], bFd[k])
                    for cl in range(2):
                        c = hf * 2 + cl
                        cs = slice(c * 512, (c + 1) * 512)
                        for f in range(nf):
                            fs = slice(f * 128, (f + 1) * 128)
                            for k in range(8):
                                pe(_I("matmul", PS[0][:, :], lhsT=Wfg[:, k, fs], rhs=hT[:, k, cs], start=(k == 0), stop=(k == 7)), reads=[bFg[k], hTb[c]], writes=[PB[0]])
                            for k in range(8):
                                pe(_I("matmul", PS[1][:, :], lhsT=Wfu[:, k, fs], rhs=hT[:, k, cs], start=(k == 0), stop=(k == 7)), reads=[bFu[k], hTb[c]], writes=[PB[1]])
                            act(_I("activation", t1, PS[0][:, :], AF.Silu), reads=[PB[0]], writes=[bt1])
                            dve(_I("tensor_tensor", aT[:, f, :], t1, PS[1][:, :], ALU.mult), reads=[bt1, PB[1]], writes=[baT])
                        for tl in range(4):
                            tt = cl * 4 + tl
                            for h2 in range(2):
                                ns = slice(h2 * 512, (h2 + 1) * 512)
                                bk = 5 + h2
                                for f in range(nf):
                                    pe(_I("matmul", PS[bk][:, :], lhsT=aT[:, f, tl * 128:(tl + 1) * 128], rhs=Wfd[:, f, ns], start=(f == 0), stop=(f == nf - 1)), reads=[baT, bFd[f]], writes=[PB[bk]])
                                dve(_I("tensor_tensor", t2, PS[bk][:, :], g2bc[:, ns], ALU.mult), reads=[PB[bk], bG], writes=[bt2])
                                dve(_I("tensor_tensor", xacc[:, tt, ns], xacc[:, tt, ns], t2, ALU.add), reads=[bt2, xab[tt]], writes=[xab[tt]])
                for tt in range(8):
                    t = hf * 8 + tt
                    S.dma(out_d[s, t * 128:(t + 1) * 128, :], xacc[:, tt, :], reads=[xab[tt]], writes=[Buf()], is_output=True)
        S.emit()
    return nc


def _prep_common(inp):
    f = lambda a: np.ascontiguousarray(np.asarray(a, dtype=np.float32))
    gn = np.concatenate([inp["g_norm1"][0].reshape(8, 128).T, inp["g_norm2"][0].reshape(8, 128).T], axis=1)
    gvec = np.concatenate([inp[k][0] for k in ("g_q_a", "g_kc_a", "g_ks_a", "g_kw_a", "g_q_b", "g_k_b")])[None, :]
    peT = np.concatenate([inp["pe_ck"][0].T, inp["pe_cv"][0].T], axis=1)
    return {
        "w_ada": f(inp["w_ada"][0]), "b_ada": f(inp["b_ada"]), "gn": f(gn), "w_in": f(inp["w_in"][0]),
        "gvec": f(gvec), "peT": f(peT), "w_ck1": f(inp["w_ck1"][0]), "w_ck2": f(inp["w_ck2"][0]),
        "w_cv1": f(inp["w_cv1"][0]), "w_cv2": f(inp["w_cv2"][0]), "w_o_a": f(inp["w_o_a"][0]),
        "w_o_b": f(inp["w_o_b"][0]), "w_out": f(inp["w_out"][0]), "w_ff_gate": f(inp["w_ff_gate"][0]),
        "w_ff_up": f(inp["w_ff_up"][0]), "w_ff_down": f(inp["w_ff_down"][0]),
    }


def _core_map(common, x, c, i, nseq):
    m = dict(common)
    m["x"] = np.ascontiguousarray(x[i * nseq:(i + 1) * nseq])
    cc = np.zeros((4, D), np.float32)
    cc[:nseq] = c[i * nseq:(i + 1) * nseq]
    m["cT"] = np.ascontiguousarray(cc.T.reshape(8, 128, 4).transpose(1, 0, 2))
    return m


def kernel(**inputs):
    x = np.asarray(inputs["x"], dtype=np.float32)
    c = np.asarray(inputs["c"], dtype=np.float32)
    n = 8
    nseq = x.shape[0] // n
    nc = build_nc(nseq)
    common = _prep_common(inputs)
    in_maps = [_core_map(common, x, c, i, nseq) for i in range(n)]
    res = run_bass_kernel_spmd(nc, in_maps, core_ids=list(range(n)))
    return np.concatenate([r["out"] for r in res.results], axis=0).astype(np.float32)
```

```python
import contextlib
import numpy as np
import concourse.bass as bass
import concourse.mybir as mybir
from concourse.bass_utils import run_bass_kernel_spmd

F32 = mybir.dt.float32
BF16 = mybir.dt.bfloat16
AF = mybir.ActivationFunctionType
ALU = mybir.AluOpType
AX = mybir.AxisListType

S_TOK = 2048
D = 1024
NT = 16
DIN = 4576
DFF = 2816
EPS = 1e-6
NBIS = 22
NEG = -30000.0


class Buf:
    __slots__ = ("name", "w", "r")

    def __init__(self, name=""):
        self.name = name
        self.w = None
        self.r = {}


class Sched:
    ENGS = ("pe", "act", "dve", "pool", "sp")
    NDMA = 24

    def __init__(self, nc):
        self.nc = nc
        self.streams = {e: [] for e in self.ENGS}
        self.count = {e: 0 for e in self.ENGS}
        self.waited = {e: {} for e in self.ENGS}
        self.dma_uses = [0] * self.NDMA
        self.dma_rr = 0
        self.out_events = []

    def _deps(self, eng, reads, writes):
        deps = {}

        def add(ev):
            if ev is None:
                return
            k, v = ev
            if deps.get(k, 0) < v:
                deps[k] = v
        for b in reads:
            add(b.w)
        for b in writes:
            if b.w is not None and b.w[0] != eng:
                add(b.w)
            for k, v in b.r.items():
                if k != eng:
                    add((k, v))
        waits = []
        for k, v in deps.items():
            if k == "pe" and eng == "pe":
                continue
            if self.waited[eng].get(k, 0) < v:
                self.waited[eng][k] = v
                waits.append((k, v))
        return waits

    def _commit(self, ev, reads, writes):
        k, v = ev
        for b in writes:
            b.w = ev
            b.r = {}
        for b in reads:
            if b.r.get(k, 0) < v:
                b.r[k] = v

    def op(self, eng, fn, reads=(), writes=()):
        waits = self._deps(eng, reads, writes)
        self.count[eng] += 1
        ev = (eng, self.count[eng])
        self.streams[eng].append((fn, waits, ev))
        self._commit(ev, reads, writes)
        return ev

    def dma(self, out, in_, reads=(), writes=(), q="sp", is_output=False, **kw):
        i = self.dma_rr
        self.dma_rr = (self.dma_rr + 1) % self.NDMA
        waits = self._deps(q, reads, writes)
        key = "dma%d" % i
        prev = self.dma_uses[i] * 16
        if prev and self.waited[q].get(key, 0) < prev:
            self.waited[q][key] = prev
            waits.append((key, prev))
        self.dma_uses[i] += 1
        ev = (key, self.dma_uses[i] * 16)
        fn = lambda e, out=out, in_=in_, kw=kw: e.dma_start(out=out, in_=in_, **kw)
        self.streams[q].append((fn, waits, ev))
        self._commit(ev, reads, writes)
        if is_output:
            self.out_events.append(ev)
        return ev

    def barrier(self):
        allv = [(e, self.count[e]) for e in self.ENGS if self.count[e]]
        allv += [("dma%d" % i, self.dma_uses[i] * 16) for i in range(self.NDMA) if self.dma_uses[i]]
        for e in self.ENGS:
            waits = []
            for k, v in allv:
                if k == e:
                    continue
                if self.waited[e].get(k, 0) < v:
                    self.waited[e][k] = v
                    waits.append((k, v))
            if waits:
                self.streams[e].append((None, waits, None))

    def emit(self):
        nc = self.nc
        with contextlib.ExitStack() as st:
            sems = {}
            for e in self.ENGS:
                sems[e] = st.enter_context(nc.semaphore("s_" + e))
            for i in range(self.NDMA):
                sems["dma%d" % i] = st.enter_context(nc.semaphore("s_dma%d" % i))
            final = {}
            for k, v in self.out_events:
                final[k] = max(final.get(k, 0), v)
            block = st.enter_context(nc.Block())

            def run(engname, e):
                for fn, waits, ev in self.streams[engname]:
                    for k, v in waits:
                        e.wait_ge(sems[k], v)
                    if fn is None:
                        continue
                    ins = fn(e)
                    k, v = ev
                    ins.then_inc(sems[k], 16 if k.startswith("dma") else 1)
                if engname == "sp":
                    for k, v in final.items():
                        e.wait_ge(sems[k], v)

            @block.tensor
            def _(e):
                run("pe", e)

            @block.scalar
            def _(e):
                run("act", e)

            @block.vector
            def _(e):
                run("dve", e)

            @block.gpsimd
            def _(e):
                run("pool", e)

            @block.sync
            def _(e):
                run("sp", e)


class Arena:
    def __init__(self, ap16):
        self.ap = ap16
        self.off = 0

    def reset(self):
        self.off = 0

    def take(self, ncols, dt=BF16):
        if dt == F32:
            self.off = (self.off + 1) // 2 * 2
            n16 = ncols * 2
        else:
            n16 = ncols
        assert self.off + n16 <= self.ap.shape[1], (self.off, n16, self.ap.shape)
        v = self.ap[:, self.off:self.off + n16]
        self.off += n16
        self.off = (self.off + 1) // 2 * 2
        return v.bitcast(F32) if dt == F32 else v


_REGS = {}


def _I(name, *args, **kw):
    if name == "affine_select":
        def thunk(e):
            a = list(args)
            key = (id(e), float(a[4]))
            if key not in _REGS:
                _REGS[key] = e.to_reg(float(a[4]))
            a[4] = _REGS[key]
            return e.affine_select(*a, **kw)
        return thunk
    return lambda e: getattr(e, name)(*args, **kw)


def build_nc(nseq=4, dbg=False):
    nc = bass.Bass("TRN2", target_bir_lowering=False)
    _REGS.clear()
    S = Sched(nc)

    def din(name, shape):
        return nc.dram_tensor(name, shape, F32, kind="ExternalInput").ap()
    x_d = din("x", [nseq, S_TOK, D])
    cT_d = din("cT", [128, 8, 4])
    wada_d = din("w_ada", [D, 6 * D])
    bada_d = din("b_ada", [1, 6 * D])
    gn_d = din("gn", [128, 16])
    win_d = din("w_in", [D, DIN])
    gv_d = din("gvec", [1, 6 * 64])
    peT_d = din("peT", [64, 64])
    wck1_d = din("w_ck1", [2048, 128])
    wck2_d = din("w_ck2", [128, 64])
    wcv1_d = din("w_cv1", [2048, 128])
    wcv2_d = din("w_cv2", [128, 64])
    woa_d = din("w_o_a", [512, D])
    wob_d = din("w_o_b", [512, D])
    wout_d = din("w_out", [D, D])
    wfg_d = din("w_ff_gate", [D, DFF])
    wfu_d = din("w_ff_up", [D, DFF])
    wfd_d = din("w_ff_down", [DFF, D])
    out_d = nc.dram_tensor("out", [nseq, S_TOK, D], F32, kind="ExternalOutput").ap()
    modrow_d = nc.dram_tensor("modrow", [4, 6 * D], F32, kind="Internal").ap()
    oT_d = nc.dram_tensor("oT_scr", [2, 4, 128, S_TOK], BF16, kind="ExternalOutput" if dbg else "Internal").ap()
    if dbg:
        hT_dbg = nc.dram_tensor("hT_dbg", [128, 8, S_TOK], BF16, kind="ExternalOutput").ap()
        x1_dbg = nc.dram_tensor("x1_dbg", [S_TOK, D], F32, kind="ExternalOutput").ap()
        mod_dbg = nc.dram_tensor("mod_dbg", [4, 6 * D], F32, kind="ExternalOutput").ap()

    st = contextlib.ExitStack()

    def sb(name, shape, dt=F32):
        return st.enter_context(nc.sbuf_tensor(name, shape, dt))

    with st:
        PS = [st.enter_context(nc.psum_tensor("ps%d" % i, [128, 512], F32)) for i in range(8)]
        PB = [Buf("ps%d" % i) for i in range(8)]

        def psb(i):
            return PS[i][:].bitcast(BF16)

        ident_b = sb("ident_b", [128, 128], BF16)
        ident_f = sb("ident_f", [128, 128], F32)
        Cm = sb("Cm", [128, 128], BF16)
        Wm = sb("Wm", [128, 128], BF16)
        cmask = sb("cmask", [128, S_TOK], BF16)
        ET = sb("ET", [68, S_TOK], BF16)
        selA = sb("selA", [128, NT, 32], F32)
        selB = sb("selB", [128, NT, 32], F32)
        aqc = sb("aqc", [128, NT, 8, 4], BF16)
        akc = sb("akc", [128, NT, 4], BF16)
        gbc = sb("gbc", [128, 6 * 64], F32)
        gains = sb("gains", [128, 4, 64], F32)
        gnc = sb("gnc", [128, 16], F32)
        modT = sb("modT", [128, 32, 4], F32)
        cb2 = sb("cb2", [128, 2], F32)
        pow2 = sb("pow2", [128, NBIS + 1], F32)
        hT = sb("hT", [128, 8, S_TOK], BF16)
        Vc = sb("Vc", [128, 2, 97], BF16)
        kc_aug = sb("kc_aug", [128, 2, 68], BF16)
        KcT = sb("KcT", [128, 2, 128], BF16)
        U_t = sb("U", [128, 65400], BF16)
        U = Arena(U_t[:])
        bC = Buf("consts")
        bOT = [Buf(), Buf()]
        bOut = Buf()

        hTb = [Buf("hT%d" % c) for c in range(4)]

        def pool(fn, reads=(), writes=()):
            return S.op("pool", fn, reads, writes)

        def dve(fn, reads=(), writes=()):
            return S.op("dve", fn, reads, writes)

        def act(fn, reads=(), writes=()):
            return S.op("act", fn, reads, writes)

        def pe(fn, reads=(), writes=()):
            return S.op("pe", fn, reads, writes)

        pool(_I("memset", ident_b[:], 1.0), writes=[bC])
        pool(_I("affine_select", ident_b[:], ident_b[:], [[1, 128]], ALU.is_equal, 0.0, base=0, channel_multiplier=-1), reads=[bC], writes=[bC])
        pool(_I("memset", ident_f[:], 1.0), writes=[bC])
        pool(_I("affine_select", ident_f[:], ident_f[:], [[1, 128]], ALU.is_equal, 0.0, base=0, channel_multiplier=-1), reads=[bC], writes=[bC])
        pool(_I("memset", Cm[:], 1.0), writes=[bC])
        pool(_I("affine_select", Cm[:], Cm[:], [[1, 128]], ALU.is_ge, 0.0, base=0, channel_multiplier=-1), reads=[bC], writes=[bC])
        pool(_I("memset", Wm[:], 1.0), writes=[bC])
        pool(_I("affine_select", Wm[:], Wm[:], [[-1, 128]], ALU.is_gt, 0.0, base=0, channel_multiplier=1), reads=[bC], writes=[bC])
        pool(_I("memset", cmask[:], 1.0), writes=[bC])
        pool(_I("affine_select", cmask[:], cmask[:], [[1, S_TOK]], ALU.is_ge, 0.0, base=-31, channel_multiplier=-16), reads=[bC], writes=[bC])
        pool(_I("memset", ET[0:32, :], 1.0), writes=[bC])
        pool(_I("memset", ET[32:64, :], 0.0), writes=[bC])
        pool(_I("memset", ET[64:68, :], 0.0), writes=[bC])
        pool(_I("affine_select", ET[0:32, :], ET[0:32, :], [[1, S_TOK]], ALU.is_ge, 0.0, base=0, channel_multiplier=-64), reads=[bC], writes=[bC])
        pool(_I("affine_select", ET[0:32, :], ET[0:32, :], [[-1, S_TOK]], ALU.is_ge, 0.0, base=63, channel_multiplier=64), reads=[bC], writes=[bC])
        for g in range(2):
            pool(_I("memset", Vc[:, g, 64:97], 1.0), writes=[bC])
            pool(_I("affine_select", Vc[:, g, 65:97], Vc[:, g, 65:97], [[-64, 32]], ALU.is_ge, 0.0, base=31, channel_multiplier=16), reads=[bC], writes=[bC])
            pool(_I("affine_select", Vc[:, g, 65:97], Vc[:, g, 65:97], [[64, 32]], ALU.is_ge, 0.0, base=63, channel_multiplier=-16), reads=[bC], writes=[bC])
        Dt = U.take(NT * 32, F32).rearrange("p (t j) -> p t j", j=32)
        jt = U.take(NT * 32, F32).rearrange("p (t j) -> p t j", j=32)
        f0 = U.take(NT * 32, F32).rearrange("p (t j) -> p t j", j=32)
        for lo_, base in ((0, 0), (64, -1)):
            pool(_I("iota", Dt[lo_:lo_ + 64], [[-2, NT], [1, 32]], base=base, channel_multiplier=0, allow_small_or_imprecise_dtypes=True), writes=[bC])
        pool(_I("iota", jt[:], [[0, NT], [1, 32]], base=0, channel_multiplier=0, allow_small_or_imprecise_dtypes=True), writes=[bC])
        dve(_I("tensor_single_scalar", f0[:], jt[:], 0.0, ALU.is_equal), reads=[bC], writes=[bC])
        dve(_I("tensor_single_scalar", jt[:], Dt[:], 0.0, ALU.is_equal), reads=[bC], writes=[bC])
        dve(_I("tensor_max", f0[:], f0[:], jt[:]), reads=[bC], writes=[bC])
        dve(_I("tensor_single_scalar", jt[:], Dt[:], -1.0, ALU.is_equal), reads=[bC], writes=[bC])
        dve(_I("tensor_max", f0[:], f0[:], jt[:]), reads=[bC], writes=[bC])
        dve(_I("tensor_single_scalar", jt[:], Dt[:], 0.0, ALU.is_le), reads=[bC], writes=[bC])
        dve(_I("tensor_sub", selA[:], jt[:], f0[:]), reads=[bC], writes=[bC])
        dve(_I("tensor_add", selB[:], jt[:], f0[:]), reads=[bC], writes=[bC])
        dve(_I("tensor_scalar", selB[:], selB[:], -1.0, 1e9, ALU.add, ALU.mult), reads=[bC], writes=[bC])
        hi_t = sb("hi_t", [128, NT], F32)
        lo_t = sb("lo_t", [128, 1], F32)
        for lo_, base in ((0, 0), (64, 64)):
            pool(_I("iota", hi_t[lo_:lo_ + 64], [[128, NT]], base=base, channel_multiplier=0, allow_small_or_imprecise_dtypes=True), writes=[bC])
            pool(_I("iota", lo_t[lo_:lo_ + 64], [[0, 1]], base=0, channel_multiplier=1, allow_small_or_imprecise_dtypes=True), writes=[bC])
        for h in range(8):
            sl = 2.0 ** -(h + 1)
            dve(_I("memset", aqc[:, :, h, 0:2], sl), reads=[bC], writes=[bC])
            dve(_I("tensor_scalar", aqc[:, :, h, 2], hi_t[:], -sl, None, ALU.mult), reads=[bC], writes=[bC])
            dve(_I("tensor_scalar", aqc[:, :, h, 3], lo_t[:].to_broadcast([128, NT]), -sl, None, ALU.mult), reads=[bC], writes=[bC])
        dve(_I("memset", akc[:, :, 2:4], 1.0), reads=[bC], writes=[bC])
        dve(_I("tensor_copy", akc[:, :, 0], hi_t[:]), reads=[bC], writes=[bC])
        dve(_I("tensor_copy", akc[:, :, 1], lo_t[:].to_broadcast([128, NT])), reads=[bC], writes=[bC])
        pn = sb("pn", [128, 1], F32)
        pool(_I("iota", pn[:], [[0, 1]], base=0, channel_multiplier=16, allow_small_or_imprecise_dtypes=True), writes=[bC])
        for g in range(2):
            dve(_I("tensor_copy", kc_aug[:, g, 64:65], pn[:]), reads=[bC], writes=[bC])
            dve(_I("memset", kc_aug[:, g, 65:66], 31.0), reads=[bC], writes=[bC])
            dve(_I("memset", kc_aug[:, g, 66:68], 1.0), reads=[bC], writes=[bC])
        for j in range(NBIS + 1):
            dve(_I("memset", pow2[:, j:j + 1], 2.0 ** -j), reads=[bC], writes=[bC])
        S.dma(gbc[:], gv_d.partition_broadcast(128), writes=[bC])
        S.dma(gnc[:], gn_d, writes=[bC])

        def gsl(i):
            return gbc[:, i * 64:(i + 1) * 64]
        for idx, (gk, gq) in enumerate(((2, 0), (3, 0), (1, 0), (5, 4))):
            dve(_I("scalar_tensor_tensor", gains[:, idx, :], gsl(gk), 0.125, gsl(gq), ALU.mult, ALU.mult), reads=[bC], writes=[bC])

        S.barrier()
        U.reset()
        scT = U.take(32, F32).rearrange("p (k b) -> p k b", b=4)
        wchunk = U.take(8 * 512, F32).rearrange("p (k n) -> p k n", k=8)
        bchunk = U.take(512, F32)
        mchunk = U.take(512, F32)
        bsc, bw, bbc, bm = Buf(), Buf(), Buf(), Buf()
        S.dma(scT, cT_d, writes=[bsc])
        act(_I("activation", scT, scT, AF.Silu), reads=[bsc], writes=[bsc])
        wada_v = wada_d.rearrange("(k p) n -> p k n", p=128)
        LNV = {0: 0, 1: 1, 3: 2, 4: 3}
        for c in range(12):
            S.dma(wchunk, wada_v[:, :, c * 512:(c + 1) * 512], writes=[bw])
            S.dma(bchunk[0:4, :], bada_d[:, c * 512:(c + 1) * 512].partition_broadcast(4), writes=[bbc])
            for k in range(8):
                pe(_I("matmul", PS[0][0:4, :], lhsT=scT[:, k, :], rhs=wchunk[:, k, :], start=(k == 0), stop=(k == 7)), reads=[bsc, bw], writes=[PB[0]])
            dve(_I("tensor_tensor", mchunk[0:4, :], PS[0][0:4, :], bchunk[0:4, :], ALU.add), reads=[PB[0], bbc], writes=[bm])
            S.dma(modrow_d[:, c * 512:(c + 1) * 512], mchunk[0:4, :], reads=[bm], writes=[bC])
            if dbg:
                S.dma(mod_dbg[:, c * 512:(c + 1) * 512], mchunk[0:4, :], reads=[bm], writes=[Buf()], is_output=True)
            vec, half = c // 2, c % 2
            if vec in LNV:
                for i in range(4):
                    col = (LNV[vec] * 8 + half * 4 + i) * 4
                    pe(_I("transpose", PS[1][:, col:col + 4], mchunk[0:4, i * 128:(i + 1) * 128], ident_f[0:4, 0:4]), reads=[bm, bC], writes=[PB[1]])
        dve(_I("tensor_copy", modT[:].rearrange("p a b -> p (a b)"), PS[1][:, 0:128]), reads=[PB[1]], writes=[bC])
        for which, gi in ((1, 0), (3, 1)):
            dve(_I("scalar_tensor_tensor",
                modT[:, which * 8:(which + 1) * 8, :], modT[:, which * 8:(which + 1) * 8, :], 1.0,
                gnc[:, gi * 8:(gi + 1) * 8].unsqueeze(2).to_broadcast([128, 8, 4]), ALU.add, ALU.mult), reads=[bC], writes=[bC])
        S.barrier()

        xt = [sb("xt%d" % i, [128, D], F32) for i in range(2)]
        xtb = [Buf() for _ in range(2)]
        xn = sb("xn", [128, D], BF16)
        xnb = Buf()
        sq = [sb("sq%d" % i, [128, 512], F32) for i in range(2)]
        sqb = [Buf(), Buf()]
        st16 = sb("st16", [128, 16], F32)
        stb = Buf()
        PT = [sb("PT%d" % i, [128, 512], BF16) for i in range(6)]
        PTb = [Buf() for _ in range(6)]
        sn = sb("sn", [128, 16], F32)
        snb = Buf()
        tmpo = sb("tmpo", [128, 4, 64], F32)
        tmpb = Buf()
        oacc = sb("oacc", [128, 8, 64], F32)
        oab = Buf()
        obf = sb("obf", [128, 512], BF16)
        obfb = Buf()
        oTs = sb("oTs", [128, 4, 128], BF16)
        oTsb = Buf()
        sm = sb("sm", [128, 64], F32)
        smb = Buf()
        state = {"pt": 0, "sb": 0}
        vstate = {}

        def layernorm(src_tile, src_buf, tl, which, b, psbank):
            act(_I("activation", sq[0][:, :].bitcast(BF16), src_tile, AF.Square, accum_out=st16[:, 0:1]), reads=[src_buf], writes=[sqb[0], stb])
            act(_I("activation", st16[:, 1:2], st16[:, 0:1], AF.Sqrt, bias=EPS, scale=1.0 / D), reads=[stb], writes=[stb])
            dve(_I("reciprocal", st16[:, 2:3], st16[:, 1:2]), reads=[stb], writes=[stb])
            dve(_I("tensor_scalar", xn[:], src_tile, st16[:, 2:3], None, ALU.mult), reads=[src_buf, stb], writes=[xnb])
            for j in range(8):
                bk = psbank + j // 4
                o0 = ((j % 4) * 2 + tl) * 128
                pe(_I("transpose", psb(bk)[:, o0:o0 + 128], xn[:, j * 128:(j + 1) * 128], ident_b[:]), reads=[xnb, bC], writes=[PB[bk]])

        def ln_evac(c2, which, b, psbank, hbuf):
            for j in range(8):
                bk = psbank + j // 4
                src = psb(bk)[:, (j % 4) * 256:(j % 4) * 256 + 256]
                Gc = modT[:, (2 * which + 1) * 8 + j, b:b + 1]
                Sc = modT[:, (2 * which) * 8 + j, b:b + 1]
                dve(_I("tensor_scalar", hT[:, j, c2 * 256:(c2 + 1) * 256], src, Gc, Sc, ALU.mult, ALU.add),
                    reads=[PB[bk], bC], writes=[hbuf])

        def load_w(dst, src, buf, q="pool"):
            S.dma(dst, src, writes=[buf], q=q)

        win_v = win_d.rearrange("(k p) n -> p k n", p=128)

        def rms_heads(psbank, nh, dst_stats):
            i = state["sb"] = (state["sb"] + 1) % 2
            act(_I("activation", sq[i][:, 0:nh * 64], PS[psbank][:, 0:nh * 64], AF.Square), reads=[PB[psbank]], writes=[sqb[i]])
            dve(_I("tensor_reduce", dst_stats, sq[i][:, 0:nh * 64].rearrange("p (h d) -> p h d", d=64), AX.X, ALU.add), reads=[sqb[i]], writes=[stb])

        def rstd_from(stats):
            act(_I("activation", stats, stats, AF.Sqrt, bias=EPS, scale=1.0 / 64), reads=[stb], writes=[stb])
            dve(_I("reciprocal", stats, stats), reads=[stb], writes=[stb])

        def make_units(QTh, qb, KTk, kb, kind, t, tiles, maskmm, pvb, slot, full_mask=None, banks=(4, 5)):
            ntl = len(tiles)
            return [dict(QTh=QTh, qb=qb, KTk=KTk, kb=kb, kind=kind, t=t, grp=tiles[g0:g0 + 4], g0=g0, ntl=ntl, maskmm=maskmm,
                         pvb=pvb, slot=slot, full_mask=full_mask, banks=banks) for g0 in range(0, ntl, 4)]

        def emit_A(u):
            qs = slice(u["t"] * 128, (u["t"] + 1) * 128)
            banks = u["banks"]
            bk = banks[state["pt"] % len(banks)]
            pi = state["pt"] % len(PT)
            state["pt"] += 1
            u["pi"] = pi
            grp = u["grp"]
            for i, (j, mt) in enumerate(grp):
                ks = slice(j * 128, (j + 1) * 128)
                pe(_I("matmul", PS[bk][:, i * 128:(i + 1) * 128], lhsT=u["KTk"][0:68, ks], rhs=u["QTh"][0:68, qs], start=True, stop=(u["maskmm"] is None)),
                   reads=[u["kb"], u["qb"]], writes=[PB[bk]])
                if u["maskmm"] is not None:
                    MTg, mb = u["maskmm"]
                    pe(_I("matmul", PS[bk][:, i * 128:(i + 1) * 128], lhsT=ET[0:68, ks], rhs=MTg[0:68, qs], start=False, stop=True),
                       reads=[mb, bC], writes=[PB[bk]])
            n = len(grp) * 128
            act(_I("activation", PT[pi][:, 0:n], PS[bk][:, 0:n], AF.Exp), reads=[PB[bk]], writes=[PTb[pi]])
            if u["full_mask"] is not None:
                mT, mTb = u["full_mask"]
                j0 = grp[0][0]
                pool(_I("tensor_tensor", PT[pi][:, 0:n], PT[pi][:, 0:n], mT[:, j0:j0 + len(grp), :].rearrange("p j c -> p (j c)"), ALU.mult),
                     reads=[mTb, PTb[pi]], writes=[PTb[pi]])
            for i, (j, mt) in enumerate(grp):
                if mt is not None:
                    mk = Cm if mt == "C" else Wm
                    pool(_I("tensor_tensor", PT[pi][:, i * 128:(i + 1) * 128], PT[pi][:, i * 128:(i + 1) * 128], mk[:], ALU.mult),
                         reads=[bC, PTb[pi]], writes=[PTb[pi]])

        def emit_B(u):
            pi = u["pi"]
            pvb = u["pvb"]
            po = PS[pvb][:, u["slot"] * 65:(u["slot"] + 1) * 65]
            for i, (j, mt) in enumerate(u["grp"]):
                gi = u["g0"] + i
                pe(_I("matmul", po, lhsT=PT[pi][:, i * 128:(i + 1) * 128], rhs=vstate["V"][:, j, u["kind"], :], start=(gi == 0), stop=(gi == u["ntl"] - 1)),
                   reads=[PTb[pi], vstate["bV"]], writes=[PB[pvb]])

        def run_units(units, L, between=None):
            n = len(units)
            for i in range(min(L, n)):
                emit_A(units[i])
            for i in range(n):
                if i + L < n:
                    emit_A(units[i + L])
                emit_B(units[i])
                if units[i].get("post") is not None:
                    units[i]["post"]()
                if between is not None:
                    between(i, n)

        def next_pv():
            state["pv"] = state.get("pv", 0) + 1
            return 6 if state["pv"] % 2 == 0 else 2

        def norm4(pvb, g, gate_view, gate_bufs, first):
            o4 = PS[pvb][:, 0:260].rearrange("p (h c) -> p h c", h=4)
            dve(_I("tensor_scalar", sn[:, 0:4], o4[:, :, 64], 1e-30, None, ALU.max), reads=[PB[pvb]], writes=[snb])
            dve(_I("reciprocal", sn[:, 4:8], sn[:, 0:4]), reads=[snb], writes=[snb])
            if gate_view is not None:
                dve(_I("tensor_tensor", sn[:, 4:8], sn[:, 4:8], gate_view, ALU.mult), reads=[snb] + gate_bufs, writes=[snb])
            wb = sn[:, 4:8].unsqueeze(2).to_broadcast([128, 4, 64])
            if first:
                dve(_I("tensor_tensor", oacc[:, 4 * g:4 * g + 4, :], o4[:, :, 0:64], wb, ALU.mult), reads=[PB[pvb], snb], writes=[oab])
            else:
                dve(_I("tensor_tensor", tmpo[:], o4[:, :, 0:64], wb, ALU.mult), reads=[PB[pvb], snb], writes=[tmpb])
                pool(_I("tensor_tensor", oacc[:, 4 * g:4 * g + 4, :], oacc[:, 4 * g:4 * g + 4, :], tmpo[:], ALU.add), reads=[tmpb, oab], writes=[oab])

        def flush_o(t, mix):
            dve(_I("tensor_copy", obf[:], oacc[:].rearrange("p h d -> p (h d)")), reads=[oab], writes=[obfb])
            for j in range(4):
                pe(_I("transpose", psb(3)[:, j * 128:(j + 1) * 128], obf[:, j * 128:(j + 1) * 128], ident_b[:]), reads=[obfb, bC], writes=[PB[3]])
            act(_I("activation", oTs[:].rearrange("p j c -> p (j c)"), psb(3)[:, 0:512], AF.Copy), reads=[PB[3]], writes=[oTsb])
            S.dma(oT_d[mix, :, :, t * 128:(t + 1) * 128].rearrange("j p c -> p j c"), oTs[:], reads=[oTsb], writes=[bOT[mix]])

        for s in range(nseq):
            b = s
            S.barrier()

            for c2 in range(8):
                for tl in range(2):
                    t = c2 * 2 + tl
                    i = t % 2
                    S.dma(xt[i][:], x_d[s, t * 128:(t + 1) * 128, :], writes=[xtb[i]])
                    layernorm(xt[i][:], xtb[i], tl, 0, b, 0)
                ln_evac(c2, 0, b, 0, hTb[c2 // 2])

            if dbg and s == 0:
                S.dma(hT_dbg, hT[:], reads=hTb, writes=[Buf()], is_output=True)
            U.reset()
            V_all = U.take(NT * 5 * 65).rearrange("p (t k c) -> p t k c", t=NT, k=5)
            bV = Buf()
            pool(_I("memset", V_all[:, :, :, 64:65], 1.0), writes=[bV])
            vstate["V"] = V_all
            vstate["bV"] = bV
            Wn = U.take(8 * 1304).rearrange("p (k n) -> p k n", k=8)
            QT = U.take(8 * S_TOK).rearrange("p (h n) -> p h n", h=8)
            KT = U.take(5 * S_TOK).rearrange("p (h n) -> p h n", h=5)
            q_aug = U.take(8 * 68).rearrange("p (h d) -> p h d", h=8)
            k_aug = U.take(4 * 68).rearrange("p (h d) -> p h d", h=4)
            kcT = U.take(S_TOK)
            vcT = U.take(S_TOK)
            MT = U.take(2 * S_TOK).rearrange("p (g n) -> p g n", g=2)
            sg = U.take(NT * 24, F32).rearrange("p (t c) -> p t c", c=24)
            bq_aug, bk_aug, bkc, bsg = Buf(), Buf(), Buf(), Buf()
            bWn = [Buf() for _ in range(8)]
            QTb = [Buf() for _ in range(8)]
            KTb = [Buf() for _ in range(5)]
            MTb = [Buf(), Buf()]
            pool(_I("memset", MT[32:64, :, :], 0.0), writes=MTb)
            pool(_I("memset", MT[64:68, :, :], 0.0), writes=MTb)
            for k in range(8):
                load_w(Wn[:, k, :], win_v[:, k, 0:1304], bWn[k])
            for c in range(4):
                for tl in range(4):
                    t = c * 4 + tl
                    ts = slice(t * 128, (t + 1) * 128)
                    pq, pk, pg = (0, 1, 2) if t % 2 == 0 else (4, 5, 6)
                    for k in range(8):
                        pe(_I("matmul", PS[pq][:, :], lhsT=hT[:, k, ts], rhs=Wn[:, k, 0:512], start=(k == 0), stop=(k == 7)), reads=[hTb[c], bWn[k]], writes=[PB[pq]])
                    for k in range(8):
                        pe(_I("matmul", PS[pk][:, :], lhsT=hT[:, k, ts], rhs=Wn[:, k, 768:1280], start=(k == 0), stop=(k == 7)), reads=[hTb[c], bWn[k]], writes=[PB[pk]])
                    for k in range(8):
                        pe(_I("matmul", PS[pg][:, 0:24], lhsT=hT[:, k, ts], rhs=Wn[:, k, 1280:1304], start=(k == 0), stop=(k == 7)), reads=[hTb[c], bWn[k]], writes=[PB[pg]])
                    rms_heads(pq, 8, st16[:, 0:8])
                    rms_heads(pk, 8, st16[:, 8:16])
                    rstd_from(st16[:, 0:16])
                    dve(_I("tensor_tensor", q_aug[:, :, 0:64], PS[pq][:, :].rearrange("p (h d) -> p h d", d=64), st16[:, 0:8].unsqueeze(2).to_broadcast([128, 8, 64]), ALU.mult),
                        reads=[PB[pq], stb], writes=[bq_aug])
                    dve(_I("tensor_copy", q_aug[:, :, 64:68], aqc[:, t, :, :]), reads=[bC], writes=[bq_aug])
                    for (c0, s0, kk, gi) in ((0, 8, 0, 0), (256, 12, 2, 1)):
                        dve(_I("tensor_tensor", sq[0][:, 0:128].rearrange("p (h d) -> p h d", d=64), PS[pk][:, c0:c0 + 128].rearrange("p (h d) -> p h d", d=64),
                                                                   st16[:, s0:s0 + 2].unsqueeze(2).to_broadcast([128, 2, 64]), ALU.mult), reads=[PB[pk], stb], writes=[sqb[0]])
                        dve(_I("tensor_tensor", k_aug[:, kk:kk + 2, 0:64], sq[0][:, 0:128].rearrange("p (h d) -> p h d", d=64),
                                                                   gains[:, gi, :].unsqueeze(1).to_broadcast([128, 2, 64]), ALU.mult), reads=[sqb[0], bC], writes=[bk_aug])
                    dve(_I("tensor_copy", k_aug[:, :, 64:68], akc[:, t, :].unsqueeze(1).to_broadcast([128, 4, 4])), reads=[bC], writes=[bk_aug])
                    act(_I("activation", V_all[:, t, 0:2, 0:64], PS[pk][:, 128:256].rearrange("p (h d) -> p h d", d=64), AF.Copy), reads=[PB[pk]], writes=[bV])
                    act(_I("activation", V_all[:, t, 2:4, 0:64], PS[pk][:, 384:512].rearrange("p (h d) -> p h d", d=64), AF.Copy), reads=[PB[pk]], writes=[bV])
                    act(_I("activation", sg[:, t, :], PS[pg][:, 0:24], AF.Sigmoid), reads=[PB[pg]], writes=[bsg])
                    for h in range(8):
                        pe(_I("transpose", psb(3)[0:68, h * 128:(h + 1) * 128], q_aug[:, h, :], ident_b[:]), reads=[bq_aug, bC], writes=[PB[3]])
                    for kk in range(4):
                        pe(_I("transpose", psb(7)[0:68, kk * 128:(kk + 1) * 128], k_aug[:, kk, :], ident_b[:]), reads=[bk_aug, bC], writes=[PB[7]])
                    act(_I("activation", QT[0:68, :, ts], psb(3)[0:68, :].rearrange("p (h c) -> p h c", h=8), AF.Copy), reads=[PB[3]], writes=QTb)
                    dve(_I("tensor_copy", KT[0:68, 0:4, ts], psb(7)[0:68, 0:512].rearrange("p (h c) -> p h c", h=4)), reads=[PB[7]], writes=KTb[0:4])
                cs = slice(c * 512, (c + 1) * 512)
                for (c0, dst) in ((512, kcT), (640, vcT)):
                    for k in range(8):
                        pe(_I("matmul", PS[0][:, :], lhsT=Wn[:, k, c0:c0 + 128], rhs=hT[:, k, cs], start=(k == 0), stop=(k == 7)), reads=[hTb[c], bWn[k]], writes=[PB[0]])
                    act(_I("activation", dst[:, cs], PS[0][:, :], AF.Copy), reads=[PB[0]], writes=[bkc])

            S.barrier()
            W1 = Wn.rearrange("p k n -> p (k n)")[:, 0:2 * 32 * 128].rearrange("p (a l n) -> p a l n", a=2, l=32)
            W2 = U.take(2 * 64).rearrange("p (a n) -> p a n", a=2)
            peT = U.take(64)
            HT = U.take(128)
            bW1, bH = Buf(), Buf()
            for a, (w1d, w2d) in enumerate(((wck1_d, wck2_d), (wcv1_d, wcv2_d))):
                for half in range(2):
                    load_w(W1[half * 64:half * 64 + 64, a, :, :], w1d.rearrange("(l d) n -> d l n", d=64), bW1)
                load_w(W2[:, a, :], w2d, bW1)
            load_w(peT[0:64, :], peT_d, bW1)
            if s == 0:
                for a in range(2):
                    for l in range(32):
                        pe(_I("matmul", PS[2][:, a:a + 1], lhsT=W1[0:64, a, l, :], rhs=peT[0:64, a * 32 + l:a * 32 + l + 1], start=(l == 0), stop=(l == 31)),
                           reads=[bW1], writes=[PB[2]])
                dve(_I("tensor_copy", cb2[:], PS[2][:, 0:2]), reads=[PB[2]], writes=[bC])
            for a, srcT in enumerate((kcT, vcT)):
                for g in range(2):
                    base = g * 64
                    v3 = srcT[base:base + 64, :].rearrange("p (n s) -> p n s", s=16)
                    for l in range(32):
                        rhs = v3[:, (l // 16):(l // 16) + 127, l % 16]
                        pe(_I("matmul", PS[0][:, 0:127], lhsT=W1[base:base + 64, a, l, :], rhs=rhs, start=(l == 0), stop=(l == 31)),
                           reads=[bW1, bkc], writes=[PB[0]])
                    act(_I("activation", HT[:, 0:127], PS[0][:, 0:127], AF.Silu, bias=cb2[:, a:a + 1]), reads=[PB[0], bC], writes=[bH])
                    pe(_I("matmul", PS[1][0:127, 0:64], lhsT=HT[:, 0:127], rhs=W2[:, a, :], start=True, stop=True), reads=[bH, bW1], writes=[PB[1]])
                    if a == 0:
                        act(_I("activation", sq[0][0:127, 0:64], PS[1][0:127, 0:64], AF.Square, accum_out=st16[0:127, 0:1]), reads=[PB[1]], writes=[sqb[0], stb])
                        rstd_from(st16[0:127, 0:1])
                        dve(_I("tensor_scalar", sq[0][0:127, 0:64], PS[1][0:127, 0:64], st16[0:127, 0:1], None, ALU.mult), reads=[PB[1], stb], writes=[sqb[0]])
                        dve(_I("tensor_tensor", kc_aug[0:127, g, 0:64], sq[0][0:127, 0:64], gains[0:127, 2, :], ALU.mult), reads=[sqb[0], bC], writes=[bC])
                        pe(_I("transpose", psb(3)[0:68, 0:127], kc_aug[0:127, g, :], ident_b[0:127, 0:127]), reads=[bC], writes=[PB[3]])
                        dve(_I("tensor_copy", KcT[0:68, g, 0:127], psb(3)[0:68, 0:127]), reads=[PB[3]], writes=[bC])
                    else:
                        act(_I("activation", Vc[0:127, g, 0:64], PS[1][0:127, 0:64], AF.Copy), reads=[PB[1]], writes=[bC])

            imp = sb("imp_%d" % s, [128, 4, 32], F32) if s == 0 else imp
            impb = Buf()
            for t in range(NT):
                qs = slice(t * 128, (t + 1) * 128)
                for g in range(2):
                    for hh in range(4):
                        h = 4 * g + hh
                        pe(_I("matmul", PS[4][0:127, hh * 128:(hh + 1) * 128], lhsT=KcT[0:68, g, 0:127], rhs=QT[0:68, h, qs], start=True, stop=True),
                           reads=[bC, QTb[h]], writes=[PB[4]])
                    dve(_I("tensor_scalar", sq[1][0:127, :], PS[4][0:127, :], 60.0, None, ALU.min), reads=[PB[4]], writes=[sqb[1]])
                    act(_I("activation", PT[0][0:127, :], sq[1][0:127, :], AF.Exp), reads=[sqb[1]], writes=[PTb[0]])
                    dve(_I("tensor_tensor", PT[0][0:127, :].rearrange("p (h c) -> p h c", h=4), PT[0][0:127, :].rearrange("p (h c) -> p h c", h=4),
                                                  cmask[0:127, qs].unsqueeze(1).to_broadcast([127, 4, 128]), ALU.mult), reads=[bC, PTb[0]], writes=[PTb[0]])
                    for hh in range(4):
                        pe(_I("matmul", PS[7][:, hh * 97:(hh + 1) * 97], lhsT=PT[0][0:127, hh * 128:(hh + 1) * 128], rhs=Vc[0:127, g, :], start=True, stop=True),
                           reads=[PTb[0], bC], writes=[PB[7]])
                    o4 = PS[7][:, 0:388].rearrange("p (h c) -> p h c", h=4)
                    dve(_I("tensor_scalar", sm[:, 0:4], o4[:, :, 64], 1e-30, None, ALU.max), reads=[PB[7]], writes=[smb])
                    dve(_I("reciprocal", sm[:, 4:8], sm[:, 0:4]), reads=[smb], writes=[smb])
                    dve(_I("tensor_tensor", sm[:, 8:12], sm[:, 4:8], sg[:, t, :].rearrange("p (h r) -> p h r", r=3)[:, 4 * g:4 * g + 4, 0], ALU.mult), reads=[smb, bsg], writes=[smb])
                    dve(_I("tensor_tensor", oacc[:, 4 * g:4 * g + 4, :], o4[:, :, 0:64], sm[:, 8:12].unsqueeze(2).to_broadcast([128, 4, 64]), ALU.mult),
                        reads=[PB[7], smb], writes=[oab])
                    dve(_I("tensor_tensor", imp[:], o4[:, :, 65:97], sm[:, 4:8].unsqueeze(2).to_broadcast([128, 4, 32]), ALU.mult), reads=[PB[7], smb], writes=[impb])
                    dve(_I("tensor_reduce", sm[:, 16:48], imp[:].rearrange("p h j -> p j h"), AX.X, ALU.add), reads=[impb], writes=[smb])
                    dve(_I("tensor_tensor", sm[:, 16:48], sm[:, 16:48], selA[:, t, :], ALU.mult), reads=[smb, bC], writes=[smb])
                    dve(_I("tensor_tensor", sm[:, 16:48], sm[:, 16:48], selB[:, t, :], ALU.add), reads=[smb, bC], writes=[smb])
                    dve(_I("max", out=sm[:, 48:56], in_=sm[:, 16:48]), reads=[smb], writes=[smb])
                    dve(_I("match_replace", out=imp[:, 0, :], in_to_replace=sm[:, 48:56], in_values=sm[:, 16:48], imm_value=-3e38), reads=[smb], writes=[impb])
                    dve(_I("max", out=sm[:, 56:64], in_=imp[:, 0, :]), reads=[impb], writes=[smb])
                    dve(_I("tensor_scalar", sm[:, 16:48], sm[:, 16:48], sm[:, 63:64], None, ALU.is_ge), reads=[smb], writes=[smb])
                    dve(_I("tensor_scalar", obf[:, 0:32], sm[:, 16:48], -1.0, -NEG, ALU.add, ALU.mult), reads=[smb], writes=[obfb])
                    pe(_I("transpose", psb(3)[0:32, 0:128], obf[:, 0:32], ident_b[:]), reads=[obfb, bC], writes=[PB[3]])
                    act(_I("activation", MT[0:32, g, qs], psb(3)[0:32, 0:128], AF.Copy), reads=[PB[3]], writes=[MTb[g]])
                sg3 = sg[:, t, :].rearrange("p (h r) -> p h r", r=3)
                units = []
                for g in range(2):
                    pvb = next_pv()
                    tiles = [(j, "C" if j == t else None) for j in range(t + 1)]
                    for hh in range(4):
                        h = 4 * g + hh
                        units += make_units(QT[:, h, :], QTb[h], KT[:, g, :], KTb[g], g, t, tiles, (MT[:, g, :], MTb[g]), pvb, hh, banks=(4, 5, 0, 1))
                    units[-1]["post"] = (lambda pvb=pvb, g=g, gv=sg3[:, 4 * g:4 * g + 4, 1]: norm4(pvb, g, gv, [bsg], False))
                    pvb = next_pv()
                    tiles = [(j, "C" if j == t else ("W" if j == t - 4 else None)) for j in range(max(0, t - 4), t + 1)]
                    for hh in range(4):
                        h = 4 * g + hh
                        units += make_units(QT[:, h, :], QTb[h], KT[:, 2 + g, :], KTb[2 + g], 2 + g, t, tiles, None, pvb, hh, banks=(4, 5, 0, 1))
                    units[-1]["post"] = (lambda pvb=pvb, g=g, gv=sg3[:, 4 * g:4 * g + 4, 2]: norm4(pvb, g, gv, [bsg], False))
                run_units(units, 2)
                flush_o(t, 0)

            S.barrier()
            U.reset()
            V_all = U.take(NT * 5 * 65).rearrange("p (t k c) -> p t k c", t=NT, k=5)
            bV = Buf()
            pool(_I("memset", V_all[:, :, :, 64:65], 1.0), writes=[bV])
            vstate["V"] = V_all
            vstate["bV"] = bV
            Wd = U.take(8 * 1352).rearrange("p (k n) -> p k n", k=8)
            QT = U.take(8 * S_TOK).rearrange("p (h n) -> p h n", h=8)
            KT = U.take(5 * S_TOK).rearrange("p (h n) -> p h n", h=5)
            q_aug = U.take(8 * 68).rearrange("p (h d) -> p h d", h=8)
            k_aug = U.take(4 * 68).rearrange("p (h d) -> p h d", h=4)
            iqT = U.take(4 * S_TOK).rearrange("p (m n) -> p m n", m=4)
            ikT = U.take(S_TOK)
            iw = U.take(NT * 8, F32).rearrange("p (t c) -> p t c", c=8)
            sc = U.take(S_TOK, F32)
            rl = U.take(512, F32)
            maskq = U.take(S_TOK)
            maskT = U.take(S_TOK).rearrange("p (j c) -> p j c", c=128)
            junk = U.take(S_TOK)
            biq, bik, biw, bsc, brl, bmq, bmT, bjk = (Buf() for _ in range(8))
            bWd = [Buf() for _ in range(8)]
            QTb = [Buf() for _ in range(8)]
            KTb = [Buf() for _ in range(5)]
            for k in range(8):
                load_w(Wd[:, k, 0:1224], win_v[:, k, 1304:2528], bWd[k])
                load_w(Wd[:, k, 1224:1288], win_v[:, k, 2456:2520], bWd[k])
                load_w(Wd[:, k, 1288:1352], win_v[:, k, 2456:2520], bWd[k])
            for c in range(4):
                cs = slice(c * 512, (c + 1) * 512)
                for tl in range(4):
                    t = c * 4 + tl
                    ts = slice(t * 128, (t + 1) * 128)
                    pq, pk, pg = (0, 1, 2) if t % 2 == 0 else (4, 5, 6)
                    for k in range(8):
                        pe(_I("matmul", PS[pq][:, :], lhsT=hT[:, k, ts], rhs=Wd[:, k, 0:512], start=(k == 0), stop=(k == 7)), reads=[hTb[c], bWd[k]], writes=[PB[pq]])
                    for k in range(8):
                        pe(_I("matmul", PS[pk][:, 0:128], lhsT=hT[:, k, ts], rhs=Wd[:, k, 512:640], start=(k == 0), stop=(k == 7)), reads=[hTb[c], bWd[k]], writes=[PB[pk]])
                    for k in range(8):
                        pe(_I("matmul", PS[pg][:, 0:8], lhsT=hT[:, k, ts], rhs=Wd[:, k, 1216:1224], start=(k == 0), stop=(k == 7)), reads=[hTb[c], bWd[k]], writes=[PB[pg]])
                    rms_heads(pq, 8, st16[:, 0:8])
                    rms_heads(pk, 1, st16[:, 8:9])
                    rstd_from(st16[:, 0:9])
                    dve(_I("tensor_tensor", q_aug[:, :, 0:64], PS[pq][:, :].rearrange("p (h d) -> p h d", d=64), st16[:, 0:8].unsqueeze(2).to_broadcast([128, 8, 64]), ALU.mult),
                        reads=[PB[pq], stb], writes=[bq_aug])
                    dve(_I("tensor_copy", q_aug[:, :, 64:68], aqc[:, t, :, :]), reads=[bC], writes=[bq_aug])
                    dve(_I("tensor_scalar", sq[0][:, 0:64], PS[pk][:, 0:64], st16[:, 8:9], None, ALU.mult), reads=[PB[pk], stb], writes=[sqb[0]])
                    dve(_I("tensor_tensor", k_aug[:, 0, 0:64], sq[0][:, 0:64], gains[:, 3, :], ALU.mult), reads=[sqb[0], bC], writes=[bk_aug])
                    dve(_I("tensor_copy", k_aug[:, 0, 64:68], akc[:, t, :]), reads=[bC], writes=[bk_aug])
                    act(_I("activation", V_all[:, t, 4, 0:64], PS[pk][:, 64:128], AF.Copy), reads=[PB[pk]], writes=[bV])
                    act(_I("activation", iw[:, t, :], PS[pg][:, 0:8], AF.Copy, scale=8.0 ** -0.5), reads=[PB[pg]], writes=[biw])
                    for h in range(8):
                        pe(_I("transpose", psb(3)[0:68, h * 128:(h + 1) * 128], q_aug[:, h, :], ident_b[:]), reads=[bq_aug, bC], writes=[PB[3]])
                    pe(_I("transpose", psb(7)[0:68, 0:128], k_aug[:, 0, :], ident_b[:]), reads=[bk_aug, bC], writes=[PB[7]])
                    act(_I("activation", QT[0:68, :, ts], psb(3)[0:68, :].rearrange("p (h c) -> p h c", h=8), AF.Copy), reads=[PB[3]], writes=QTb)
                    dve(_I("tensor_copy", KT[0:68, 4, ts], psb(7)[0:68, 0:128]), reads=[PB[7]], writes=[KTb[4]])
                for m in range(4):
                    for k in range(8):
                        pe(_I("matmul", PS[0][:, :], lhsT=Wd[:, k, 640 + m * 128:640 + (m + 1) * 128], rhs=hT[:, k, cs], start=(k == 0), stop=(k == 7)), reads=[hTb[c], bWd[k]], writes=[PB[0]])
                    act(_I("activation", iqT[:, m, cs], PS[0][:, :], AF.Copy, scale=0.125), reads=[PB[0]], writes=[biq])
                for k in range(8):
                    pe(_I("matmul", PS[1][:, :], lhsT=Wd[:, k, 1224:1352], rhs=hT[:, k, cs], start=(k == 0), stop=(k == 7)), reads=[hTb[c], bWd[k]], writes=[PB[1]])
                act(_I("activation", ikT[:, cs], PS[1][:, :], AF.Copy), reads=[PB[1]], writes=[bik])

            S.barrier()
            Wd_flat = Wd.rearrange("p k n -> p (k n)")
            scs = [sc, Wd_flat[:, 0:4096].bitcast(F32)]
            maskqs = [maskq, Wd_flat[:, 4096:6144]]
            maskTs = [maskT, Wd_flat[:, 6144:8192].rearrange("p (j c) -> p j c", c=128)]
            bscs, bmqs, bmTs = [Buf(), Buf()], [Buf(), Buf()], [Buf(), Buf()]
            smx = [sb("smx%d_%d" % (s, i), [128, 40], F32) for i in range(2)] if s == 0 else smx
            smxb = [Buf(), Buf()]

            def indexer(t):
                p = t % 2
                sc_, bsc_, sm_, smb_ = scs[p], bscs[p], smx[p], smxb[p]
                qs = slice(t * 128, (t + 1) * 128)
                nk = (t + 1) * 128
                nch = (nk + 511) // 512
                for h in range(8):
                    base = (h % 2) * 64
                    for cc in range(nch):
                        w = min(512, nk - cc * 512)
                        cs = slice(cc * 512, cc * 512 + w)
                        bk = cc % 2
                        pe(_I("matmul", PS[bk][:, 0:w], lhsT=iqT[base:base + 64, h // 2, qs], rhs=ikT[base:base + 64, cs], start=True, stop=True),
                           reads=[biq, bik], writes=[PB[bk]])
                        act(_I("activation", rl[:, 0:w], PS[bk][:, 0:w], AF.Relu), reads=[PB[bk]], writes=[brl])
                        if h == 0:
                            dve(_I("tensor_scalar", sc_[:, cs], rl[:, 0:w], iw[:, t, 0:1], None, ALU.mult), reads=[brl, biw], writes=[bsc_])
                        else:
                            dve(_I("scalar_tensor_tensor", sc_[:, cs], rl[:, 0:w], iw[:, t, h:h + 1], sc_[:, cs], ALU.mult, ALU.add), reads=[brl, biw, bsc_], writes=[bsc_])
                dve(_I("tensor_reduce", sm_[:, 0:1], sc_[:, 0:nk], AX.X, ALU.max, apply_absolute_value=True), reads=[bsc_], writes=[smb_])
                pool(_I("affine_select", sc_[:, t * 128:(t + 1) * 128], sc_[:, t * 128:(t + 1) * 128], [[-1, 128]], ALU.is_ge, -3e38, base=0, channel_multiplier=1), reads=[bsc_, smb_], writes=[bsc_])
                dve(_I("tensor_scalar", sm_[:, 8:8 + NBIS + 1], pow2[:], sm_[:, 0:1], None, ALU.mult), reads=[smb_, bC], writes=[smb_])
                dve(_I("memset", sm_[:, 1:2], 0.0), reads=[smb_], writes=[smb_])

            def bisect_iter(t, j):
                p = t % 2
                sc_, bsc_, sm_, smb_ = scs[p], bscs[p], smx[p], smxb[p]
                nk = (t + 1) * 128
                dve(_I("tensor_scalar", maskqs[p][:, 0:nk], sc_[:, 0:nk], sm_[:, 1:2], None, ALU.is_ge, ALU.add, accum_out=sm_[:, 2:3]), reads=[bsc_, smb_], writes=[bmqs[p], smb_])
                dve(_I("tensor_scalar", sm_[:, 3:4], sm_[:, 2:3], 255.5, -0.5, ALU.is_ge, ALU.add), reads=[smb_], writes=[smb_])
                dve(_I("scalar_tensor_tensor", sm_[:, 1:2], sm_[:, 3:4], sm_[:, 8 + j:9 + j], sm_[:, 1:2], ALU.mult, ALU.add), reads=[smb_], writes=[smb_])

            def finish_mask(t):
                p = t % 2
                sc_, bsc_, sm_, smb_ = scs[p], bscs[p], smx[p], smxb[p]
                nk = (t + 1) * 128
                dve(_I("tensor_tensor", sm_[:, 1:2], sm_[:, 1:2], sm_[:, 8 + NBIS:9 + NBIS], ALU.subtract), reads=[smb_], writes=[smb_])
                dve(_I("tensor_scalar", maskqs[p][:, 0:nk], sc_[:, 0:nk], sm_[:, 1:2], None, ALU.is_ge), reads=[bsc_, smb_], writes=[bmqs[p]])
                for j in range(t + 1):
                    bk = 3 if (j // 8) % 2 == 0 else 7
                    pe(_I("transpose", psb(bk)[:, (j % 8) * 128:(j % 8 + 1) * 128], maskqs[p][:, j * 128:(j + 1) * 128], ident_b[:]), reads=[bmqs[p], bC], writes=[PB[bk]])
                    if j % 8 == 7 or j == t:
                        j0 = (j // 8) * 8
                        n = j - j0 + 1
                        act(_I("activation", maskTs[p][:, j0:j0 + n, :].rearrange("p j c -> p (j c)"), psb(bk)[:, 0:n * 128], AF.Copy), reads=[PB[bk]], writes=[bmTs[p]])

            indexer(0)
            for j in range(NBIS):
                bisect_iter(0, j)
            finish_mask(0)
            for t in range(NT):
                if t + 1 < NT:
                    indexer(t + 1)
                tiles = [(j, None) for j in range(t + 1)]
                units = []
                for g2 in range(2):
                    pvb = next_pv()
                    for hh in range(4):
                        h = 4 * g2 + hh
                        units += make_units(QT[:, h, :], QTb[h], KT[:, 4, :], KTb[4], 4, t, tiles, None, pvb, hh, full_mask=(maskTs[t % 2], bmTs[t % 2]))
                    units[-1]["post"] = (lambda pvb=pvb, g2=g2: norm4(pvb, g2, None, [], True))
                done = [0]

                def between(i, n, t=t, done=done):
                    if t + 1 >= NT or done[0] > NBIS:
                        return
                    target = min(NBIS, ((i + 1) * NBIS * 10) // (n * 7) + 1)
                    while done[0] < target:
                        bisect_iter(t + 1, done[0])
                        done[0] += 1
                    if done[0] == NBIS:
                        finish_mask(t + 1)
                        done[0] = NBIS + 1
                run_units(units, 1, between)
                if t + 1 < NT and done[0] <= NBIS:
                    while done[0] < NBIS:
                        bisect_iter(t + 1, done[0])
                        done[0] += 1
                    finish_mask(t + 1)
                flush_o(t, 1)

            for hf in range(2):
                S.barrier()
                U.reset()
                Wg = U.take(8 * 2048).rearrange("p (k n) -> p k n", k=8)
                Woa = U.take(4 * D).rearrange("p (k n) -> p k n", k=4)
                Wob = U.take(4 * D).rearrange("p (k n) -> p k n", k=4)
                Wout = U.take(8 * D).rearrange("p (k n) -> p k n", k=8)
                Wff_region = (Wg, Woa, Wob, Wout)
                xacc = U.take(8 * D, F32).rearrange("p (t n) -> p t n", t=8)
                yT = U.take(8 * 512).rearrange("p (k n) -> p k n", k=8)
                oaT = U.take(4 * 512).rearrange("p (k n) -> p k n", k=4)
                obT = U.take(4 * 512).rearrange("p (k n) -> p k n", k=4)
                sga = U.take(512)
                sgb = U.take(512)
                t1 = U.take(512, F32)
                t2 = U.take(512, F32)
                aT = yT
                g1bc = U.take(D, F32)
                g2bc = U.take(D, F32)
                bG = Buf()
                S.dma(g1bc, modrow_d[b:b + 1, 2 * D:3 * D].partition_broadcast(128), writes=[bG])
                S.dma(g2bc, modrow_d[b:b + 1, 5 * D:6 * D].partition_broadcast(128), writes=[bG])
                bya, byT, boa, bob, bsga, bsgb, bt1, bt2, baT = (Buf() for _ in range(9))
                bWg = [Buf() for _ in range(8)]
                bWo = [Buf() for _ in range(8)]
                bWa = [Buf() for _ in range(4)]
                bWb = [Buf() for _ in range(4)]
                xab = [Buf() for _ in range(8)]
                for k in range(8):
                    load_w(Wg[:, k, :], win_v[:, k, 2528:4576], bWg[k])
                    load_w(Wout[:, k, :], wout_d.rearrange("(k p) n -> p k n", p=128)[:, k, :], bWo[k])
                for k in range(4):
                    load_w(Woa[:, k, :], woa_d.rearrange("(k p) n -> p k n", p=128)[:, k, :], bWa[k])
                    load_w(Wob[:, k, :], wob_d.rearrange("(k p) n -> p k n", p=128)[:, k, :], bWb[k])
                for cl in range(2):
                    c = hf * 2 + cl
                    cs = slice(c * 512, (c + 1) * 512)
                    S.dma(oaT, oT_d[0, :, :, cs].rearrange("j p c -> p j c"), reads=[bOT[0]], writes=[boa])
                    S.dma(obT, oT_d[1, :, :, cs].rearrange("j p c -> p j c"), reads=[bOT[1]], writes=[bob])
                    for f in range(8):
                        fs = slice(f * 128, (f + 1) * 128)
                        for k in range(8):
                            pe(_I("matmul", PS[0][:, :], lhsT=Wg[:, k, fs], rhs=hT[:, k, cs], start=(k == 0), stop=(k == 7)), reads=[bWg[k], hTb[c]], writes=[PB[0]])
                        act(_I("activation", sga, PS[0][:, :], AF.Sigmoid), reads=[PB[0]], writes=[bsga])
                        for k in range(8):
                            pe(_I("matmul", PS[1][:, :], lhsT=Wg[:, k, 1024 + f * 128:1024 + (f + 1) * 128], rhs=hT[:, k, cs], start=(k == 0), stop=(k == 7)), reads=[bWg[k], hTb[c]], writes=[PB[1]])
                        act(_I("activation", sgb, PS[1][:, :], AF.Sigmoid), reads=[PB[1]], writes=[bsgb])
                        for k in range(4):
                            pe(_I("matmul", PS[2][:, :], lhsT=Woa[:, k, fs], rhs=oaT[:, k, :], start=(k == 0), stop=(k == 3)), reads=[bWa[k], boa], writes=[PB[2]])
                        for k in range(4):
                            pe(_I("matmul", PS[4][:, :], lhsT=Wob[:, k, fs], rhs=obT[:, k, :], start=(k == 0), stop=(k == 3)), reads=[bWb[k], bob], writes=[PB[4]])
                        dve(_I("tensor_tensor", t1, PS[2][:, :], sga, ALU.mult), reads=[PB[2], bsga], writes=[bt1])
                        dve(_I("tensor_tensor", t2, PS[4][:, :], sgb, ALU.mult), reads=[PB[4], bsgb], writes=[bt2])
                        dve(_I("tensor_tensor", yT[:, f, :], t1, t2, ALU.add), reads=[bt1, bt2], writes=[byT])
                    for tl in range(4):
                        t = c * 4 + tl
                        tt = t - hf * 8
                        i = t % 2
                        S.dma(xt[i][:], x_d[s, t * 128:(t + 1) * 128, :], writes=[xtb[i]])
                        for h2 in range(2):
                            ns = slice(h2 * 512, (h2 + 1) * 512)
                            bk = 5 + h2
                            for k in range(8):
                                pe(_I("matmul", PS[bk][:, :], lhsT=yT[:, k, tl * 128:(tl + 1) * 128], rhs=Wout[:, k, ns], start=(k == 0), stop=(k == 7)), reads=[byT, bWo[k]], writes=[PB[bk]])
                            dve(_I("tensor_tensor", t1, PS[bk][:, :], g1bc[:, ns], ALU.mult), reads=[PB[bk], bG], writes=[bt1])
                            dve(_I("tensor_tensor", xacc[:, tt, ns], t1, xt[i][:, ns], ALU.add), reads=[bt1, xtb[i]], writes=[xab[tt]])
                        if dbg and s == 0:
                            S.dma(x1_dbg[t * 128:(t + 1) * 128, :], xacc[:, tt, :], reads=[xab[tt]], writes=[Buf()], is_output=True)
                        layernorm(xacc[:, tt, :], xab[tt], tl % 2, 1, b, 0)
                        if tl % 2 == 1:
                            ln_evac(t // 2, 1, b, 0, hTb[c])
                S.barrier()
                for (f0_, nf) in ((0, 8), (8, 8), (16, 6)):
                    Wfg = Wg.rearrange("p k n -> p (k n)")[:, 0:8 * 1024].rearrange("p (k n) -> p k n", k=8)
                    Wfu = Wg.rearrange("p k n -> p (k n)")[:, 8 * 1024:16 * 1024].rearrange("p (k n) -> p k n", k=8)
                    Wfd = Wout.rearrange("p k n -> p (k n)")[:, 0:8 * D].rearrange("p (k n) -> p k n", k=8)
                    nfc = nf * 128
                    if f0_ == 0:
                        bFg = [Buf() for _ in range(8)]
                        bFu = [Buf() for _ in range(8)]
                        bFd = [Buf() for _ in range(8)]
                    for k in range(8):
                        load_w(Wfg[:, k, 0:nfc], wfg_d.rearrange("(k p) n -> p k n", p=128)[:, k, f0_ * 128:f0_ * 128 + nfc], bFg[k])
                        load_w(Wfu[:, k, 0:nfc], wfu_d.rearrange("(k p) n -> p k n", p=128)[:, k, f0_ * 128:f0_ * 128 + nfc], bFu[k])
                    for k in range(nf):
                        load_w(Wfd[:, k, :], wfd_d[(f0_ + k) * 128:(f0_ + k + 1) * 128, :], bFd[k])
                    for cl in range(2):
                        c = hf * 2 + cl
                        cs = slice(c * 512, (c + 1) * 512)
                        for f in range(nf):
                            fs = slice(f * 128, (f + 1) * 128)
                            for k in range(8):
                                pe(_I("matmul", PS[0][:, :], lhsT=Wfg[:, k, fs], rhs=hT[:, k, cs], start=(k == 0), stop=(k == 7)), reads=[bFg[k], hTb[c]], writes=[PB[0]])
                            for k in range(8):
                                pe(_I("matmul", PS[1][:, :], lhsT=Wfu[:, k, fs], rhs=hT[:, k, cs], start=(k == 0), stop=(k == 7)), reads=[bFu[k], hTb[c]], writes=[PB[1]])
                            act(_I("activation", t1, PS[0][:, :], AF.Silu), reads=[PB[0]], writes=[bt1])
                            dve(_I("tensor_tensor", aT[:, f, :], t1, PS[1][:, :], ALU.mult), reads=[bt1, PB[1]], writes=[baT])
                        for tl in range(4):
                            tt = cl * 4 + tl
                            for h2 in range(2):
                                ns = slice(h2 * 512, (h2 + 1) * 512)
                                bk = 5 + h2
                                for f in range(nf):
                                    pe(_I("matmul", PS[bk][:, :], lhsT=aT[:, f, tl * 128:(tl + 1) * 128], rhs=Wfd[:, f, ns], start=(f == 0), stop=(f == nf - 1)), reads=[baT, bFd[f]], writes=[PB[bk]])
                                dve(_I("tensor_tensor", t2, PS[bk][:, :], g2bc[:, ns], ALU.mult), reads=[PB[bk], bG], writes=[bt2])
                                dve(_I("tensor_tensor", xacc[:, tt, ns], xacc[:, tt, ns], t2, ALU.add), reads=[bt2, xab[tt]], writes=[xab[tt]])
                for tt in range(8):
                    t = hf * 8 + tt
                    S.dma(out_d[s, t * 128:(t + 1) * 128, :], xacc[:, tt, :], reads=[xab[tt]], writes=[Buf()], is_output=True)
        S.emit()
    return nc


def _prep_common(inp):
    f = lambda a: np.ascontiguousarray(np.asarray(a, dtype=np.float32))
    gn = np.concatenate([inp["g_norm1"][0].reshape(8, 128).T, inp["g_norm2"][0].reshape(8, 128).T], axis=1)
    gvec = np.concatenate([inp[k][0] for k in ("g_q_a", "g_kc_a", "g_ks_a", "g_kw_a", "g_q_b", "g_k_b")])[None, :]
    peT = np.concatenate([inp["pe_ck"][0].T, inp["pe_cv"][0].T], axis=1)
    return {
        "w_ada": f(inp["w_ada"][0]), "b_ada": f(inp["b_ada"]), "gn": f(gn), "w_in": f(inp["w_in"][0]),
        "gvec": f(gvec), "peT": f(peT), "w_ck1": f(inp["w_ck1"][0]), "w_ck2": f(inp["w_ck2"][0]),
        "w_cv1": f(inp["w_cv1"][0]), "w_cv2": f(inp["w_cv2"][0]), "w_o_a": f(inp["w_o_a"][0]),
        "w_o_b": f(inp["w_o_b"][0]), "w_out": f(inp["w_out"][0]), "w_ff_gate": f(inp["w_ff_gate"][0]),
        "w_ff_up": f(inp["w_ff_up"][0]), "w_ff_down": f(inp["w_ff_down"][0]),
    }


def _core_map(common, x, c, i, nseq):
    m = dict(common)
    m["x"] = np.ascontiguousarray(x[i * nseq:(i + 1) * nseq])
    cc = np.zeros((4, D), np.float32)
    cc[:nseq] = c[i * nseq:(i + 1) * nseq]
    m["cT"] = np.ascontiguousarray(cc.T.reshape(8, 128, 4).transpose(1, 0, 2))
    return m


def kernel(**inputs):
    x = np.asarray(inputs["x"], dtype=np.float32)
    c = np.asarray(inputs["c"], dtype=np.float32)
    n = 8
    nseq = x.shape[0] // n
    nc = build_nc(nseq)
    common = _prep_common(inputs)
    in_maps = [_core_map(common, x, c, i, nseq) for i in range(n)]
    res = run_bass_kernel_spmd(nc, in_maps, core_ids=list(range(n)))
    return np.concatenate([r["out"] for r in res.results], axis=0).astype(np.float32)
```

```python
import contextlib
import numpy as np
import concourse.bass as bass
import concourse.mybir as mybir
from concourse.bass_utils import run_bass_kernel_spmd

F32 = mybir.dt.float32
BF16 = mybir.dt.bfloat16
AF = mybir.ActivationFunctionType
ALU = mybir.AluOpType
AX = mybir.AxisListType

S_TOK = 2048
D = 1024
NT = 16
DIN = 4576
DFF = 2816
EPS = 1e-6
NBIS = 22
NEG = -30000.0


class Buf:
    __slots__ = ("name", "w", "r")

    def __init__(self, name=""):
        self.name = name
        self.w = None
        self.r = {}


class Sched:
    ENGS = ("pe", "act", "dve", "pool", "sp")
    NDMA = 24

    def __init__(self, nc):
        self.nc = nc
        self.streams = {e: [] for e in self.ENGS}
        self.count = {e: 0 for e in self.ENGS}
        self.waited = {e: {} for e in self.ENGS}
        self.dma_uses = [0] * self.NDMA
        self.dma_rr = 0
        self.out_events = []

    def _deps(self, eng, reads, writes):
        deps = {}

        def add(ev):
            if ev is None:
                return
            k, v = ev
            if deps.get(k, 0) < v:
                deps[k] = v
        for b in reads:
            add(b.w)
        for b in writes:
            if b.w is not None and b.w[0] != eng:
                add(b.w)
            for k, v in b.r.items():
                if k != eng:
                    add((k, v))
        waits = []
        for k, v in deps.items():
            if k == "pe" and eng == "pe":
                continue
            if self.waited[eng].get(k, 0) < v:
                self.waited[eng][k] = v
                waits.append((k, v))
        return waits

    def _commit(self, ev, reads, writes):
        k, v = ev
        for b in writes:
            b.w = ev
            b.r = {}
        for b in reads:
            if b.r.get(k, 0) < v:
                b.r[k] = v

    def op(self, eng, fn, reads=(), writes=()):
        waits = self._deps(eng, reads, writes)
        self.count[eng] += 1
        ev = (eng, self.count[eng])
        self.streams[eng].append((fn, waits, ev))
        self._commit(ev, reads, writes)
        return ev

    def dma(self, out, in_, reads=(), writes=(), q="sp", is_output=False, **kw):
        i = self.dma_rr
        self.dma_rr = (self.dma_rr + 1) % self.NDMA
        waits = self._deps(q, reads, writes)
        key = "dma%d" % i
        prev = self.dma_uses[i] * 16
        if prev and self.waited[q].get(key, 0) < prev:
            self.waited[q][key] = prev
            waits.append((key, prev))
        self.dma_uses[i] += 1
        ev = (key, self.dma_uses[i] * 16)
        fn = lambda e, out=out, in_=in_, kw=kw: e.dma_start(out=out, in_=in_, **kw)
        self.streams[q].append((fn, waits, ev))
        self._commit(ev, reads, writes)
        if is_output:
            self.out_events.append(ev)
        return ev

    def barrier(self):
        allv = [(e, self.count[e]) for e in self.ENGS if self.count[e]]
        allv += [("dma%d" % i, self.dma_uses[i] * 16) for i in range(self.NDMA) if self.dma_uses[i]]
        for e in self.ENGS:
            waits = []
            for k, v in allv:
                if k == e:
                    continue
                if self.waited[e].get(k, 0) < v:
                    self.waited[e][k] = v
                    waits.append((k, v))
            if waits:
                self.streams[e].append((None, waits, None))

    def emit(self):
        nc = self.nc
        with contextlib.ExitStack() as st:
            sems = {}
            for e in self.ENGS:
                sems[e] = st.enter_context(nc.semaphore("s_" + e))
            for i in range(self.NDMA):
                sems["dma%d" % i] = st.enter_context(nc.semaphore("s_dma%d" % i))
            final = {}
            for k, v in self.out_events:
                final[k] = max(final.get(k, 0), v)
            block = st.enter_context(nc.Block())

            def run(engname, e):
                for fn, waits, ev in self.streams[engname]:
                    for k, v in waits:
                        e.wait_ge(sems[k], v)
                    if fn is None:
                        continue
                    ins = fn(e)
                    k, v = ev
                    ins.then_inc(sems[k], 16 if k.startswith("dma") else 1)
                if engname == "sp":
                    for k, v in final.items():
                        e.wait_ge(sems[k], v)

            @block.tensor
            def _(e):
                run("pe", e)

            @block.scalar
            def _(e):
                run("act", e)

            @block.vector
            def _(e):
                run("dve", e)

            @block.gpsimd
            def _(e):
                run("pool", e)

            @block.sync
            def _(e):
                run("sp", e)


class Arena:
    def __init__(self, ap16):
        self.ap = ap16
        self.off = 0

    def reset(self):
        self.off = 0

    def take(self, ncols, dt=BF16):
        if dt == F32:
            self.off = (self.off + 1) // 2 * 2
            n16 = ncols * 2
        else:
            n16 = ncols
        assert self.off + n16 <= self.ap.shape[1], (self.off, n16, self.ap.shape)
        v = self.ap[:, self.off:self.off + n16]
        self.off += n16
        self.off = (self.off + 1) // 2 * 2
        return v.bitcast(F32) if dt == F32 else v


_REGS = {}


def _I(name, *args, **kw):
    if name == "affine_select":
        def thunk(e):
            a = list(args)
            key = (id(e), float(a[4]))
            if key not in _REGS:
                _REGS[key] = e.to_reg(float(a[4]))
            a[4] = _REGS[key]
            return e.affine_select(*a, **kw)
        return thunk
    return lambda e: getattr(e, name)(*args, **kw)


def build_nc(nseq=4, dbg=False):
    nc = bass.Bass("TRN2", target_bir_lowering=False)
    _REGS.clear()
    S = Sched(nc)

    def din(name, shape):
        return nc.dram_tensor(name, shape, F32, kind="ExternalInput").ap()
    x_d = din("x", [nseq, S_TOK, D])
    cT_d = din("cT", [128, 8, 4])
    wada_d = din("w_ada", [D, 6 * D])
    bada_d = din("b_ada", [1, 6 * D])
    gn_d = din("gn", [128, 16])
    win_d = din("w_in", [D, DIN])
    gv_d = din("gvec", [1, 6 * 64])
    peT_d = din("peT", [64, 64])
    wck1_d = din("w_ck1", [2048, 128])
    wck2_d = din("w_ck2", [128, 64])
    wcv1_d = din("w_cv1", [2048, 128])
    wcv2_d = din("w_cv2", [128, 64])
    woa_d = din("w_o_a", [512, D])
    wob_d = din("w_o_b", [512, D])
    wout_d = din("w_out", [D, D])
    wfg_d = din("w_ff_gate", [D, DFF])
    wfu_d = din("w_ff_up", [D, DFF])
    wfd_d = din("w_ff_down", [DFF, D])
    out_d = nc.dram_tensor("out", [nseq, S_TOK, D], F32, kind="ExternalOutput").ap()
    modrow_d = nc.dram_tensor("modrow", [4, 6 * D], F32, kind="Internal").ap()
    oT_d = nc.dram_tensor("oT_scr", [2, 4, 128, S_TOK], BF16, kind="ExternalOutput" if dbg else "Internal").ap()
    if dbg:
        hT_dbg = nc.dram_tensor("hT_dbg", [128, 8, S_TOK], BF16, kind="ExternalOutput").ap()
        x1_dbg = nc.dram_tensor("x1_dbg", [S_TOK, D], F32, kind="ExternalOutput").ap()
        mod_dbg = nc.dram_tensor("mod_dbg", [4, 6 * D], F32, kind="ExternalOutput").ap()

    st = contextlib.ExitStack()

    def sb(name, shape, dt=F32):
        return st.enter_context(nc.sbuf_tensor(name, shape, dt))

    with st:
        PS = [st.enter_context(nc.psum_tensor("ps%d" % i, [128, 512], F32)) for i in range(8)]
        PB = [Buf("ps%d" % i) for i in range(8)]

        def psb(i):
            return PS[i][:].bitcast(BF16)

        ident_b = sb("ident_b", [128, 128], BF16)
        ident_f = sb("ident_f", [128, 128], F32)
        Cm = sb("Cm", [128, 128], BF16)
        Wm = sb("Wm", [128, 128], BF16)
        cmask = sb("cmask", [128, S_TOK], BF16)
        ET = sb("ET", [68, S_TOK], BF16)
        selA = sb("selA", [128, NT, 32], F32)
        selB = sb("selB", [128, NT, 32], F32)
        aqc = sb("aqc", [128, NT, 8, 4], BF16)
        akc = sb("akc", [128, NT, 4], BF16)
        gbc = sb("gbc", [128, 6 * 64], F32)
        gains = sb("gains", [128, 4, 64], F32)
        gnc = sb("gnc", [128, 16], F32)
        modT = sb("modT", [128, 32, 4], F32)
        cb2 = sb("cb2", [128, 2], F32)
        pow2 = sb("pow2", [128, NBIS + 1], F32)
        hT = sb("hT", [128, 8, S_TOK], BF16)
        Vc = sb("Vc", [128, 2, 97], BF16)
        kc_aug = sb("kc_aug", [128, 2, 68], BF16)
        KcT = sb("KcT", [128, 2, 128], BF16)
        U_t = sb("U", [128, 65400], BF16)
        U = Arena(U_t[:])
        bC = Buf("consts")
        bOT = [Buf(), Buf()]
        bOut = Buf()

        hTb = [Buf("hT%d" % c) for c in range(4)]

        def pool(fn, reads=(), writes=()):
            return S.op("pool", fn, reads, writes)

        def dve(fn, reads=(), writes=()):
            return S.op("dve", fn, reads, writes)

        def act(fn, reads=(), writes=()):
            return S.op("act", fn, reads, writes)

        def pe(fn, reads=(), writes=()):
            return S.op("pe", fn, reads, writes)

        pool(_I("memset", ident_b[:], 1.0), writes=[bC])
        pool(_I("affine_select", ident_b[:], ident_b[:], [[1, 128]], ALU.is_equal, 0.0, base=0, channel_multiplier=-1), reads=[bC], writes=[bC])
        pool(_I("memset", ident_f[:], 1.0), writes=[bC])
        pool(_I("affine_select", ident_f[:], ident_f[:], [[1, 128]], ALU.is_equal, 0.0, base=0, channel_multiplier=-1), reads=[bC], writes=[bC])
        pool(_I("memset", Cm[:], 1.0), writes=[bC])
        pool(_I("affine_select", Cm[:], Cm[:], [[1, 128]], ALU.is_ge, 0.0, base=0, channel_multiplier=-1), reads=[bC], writes=[bC])
        pool(_I("memset", Wm[:], 1.0), writes=[bC])
        pool(_I("affine_select", Wm[:], Wm[:], [[-1, 128]], ALU.is_gt, 0.0, base=0, channel_multiplier=1), reads=[bC], writes=[bC])
        pool(_I("memset", cmask[:], 1.0), writes=[bC])
        pool(_I("affine_select", cmask[:], cmask[:], [[1, S_TOK]], ALU.is_ge, 0.0, base=-31, channel_multiplier=-16), reads=[bC], writes=[bC])
        pool(_I("memset", ET[0:32, :], 1.0), writes=[bC])
        pool(_I("memset", ET[32:64, :], 0.0), writes=[bC])
        pool(_I("memset", ET[64:68, :], 0.0), writes=[bC])
        pool(_I("affine_select", ET[0:32, :], ET[0:32, :], [[1, S_TOK]], ALU.is_ge, 0.0, base=0, channel_multiplier=-64), reads=[bC], writes=[bC])
        pool(_I("affine_select", ET[0:32, :], ET[0:32, :], [[-1, S_TOK]], ALU.is_ge, 0.0, base=63, channel_multiplier=64), reads=[bC], writes=[bC])
        for g in range(2):
            pool(_I("memset", Vc[:, g, 64:97], 1.0), writes=[bC])
            pool(_I("affine_select", Vc[:, g, 65:97], Vc[:, g, 65:97], [[-64, 32]], ALU.is_ge, 0.0, base=31, channel_multiplier=16), reads=[bC], writes=[bC])
            pool(_I("affine_select", Vc[:, g, 65:97], Vc[:, g, 65:97], [[64, 32]], ALU.is_ge, 0.0, base=63, channel_multiplier=-16), reads=[bC], writes=[bC])
        Dt = U.take(NT * 32, F32).rearrange("p (t j) -> p t j", j=32)
        jt = U.take(NT * 32, F32).rearrange("p (t j) -> p t j", j=32)
        f0 = U.take(NT * 32, F32).rearrange("p (t j) -> p t j", j=32)
        for lo_, base in ((0, 0), (64, -1)):
            pool(_I("iota", Dt[lo_:lo_ + 64], [[-2, NT], [1, 32]], base=base, channel_multiplier=0, allow_small_or_imprecise_dtypes=True), writes=[bC])
        pool(_I("iota", jt[:], [[0, NT], [1, 32]], base=0, channel_multiplier=0, allow_small_or_imprecise_dtypes=True), writes=[bC])
        dve(_I("tensor_single_scalar", f0[:], jt[:], 0.0, ALU.is_equal), reads=[bC], writes=[bC])
        dve(_I("tensor_single_scalar", jt[:], Dt[:], 0.0, ALU.is_equal), reads=[bC], writes=[bC])
        dve(_I("tensor_max", f0[:], f0[:], jt[:]), reads=[bC], writes=[bC])
        dve(_I("tensor_single_scalar", jt[:], Dt[:], -1.0, ALU.is_equal), reads=[bC], writes=[bC])
        dve(_I("tensor_max", f0[:], f0[:], jt[:]), reads=[bC], writes=[bC])
        dve(_I("tensor_single_scalar", jt[:], Dt[:], 0.0, ALU.is_le), reads=[bC], writes=[bC])
        dve(_I("tensor_sub", selA[:], jt[:], f0[:]), reads=[bC], writes=[bC])
        dve(_I("tensor_add", selB[:], jt[:], f0[:]), reads=[bC], writes=[bC])
        dve(_I("tensor_scalar", selB[:], selB[:], -1.0, 1e9, ALU.add, ALU.mult), reads=[bC], writes=[bC])
        hi_t = sb("hi_t", [128, NT], F32)
        lo_t = sb("lo_t", [128, 1], F32)
        for lo_, base in ((0, 0), (64, 64)):
            pool(_I("iota", hi_t[lo_:lo_ + 64], [[128, NT]], base=base, channel_multiplier=0, allow_small_or_imprecise_dtypes=True), writes=[bC])
            pool(_I("iota", lo_t[lo_:lo_ + 64], [[0, 1]], base=0, channel_multiplier=1, allow_small_or_imprecise_dtypes=True), writes=[bC])
        for h in range(8):
            sl = 2.0 ** -(h + 1)
            dve(_I("memset", aqc[:, :, h, 0:2], sl), reads=[bC], writes=[bC])
            dve(_I("tensor_scalar", aqc[:, :, h, 2], hi_t[:], -sl, None, ALU.mult), reads=[bC], writes=[bC])
            dve(_I("tensor_scalar", aqc[:, :, h, 3], lo_t[:].to_broadcast([128, NT]), -sl, None, ALU.mult), reads=[bC], writes=[bC])
        dve(_I("memset", akc[:, :, 2:4], 1.0), reads=[bC], writes=[bC])
        dve(_I("tensor_copy", akc[:, :, 0], hi_t[:]), reads=[bC], writes=[bC])
        dve(_I("tensor_copy", akc[:, :, 1], lo_t[:].to_broadcast([128, NT])), reads=[bC], writes=[bC])
        pn = sb("pn", [128, 1], F32)
        pool(_I("iota", pn[:], [[0, 1]], base=0, channel_multiplier=16, allow_small_or_imprecise_dtypes=True), writes=[bC])
        for g in range(2):
            dve(_I("tensor_copy", kc_aug[:, g, 64:65], pn[:]), reads=[bC], writes=[bC])
            dve(_I("memset", kc_aug[:, g, 65:66], 31.0), reads=[bC], writes=[bC])
            dve(_I("memset", kc_aug[:, g, 66:68], 1.0), reads=[bC], writes=[bC])
        for j in range(NBIS + 1):
            dve(_I("memset", pow2[:, j:j + 1], 2.0 ** -j), reads=[bC], writes=[bC])
        S.dma(gbc[:], gv_d.partition_broadcast(128), writes=[bC])
        S.dma(gnc[:], gn_d, writes=[bC])

        def gsl(i):
            return gbc[:, i * 64:(i + 1) * 64]
        for idx, (gk, gq) in enumerate(((2, 0), (3, 0), (1, 0), (5, 4))):
            dve(_I("scalar_tensor_tensor", gains[:, idx, :], gsl(gk), 0.125, gsl(gq), ALU.mult, ALU.mult), reads=[bC], writes=[bC])

        S.barrier()
        U.reset()
        scT = U.take(32, F32).rearrange("p (k b) -> p k b", b=4)
        wchunk = U.take(8 * 512, F32).rearrange("p (k n) -> p k n", k=8)
        bchunk = U.take(512, F32)
        mchunk = U.take(512, F32)
        bsc, bw, bbc, bm = Buf(), Buf(), Buf(), Buf()
        S.dma(scT, cT_d, writes=[bsc])
        act(_I("activation", scT, scT, AF.Silu), reads=[bsc], writes=[bsc])
        wada_v = wada_d.rearrange("(k p) n -> p k n", p=128)
        LNV = {0: 0, 1: 1, 3: 2, 4: 3}
        for c in range(12):
            S.dma(wchunk, wada_v[:, :, c * 512:(c + 1) * 512], writes=[bw])
            S.dma(bchunk[0:4, :], bada_d[:, c * 512:(c + 1) * 512].partition_broadcast(4), writes=[bbc])
            for k in range(8):
                pe(_I("matmul", PS[0][0:4, :], lhsT=scT[:, k, :], rhs=wchunk[:, k, :], start=(k == 0), stop=(k == 7)), reads=[bsc, bw], writes=[PB[0]])
            dve(_I("tensor_tensor", mchunk[0:4, :], PS[0][0:4, :], bchunk[0:4, :], ALU.add), reads=[PB[0], bbc], writes=[bm])
            S.dma(modrow_d[:, c * 512:(c + 1) * 512], mchunk[0:4, :], reads=[bm], writes=[bC])
            if dbg:
                S.dma(mod_dbg[:, c * 512:(c + 1) * 512], mchunk[0:4, :], reads=[bm], writes=[Buf()], is_output=True)
            vec, half = c // 2, c % 2
            if vec in LNV:
                for i in range(4):
                    col = (LNV[vec] * 8 + half * 4 + i) * 4
                    pe(_I("transpose", PS[1][:, col:col + 4], mchunk[0:4, i * 128:(i + 1) * 128], ident_f[0:4, 0:4]), reads=[bm, bC], writes=[PB[1]])
        dve(_I("tensor_copy", modT[:].rearrange("p a b -> p (a b)"), PS[1][:, 0:128]), reads=[PB[1]], writes=[bC])
        for which, gi in ((1, 0), (3, 1)):
            dve(_I("scalar_tensor_tensor",
                modT[:, which * 8:(which + 1) * 8, :], modT[:, which * 8:(which + 1) * 8, :], 1.0,
                gnc[:, gi * 8:(gi + 1) * 8].unsqueeze(2).to_broadcast([128, 8, 4]), ALU.add, ALU.mult), reads=[bC], writes=[bC])
        S.barrier()

        xt = [sb("xt%d" % i, [128, D], F32) for i in range(2)]
        xtb = [Buf() for _ in range(2)]
        xn = sb("xn", [128, D], BF16)
        xnb = Buf()
        sq = [sb("sq%d" % i, [128, 512], F32) for i in range(2)]
        sqb = [Buf(), Buf()]
        st16 = sb("st16", [128, 16], F32)
        stb = Buf()
        PT = [sb("PT%d" % i, [128, 512], BF16) for i in range(6)]
        PTb = [Buf() for _ in range(6)]
        sn = sb("sn", [128, 16], F32)
        snb = Buf()
        tmpo = sb("tmpo", [128, 4, 64], F32)
        tmpb = Buf()
        oacc = sb("oacc", [128, 8, 64], F32)
        oab = Buf()
        obf = sb("obf", [128, 512], BF16)
        obfb = Buf()
        oTs = sb("oTs", [128, 4, 128], BF16)
        oTsb = Buf()
        sm = sb("sm", [128, 64], F32)
        smb = Buf()
        state = {"pt": 0, "sb": 0}
        vstate = {}

        def layernorm(src_tile, src_buf, tl, which, b, psbank):
            act(_I("activation", sq[0][:, :].bitcast(BF16), src_tile, AF.Square, accum_out=st16[:, 0:1]), reads=[src_buf], writes=[sqb[0], stb])
            act(_I("activation", st16[:, 1:2], st16[:, 0:1], AF.Sqrt, bias=EPS, scale=1.0 / D), reads=[stb], writes=[stb])
            dve(_I("reciprocal", st16[:, 2:3], st16[:, 1:2]), reads=[stb], writes=[stb])
            dve(_I("tensor_scalar", xn[:], src_tile, st16[:, 2:3], None, ALU.mult), reads=[src_buf, stb], writes=[xnb])
            for j in range(8):
                bk = psbank + j // 4
                o0 = ((j % 4) * 2 + tl) * 128
                pe(_I("transpose", psb(bk)[:, o0:o0 + 128], xn[:, j * 128:(j + 1) * 128], ident_b[:]), reads=[xnb, bC], writes=[PB[bk]])

        def ln_evac(c2, which, b, psbank, hbuf):
            for j in range(8):
                bk = psbank + j // 4
                src = psb(bk)[:, (j % 4) * 256:(j % 4) * 256 + 256]
                Gc = modT[:, (2 * which + 1) * 8 + j, b:b + 1]
                Sc = modT[:, (2 * which) * 8 + j, b:b + 1]
                dve(_I("tensor_scalar", hT[:, j, c2 * 256:(c2 + 1) * 256], src, Gc, Sc, ALU.mult, ALU.add),
                    reads=[PB[bk], bC], writes=[hbuf])

        def load_w(dst, src, buf, q="pool"):
            S.dma(dst, src, writes=[buf], q=q)

        win_v = win_d.rearrange("(k p) n -> p k n", p=128)

        def rms_heads(psbank, nh, dst_stats):
            i = state["sb"] = (state["sb"] + 1) % 2
            act(_I("activation", sq[i][:, 0:nh * 64], PS[psbank][:, 0:nh * 64], AF.Square), reads=[PB[psbank]], writes=[sqb[i]])
            dve(_I("tensor_reduce", dst_stats, sq[i][:, 0:nh * 64].rearrange("p (h d) -> p h d", d=64), AX.X, ALU.add), reads=[sqb[i]], writes=[stb])

        def rstd_from(stats):
            act(_I("activation", stats, stats, AF.Sqrt, bias=EPS, scale=1.0 / 64), reads=[stb], writes=[stb])
            dve(_I("reciprocal", stats, stats), reads=[stb], writes=[stb])

        def make_units(QTh, qb, KTk, kb, kind, t, tiles, maskmm, pvb, slot, full_mask=None, banks=(4, 5)):
            ntl = len(tiles)
            return [dict(QTh=QTh, qb=qb, KTk=KTk, kb=kb, kind=kind, t=t, grp=tiles[g0:g0 + 4], g0=g0, ntl=ntl, maskmm=maskmm,
                         pvb=pvb, slot=slot, full_mask=full_mask, banks=banks) for g0 in range(0, ntl, 4)]

        def emit_A(u):
            qs = slice(u["t"] * 128, (u["t"] + 1) * 128)
            banks = u["banks"]
            bk = banks[state["pt"] % len(banks)]
            pi = state["pt"] % len(PT)
            state["pt"] += 1
            u["pi"] = pi
            grp = u["grp"]
            for i, (j, mt) in enumerate(grp):
                ks = slice(j * 128, (j + 1) * 128)
                pe(_I("matmul", PS[bk][:, i * 128:(i + 1) * 128], lhsT=u["KTk"][0:68, ks], rhs=u["QTh"][0:68, qs], start=True, stop=(u["maskmm"] is None)),
                   reads=[u["kb"], u["qb"]], writes=[PB[bk]])
                if u["maskmm"] is not None:
                    MTg, mb = u["maskmm"]
                    pe(_I("matmul", PS[bk][:, i * 128:(i + 1) * 128], lhsT=ET[0:68, ks], rhs=MTg[0:68, qs], start=False, stop=True),
                       reads=[mb, bC], writes=[PB[bk]])
            n = len(grp) * 128
            act(_I("activation", PT[pi][:, 0:n], PS[bk][:, 0:n], AF.Exp), reads=[PB[bk]], writes=[PTb[pi]])
            if u["full_mask"] is not None:
                mT, mTb = u["full_mask"]
                j0 = grp[0][0]
                pool(_I("tensor_tensor", PT[pi][:, 0:n], PT[pi][:, 0:n], mT[:, j0:j0 + len(grp), :].rearrange("p j c -> p (j c)"), ALU.mult),
                     reads=[mTb, PTb[pi]], writes=[PTb[pi]])
            for i, (j, mt) in enumerate(grp):
                if mt is not None:
                    mk = Cm if mt == "C" else Wm
                    pool(_I("tensor_tensor", PT[pi][:, i * 128:(i + 1) * 128], PT[pi][:, i * 128:(i + 1) * 128], mk[:], ALU.mult),
                         reads=[bC, PTb[pi]], writes=[PTb[pi]])

        def emit_B(u):
            pi = u["pi"]
            pvb = u["pvb"]
            po = PS[pvb][:, u["slot"] * 65:(u["slot"] + 1) * 65]
            for i, (j, mt) in enumerate(u["grp"]):
                gi = u["g0"] + i
                pe(_I("matmul", po, lhsT=PT[pi][:, i * 128:(i + 1) * 128], rhs=vstate["V"][:, j, u["kind"], :], start=(gi == 0), stop=(gi == u["ntl"] - 1)),
                   reads=[PTb[pi], vstate["bV"]], writes=[PB[pvb]])

        def run_units(units, L, between=None):
            n = len(units)
            for i in range(min(L, n)):
                emit_A(units[i])
            for i in range(n):
                if i + L < n:
                    emit_A(units[i + L])
                emit_B(units[i])
                if units[i].get("post") is not None:
                    units[i]["post"]()
                if between is not None:
                    between(i, n)

        def next_pv():
            state["pv"] = state.get("pv", 0) + 1
            return 6 if state["pv"] % 2 == 0 else 2

        def norm4(pvb, g, gate_view, gate_bufs, first):
            o4 = PS[pvb][:, 0:260].rearrange("p (h c) -> p h c", h=4)
            dve(_I("tensor_scalar", sn[:, 0:4], o4[:, :, 64], 1e-30, None, ALU.max), reads=[PB[pvb]], writes=[snb])
            dve(_I("reciprocal", sn[:, 4:8], sn[:, 0:4]), reads=[snb], writes=[snb])
            if gate_view is not None:
                dve(_I("tensor_tensor", sn[:, 4:8], sn[:, 4:8], gate_view, ALU.mult), reads=[snb] + gate_bufs, writes=[snb])
            wb = sn[:, 4:8].unsqueeze(2).to_broadcast([128, 4, 64])
            if first:
                dve(_I("tensor_tensor", oacc[:, 4 * g:4 * g + 4, :], o4[:, :, 0:64], wb, ALU.mult), reads=[PB[pvb], snb], writes=[oab])
            else:
                dve(_I("tensor_tensor", tmpo[:], o4[:, :, 0:64], wb, ALU.mult), reads=[PB[pvb], snb], writes=[tmpb])
                pool(_I("tensor_tensor", oacc[:, 4 * g:4 * g + 4, :], oacc[:, 4 * g:4 * g + 4, :], tmpo[:], ALU.add), reads=[tmpb, oab], writes=[oab])

        def flush_o(t, mix):
            dve(_I("tensor_copy", obf[:], oacc[:].rearrange("p h d -> p (h d)")), reads=[oab], writes=[obfb])
            for j in range(4):
                pe(_I("transpose", psb(3)[:, j * 128:(j + 1) * 128], obf[:, j * 128:(j + 1) * 128], ident_b[:]), reads=[obfb, bC], writes=[PB[3]])
            act(_I("activation", oTs[:].rearrange("p j c -> p (j c)"), psb(3)[:, 0:512], AF.Copy), reads=[PB[3]], writes=[oTsb])
            S.dma(oT_d[mix, :, :, t * 128:(t + 1) * 128].rearrange("j p c -> p j c"), oTs[:], reads=[oTsb], writes=[bOT[mix]])

        for s in range(nseq):
            b = s
            S.barrier()

            for c2 in range(8):
                for tl in range(2):
                    t = c2 * 2 + tl
                    i = t % 2
                    S.dma(xt[i][:], x_d[s, t * 128:(t + 1) * 128, :], writes=[xtb[i]])
                    layernorm(xt[i][:], xtb[i], tl, 0, b, 0)
                ln_evac(c2, 0, b, 0, hTb[c2 // 2])

            if dbg and s == 0:
                S.dma(hT_dbg, hT[:], reads=hTb, writes=[Buf()], is_output=True)
            U.reset()
            V_all = U.take(NT * 5 * 65).rearrange("p (t k c) -> p t k c", t=NT, k=5)
            bV = Buf()
            pool(_I("memset", V_all[:, :, :, 64:65], 1.0), writes=[bV])
            vstate["V"] = V_all
            vstate["bV"] = bV
            Wn = U.take(8 * 1304).rearrange("p (k n) -> p k n", k=8)
            QT = U.take(8 * S_TOK).rearrange("p (h n) -> p h n", h=8)
            KT = U.take(5 * S_TOK).rearrange("p (h n) -> p h n", h=5)
            q_aug = U.take(8 * 68).rearrange("p (h d) -> p h d", h=8)
            k_aug = U.take(4 * 68).rearrange("p (h d) -> p h d", h=4)
            kcT = U.take(S_TOK)
            vcT = U.take(S_TOK)
            MT = U.take(2 * S_TOK).rearrange("p (g n) -> p g n", g=2)
            sg = U.take(NT * 24, F32).rearrange("p (t c) -> p t c", c=24)
            bq_aug, bk_aug, bkc, bsg = Buf(), Buf(), Buf(), Buf()
            bWn = [Buf() for _ in range(8)]
            QTb = [Buf() for _ in range(8)]
            KTb = [Buf() for _ in range(5)]
            MTb = [Buf(), Buf()]
            pool(_I("memset", MT[32:64, :, :], 0.0), writes=MTb)
            pool(_I("memset", MT[64:68, :, :], 0.0), writes=MTb)
            for k in range(8):
                load_w(Wn[:, k, :], win_v[:, k, 0:1304], bWn[k])
            for c in range(4):
                for tl in range(4):
                    t = c * 4 + tl
                    ts = slice(t * 128, (t + 1) * 128)
                    pq, pk, pg = (0, 1, 2) if t % 2 == 0 else (4, 5, 6)
                    for k in range(8):
                        pe(_I("matmul", PS[pq][:, :], lhsT=hT[:, k, ts], rhs=Wn[:, k, 0:512], start=(k == 0), stop=(k == 7)), reads=[hTb[c], bWn[k]], writes=[PB[pq]])
                    for k in range(8):
                        pe(_I("matmul", PS[pk][:, :], lhsT=hT[:, k, ts], rhs=Wn[:, k, 768:1280], start=(k == 0), stop=(k == 7)), reads=[hTb[c], bWn[k]], writes=[PB[pk]])
                    for k in range(8):
                        pe(_I("matmul", PS[pg][:, 0:24], lhsT=hT[:, k, ts], rhs=Wn[:, k, 1280:1304], start=(k == 0), stop=(k == 7)), reads=[hTb[c], bWn[k]], writes=[PB[pg]])
                    rms_heads(pq, 8, st16[:, 0:8])
                    rms_heads(pk, 8, st16[:, 8:16])
                    rstd_from(st16[:, 0:16])
                    dve(_I("tensor_tensor", q_aug[:, :, 0:64], PS[pq][:, :].rearrange("p (h d) -> p h d", d=64), st16[:, 0:8].unsqueeze(2).to_broadcast([128, 8, 64]), ALU.mult),
                        reads=[PB[pq], stb], writes=[bq_aug])
                    dve(_I("tensor_copy", q_aug[:, :, 64:68], aqc[:, t, :, :]), reads=[bC], writes=[bq_aug])
                    for (c0, s0, kk, gi) in ((0, 8, 0, 0), (256, 12, 2, 1)):
                        dve(_I("tensor_tensor", sq[0][:, 0:128].rearrange("p (h d) -> p h d", d=64), PS[pk][:, c0:c0 + 128].rearrange("p (h d) -> p h d", d=64),
                                                                   st16[:, s0:s0 + 2].unsqueeze(2).to_broadcast([128, 2, 64]), ALU.mult), reads=[PB[pk], stb], writes=[sqb[0]])
                        dve(_I("tensor_tensor", k_aug[:, kk:kk + 2, 0:64], sq[0][:, 0:128].rearrange("p (h d) -> p h d", d=64),
                                                                   gains[:, gi, :].unsqueeze(1).to_broadcast([128, 2, 64]), ALU.mult), reads=[sqb[0], bC], writes=[bk_aug])
                    dve(_I("tensor_copy", k_aug[:, :, 64:68], akc[:, t, :].unsqueeze(1).to_broadcast([128, 4, 4])), reads=[bC], writes=[bk_aug])
                    act(_I("activation", V_all[:, t, 0:2, 0:64], PS[pk][:, 128:256].rearrange("p (h d) -> p h d", d=64), AF.Copy), reads=[PB[pk]], writes=[bV])
                    act(_I("activation", V_all[:, t, 2:4, 0:64], PS[pk][:, 384:512].rearrange("p (h d) -> p h d", d=64), AF.Copy), reads=[PB[pk]], writes=[bV])
                    act(_I("activation", sg[:, t, :], PS[pg][:, 0:24], AF.Sigmoid), reads=[PB[pg]], writes=[bsg])
                    for h in range(8):
                        pe(_I("transpose", psb(3)[0:68, h * 128:(h + 1) * 128], q_aug[:, h, :], ident_b[:]), reads=[bq_aug, bC], writes=[PB[3]])
                    for kk in range(4):
                        pe(_I("transpose", psb(7)[0:68, kk * 128:(kk + 1) * 128], k_aug[:, kk, :], ident_b[:]), reads=[bk_aug, bC], writes=[PB[7]])
                    act(_I("activation", QT[0:68, :, ts], psb(3)[0:68, :].rearrange("p (h c) -> p h c", h=8), AF.Copy), reads=[PB[3]], writes=QTb)
                    dve(_I("tensor_copy", KT[0:68, 0:4, ts], psb(7)[0:68, 0:512].rearrange("p (h c) -> p h c", h=4)), reads=[PB[7]], writes=KTb[0:4])
                cs = slice(c * 512, (c + 1) * 512)
                for (c0, dst) in ((512, kcT), (640, vcT)):
                    for k in range(8):
                        pe(_I("matmul", PS[0][:, :], lhsT=Wn[:, k, c0:c0 + 128], rhs=hT[:, k, cs], start=(k == 0), stop=(k == 7)), reads=[hTb[c], bWn[k]], writes=[PB[0]])
                    act(_I("activation", dst[:, cs], PS[0][:, :], AF.Copy), reads=[PB[0]], writes=[bkc])

            S.barrier()
            W1 = Wn.rearrange("p k n -> p (k n)")[:, 0:2 * 32 * 128].rearrange("p (a l n) -> p a l n", a=2, l=32)
            W2 = U.take(2 * 64).rearrange("p (a n) -> p a n", a=2)
            peT = U.take(64)
            HT = U.take(128)
            bW1, bH = Buf(), Buf()
            for a, (w1d, w2d) in enumerate(((wck1_d, wck2_d), (wcv1_d, wcv2_d))):
                for half in range(2):
                    load_w(W1[half * 64:half * 64 + 64, a, :, :], w1d.rearrange("(l d) n -> d l n", d=64), bW1)
                load_w(W2[:, a, :], w2d, bW1)
            load_w(peT[0:64, :], peT_d, bW1)
            if s == 0:
                for a in range(2):
                    for l in range(32):
                        pe(_I("matmul", PS[2][:, a:a + 1], lhsT=W1[0:64, a, l, :], rhs=peT[0:64, a * 32 + l:a * 32 + l + 1], start=(l == 0), stop=(l == 31)),
                           reads=[bW1], writes=[PB[2]])
                dve(_I("tensor_copy", cb2[:], PS[2][:, 0:2]), reads=[PB[2]], writes=[bC])
            for a, srcT in enumerate((kcT, vcT)):
                for g in range(2):
                    base = g * 64
                    v3 = srcT[base:base + 64, :].rearrange("p (n s) -> p n s", s=16)
                    for l in range(32):
                        rhs = v3[:, (l // 16):(l // 16) + 127, l % 16]
                        pe(_I("matmul", PS[0][:, 0:127], lhsT=W1[base:base + 64, a, l, :], rhs=rhs, start=(l == 0), stop=(l == 31)),
                           reads=[bW1, bkc], writes=[PB[0]])
                    act(_I("activation", HT[:, 0:127], PS[0][:, 0:127], AF.Silu, bias=cb2[:, a:a + 1]), reads=[PB[0], bC], writes=[bH])
                    pe(_I("matmul", PS[1][0:127, 0:64], lhsT=HT[:, 0:127], rhs=W2[:, a, :], start=True, stop=True), reads=[bH, bW1], writes=[PB[1]])
                    if a == 0:
                        act(_I("activation", sq[0][0:127, 0:64], PS[1][0:127, 0:64], AF.Square, accum_out=st16[0:127, 0:1]), reads=[PB[1]], writes=[sqb[0], stb])
                        rstd_from(st16[0:127, 0:1])
                        dve(_I("tensor_scalar", sq[0][0:127, 0:64], PS[1][0:127, 0:64], st16[0:127, 0:1], None, ALU.mult), reads=[PB[1], stb], writes=[sqb[0]])
                        dve(_I("tensor_tensor", kc_aug[0:127, g, 0:64], sq[0][0:127, 0:64], gains[0:127, 2, :], ALU.mult), reads=[sqb[0], bC], writes=[bC])
                        pe(_I("transpose", psb(3)[0:68, 0:127], kc_aug[0:127, g, :], ident_b[0:127, 0:127]), reads=[bC], writes=[PB[3]])
                        dve(_I("tensor_copy", KcT[0:68, g, 0:127], psb(3)[0:68, 0:127]), reads=[PB[3]], writes=[bC])
                    else:
                        act(_I("activation", Vc[0:127, g, 0:64], PS[1][0:127, 0:64], AF.Copy), reads=[PB[1]], writes=[bC])

            imp = sb("imp_%d" % s, [128, 4, 32], F32) if s == 0 else imp
            impb = Buf()
            for t in range(NT):
                qs = slice(t * 128, (t + 1) * 128)
                for g in range(2):
                    for hh in range(4):
                        h = 4 * g + hh
                        pe(_I("matmul", PS[4][0:127, hh * 128:(hh + 1) * 128], lhsT=KcT[0:68, g, 0:127], rhs=QT[0:68, h, qs], start=True, stop=True),
                           reads=[bC, QTb[h]], writes=[PB[4]])
                    dve(_I("tensor_scalar", sq[1][0:127, :], PS[4][0:127, :], 60.0, None, ALU.min), reads=[PB[4]], writes=[sqb[1]])
                    act(_I("activation", PT[0][0:127, :], sq[1][0:127, :], AF.Exp), reads=[sqb[1]], writes=[PTb[0]])
                    dve(_I("tensor_tensor", PT[0][0:127, :].rearrange("p (h c) -> p h c", h=4), PT[0][0:127, :].rearrange("p (h c) -> p h c", h=4),
                                                  cmask[0:127, qs].unsqueeze(1).to_broadcast([127, 4, 128]), ALU.mult), reads=[bC, PTb[0]], writes=[PTb[0]])
                    for hh in range(4):
                        pe(_I("matmul", PS[7][:, hh * 97:(hh + 1) * 97], lhsT=PT[0][0:127, hh * 128:(hh + 1) * 128], rhs=Vc[0:127, g, :], start=True, stop=True),
                           reads=[PTb[0], bC], writes=[PB[7]])
                    o4 = PS[7][:, 0:388].rearrange("p (h c) -> p h c", h=4)
                    dve(_I("tensor_scalar", sm[:, 0:4], o4[:, :, 64], 1e-30, None, ALU.max), reads=[PB[7]], writes=[smb])
                    dve(_I("reciprocal", sm[:, 4:8], sm[:, 0:4]), reads=[smb], writes=[smb])
                    dve(_I("tensor_tensor", sm[:, 8:12], sm[:, 4:8], sg[:, t, :].rearrange("p (h r) -> p h r", r=3)[:, 4 * g:4 * g + 4, 0], ALU.mult), reads=[smb, bsg], writes=[smb])
                    dve(_I("tensor_tensor", oacc[:, 4 * g:4 * g + 4, :], o4[:, :, 0:64], sm[:, 8:12].unsqueeze(2).to_broadcast([128, 4, 64]), ALU.mult),
                        reads=[PB[7], smb], writes=[oab])
                    dve(_I("tensor_tensor", imp[:], o4[:, :, 65:97], sm[:, 4:8].unsqueeze(2).to_broadcast([128, 4, 32]), ALU.mult), reads=[PB[7], smb], writes=[impb])
                    dve(_I("tensor_reduce", sm[:, 16:48], imp[:].rearrange("p h j -> p j h"), AX.X, ALU.add), reads=[impb], writes=[smb])
                    dve(_I("tensor_tensor", sm[:, 16:48], sm[:, 16:48], selA[:, t, :], ALU.mult), reads=[smb, bC], writes=[smb])
                    dve(_I("tensor_tensor", sm[:, 16:48], sm[:, 16:48], selB[:, t, :], ALU.add), reads=[smb, bC], writes=[smb])
                    dve(_I("max", out=sm[:, 48:56], in_=sm[:, 16:48]), reads=[smb], writes=[smb])
                    dve(_I("match_replace", out=imp[:, 0, :], in_to_replace=sm[:, 48:56], in_values=sm[:, 16:48], imm_value=-3e38), reads=[smb], writes=[impb])
                    dve(_I("max", out=sm[:, 56:64], in_=imp[:, 0, :]), reads=[impb], writes=[smb])
                    dve(_I("tensor_scalar", sm[:, 16:48], sm[:, 16:48], sm[:, 63:64], None, ALU.is_ge), reads=[smb], writes=[smb])
                    dve(_I("tensor_scalar", obf[:, 0:32], sm[:, 16:48], -1.0, -NEG, ALU.add, ALU.mult), reads=[smb], writes=[obfb])
                    pe(_I("transpose", psb(3)[0:32, 0:128], obf[:, 0:32], ident_b[:]), reads=[obfb, bC], writes=[PB[3]])
                    act(_I("activation", MT[0:32, g, qs], psb(3)[0:32, 0:128], AF.Copy), reads=[PB[3]], writes=[MTb[g]])
                sg3 = sg[:, t, :].rearrange("p (h r) -> p h r", r=3)
                units = []
                for g in range(2):
                    pvb = next_pv()
                    tiles = [(j, "C" if j == t else None) for j in range(t + 1)]
                    for hh in range(4):
                        h = 4 * g + hh
                        units += make_units(QT[:, h, :], QTb[h], KT[:, g, :], KTb[g], g, t, tiles, (MT[:, g, :], MTb[g]), pvb, hh, banks=(4, 5, 0, 1))
                    units[-1]["post"] = (lambda pvb=pvb, g=g, gv=sg3[:, 4 * g:4 * g + 4, 1]: norm4(pvb, g, gv, [bsg], False))
                    pvb = next_pv()
                    tiles = [(j, "C" if j == t else ("W" if j == t - 4 else None)) for j in range(max(0, t - 4), t + 1)]
                    for hh in range(4):
                        h = 4 * g + hh
                        units += make_units(QT[:, h, :], QTb[h], KT[:, 2 + g, :], KTb[2 + g], 2 + g, t, tiles, None, pvb, hh, banks=(4, 5, 0, 1))
                    units[-1]["post"] = (lambda pvb=pvb, g=g, gv=sg3[:, 4 * g:4 * g + 4, 2]: norm4(pvb, g, gv, [bsg], False))
                run_units(units, 2)
                flush_o(t, 0)

            S.barrier()
            U.reset()
            V_all = U.take(NT * 5 * 65).rearrange("p (t k c) -> p t k c", t=NT, k=5)
            bV = Buf()
            pool(_I("memset", V_all[:, :, :, 64:65], 1.0), writes=[bV])
            vstate["V"] = V_all
            vstate["bV"] = bV
            Wd = U.take(8 * 1352).rearrange("p (k n) -> p k n", k=8)
            QT = U.take(8 * S_TOK).rearrange("p (h n) -> p h n", h=8)
            KT = U.take(5 * S_TOK).rearrange("p (h n) -> p h n", h=5)
            q_aug = U.take(8 * 68).rearrange("p (h d) -> p h d", h=8)
            k_aug = U.take(4 * 68).rearrange("p (h d) -> p h d", h=4)
            iqT = U.take(4 * S_TOK).rearrange("p (m n) -> p m n", m=4)
            ikT = U.take(S_TOK)
            iw = U.take(NT * 8, F32).rearrange("p (t c) -> p t c", c=8)
            sc = U.take(S_TOK, F32)
            rl = U.take(512, F32)
            maskq = U.take(S_TOK)
            maskT = U.take(S_TOK).rearrange("p (j c) -> p j c", c=128)
            junk = U.take(S_TOK)
            biq, bik, biw, bsc, brl, bmq, bmT, bjk = (Buf() for _ in range(8))
            bWd = [Buf() for _ in range(8)]
            QTb = [Buf() for _ in range(8)]
            KTb = [Buf() for _ in range(5)]
            for k in range(8):
                load_w(Wd[:, k, 0:1224], win_v[:, k, 1304:2528], bWd[k])
                load_w(Wd[:, k, 1224:1288], win_v[:, k, 2456:2520], bWd[k])
                load_w(Wd[:, k, 1288:1352], win_v[:, k, 2456:2520], bWd[k])
            for c in range(4):
                cs = slice(c * 512, (c + 1) * 512)
                for tl in range(4):
                    t = c * 4 + tl
                    ts = slice(t * 128, (t + 1) * 128)
                    pq, pk, pg = (0, 1, 2) if t % 2 == 0 else (4, 5, 6)
                    for k in range(8):
                        pe(_I("matmul", PS[pq][:, :], lhsT=hT[:, k, ts], rhs=Wd[:, k, 0:512], start=(k == 0), stop=(k == 7)), reads=[hTb[c], bWd[k]], writes=[PB[pq]])
                    for k in range(8):
                        pe(_I("matmul", PS[pk][:, 0:128], lhsT=hT[:, k, ts], rhs=Wd[:, k, 512:640], start=(k == 0), stop=(k == 7)), reads=[hTb[c], bWd[k]], writes=[PB[pk]])
                    for k in range(8):
                        pe(_I("matmul", PS[pg][:, 0:8], lhsT=hT[:, k, ts], rhs=Wd[:, k, 1216:1224], start=(k == 0), stop=(k == 7)), reads=[hTb[c], bWd[k]], writes=[PB[pg]])
                    rms_heads(pq, 8, st16[:, 0:8])
                    rms_heads(pk, 1, st16[:, 8:9])
                    rstd_from(st16[:, 0:9])
                    dve(_I("tensor_tensor", q_aug[:, :, 0:64], PS[pq][:, :].rearrange("p (h d) -> p h d", d=64), st16[:, 0:8].unsqueeze(2).to_broadcast([128, 8, 64]), ALU.mult),
                        reads=[PB[pq], stb], writes=[bq_aug])
                    dve(_I("tensor_copy", q_aug[:, :, 64:68], aqc[:, t, :, :]), reads=[bC], writes=[bq_aug])
                    dve(_I("tensor_scalar", sq[0][:, 0:64], PS[pk][:, 0:64], st16[:, 8:9], None, ALU.mult), reads=[PB[pk], stb], writes=[sqb[0]])
                    dve(_I("tensor_tensor", k_aug[:, 0, 0:64], sq[0][:, 0:64], gains[:, 3, :], ALU.mult), reads=[sqb[0], bC], writes=[bk_aug])
                    dve(_I("tensor_copy", k_aug[:, 0, 64:68], akc[:, t, :]), reads=[bC], writes=[bk_aug])
                    act(_I("activation", V_all[:, t, 4, 0:64], PS[pk][:, 64:128], AF.Copy), reads=[PB[pk]], writes=[bV])
                    act(_I("activation", iw[:, t, :], PS[pg][:, 0:8], AF.Copy, scale=8.0 ** -0.5), reads=[PB[pg]], writes=[biw])
                    for h in range(8):
                        pe(_I("transpose", psb(3)[0:68, h * 128:(h + 1) * 128], q_aug[:, h, :], ident_b[:]), reads=[bq_aug, bC], writes=[PB[3]])
                    pe(_I("transpose", psb(7)[0:68, 0:128], k_aug[:, 0, :], ident_b[:]), reads=[bk_aug, bC], writes=[PB[7]])
                    act(_I("activation", QT[0:68, :, ts], psb(3)[0:68, :].rearrange("p (h c) -> p h c", h=8), AF.Copy), reads=[PB[3]], writes=QTb)
                    dve(_I("tensor_copy", KT[0:68, 4, ts], psb(7)[0:68, 0:128]), reads=[PB[7]], writes=[KTb[4]])
                for m in range(4):
                    for k in range(8):
                        pe(_I("matmul", PS[0][:, :], lhsT=Wd[:, k, 640 + m * 128:640 + (m + 1) * 128], rhs=hT[:, k, cs], start=(k == 0), stop=(k == 7)), reads=[hTb[c], bWd[k]], writes=[PB[0]])
                    act(_I("activation", iqT[:, m, cs], PS[0][:, :], AF.Copy, scale=0.125), reads=[PB[0]], writes=[biq])
                for k in range(8):
                    pe(_I("matmul", PS[1][:, :], lhsT=Wd[:, k, 1224:1352], rhs=hT[:, k, cs], start=(k == 0), stop=(k == 7)), reads=[hTb[c], bWd[k]], writes=[PB[1]])
                act(_I("activation", ikT[:, cs], PS[1][:, :], AF.Copy), reads=[PB[1]], writes=[bik])

            S.barrier()
            Wd_flat = Wd.rearrange("p k n -> p (k n)")
            scs = [sc, Wd_flat[:, 0:4096].bitcast(F32)]
            maskqs = [maskq, Wd_flat[:, 4096:6144]]
            maskTs = [maskT, Wd_flat[:, 6144:8192].rearrange("p (j c) -> p j c", c=128)]
            bscs, bmqs, bmTs = [Buf(), Buf()], [Buf(), Buf()], [Buf(), Buf()]
            smx = [sb("smx%d_%d" % (s, i), [128, 40], F32) for i in range(2)] if s == 0 else smx
            smxb = [Buf(), Buf()]

            rls = [rl, Wd_flat[:, 8192:9216].bitcast(F32)]
            brls = [brl, Buf()]

            def indexer_work(t):
                p = t % 2
                sc_, bsc_, sm_, smb_ = scs[p], bscs[p], smx[p], smxb[p]
                qs = slice(t * 128, (t + 1) * 128)
                nk = (t + 1) * 128
                nch = (nk + 511) // 512
                items = []
                idx = 0
                for h in range(8):
                    base = (h % 2) * 64
                    for cc in range(nch):
                        w = min(512, nk - cc * 512)
                        cs = slice(cc * 512, cc * 512 + w)

                        def piece(h=h, base=base, w=w, cs=cs, k=idx):
                            bk = k % 2
                            rl_, brl_ = rls[k % 2], brls[k % 2]
                            pe(_I("matmul", PS[bk][:, 0:w], lhsT=iqT[base:base + 64, h // 2, qs], rhs=ikT[base:base + 64, cs], start=True, stop=True),
                               reads=[biq, bik], writes=[PB[bk]])
                            act(_I("activation", rl_[:, 0:w], PS[bk][:, 0:w], AF.Relu), reads=[PB[bk]], writes=[brl_])
                            if h == 0:
                                dve(_I("tensor_scalar", sc_[:, cs], rl_[:, 0:w], iw[:, t, 0:1], None, ALU.mult), reads=[brl_, biw], writes=[bsc_])
                            else:
                                dve(_I("scalar_tensor_tensor", sc_[:, cs], rl_[:, 0:w], iw[:, t, h:h + 1], sc_[:, cs], ALU.mult, ALU.add), reads=[brl_, biw, bsc_], writes=[bsc_])
                        items.append(piece)
                        idx += 1

                def post():
                    dve(_I("tensor_reduce", sm_[:, 0:1], sc_[:, 0:nk], AX.X, ALU.max, apply_absolute_value=True), reads=[bsc_], writes=[smb_])
                    pool(_I("affine_select", sc_[:, t * 128:(t + 1) * 128], sc_[:, t * 128:(t + 1) * 128], [[-1, 128]], ALU.is_ge, -3e38, base=0, channel_multiplier=1), reads=[bsc_, smb_], writes=[bsc_])
                    dve(_I("tensor_scalar", sm_[:, 8:8 + NBIS + 1], pow2[:], sm_[:, 0:1], None, ALU.mult), reads=[smb_, bC], writes=[smb_])
                    dve(_I("memset", sm_[:, 1:2], 0.0), reads=[smb_], writes=[smb_])
                items.append(post)
                return items

            def tile_work(t):
                return indexer_work(t) + [(lambda j=j: bisect_iter(t, j)) for j in range(NBIS)] + [lambda: finish_mask(t)]

            def bisect_iter(t, j):
                p = t % 2
                sc_, bsc_, sm_, smb_ = scs[p], bscs[p], smx[p], smxb[p]
                nk = (t + 1) * 128
                dve(_I("tensor_scalar", maskqs[p][:, 0:nk], sc_[:, 0:nk], sm_[:, 1:2], None, ALU.is_ge, ALU.add, accum_out=sm_[:, 2:3]), reads=[bsc_, smb_], writes=[bmqs[p], smb_])
                dve(_I("tensor_scalar", sm_[:, 3:4], sm_[:, 2:3], 255.5, -0.5, ALU.is_ge, ALU.add), reads=[smb_], writes=[smb_])
                dve(_I("scalar_tensor_tensor", sm_[:, 1:2], sm_[:, 3:4], sm_[:, 8 + j:9 + j], sm_[:, 1:2], ALU.mult, ALU.add), reads=[smb_], writes=[smb_])

            def finish_mask(t):
                p = t % 2
                sc_, bsc_, sm_, smb_ = scs[p], bscs[p], smx[p], smxb[p]
                nk = (t + 1) * 128
                dve(_I("tensor_tensor", sm_[:, 1:2], sm_[:, 1:2], sm_[:, 8 + NBIS:9 + NBIS], ALU.subtract), reads=[smb_], writes=[smb_])
                dve(_I("tensor_scalar", maskqs[p][:, 0:nk], sc_[:, 0:nk], sm_[:, 1:2], None, ALU.is_ge), reads=[bsc_, smb_], writes=[bmqs[p]])
                for j in range(t + 1):
                    bk = 3 if (j // 8) % 2 == 0 else 7
                    pe(_I("transpose", psb(bk)[:, (j % 8) * 128:(j % 8 + 1) * 128], maskqs[p][:, j * 128:(j + 1) * 128], ident_b[:]), reads=[bmqs[p], bC], writes=[PB[bk]])
                    if j % 8 == 7 or j == t:
                        j0 = (j // 8) * 8
                        n = j - j0 + 1
                        act(_I("activation", maskTs[p][:, j0:j0 + n, :].rearrange("p j c -> p (j c)"), psb(bk)[:, 0:n * 128], AF.Copy), reads=[PB[bk]], writes=[bmTs[p]])

            for item in tile_work(0):
                item()
            for t in range(NT):
                tiles = [(j, None) for j in range(t + 1)]
                units = []
                for g2 in range(2):
                    pvb = next_pv()
                    for hh in range(4):
                        h = 4 * g2 + hh
                        units += make_units(QT[:, h, :], QTb[h], KT[:, 4, :], KTb[4], 4, t, tiles, None, pvb, hh, full_mask=(maskTs[t % 2], bmTs[t % 2]))
                    units[-1]["post"] = (lambda pvb=pvb, g2=g2: norm4(pvb, g2, None, [], True))
                W = tile_work(t + 1) if t + 1 < NT else []
                done = [0]

                def between(i, n, W=W, done=done):
                    target = min(len(W), ((i + 1) * len(W) * 10) // (n * 9) + 1)
                    while done[0] < target:
                        W[done[0]]()
                        done[0] += 1
                run_units(units, 1, between)
                while done[0] < len(W):
                    W[done[0]]()
                    done[0] += 1
                flush_o(t, 1)

            for hf in range(2):
                S.barrier()
                U.reset()
                Wg = U.take(8 * 2048).rearrange("p (k n) -> p k n", k=8)
                Woa = U.take(4 * D).rearrange("p (k n) -> p k n", k=4)
                Wob = U.take(4 * D).rearrange("p (k n) -> p k n", k=4)
                Wout = U.take(8 * D).rearrange("p (k n) -> p k n", k=8)
                Wff_region = (Wg, Woa, Wob, Wout)
                xacc = U.take(8 * D, F32).rearrange("p (t n) -> p t n", t=8)
                yT = U.take(8 * 512).rearrange("p (k n) -> p k n", k=8)
                oaT = U.take(4 * 512).rearrange("p (k n) -> p k n", k=4)
                obT = U.take(4 * 512).rearrange("p (k n) -> p k n", k=4)
                sga = U.take(512)
                sgb = U.take(512)
                t1 = U.take(512, F32)
                t2 = U.take(512, F32)
                aT = yT
                g1bc = U.take(D, F32)
                g2bc = U.take(D, F32)
                bG = Buf()
                S.dma(g1bc, modrow_d[b:b + 1, 2 * D:3 * D].partition_broadcast(128), writes=[bG])
                S.dma(g2bc, modrow_d[b:b + 1, 5 * D:6 * D].partition_broadcast(128), writes=[bG])
                bya, byT, boa, bob, bsga, bsgb, bt1, bt2, baT = (Buf() for _ in range(9))
                bWg = [Buf() for _ in range(8)]
                bWo = [Buf() for _ in range(8)]
                bWa = [Buf() for _ in range(4)]
                bWb = [Buf() for _ in range(4)]
                xab = [Buf() for _ in range(8)]
                for k in range(8):
                    load_w(Wg[:, k, :], win_v[:, k, 2528:4576], bWg[k])
                    load_w(Wout[:, k, :], wout_d.rearrange("(k p) n -> p k n", p=128)[:, k, :], bWo[k])
                for k in range(4):
                    load_w(Woa[:, k, :], woa_d.rearrange("(k p) n -> p k n", p=128)[:, k, :], bWa[k])
                    load_w(Wob[:, k, :], wob_d.rearrange("(k p) n -> p k n", p=128)[:, k, :], bWb[k])
                for cl in range(2):
                    c = hf * 2 + cl
                    cs = slice(c * 512, (c + 1) * 512)
                    S.dma(oaT, oT_d[0, :, :, cs].rearrange("j p c -> p j c"), reads=[bOT[0]], writes=[boa])
                    S.dma(obT, oT_d[1, :, :, cs].rearrange("j p c -> p j c"), reads=[bOT[1]], writes=[bob])
                    for f in range(8):
                        fs = slice(f * 128, (f + 1) * 128)
                        for k in range(8):
                            pe(_I("matmul", PS[0][:, :], lhsT=Wg[:, k, fs], rhs=hT[:, k, cs], start=(k == 0), stop=(k == 7)), reads=[bWg[k], hTb[c]], writes=[PB[0]])
                        act(_I("activation", sga, PS[0][:, :], AF.Sigmoid), reads=[PB[0]], writes=[bsga])
                        for k in range(8):
                            pe(_I("matmul", PS[1][:, :], lhsT=Wg[:, k, 1024 + f * 128:1024 + (f + 1) * 128], rhs=hT[:, k, cs], start=(k == 0), stop=(k == 7)), reads=[bWg[k], hTb[c]], writes=[PB[1]])
                        act(_I("activation", sgb, PS[1][:, :], AF.Sigmoid), reads=[PB[1]], writes=[bsgb])
                        for k in range(4):
                            pe(_I("matmul", PS[2][:, :], lhsT=Woa[:, k, fs], rhs=oaT[:, k, :], start=(k == 0), stop=(k == 3)), reads=[bWa[k], boa], writes=[PB[2]])
                        for k in range(4):
                            pe(_I("matmul", PS[4][:, :], lhsT=Wob[:, k, fs], rhs=obT[:, k, :], start=(k == 0), stop=(k == 3)), reads=[bWb[k], bob], writes=[PB[4]])
                        dve(_I("tensor_tensor", t1, PS[2][:, :], sga, ALU.mult), reads=[PB[2], bsga], writes=[bt1])
                        dve(_I("tensor_tensor", t2, PS[4][:, :], sgb, ALU.mult), reads=[PB[4], bsgb], writes=[bt2])
                        dve(_I("tensor_tensor", yT[:, f, :], t1, t2, ALU.add), reads=[bt1, bt2], writes=[byT])
                    for tl in range(4):
                        t = c * 4 + tl
                        tt = t - hf * 8
                        i = t % 2
                        S.dma(xt[i][:], x_d[s, t * 128:(t + 1) * 128, :], writes=[xtb[i]])
                        for h2 in range(2):
                            ns = slice(h2 * 512, (h2 + 1) * 512)
                            bk = 5 + h2
                            for k in range(8):
                                pe(_I("matmul", PS[bk][:, :], lhsT=yT[:, k, tl * 128:(tl + 1) * 128], rhs=Wout[:, k, ns], start=(k == 0), stop=(k == 7)), reads=[byT, bWo[k]], writes=[PB[bk]])
                            dve(_I("tensor_tensor", t1, PS[bk][:, :], g1bc[:, ns], ALU.mult), reads=[PB[bk], bG], writes=[bt1])
                            dve(_I("tensor_tensor", xacc[:, tt, ns], t1, xt[i][:, ns], ALU.add), reads=[bt1, xtb[i]], writes=[xab[tt]])
                        if dbg and s == 0:
                            S.dma(x1_dbg[t * 128:(t + 1) * 128, :], xacc[:, tt, :], reads=[xab[tt]], writes=[Buf()], is_output=True)
                        layernorm(xacc[:, tt, :], xab[tt], tl % 2, 1, b, 0)
                        if tl % 2 == 1:
                            ln_evac(t // 2, 1, b, 0, hTb[c])
                S.barrier()
                for (f0_, nf) in ((0, 8), (8, 8), (16, 6)):
                    Wfg = Wg.rearrange("p k n -> p (k n)")[:, 0:8 * 1024].rearrange("p (k n) -> p k n", k=8)
                    Wfu = Wg.rearrange("p k n -> p (k n)")[:, 8 * 1024:16 * 1024].rearrange("p (k n) -> p k n", k=8)
                    Wfd = Wout.rearrange("p k n -> p (k n)")[:, 0:8 * D].rearrange("p (k n) -> p k n", k=8)
                    nfc = nf * 128
                    if f0_ == 0:
                        bFg = [Buf() for _ in range(8)]
                        bFu = [Buf() for _ in range(8)]
                        bFd = [Buf() for _ in range(8)]
                    for k in range(8):
                        load_w(Wfg[:, k, 0:nfc], wfg_d.rearrange("(k p) n -> p k n", p=128)[:, k, f0_ * 128:f0_ * 128 + nfc], bFg[k])
                        load_w(Wfu[:, k, 0:nfc], wfu_d.rearrange("(k p) n -> p k n", p=128)[:, k, f0_ * 128:f0_ * 128 + nfc], bFu[k])
                    for k in range(nf):
                        load_w(Wfd[:, k, :], wfd_d[(f0_ + k) * 128:(f0_ + k + 1) * 128, :], bFd[k])
                    for cl in range(2):
                        c = hf * 2 + cl
                        cs = slice(c * 512, (c + 1) * 512)
                        for f in range(nf):
                            fs = slice(f * 128, (f + 1) * 128)
                            for k in range(8):
                                pe(_I("matmul", PS[0][:, :], lhsT=Wfg[:, k, fs], rhs=hT[:, k, cs], start=(k == 0), stop=(k == 7)), reads=[bFg[k], hTb[c]], writes=[PB[0]])
                            for k in range(8):
                                pe(_I("matmul", PS[1][:, :], lhsT=Wfu[:, k, fs], rhs=hT[:, k, cs], start=(k == 0), stop=(k == 7)), reads=[bFu[k], hTb[c]], writes=[PB[1]])
                            act(_I("activation", t1, PS[0][:, :], AF.Silu), reads=[PB[0]], writes=[bt1])
                            dve(_I("tensor_tensor", aT[:, f, :], t1, PS[1][:, :], ALU.mult), reads=[bt1, PB[1]], writes=[baT])
                        for tl in range(4):
                            tt = cl * 4 + tl
                            for h2 in range(2):
                                ns = slice(h2 * 512, (h2 + 1) * 512)
                                bk = 5 + h2
                                for f in range(nf):
                                    pe(_I("matmul", PS[bk][:, :], lhsT=aT[:, f, tl * 128:(tl + 1) * 128], rhs=Wfd[:, f, ns], start=(f == 0), stop=(f == nf - 1)), reads=[baT, bFd[f]], writes=[PB[bk]])
                                dve(_I("tensor_tensor", t2, PS[bk][:, :], g2bc[:, ns], ALU.mult), reads=[PB[bk], bG], writes=[bt2])
                                dve(_I("tensor_tensor", xacc[:, tt, ns], xacc[:, tt, ns], t2, ALU.add), reads=[bt2, xab[tt]], writes=[xab[tt]])
                for tt in range(8):
                    t = hf * 8 + tt
                    S.dma(out_d[s, t * 128:(t + 1) * 128, :], xacc[:, tt, :], reads=[xab[tt]], writes=[Buf()], is_output=True)
        S.emit()
    return nc


def _prep_common(inp):
    f = lambda a: np.ascontiguousarray(np.asarray(a, dtype=np.float32))
    gn = np.concatenate([inp["g_norm1"][0].reshape(8, 128).T, inp["g_norm2"][0].reshape(8, 128).T], axis=1)
    gvec = np.concatenate([inp[k][0] for k in ("g_q_a", "g_kc_a", "g_ks_a", "g_kw_a", "g_q_b", "g_k_b")])[None, :]
    peT = np.concatenate([inp["pe_ck"][0].T, inp["pe_cv"][0].T], axis=1)
    return {
        "w_ada": f(inp["w_ada"][0]), "b_ada": f(inp["b_ada"]), "gn": f(gn), "w_in": f(inp["w_in"][0]),
        "gvec": f(gvec), "peT": f(peT), "w_ck1": f(inp["w_ck1"][0]), "w_ck2": f(inp["w_ck2"][0]),
        "w_cv1": f(inp["w_cv1"][0]), "w_cv2": f(inp["w_cv2"][0]), "w_o_a": f(inp["w_o_a"][0]),
        "w_o_b": f(inp["w_o_b"][0]), "w_out": f(inp["w_out"][0]), "w_ff_gate": f(inp["w_ff_gate"][0]),
        "w_ff_up": f(inp["w_ff_up"][0]), "w_ff_down": f(inp["w_ff_down"][0]),
    }


def _core_map(common, x, c, i, nseq):
    m = dict(common)
    m["x"] = np.ascontiguousarray(x[i * nseq:(i + 1) * nseq])
    cc = np.zeros((4, D), np.float32)
    cc[:nseq] = c[i * nseq:(i + 1) * nseq]
    m["cT"] = np.ascontiguousarray(cc.T.reshape(8, 128, 4).transpose(1, 0, 2))
    return m


def kernel(**inputs):
    x = np.asarray(inputs["x"], dtype=np.float32)
    c = np.asarray(inputs["c"], dtype=np.float32)
    n = 8
    nseq = x.shape[0] // n
    nc = build_nc(nseq)
    common = _prep_common(inputs)
    in_maps = [_core_map(common, x, c, i, nseq) for i in range(n)]
    res = run_bass_kernel_spmd(nc, in_maps, core_ids=list(range(n)))
    return np.concatenate([r["out"] for r in res.results], axis=0).astype(np.float32)
```

```python
import contextlib
import numpy as np
import concourse.bass as bass
import concourse.mybir as mybir
from concourse.bass_utils import run_bass_kernel_spmd

F32 = mybir.dt.float32
BF16 = mybir.dt.bfloat16
AF = mybir.ActivationFunctionType
ALU = mybir.AluOpType
AX = mybir.AxisListType

S_TOK = 2048
D = 1024
NT = 16
DIN = 4576
DFF = 2816
EPS = 1e-6
NBIS = 16
NEG = -30000.0


class Buf:
    __slots__ = ("name", "w", "r")

    def __init__(self, name=""):
        self.name = name
        self.w = None
        self.r = {}


class Sched:
    ENGS = ("pe", "act", "dve", "pool", "sp")
    NDMA = 24

    def __init__(self, nc):
        self.nc = nc
        self.streams = {e: [] for e in self.ENGS}
        self.count = {e: 0 for e in self.ENGS}
        self.waited = {e: {} for e in self.ENGS}
        self.dma_uses = [0] * self.NDMA
        self.dma_rr = 0
        self.out_events = []

    def _deps(self, eng, reads, writes):
        deps = {}

        def add(ev):
            if ev is None:
                return
            k, v = ev
            if deps.get(k, 0) < v:
                deps[k] = v
        for b in reads:
            add(b.w)
        for b in writes:
            if b.w is not None and b.w[0] != eng:
                add(b.w)
            for k, v in b.r.items():
                if k != eng:
                    add((k, v))
        waits = []
        for k, v in deps.items():
            if k == "pe" and eng == "pe":
                continue
            if self.waited[eng].get(k, 0) < v:
                self.waited[eng][k] = v
                waits.append((k, v))
        return waits

    def _commit(self, ev, reads, writes):
        k, v = ev
        for b in writes:
            b.w = ev
            b.r = {}
        for b in reads:
            if b.r.get(k, 0) < v:
                b.r[k] = v

    def op(self, eng, fn, reads=(), writes=()):
        waits = self._deps(eng, reads, writes)
        self.count[eng] += 1
        ev = (eng, self.count[eng])
        self.streams[eng].append((fn, waits, ev))
        self._commit(ev, reads, writes)
        return ev

    def dma(self, out, in_, reads=(), writes=(), q="sp", is_output=False, **kw):
        i = self.dma_rr
        self.dma_rr = (self.dma_rr + 1) % self.NDMA
        waits = self._deps(q, reads, writes)
        key = "dma%d" % i
        prev = self.dma_uses[i] * 16
        if prev and self.waited[q].get(key, 0) < prev:
            self.waited[q][key] = prev
            waits.append((key, prev))
        self.dma_uses[i] += 1
        ev = (key, self.dma_uses[i] * 16)
        fn = lambda e, out=out, in_=in_, kw=kw: e.dma_start(out=out, in_=in_, **kw)
        self.streams[q].append((fn, waits, ev))
        self._commit(ev, reads, writes)
        if is_output:
            self.out_events.append(ev)
        return ev

    def barrier(self):
        allv = [(e, self.count[e]) for e in self.ENGS if self.count[e]]
        allv += [("dma%d" % i, self.dma_uses[i] * 16) for i in range(self.NDMA) if self.dma_uses[i]]
        for e in self.ENGS:
            waits = []
            for k, v in allv:
                if k == e:
                    continue
                if self.waited[e].get(k, 0) < v:
                    self.waited[e][k] = v
                    waits.append((k, v))
            if waits:
                self.streams[e].append((None, waits, None))

    def emit(self):
        nc = self.nc
        with contextlib.ExitStack() as st:
            sems = {}
            for e in self.ENGS:
                sems[e] = st.enter_context(nc.semaphore("s_" + e))
            for i in range(self.NDMA):
                sems["dma%d" % i] = st.enter_context(nc.semaphore("s_dma%d" % i))
            final = {}
            for k, v in self.out_events:
                final[k] = max(final.get(k, 0), v)
            block = st.enter_context(nc.Block())

            def run(engname, e):
                for fn, waits, ev in self.streams[engname]:
                    for k, v in waits:
                        e.wait_ge(sems[k], v)
                    if fn is None:
                        continue
                    ins = fn(e)
                    k, v = ev
                    ins.then_inc(sems[k], 16 if k.startswith("dma") else 1)
                if engname == "sp":
                    for k, v in final.items():
                        e.wait_ge(sems[k], v)

            @block.tensor
            def _(e):
                run("pe", e)

            @block.scalar
            def _(e):
                run("act", e)

            @block.vector
            def _(e):
                run("dve", e)

            @block.gpsimd
            def _(e):
                run("pool", e)

            @block.sync
            def _(e):
                run("sp", e)


class Arena:
    def __init__(self, ap16):
        self.ap = ap16
        self.off = 0

    def reset(self):
        self.off = 0

    def take(self, ncols, dt=BF16):
        if dt == F32:
            self.off = (self.off + 1) // 2 * 2
            n16 = ncols * 2
        else:
            n16 = ncols
        assert self.off + n16 <= self.ap.shape[1], (self.off, n16, self.ap.shape)
        v = self.ap[:, self.off:self.off + n16]
        self.off += n16
        self.off = (self.off + 1) // 2 * 2
        return v.bitcast(F32) if dt == F32 else v


_REGS = {}


def _I(name, *args, **kw):
    if name == "affine_select":
        def thunk(e):
            a = list(args)
            key = (id(e), float(a[4]))
            if key not in _REGS:
                _REGS[key] = e.to_reg(float(a[4]))
            a[4] = _REGS[key]
            return e.affine_select(*a, **kw)
        return thunk
    return lambda e: getattr(e, name)(*args, **kw)


def build_nc(nseq=4, dbg=False):
    nc = bass.Bass("TRN2", target_bir_lowering=False)
    _REGS.clear()
    S = Sched(nc)

    def din(name, shape):
        return nc.dram_tensor(name, shape, F32, kind="ExternalInput").ap()
    x_d = din("x", [nseq, S_TOK, D])
    cT_d = din("cT", [128, 8, 4])
    wada_d = din("w_ada", [D, 6 * D])
    bada_d = din("b_ada", [1, 6 * D])
    gn_d = din("gn", [128, 16])
    win_d = din("w_in", [D, DIN])
    gv_d = din("gvec", [1, 6 * 64])
    peT_d = din("peT", [64, 64])
    wck1_d = din("w_ck1", [2048, 128])
    wck2_d = din("w_ck2", [128, 64])
    wcv1_d = din("w_cv1", [2048, 128])
    wcv2_d = din("w_cv2", [128, 64])
    woa_d = din("w_o_a", [512, D])
    wob_d = din("w_o_b", [512, D])
    wout_d = din("w_out", [D, D])
    wfg_d = din("w_ff_gate", [D, DFF])
    wfu_d = din("w_ff_up", [D, DFF])
    wfd_d = din("w_ff_down", [DFF, D])
    out_d = nc.dram_tensor("out", [nseq, S_TOK, D], F32, kind="ExternalOutput").ap()
    modrow_d = nc.dram_tensor("modrow", [4, 6 * D], F32, kind="Internal").ap()
    oT_d = nc.dram_tensor("oT_scr", [2, 4, 128, S_TOK], BF16, kind="ExternalOutput" if dbg else "Internal").ap()
    if dbg:
        hT_dbg = nc.dram_tensor("hT_dbg", [128, 8, S_TOK], BF16, kind="ExternalOutput").ap()
        x1_dbg = nc.dram_tensor("x1_dbg", [S_TOK, D], F32, kind="ExternalOutput").ap()
        mod_dbg = nc.dram_tensor("mod_dbg", [4, 6 * D], F32, kind="ExternalOutput").ap()

    st = contextlib.ExitStack()

    def sb(name, shape, dt=F32):
        return st.enter_context(nc.sbuf_tensor(name, shape, dt))

    with st:
        PS = [st.enter_context(nc.psum_tensor("ps%d" % i, [128, 512], F32)) for i in range(8)]
        PB = [Buf("ps%d" % i) for i in range(8)]

        def psb(i):
            return PS[i][:].bitcast(BF16)

        ident_b = sb("ident_b", [128, 128], BF16)
        ident_f = sb("ident_f", [128, 128], F32)
        Cm = sb("Cm", [128, 128], BF16)
        Wm = sb("Wm", [128, 128], BF16)
        cmask = sb("cmask", [128, S_TOK], BF16)
        ET = sb("ET", [68, S_TOK], BF16)
        selA = sb("selA", [128, NT, 32], F32)
        selB = sb("selB", [128, NT, 32], F32)
        aqc = sb("aqc", [128, NT, 8, 4], BF16)
        akc = sb("akc", [128, NT, 4], BF16)
        gbc = sb("gbc", [128, 6 * 64], F32)
        gains = sb("gains", [128, 4, 64], F32)
        gnc = sb("gnc", [128, 16], F32)
        modT = sb("modT", [128, 32, 4], F32)
        cb2 = sb("cb2", [128, 2], F32)
        pow2 = sb("pow2", [128, NBIS + 1], F32)
        hT = sb("hT", [128, 8, S_TOK], BF16)
        Vc = sb("Vc", [128, 2, 97], BF16)
        kc_aug = sb("kc_aug", [128, 2, 68], BF16)
        KcT = sb("KcT", [128, 2, 128], BF16)
        U_t = sb("U", [128, 65400], BF16)
        U = Arena(U_t[:])
        bC = Buf("consts")
        bOT = [Buf(), Buf()]
        bOut = Buf()

        hTb = [Buf("hT%d" % c) for c in range(4)]

        def pool(fn, reads=(), writes=()):
            return S.op("pool", fn, reads, writes)

        def dve(fn, reads=(), writes=()):
            return S.op("dve", fn, reads, writes)

        def act(fn, reads=(), writes=()):
            return S.op("act", fn, reads, writes)

        def pe(fn, reads=(), writes=()):
            return S.op("pe", fn, reads, writes)

        pool(_I("memset", ident_b[:], 1.0), writes=[bC])
        pool(_I("affine_select", ident_b[:], ident_b[:], [[1, 128]], ALU.is_equal, 0.0, base=0, channel_multiplier=-1), reads=[bC], writes=[bC])
        pool(_I("memset", ident_f[:], 1.0), writes=[bC])
        pool(_I("affine_select", ident_f[:], ident_f[:], [[1, 128]], ALU.is_equal, 0.0, base=0, channel_multiplier=-1), reads=[bC], writes=[bC])
        pool(_I("memset", Cm[:], 1.0), writes=[bC])
        pool(_I("affine_select", Cm[:], Cm[:], [[1, 128]], ALU.is_ge, 0.0, base=0, channel_multiplier=-1), reads=[bC], writes=[bC])
        pool(_I("memset", Wm[:], 1.0), writes=[bC])
        pool(_I("affine_select", Wm[:], Wm[:], [[-1, 128]], ALU.is_gt, 0.0, base=0, channel_multiplier=1), reads=[bC], writes=[bC])
        pool(_I("memset", cmask[:], 1.0), writes=[bC])
        pool(_I("affine_select", cmask[:], cmask[:], [[1, S_TOK]], ALU.is_ge, 0.0, base=-31, channel_multiplier=-16), reads=[bC], writes=[bC])
        pool(_I("memset", ET[0:32, :], 1.0), writes=[bC])
        pool(_I("memset", ET[32:64, :], 0.0), writes=[bC])
        pool(_I("memset", ET[64:68, :], 0.0), writes=[bC])
        pool(_I("affine_select", ET[0:32, :], ET[0:32, :], [[1, S_TOK]], ALU.is_ge, 0.0, base=0, channel_multiplier=-64), reads=[bC], writes=[bC])
        pool(_I("affine_select", ET[0:32, :], ET[0:32, :], [[-1, S_TOK]], ALU.is_ge, 0.0, base=63, channel_multiplier=64), reads=[bC], writes=[bC])
        for g in range(2):
            pool(_I("memset", Vc[:, g, 64:97], 1.0), writes=[bC])
            pool(_I("affine_select", Vc[:, g, 65:97], Vc[:, g, 65:97], [[-64, 32]], ALU.is_ge, 0.0, base=31, channel_multiplier=16), reads=[bC], writes=[bC])
            pool(_I("affine_select", Vc[:, g, 65:97], Vc[:, g, 65:97], [[64, 32]], ALU.is_ge, 0.0, base=63, channel_multiplier=-16), reads=[bC], writes=[bC])
        Dt = U.take(NT * 32, F32).rearrange("p (t j) -> p t j", j=32)
        jt = U.take(NT * 32, F32).rearrange("p (t j) -> p t j", j=32)
        f0 = U.take(NT * 32, F32).rearrange("p (t j) -> p t j", j=32)
        for lo_, base in ((0, 0), (64, -1)):
            pool(_I("iota", Dt[lo_:lo_ + 64], [[-2, NT], [1, 32]], base=base, channel_multiplier=0, allow_small_or_imprecise_dtypes=True), writes=[bC])
        pool(_I("iota", jt[:], [[0, NT], [1, 32]], base=0, channel_multiplier=0, allow_small_or_imprecise_dtypes=True), writes=[bC])
        dve(_I("tensor_single_scalar", f0[:], jt[:], 0.0, ALU.is_equal), reads=[bC], writes=[bC])
        dve(_I("tensor_single_scalar", jt[:], Dt[:], 0.0, ALU.is_equal), reads=[bC], writes=[bC])
        dve(_I("tensor_max", f0[:], f0[:], jt[:]), reads=[bC], writes=[bC])
        dve(_I("tensor_single_scalar", jt[:], Dt[:], -1.0, ALU.is_equal), reads=[bC], writes=[bC])
        dve(_I("tensor_max", f0[:], f0[:], jt[:]), reads=[bC], writes=[bC])
        dve(_I("tensor_single_scalar", jt[:], Dt[:], 0.0, ALU.is_le), reads=[bC], writes=[bC])
        dve(_I("tensor_sub", selA[:], jt[:], f0[:]), reads=[bC], writes=[bC])
        dve(_I("tensor_add", selB[:], jt[:], f0[:]), reads=[bC], writes=[bC])
        dve(_I("tensor_scalar", selB[:], selB[:], -1.0, 1e9, ALU.add, ALU.mult), reads=[bC], writes=[bC])
        hi_t = sb("hi_t", [128, NT], F32)
        lo_t = sb("lo_t", [128, 1], F32)
        for lo_, base in ((0, 0), (64, 64)):
            pool(_I("iota", hi_t[lo_:lo_ + 64], [[128, NT]], base=base, channel_multiplier=0, allow_small_or_imprecise_dtypes=True), writes=[bC])
            pool(_I("iota", lo_t[lo_:lo_ + 64], [[0, 1]], base=0, channel_multiplier=1, allow_small_or_imprecise_dtypes=True), writes=[bC])
        for h in range(8):
            sl = 2.0 ** -(h + 1)
            dve(_I("memset", aqc[:, :, h, 0:2], sl), reads=[bC], writes=[bC])
            dve(_I("tensor_scalar", aqc[:, :, h, 2], hi_t[:], -sl, None, ALU.mult), reads=[bC], writes=[bC])
            dve(_I("tensor_scalar", aqc[:, :, h, 3], lo_t[:].to_broadcast([128, NT]), -sl, None, ALU.mult), reads=[bC], writes=[bC])
        dve(_I("memset", akc[:, :, 2:4], 1.0), reads=[bC], writes=[bC])
        dve(_I("tensor_copy", akc[:, :, 0], hi_t[:]), reads=[bC], writes=[bC])
        dve(_I("tensor_copy", akc[:, :, 1], lo_t[:].to_broadcast([128, NT])), reads=[bC], writes=[bC])
        pn = sb("pn", [128, 1], F32)
        pool(_I("iota", pn[:], [[0, 1]], base=0, channel_multiplier=16, allow_small_or_imprecise_dtypes=True), writes=[bC])
        for g in range(2):
            dve(_I("tensor_copy", kc_aug[:, g, 64:65], pn[:]), reads=[bC], writes=[bC])
            dve(_I("memset", kc_aug[:, g, 65:66], 31.0), reads=[bC], writes=[bC])
            dve(_I("memset", kc_aug[:, g, 66:68], 1.0), reads=[bC], writes=[bC])
        for j in range(NBIS + 1):
            dve(_I("memset", pow2[:, j:j + 1], 2.0 ** -j * (1.01 if j == NBIS else 1.0)), reads=[bC], writes=[bC])
        S.dma(gbc[:], gv_d.partition_broadcast(128), writes=[bC])
        S.dma(gnc[:], gn_d, writes=[bC])

        def gsl(i):
            return gbc[:, i * 64:(i + 1) * 64]
        for idx, (gk, gq) in enumerate(((2, 0), (3, 0), (1, 0), (5, 4))):
            dve(_I("scalar_tensor_tensor", gains[:, idx, :], gsl(gk), 0.125, gsl(gq), ALU.mult, ALU.mult), reads=[bC], writes=[bC])

        S.barrier()
        U.reset()
        scT = U.take(32, F32).rearrange("p (k b) -> p k b", b=4)
        wchunk = U.take(8 * 512, F32).rearrange("p (k n) -> p k n", k=8)
        bchunk = U.take(512, F32)
        mchunk = U.take(512, F32)
        bsc, bw, bbc, bm = Buf(), Buf(), Buf(), Buf()
        S.dma(scT, cT_d, writes=[bsc])
        act(_I("activation", scT, scT, AF.Silu), reads=[bsc], writes=[bsc])
        wada_v = wada_d.rearrange("(k p) n -> p k n", p=128)
        LNV = {0: 0, 1: 1, 3: 2, 4: 3}
        for c in range(12):
            S.dma(wchunk, wada_v[:, :, c * 512:(c + 1) * 512], writes=[bw])
            S.dma(bchunk[0:4, :], bada_d[:, c * 512:(c + 1) * 512].partition_broadcast(4), writes=[bbc])
            for k in range(8):
                pe(_I("matmul", PS[0][0:4, :], lhsT=scT[:, k, :], rhs=wchunk[:, k, :], start=(k == 0), stop=(k == 7)), reads=[bsc, bw], writes=[PB[0]])
            dve(_I("tensor_tensor", mchunk[0:4, :], PS[0][0:4, :], bchunk[0:4, :], ALU.add), reads=[PB[0], bbc], writes=[bm])
            S.dma(modrow_d[:, c * 512:(c + 1) * 512], mchunk[0:4, :], reads=[bm], writes=[bC])
            if dbg:
                S.dma(mod_dbg[:, c * 512:(c + 1) * 512], mchunk[0:4, :], reads=[bm], writes=[Buf()], is_output=True)
            vec, half = c // 2, c % 2
            if vec in LNV:
                for i in range(4):
                    col = (LNV[vec] * 8 + half * 4 + i) * 4
                    pe(_I("transpose", PS[1][:, col:col + 4], mchunk[0:4, i * 128:(i + 1) * 128], ident_f[0:4, 0:4]), reads=[bm, bC], writes=[PB[1]])
        dve(_I("tensor_copy", modT[:].rearrange("p a b -> p (a b)"), PS[1][:, 0:128]), reads=[PB[1]], writes=[bC])
        for which, gi in ((1, 0), (3, 1)):
            dve(_I("scalar_tensor_tensor",
                modT[:, which * 8:(which + 1) * 8, :], modT[:, which * 8:(which + 1) * 8, :], 1.0,
                gnc[:, gi * 8:(gi + 1) * 8].unsqueeze(2).to_broadcast([128, 8, 4]), ALU.add, ALU.mult), reads=[bC], writes=[bC])
        S.barrier()

        xt = [sb("xt%d" % i, [128, D], F32) for i in range(2)]
        xtb = [Buf() for _ in range(2)]
        xn = sb("xn", [128, D], BF16)
        xnb = Buf()
        sq = [sb("sq%d" % i, [128, 512], F32) for i in range(2)]
        sqb = [Buf(), Buf()]
        st16 = sb("st16", [128, 16], F32)
        stb = Buf()
        PT = [sb("PT%d" % i, [128, 512], BF16) for i in range(6)]
        PTb = [Buf() for _ in range(6)]
        sn = sb("sn", [128, 16], F32)
        snb = Buf()
        tmpo = sb("tmpo", [128, 4, 64], F32)
        tmpb = Buf()
        oacc = sb("oacc", [128, 8, 64], F32)
        oab = Buf()
        obf = sb("obf", [128, 512], BF16)
        obfb = Buf()
        oTs = sb("oTs", [128, 4, 128], BF16)
        oTsb = Buf()
        sm = sb("sm", [128, 64], F32)
        smb = Buf()
        state = {"pt": 0, "sb": 0}
        vstate = {}

        def layernorm(src_tile, src_buf, tl, which, b, psbank):
            act(_I("activation", sq[0][:, :].bitcast(BF16), src_tile, AF.Square, accum_out=st16[:, 0:1]), reads=[src_buf], writes=[sqb[0], stb])
            act(_I("activation", st16[:, 1:2], st16[:, 0:1], AF.Sqrt, bias=EPS, scale=1.0 / D), reads=[stb], writes=[stb])
            dve(_I("reciprocal", st16[:, 2:3], st16[:, 1:2]), reads=[stb], writes=[stb])
            dve(_I("tensor_scalar", xn[:], src_tile, st16[:, 2:3], None, ALU.mult), reads=[src_buf, stb], writes=[xnb])
            for j in range(8):
                bk = psbank + j // 4
                o0 = ((j % 4) * 2 + tl) * 128
                pe(_I("transpose", psb(bk)[:, o0:o0 + 128], xn[:, j * 128:(j + 1) * 128], ident_b[:]), reads=[xnb, bC], writes=[PB[bk]])

        def ln_evac(c2, which, b, psbank, hbuf):
            for j in range(8):
                bk = psbank + j // 4
                src = psb(bk)[:, (j % 4) * 256:(j % 4) * 256 + 256]
                Gc = modT[:, (2 * which + 1) * 8 + j, b:b + 1]
                Sc = modT[:, (2 * which) * 8 + j, b:b + 1]
                dve(_I("tensor_scalar", hT[:, j, c2 * 256:(c2 + 1) * 256], src, Gc, Sc, ALU.mult, ALU.add),
                    reads=[PB[bk], bC], writes=[hbuf])

        def load_w(dst, src, buf, q="pool"):
            S.dma(dst, src, writes=[buf], q=q)

        win_v = win_d.rearrange("(k p) n -> p k n", p=128)

        def rms_heads(psbank, nh, dst_stats):
            i = state["sb"] = (state["sb"] + 1) % 2
            act(_I("activation", sq[i][:, 0:nh * 64], PS[psbank][:, 0:nh * 64], AF.Square), reads=[PB[psbank]], writes=[sqb[i]])
            dve(_I("tensor_reduce", dst_stats, sq[i][:, 0:nh * 64].rearrange("p (h d) -> p h d", d=64), AX.X, ALU.add), reads=[sqb[i]], writes=[stb])

        def rstd_from(stats):
            act(_I("activation", stats, stats, AF.Sqrt, bias=EPS, scale=1.0 / 64), reads=[stb], writes=[stb])
            dve(_I("reciprocal", stats, stats), reads=[stb], writes=[stb])

        def make_units(QTh, qb, KTk, kb, kind, t, tiles, maskmm, pvb, slot, full_mask=None, banks=(4, 5)):
            ntl = len(tiles)
            return [dict(QTh=QTh, qb=qb, KTk=KTk, kb=kb, kind=kind, t=t, grp=tiles[g0:g0 + 4], g0=g0, ntl=ntl, maskmm=maskmm,
                         pvb=pvb, slot=slot, full_mask=full_mask, banks=banks) for g0 in range(0, ntl, 4)]

        def emit_A(u):
            qs = slice(u["t"] * 128, (u["t"] + 1) * 128)
            banks = u["banks"]
            bk = banks[state["pt"] % len(banks)]
            pi = state["pt"] % len(PT)
            state["pt"] += 1
            u["pi"] = pi
            grp = u["grp"]
            for i, (j, mt) in enumerate(grp):
                ks = slice(j * 128, (j + 1) * 128)
                pe(_I("matmul", PS[bk][:, i * 128:(i + 1) * 128], lhsT=u["KTk"][0:68, ks], rhs=u["QTh"][0:68, qs], start=True, stop=(u["maskmm"] is None)),
                   reads=[u["kb"], u["qb"]], writes=[PB[bk]])
                if u["maskmm"] is not None:
                    MTg, mb = u["maskmm"]
                    pe(_I("matmul", PS[bk][:, i * 128:(i + 1) * 128], lhsT=ET[0:68, ks], rhs=MTg[0:68, qs], start=False, stop=True),
                       reads=[mb, bC], writes=[PB[bk]])
            n = len(grp) * 128
            act(_I("activation", PT[pi][:, 0:n], PS[bk][:, 0:n], AF.Exp), reads=[PB[bk]], writes=[PTb[pi]])
            if u["full_mask"] is not None:
                mT, mTb = u["full_mask"]
                j0 = grp[0][0]
                pool(_I("tensor_tensor", PT[pi][:, 0:n], PT[pi][:, 0:n], mT[:, j0:j0 + len(grp), :].rearrange("p j c -> p (j c)"), ALU.mult),
                     reads=[mTb, PTb[pi]], writes=[PTb[pi]])
            for i, (j, mt) in enumerate(grp):
                if mt is not None:
                    mk = Cm if mt == "C" else Wm
                    pool(_I("tensor_tensor", PT[pi][:, i * 128:(i + 1) * 128], PT[pi][:, i * 128:(i + 1) * 128], mk[:], ALU.mult),
                         reads=[bC, PTb[pi]], writes=[PTb[pi]])

        def emit_B(u):
            pi = u["pi"]
            pvb = u["pvb"]
            po = PS[pvb][:, u["slot"] * 65:(u["slot"] + 1) * 65]
            for i, (j, mt) in enumerate(u["grp"]):
                gi = u["g0"] + i
                pe(_I("matmul", po, lhsT=PT[pi][:, i * 128:(i + 1) * 128], rhs=vstate["V"][:, j, u["kind"], :], start=(gi == 0), stop=(gi == u["ntl"] - 1)),
                   reads=[PTb[pi], vstate["bV"]], writes=[PB[pvb]])

        def run_units(units, L, between=None):
            n = len(units)
            for i in range(min(L, n)):
                emit_A(units[i])
            for i in range(n):
                if i + L < n:
                    emit_A(units[i + L])
                emit_B(units[i])
                if units[i].get("post") is not None:
                    units[i]["post"]()
                if between is not None:
                    between(i, n)

        def next_pv():
            state["pv"] = state.get("pv", 0) + 1
            return 6 if state["pv"] % 2 == 0 else 2

        def norm4(pvb, g, gate_view, gate_bufs, first):
            o4 = PS[pvb][:, 0:260].rearrange("p (h c) -> p h c", h=4)
            dve(_I("tensor_scalar", sn[:, 0:4], o4[:, :, 64], 1e-30, None, ALU.max), reads=[PB[pvb]], writes=[snb])
            dve(_I("reciprocal", sn[:, 4:8], sn[:, 0:4]), reads=[snb], writes=[snb])
            if gate_view is not None:
                dve(_I("tensor_tensor", sn[:, 4:8], sn[:, 4:8], gate_view, ALU.mult), reads=[snb] + gate_bufs, writes=[snb])
            wb = sn[:, 4:8].unsqueeze(2).to_broadcast([128, 4, 64])
            if first:
                dve(_I("tensor_tensor", oacc[:, 4 * g:4 * g + 4, :], o4[:, :, 0:64], wb, ALU.mult), reads=[PB[pvb], snb], writes=[oab])
            else:
                dve(_I("tensor_tensor", tmpo[:], o4[:, :, 0:64], wb, ALU.mult), reads=[PB[pvb], snb], writes=[tmpb])
                pool(_I("tensor_tensor", oacc[:, 4 * g:4 * g + 4, :], oacc[:, 4 * g:4 * g + 4, :], tmpo[:], ALU.add), reads=[tmpb, oab], writes=[oab])

        def flush_o(t, mix):
            dve(_I("tensor_copy", obf[:], oacc[:].rearrange("p h d -> p (h d)")), reads=[oab], writes=[obfb])
            for j in range(4):
                pe(_I("transpose", psb(3)[:, j * 128:(j + 1) * 128], obf[:, j * 128:(j + 1) * 128], ident_b[:]), reads=[obfb, bC], writes=[PB[3]])
            act(_I("activation", oTs[:].rearrange("p j c -> p (j c)"), psb(3)[:, 0:512], AF.Copy), reads=[PB[3]], writes=[oTsb])
            S.dma(oT_d[mix, :, :, t * 128:(t + 1) * 128].rearrange("j p c -> p j c"), oTs[:], reads=[oTsb], writes=[bOT[mix]])

        for s in range(nseq):
            b = s
            S.barrier()

            for c2 in range(8):
                for tl in range(2):
                    t = c2 * 2 + tl
                    i = t % 2
                    S.dma(xt[i][:], x_d[s, t * 128:(t + 1) * 128, :], writes=[xtb[i]])
                    layernorm(xt[i][:], xtb[i], tl, 0, b, 0)
                ln_evac(c2, 0, b, 0, hTb[c2 // 2])

            if dbg and s == 0:
                S.dma(hT_dbg, hT[:], reads=hTb, writes=[Buf()], is_output=True)
            U.reset()
            V_all = U.take(NT * 5 * 65).rearrange("p (t k c) -> p t k c", t=NT, k=5)
            bV = Buf()
            pool(_I("memset", V_all[:, :, :, 64:65], 1.0), writes=[bV])
            vstate["V"] = V_all
            vstate["bV"] = bV
            Wn = U.take(8 * 1304).rearrange("p (k n) -> p k n", k=8)
            QT = U.take(8 * S_TOK).rearrange("p (h n) -> p h n", h=8)
            KT = U.take(5 * S_TOK).rearrange("p (h n) -> p h n", h=5)
            q_aug = U.take(8 * 68).rearrange("p (h d) -> p h d", h=8)
            k_aug = U.take(4 * 68).rearrange("p (h d) -> p h d", h=4)
            kcT = U.take(S_TOK)
            vcT = U.take(S_TOK)
            MT = U.take(2 * S_TOK).rearrange("p (g n) -> p g n", g=2)
            sg = U.take(NT * 24, F32).rearrange("p (t c) -> p t c", c=24)
            bq_aug, bk_aug, bkc, bsg = Buf(), Buf(), Buf(), Buf()
            bWn = [Buf() for _ in range(8)]
            QTb = [Buf() for _ in range(8)]
            KTb = [Buf() for _ in range(5)]
            MTb = [Buf(), Buf()]
            pool(_I("memset", MT[32:64, :, :], 0.0), writes=MTb)
            pool(_I("memset", MT[64:68, :, :], 0.0), writes=MTb)
            for k in range(8):
                load_w(Wn[:, k, :], win_v[:, k, 0:1304], bWn[k])
            for c in range(4):
                for tl in range(4):
                    t = c * 4 + tl
                    ts = slice(t * 128, (t + 1) * 128)
                    pq, pk, pg = (0, 1, 2) if t % 2 == 0 else (4, 5, 6)
                    for k in range(8):
                        pe(_I("matmul", PS[pq][:, :], lhsT=hT[:, k, ts], rhs=Wn[:, k, 0:512], start=(k == 0), stop=(k == 7)), reads=[hTb[c], bWn[k]], writes=[PB[pq]])
                    for k in range(8):
                        pe(_I("matmul", PS[pk][:, :], lhsT=hT[:, k, ts], rhs=Wn[:, k, 768:1280], start=(k == 0), stop=(k == 7)), reads=[hTb[c], bWn[k]], writes=[PB[pk]])
                    for k in range(8):
                        pe(_I("matmul", PS[pg][:, 0:24], lhsT=hT[:, k, ts], rhs=Wn[:, k, 1280:1304], start=(k == 0), stop=(k == 7)), reads=[hTb[c], bWn[k]], writes=[PB[pg]])
                    rms_heads(pq, 8, st16[:, 0:8])
                    rms_heads(pk, 8, st16[:, 8:16])
                    rstd_from(st16[:, 0:16])
                    dve(_I("tensor_tensor", q_aug[:, :, 0:64], PS[pq][:, :].rearrange("p (h d) -> p h d", d=64), st16[:, 0:8].unsqueeze(2).to_broadcast([128, 8, 64]), ALU.mult),
                        reads=[PB[pq], stb], writes=[bq_aug])
                    dve(_I("tensor_copy", q_aug[:, :, 64:68], aqc[:, t, :, :]), reads=[bC], writes=[bq_aug])
                    for (c0, s0, kk, gi) in ((0, 8, 0, 0), (256, 12, 2, 1)):
                        dve(_I("tensor_tensor", sq[0][:, 0:128].rearrange("p (h d) -> p h d", d=64), PS[pk][:, c0:c0 + 128].rearrange("p (h d) -> p h d", d=64),
                                                                   st16[:, s0:s0 + 2].unsqueeze(2).to_broadcast([128, 2, 64]), ALU.mult), reads=[PB[pk], stb], writes=[sqb[0]])
                        dve(_I("tensor_tensor", k_aug[:, kk:kk + 2, 0:64], sq[0][:, 0:128].rearrange("p (h d) -> p h d", d=64),
                                                                   gains[:, gi, :].unsqueeze(1).to_broadcast([128, 2, 64]), ALU.mult), reads=[sqb[0], bC], writes=[bk_aug])
                    dve(_I("tensor_copy", k_aug[:, :, 64:68], akc[:, t, :].unsqueeze(1).to_broadcast([128, 4, 4])), reads=[bC], writes=[bk_aug])
                    act(_I("activation", V_all[:, t, 0:2, 0:64], PS[pk][:, 128:256].rearrange("p (h d) -> p h d", d=64), AF.Copy), reads=[PB[pk]], writes=[bV])
                    act(_I("activation", V_all[:, t, 2:4, 0:64], PS[pk][:, 384:512].rearrange("p (h d) -> p h d", d=64), AF.Copy), reads=[PB[pk]], writes=[bV])
                    act(_I("activation", sg[:, t, :], PS[pg][:, 0:24], AF.Sigmoid), reads=[PB[pg]], writes=[bsg])
                    for h in range(8):
                        pe(_I("transpose", psb(3)[0:68, h * 128:(h + 1) * 128], q_aug[:, h, :], ident_b[:]), reads=[bq_aug, bC], writes=[PB[3]])
                    for kk in range(4):
                        pe(_I("transpose", psb(7)[0:68, kk * 128:(kk + 1) * 128], k_aug[:, kk, :], ident_b[:]), reads=[bk_aug, bC], writes=[PB[7]])
                    act(_I("activation", QT[0:68, :, ts], psb(3)[0:68, :].rearrange("p (h c) -> p h c", h=8), AF.Copy), reads=[PB[3]], writes=QTb)
                    dve(_I("tensor_copy", KT[0:68, 0:4, ts], psb(7)[0:68, 0:512].rearrange("p (h c) -> p h c", h=4)), reads=[PB[7]], writes=KTb[0:4])
                cs = slice(c * 512, (c + 1) * 512)
                for (c0, dst) in ((512, kcT), (640, vcT)):
                    for k in range(8):
                        pe(_I("matmul", PS[0][:, :], lhsT=Wn[:, k, c0:c0 + 128], rhs=hT[:, k, cs], start=(k == 0), stop=(k == 7)), reads=[hTb[c], bWn[k]], writes=[PB[0]])
                    act(_I("activation", dst[:, cs], PS[0][:, :], AF.Copy), reads=[PB[0]], writes=[bkc])

            S.barrier()
            W1 = Wn.rearrange("p k n -> p (k n)")[:, 0:2 * 32 * 128].rearrange("p (a l n) -> p a l n", a=2, l=32)
            W2 = U.take(2 * 64).rearrange("p (a n) -> p a n", a=2)
            peT = U.take(64)
            HT = U.take(128)
            bW1, bH = Buf(), Buf()
            for a, (w1d, w2d) in enumerate(((wck1_d, wck2_d), (wcv1_d, wcv2_d))):
                for half in range(2):
                    load_w(W1[half * 64:half * 64 + 64, a, :, :], w1d.rearrange("(l d) n -> d l n", d=64), bW1)
                load_w(W2[:, a, :], w2d, bW1)
            load_w(peT[0:64, :], peT_d, bW1)
            if s == 0:
                for a in range(2):
                    for l in range(32):
                        pe(_I("matmul", PS[2][:, a:a + 1], lhsT=W1[0:64, a, l, :], rhs=peT[0:64, a * 32 + l:a * 32 + l + 1], start=(l == 0), stop=(l == 31)),
                           reads=[bW1], writes=[PB[2]])
                dve(_I("tensor_copy", cb2[:], PS[2][:, 0:2]), reads=[PB[2]], writes=[bC])
            for a, srcT in enumerate((kcT, vcT)):
                for g in range(2):
                    base = g * 64
                    v3 = srcT[base:base + 64, :].rearrange("p (n s) -> p n s", s=16)
                    for l in range(32):
                        rhs = v3[:, (l // 16):(l // 16) + 127, l % 16]
                        pe(_I("matmul", PS[0][:, 0:127], lhsT=W1[base:base + 64, a, l, :], rhs=rhs, start=(l == 0), stop=(l == 31)),
                           reads=[bW1, bkc], writes=[PB[0]])
                    act(_I("activation", HT[:, 0:127], PS[0][:, 0:127], AF.Silu, bias=cb2[:, a:a + 1]), reads=[PB[0], bC], writes=[bH])
                    pe(_I("matmul", PS[1][0:127, 0:64], lhsT=HT[:, 0:127], rhs=W2[:, a, :], start=True, stop=True), reads=[bH, bW1], writes=[PB[1]])
                    if a == 0:
                        act(_I("activation", sq[0][0:127, 0:64], PS[1][0:127, 0:64], AF.Square, accum_out=st16[0:127, 0:1]), reads=[PB[1]], writes=[sqb[0], stb])
                        rstd_from(st16[0:127, 0:1])
                        dve(_I("tensor_scalar", sq[0][0:127, 0:64], PS[1][0:127, 0:64], st16[0:127, 0:1], None, ALU.mult), reads=[PB[1], stb], writes=[sqb[0]])
                        dve(_I("tensor_tensor", kc_aug[0:127, g, 0:64], sq[0][0:127, 0:64], gains[0:127, 2, :], ALU.mult), reads=[sqb[0], bC], writes=[bC])
                        pe(_I("transpose", psb(3)[0:68, 0:127], kc_aug[0:127, g, :], ident_b[0:127, 0:127]), reads=[bC], writes=[PB[3]])
                        dve(_I("tensor_copy", KcT[0:68, g, 0:127], psb(3)[0:68, 0:127]), reads=[PB[3]], writes=[bC])
                    else:
                        act(_I("activation", Vc[0:127, g, 0:64], PS[1][0:127, 0:64], AF.Copy), reads=[PB[1]], writes=[bC])

            imp = sb("imp_%d" % s, [128, 4, 32], F32) if s == 0 else imp
            impb = Buf()
            for t in range(NT):
                qs = slice(t * 128, (t + 1) * 128)
                for g in range(2):
                    for hh in range(4):
                        h = 4 * g + hh
                        pe(_I("matmul", PS[4][0:127, hh * 128:(hh + 1) * 128], lhsT=KcT[0:68, g, 0:127], rhs=QT[0:68, h, qs], start=True, stop=True),
                           reads=[bC, QTb[h]], writes=[PB[4]])
                    dve(_I("tensor_scalar", sq[1][0:127, :], PS[4][0:127, :], 60.0, None, ALU.min), reads=[PB[4]], writes=[sqb[1]])
                    act(_I("activation", PT[0][0:127, :], sq[1][0:127, :], AF.Exp), reads=[sqb[1]], writes=[PTb[0]])
                    dve(_I("tensor_tensor", PT[0][0:127, :].rearrange("p (h c) -> p h c", h=4), PT[0][0:127, :].rearrange("p (h c) -> p h c", h=4),
                                                  cmask[0:127, qs].unsqueeze(1).to_broadcast([127, 4, 128]), ALU.mult), reads=[bC, PTb[0]], writes=[PTb[0]])
                    for hh in range(4):
                        pe(_I("matmul", PS[7][:, hh * 97:(hh + 1) * 97], lhsT=PT[0][0:127, hh * 128:(hh + 1) * 128], rhs=Vc[0:127, g, :], start=True, stop=True),
                           reads=[PTb[0], bC], writes=[PB[7]])
                    o4 = PS[7][:, 0:388].rearrange("p (h c) -> p h c", h=4)
                    dve(_I("tensor_scalar", sm[:, 0:4], o4[:, :, 64], 1e-30, None, ALU.max), reads=[PB[7]], writes=[smb])
                    dve(_I("reciprocal", sm[:, 4:8], sm[:, 0:4]), reads=[smb], writes=[smb])
                    dve(_I("tensor_tensor", sm[:, 8:12], sm[:, 4:8], sg[:, t, :].rearrange("p (h r) -> p h r", r=3)[:, 4 * g:4 * g + 4, 0], ALU.mult), reads=[smb, bsg], writes=[smb])
                    dve(_I("tensor_tensor", oacc[:, 4 * g:4 * g + 4, :], o4[:, :, 0:64], sm[:, 8:12].unsqueeze(2).to_broadcast([128, 4, 64]), ALU.mult),
                        reads=[PB[7], smb], writes=[oab])
                    dve(_I("tensor_tensor", imp[:], o4[:, :, 65:97], sm[:, 4:8].unsqueeze(2).to_broadcast([128, 4, 32]), ALU.mult), reads=[PB[7], smb], writes=[impb])
                    dve(_I("tensor_reduce", sm[:, 16:48], imp[:].rearrange("p h j -> p j h"), AX.X, ALU.add), reads=[impb], writes=[smb])
                    dve(_I("tensor_tensor", sm[:, 16:48], sm[:, 16:48], selA[:, t, :], ALU.mult), reads=[smb, bC], writes=[smb])
                    dve(_I("tensor_tensor", sm[:, 16:48], sm[:, 16:48], selB[:, t, :], ALU.add), reads=[smb, bC], writes=[smb])
                    dve(_I("max", out=sm[:, 48:56], in_=sm[:, 16:48]), reads=[smb], writes=[smb])
                    dve(_I("match_replace", out=imp[:, 0, :], in_to_replace=sm[:, 48:56], in_values=sm[:, 16:48], imm_value=-3e38), reads=[smb], writes=[impb])
                    dve(_I("max", out=sm[:, 56:64], in_=imp[:, 0, :]), reads=[impb], writes=[smb])
                    dve(_I("tensor_scalar", sm[:, 16:48], sm[:, 16:48], sm[:, 63:64], None, ALU.is_ge), reads=[smb], writes=[smb])
                    dve(_I("tensor_scalar", obf[:, 0:32], sm[:, 16:48], -1.0, -NEG, ALU.add, ALU.mult), reads=[smb], writes=[obfb])
                    pe(_I("transpose", psb(3)[0:32, 0:128], obf[:, 0:32], ident_b[:]), reads=[obfb, bC], writes=[PB[3]])
                    act(_I("activation", MT[0:32, g, qs], psb(3)[0:32, 0:128], AF.Copy), reads=[PB[3]], writes=[MTb[g]])
                sg3 = sg[:, t, :].rearrange("p (h r) -> p h r", r=3)
                units = []
                for g in range(2):
                    pvb = next_pv()
                    tiles = [(j, "C" if j == t else None) for j in range(t + 1)]
                    for hh in range(4):
                        h = 4 * g + hh
                        units += make_units(QT[:, h, :], QTb[h], KT[:, g, :], KTb[g], g, t, tiles, (MT[:, g, :], MTb[g]), pvb, hh, banks=(4, 5, 0, 1))
                    units[-1]["post"] = (lambda pvb=pvb, g=g, gv=sg3[:, 4 * g:4 * g + 4, 1]: norm4(pvb, g, gv, [bsg], False))
                    pvb = next_pv()
                    tiles = [(j, "C" if j == t else ("W" if j == t - 4 else None)) for j in range(max(0, t - 4), t + 1)]
                    for hh in range(4):
                        h = 4 * g + hh
                        units += make_units(QT[:, h, :], QTb[h], KT[:, 2 + g, :], KTb[2 + g], 2 + g, t, tiles, None, pvb, hh, banks=(4, 5, 0, 1))
                    units[-1]["post"] = (lambda pvb=pvb, g=g, gv=sg3[:, 4 * g:4 * g + 4, 2]: norm4(pvb, g, gv, [bsg], False))
                run_units(units, 3)
                flush_o(t, 0)

            S.barrier()
            U.reset()
            V_all = U.take(NT * 5 * 65).rearrange("p (t k c) -> p t k c", t=NT, k=5)
            bV = Buf()
            pool(_I("memset", V_all[:, :, :, 64:65], 1.0), writes=[bV])
            vstate["V"] = V_all
            vstate["bV"] = bV
            Wd = U.take(8 * 1352).rearrange("p (k n) -> p k n", k=8)
            QT = U.take(8 * S_TOK).rearrange("p (h n) -> p h n", h=8)
            KT = U.take(5 * S_TOK).rearrange("p (h n) -> p h n", h=5)
            q_aug = U.take(8 * 68).rearrange("p (h d) -> p h d", h=8)
            k_aug = U.take(4 * 68).rearrange("p (h d) -> p h d", h=4)
            iqT = U.take(4 * S_TOK).rearrange("p (m n) -> p m n", m=4)
            ikT = U.take(S_TOK)
            iw = U.take(NT * 8, F32).rearrange("p (t c) -> p t c", c=8)
            sc = U.take(S_TOK, F32)
            rl = U.take(512, F32)
            maskq = U.take(S_TOK)
            maskT = U.take(S_TOK).rearrange("p (j c) -> p j c", c=128)
            junk = U.take(S_TOK)
            biq, bik, biw, bsc, brl, bmq, bmT, bjk = (Buf() for _ in range(8))
            bWd = [Buf() for _ in range(8)]
            QTb = [Buf() for _ in range(8)]
            KTb = [Buf() for _ in range(5)]
            for k in range(8):
                load_w(Wd[:, k, 0:1224], win_v[:, k, 1304:2528], bWd[k])
                load_w(Wd[:, k, 1224:1288], win_v[:, k, 2456:2520], bWd[k])
                load_w(Wd[:, k, 1288:1352], win_v[:, k, 2456:2520], bWd[k])
            for c in range(4):
                cs = slice(c * 512, (c + 1) * 512)
                for tl in range(4):
                    t = c * 4 + tl
                    ts = slice(t * 128, (t + 1) * 128)
                    pq, pk, pg = (0, 1, 2) if t % 2 == 0 else (4, 5, 6)
                    for k in range(8):
                        pe(_I("matmul", PS[pq][:, :], lhsT=hT[:, k, ts], rhs=Wd[:, k, 0:512], start=(k == 0), stop=(k == 7)), reads=[hTb[c], bWd[k]], writes=[PB[pq]])
                    for k in range(8):
                        pe(_I("matmul", PS[pk][:, 0:128], lhsT=hT[:, k, ts], rhs=Wd[:, k, 512:640], start=(k == 0), stop=(k == 7)), reads=[hTb[c], bWd[k]], writes=[PB[pk]])
                    for k in range(8):
                        pe(_I("matmul", PS[pg][:, 0:8], lhsT=hT[:, k, ts], rhs=Wd[:, k, 1216:1224], start=(k == 0), stop=(k == 7)), reads=[hTb[c], bWd[k]], writes=[PB[pg]])
                    rms_heads(pq, 8, st16[:, 0:8])
                    rms_heads(pk, 1, st16[:, 8:9])
                    rstd_from(st16[:, 0:9])
                    dve(_I("tensor_tensor", q_aug[:, :, 0:64], PS[pq][:, :].rearrange("p (h d) -> p h d", d=64), st16[:, 0:8].unsqueeze(2).to_broadcast([128, 8, 64]), ALU.mult),
                        reads=[PB[pq], stb], writes=[bq_aug])
                    dve(_I("tensor_copy", q_aug[:, :, 64:68], aqc[:, t, :, :]), reads=[bC], writes=[bq_aug])
                    dve(_I("tensor_scalar", sq[0][:, 0:64], PS[pk][:, 0:64], st16[:, 8:9], None, ALU.mult), reads=[PB[pk], stb], writes=[sqb[0]])
                    dve(_I("tensor_tensor", k_aug[:, 0, 0:64], sq[0][:, 0:64], gains[:, 3, :], ALU.mult), reads=[sqb[0], bC], writes=[bk_aug])
                    dve(_I("tensor_copy", k_aug[:, 0, 64:68], akc[:, t, :]), reads=[bC], writes=[bk_aug])
                    act(_I("activation", V_all[:, t, 4, 0:64], PS[pk][:, 64:128], AF.Copy), reads=[PB[pk]], writes=[bV])
                    act(_I("activation", iw[:, t, :], PS[pg][:, 0:8], AF.Copy, scale=8.0 ** -0.5), reads=[PB[pg]], writes=[biw])
                    for h in range(8):
                        pe(_I("transpose", psb(3)[0:68, h * 128:(h + 1) * 128], q_aug[:, h, :], ident_b[:]), reads=[bq_aug, bC], writes=[PB[3]])
                    pe(_I("transpose", psb(7)[0:68, 0:128], k_aug[:, 0, :], ident_b[:]), reads=[bk_aug, bC], writes=[PB[7]])
                    act(_I("activation", QT[0:68, :, ts], psb(3)[0:68, :].rearrange("p (h c) -> p h c", h=8), AF.Copy), reads=[PB[3]], writes=QTb)
                    dve(_I("tensor_copy", KT[0:68, 4, ts], psb(7)[0:68, 0:128]), reads=[PB[7]], writes=[KTb[4]])
                for m in range(4):
                    for k in range(8):
                        pe(_I("matmul", PS[0][:, :], lhsT=Wd[:, k, 640 + m * 128:640 + (m + 1) * 128], rhs=hT[:, k, cs], start=(k == 0), stop=(k == 7)), reads=[hTb[c], bWd[k]], writes=[PB[0]])
                    act(_I("activation", iqT[:, m, cs], PS[0][:, :], AF.Copy, scale=0.125), reads=[PB[0]], writes=[biq])
                for k in range(8):
                    pe(_I("matmul", PS[1][:, :], lhsT=Wd[:, k, 1224:1352], rhs=hT[:, k, cs], start=(k == 0), stop=(k == 7)), reads=[hTb[c], bWd[k]], writes=[PB[1]])
                act(_I("activation", ikT[:, cs], PS[1][:, :], AF.Copy), reads=[PB[1]], writes=[bik])

            S.barrier()
            Wd_flat = Wd.rearrange("p k n -> p (k n)")
            scs = [sc, Wd_flat[:, 0:4096].bitcast(F32)]
            maskqs = [maskq, Wd_flat[:, 4096:6144]]
            maskTs = [maskT, Wd_flat[:, 6144:8192].rearrange("p (j c) -> p j c", c=128)]
            bscs, bmqs, bmTs = [Buf(), Buf()], [Buf(), Buf()], [Buf(), Buf()]
            smx = [sb("smx%d_%d" % (s, i), [128, 40], F32) for i in range(2)] if s == 0 else smx
            smxb = [Buf(), Buf()]

            rls = [rl, Wd_flat[:, 8192:9216].bitcast(F32)]
            brls = [brl, Buf()]

            def indexer_work(t):
                p = t % 2
                sc_, bsc_, sm_, smb_ = scs[p], bscs[p], smx[p], smxb[p]
                qs = slice(t * 128, (t + 1) * 128)
                nk = (t + 1) * 128
                nch = (nk + 511) // 512
                items = []
                idx = 0
                for h in range(8):
                    base = (h % 2) * 64
                    for cc in range(nch):
                        w = min(512, nk - cc * 512)
                        cs = slice(cc * 512, cc * 512 + w)

                        def piece(h=h, base=base, w=w, cs=cs, k=idx):
                            bk = k % 2
                            rl_, brl_ = rls[k % 2], brls[k % 2]
                            pe(_I("matmul", PS[bk][:, 0:w], lhsT=iqT[base:base + 64, h // 2, qs], rhs=ikT[base:base + 64, cs], start=True, stop=True),
                               reads=[biq, bik], writes=[PB[bk]])
                            act(_I("activation", rl_[:, 0:w], PS[bk][:, 0:w], AF.Relu), reads=[PB[bk]], writes=[brl_])
                            if h == 0:
                                dve(_I("tensor_scalar", sc_[:, cs], rl_[:, 0:w], iw[:, t, 0:1], None, ALU.mult), reads=[brl_, biw], writes=[bsc_])
                            else:
                                dve(_I("scalar_tensor_tensor", sc_[:, cs], rl_[:, 0:w], iw[:, t, h:h + 1], sc_[:, cs], ALU.mult, ALU.add), reads=[brl_, biw, bsc_], writes=[bsc_])
                        items.append(piece)
                        idx += 1

                def post():
                    dve(_I("tensor_reduce", sm_[:, 0:1], sc_[:, 0:nk], AX.X, ALU.max, apply_absolute_value=True), reads=[bsc_], writes=[smb_])
                    pool(_I("affine_select", sc_[:, t * 128:(t + 1) * 128], sc_[:, t * 128:(t + 1) * 128], [[-1, 128]], ALU.is_ge, -3e38, base=0, channel_multiplier=1), reads=[bsc_, smb_], writes=[bsc_])
                    dve(_I("tensor_scalar", sm_[:, 8:8 + NBIS + 1], pow2[:], sm_[:, 0:1], None, ALU.mult), reads=[smb_, bC], writes=[smb_])
                    dve(_I("memset", sm_[:, 1:2], 0.0), reads=[smb_], writes=[smb_])
                items.append(post)
                return items

            def tile_work(t):
                return indexer_work(t) + [(lambda j=j: bisect_iter(t, j)) for j in range(NBIS)] + [lambda: finish_mask(t)]

            def bisect_iter(t, j):
                p = t % 2
                sc_, bsc_, sm_, smb_ = scs[p], bscs[p], smx[p], smxb[p]
                nk = (t + 1) * 128
                dve(_I("tensor_scalar", maskqs[p][:, 0:nk], sc_[:, 0:nk], sm_[:, 1:2], None, ALU.is_ge, ALU.add, accum_out=sm_[:, 2:3]), reads=[bsc_, smb_], writes=[bmqs[p], smb_])
                dve(_I("tensor_scalar", sm_[:, 3:4], sm_[:, 2:3], 255.5, -0.5, ALU.is_ge, ALU.add), reads=[smb_], writes=[smb_])
                dve(_I("scalar_tensor_tensor", sm_[:, 1:2], sm_[:, 3:4], sm_[:, 8 + j:9 + j], sm_[:, 1:2], ALU.mult, ALU.add), reads=[smb_], writes=[smb_])

            def finish_mask(t):
                p = t % 2
                sc_, bsc_, sm_, smb_ = scs[p], bscs[p], smx[p], smxb[p]
                nk = (t + 1) * 128
                dve(_I("tensor_tensor", sm_[:, 1:2], sm_[:, 1:2], sm_[:, 8 + NBIS:9 + NBIS], ALU.subtract), reads=[smb_], writes=[smb_])
                dve(_I("tensor_scalar", maskqs[p][:, 0:nk], sc_[:, 0:nk], sm_[:, 1:2], None, ALU.is_ge), reads=[bsc_, smb_], writes=[bmqs[p]])
                for j in range(t + 1):
                    bk = 3 if (j // 8) % 2 == 0 else 7
                    pe(_I("transpose", psb(bk)[:, (j % 8) * 128:(j % 8 + 1) * 128], maskqs[p][:, j * 128:(j + 1) * 128], ident_b[:]), reads=[bmqs[p], bC], writes=[PB[bk]])
                    if j % 8 == 7 or j == t:
                        j0 = (j // 8) * 8
                        n = j - j0 + 1
                        act(_I("activation", maskTs[p][:, j0:j0 + n, :].rearrange("p j c -> p (j c)"), psb(bk)[:, 0:n * 128], AF.Copy), reads=[PB[bk]], writes=[bmTs[p]])

            for item in tile_work(0):
                item()
            for t in range(NT):
                tiles = [(j, None) for j in range(t + 1)]
                units = []
                for g2 in range(2):
                    pvb = next_pv()
                    for hh in range(4):
                        h = 4 * g2 + hh
                        units += make_units(QT[:, h, :], QTb[h], KT[:, 4, :], KTb[4], 4, t, tiles, None, pvb, hh, full_mask=(maskTs[t % 2], bmTs[t % 2]))
                    units[-1]["post"] = (lambda pvb=pvb, g2=g2: norm4(pvb, g2, None, [], True))
                W = tile_work(t + 1) if t + 1 < NT else []
                done = [0]

                def between(i, n, W=W, done=done):
                    target = min(len(W), ((i + 1) * len(W) * 10) // (n * 9) + 1)
                    while done[0] < target:
                        W[done[0]]()
                        done[0] += 1
                run_units(units, 1, between)
                while done[0] < len(W):
                    W[done[0]]()
                    done[0] += 1
                flush_o(t, 1)

            for hf in range(2):
                S.barrier()
                U.reset()
                Wg = U.take(8 * 2048).rearrange("p (k n) -> p k n", k=8)
                Woa = U.take(4 * D).rearrange("p (k n) -> p k n", k=4)
                Wob = U.take(4 * D).rearrange("p (k n) -> p k n", k=4)
                Wout = U.take(8 * D).rearrange("p (k n) -> p k n", k=8)
                Wff_region = (Wg, Woa, Wob, Wout)
                xacc = U.take(8 * D, F32).rearrange("p (t n) -> p t n", t=8)
                yT = U.take(8 * 512).rearrange("p (k n) -> p k n", k=8)
                oaT = U.take(4 * 512).rearrange("p (k n) -> p k n", k=4)
                obT = U.take(4 * 512).rearrange("p (k n) -> p k n", k=4)
                sga = U.take(512)
                sgb = U.take(512)
                t1 = U.take(512, F32)
                t2 = U.take(512, F32)
                aT = yT
                g1bc = U.take(D, F32)
                g2bc = U.take(D, F32)
                bG = Buf()
                S.dma(g1bc, modrow_d[b:b + 1, 2 * D:3 * D].partition_broadcast(128), writes=[bG])
                S.dma(g2bc, modrow_d[b:b + 1, 5 * D:6 * D].partition_broadcast(128), writes=[bG])
                bya, byT, boa, bob, bsga, bsgb, bt1, bt2, baT = (Buf() for _ in range(9))
                bWg = [Buf() for _ in range(8)]
                bWo = [Buf() for _ in range(8)]
                bWa = [Buf() for _ in range(4)]
                bWb = [Buf() for _ in range(4)]
                xab = [Buf() for _ in range(8)]
                for k in range(8):
                    load_w(Wg[:, k, :], win_v[:, k, 2528:4576], bWg[k])
                    load_w(Wout[:, k, :], wout_d.rearrange("(k p) n -> p k n", p=128)[:, k, :], bWo[k])
                for k in range(4):
                    load_w(Woa[:, k, :], woa_d.rearrange("(k p) n -> p k n", p=128)[:, k, :], bWa[k])
                    load_w(Wob[:, k, :], wob_d.rearrange("(k p) n -> p k n", p=128)[:, k, :], bWb[k])
                for cl in range(2):
                    c = hf * 2 + cl
                    cs = slice(c * 512, (c + 1) * 512)
                    S.dma(oaT, oT_d[0, :, :, cs].rearrange("j p c -> p j c"), reads=[bOT[0]], writes=[boa])
                    S.dma(obT, oT_d[1, :, :, cs].rearrange("j p c -> p j c"), reads=[bOT[1]], writes=[bob])
                    for f in range(8):
                        fs = slice(f * 128, (f + 1) * 128)
                        for k in range(8):
                            pe(_I("matmul", PS[0][:, :], lhsT=Wg[:, k, fs], rhs=hT[:, k, cs], start=(k == 0), stop=(k == 7)), reads=[bWg[k], hTb[c]], writes=[PB[0]])
                        act(_I("activation", sga, PS[0][:, :], AF.Sigmoid), reads=[PB[0]], writes=[bsga])
                        for k in range(8):
                            pe(_I("matmul", PS[1][:, :], lhsT=Wg[:, k, 1024 + f * 128:1024 + (f + 1) * 128], rhs=hT[:, k, cs], start=(k == 0), stop=(k == 7)), reads=[bWg[k], hTb[c]], writes=[PB[1]])
                        act(_I("activation", sgb, PS[1][:, :], AF.Sigmoid), reads=[PB[1]], writes=[bsgb])
                        for k in range(4):
                            pe(_I("matmul", PS[2][:, :], lhsT=Woa[:, k, fs], rhs=oaT[:, k, :], start=(k == 0), stop=(k == 3)), reads=[bWa[k], boa], writes=[PB[2]])
                        for k in range(4):
                            pe(_I("matmul", PS[4][:, :], lhsT=Wob[:, k, fs], rhs=obT[:, k, :], start=(k == 0), stop=(k == 3)), reads=[bWb[k], bob], writes=[PB[4]])
                        dve(_I("tensor_tensor", t1, PS[2][:, :], sga, ALU.mult), reads=[PB[2], bsga], writes=[bt1])
                        dve(_I("tensor_tensor", t2, PS[4][:, :], sgb, ALU.mult), reads=[PB[4], bsgb], writes=[bt2])
                        dve(_I("tensor_tensor", yT[:, f, :], t1, t2, ALU.add), reads=[bt1, bt2], writes=[byT])
                    for tl in range(4):
                        t = c * 4 + tl
                        tt = t - hf * 8
                        i = t % 2
                        S.dma(xt[i][:], x_d[s, t * 128:(t + 1) * 128, :], writes=[xtb[i]])
                        for h2 in range(2):
                            ns = slice(h2 * 512, (h2 + 1) * 512)
                            bk = 5 + h2
                            for k in range(8):
                                pe(_I("matmul", PS[bk][:, :], lhsT=yT[:, k, tl * 128:(tl + 1) * 128], rhs=Wout[:, k, ns], start=(k == 0), stop=(k == 7)), reads=[byT, bWo[k]], writes=[PB[bk]])
                            dve(_I("tensor_tensor", t1, PS[bk][:, :], g1bc[:, ns], ALU.mult), reads=[PB[bk], bG], writes=[bt1])
                            dve(_I("tensor_tensor", xacc[:, tt, ns], t1, xt[i][:, ns], ALU.add), reads=[bt1, xtb[i]], writes=[xab[tt]])
                        if dbg and s == 0:
                            S.dma(x1_dbg[t * 128:(t + 1) * 128, :], xacc[:, tt, :], reads=[xab[tt]], writes=[Buf()], is_output=True)
                        layernorm(xacc[:, tt, :], xab[tt], tl % 2, 1, b, 0)
                        if tl % 2 == 1:
                            ln_evac(t // 2, 1, b, 0, hTb[c])
                S.barrier()
                for (f0_, nf) in ((0, 8), (8, 8), (16, 6)):
                    Wfg = Wg.rearrange("p k n -> p (k n)")[:, 0:8 * 1024].rearrange("p (k n) -> p k n", k=8)
                    Wfu = Wg.rearrange("p k n -> p (k n)")[:, 8 * 1024:16 * 1024].rearrange("p (k n) -> p k n", k=8)
                    Wfd = Wout.rearrange("p k n -> p (k n)")[:, 0:8 * D].rearrange("p (k n) -> p k n", k=8)
                    nfc = nf * 128
                    if f0_ == 0:
                        bFg = [Buf() for _ in range(8)]
                        bFu = [Buf() for _ in range(8)]
                        bFd = [Buf() for _ in range(8)]
                    for k in range(8):
                        load_w(Wfg[:, k, 0:nfc], wfg_d.rearrange("(k p) n -> p k n", p=128)[:, k, f0_ * 128:f0_ * 128 + nfc], bFg[k])
                        load_w(Wfu[:, k, 0:nfc], wfu_d.rearrange("(k p) n -> p k n", p=128)[:, k, f0_ * 128:f0_ * 128 + nfc], bFu[k])
                    for k in range(nf):
                        load_w(Wfd[:, k, :], wfd_d[(f0_ + k) * 128:(f0_ + k + 1) * 128, :], bFd[k])
                    for cl in range(2):
                        c = hf * 2 + cl
                        cs = slice(c * 512, (c + 1) * 512)
                        for f in range(nf):
                            fs = slice(f * 128, (f + 1) * 128)
                            for k in range(8):
                                pe(_I("matmul", PS[0][:, :], lhsT=Wfg[:, k, fs], rhs=hT[:, k, cs], start=(k == 0), stop=(k == 7)), reads=[bFg[k], hTb[c]], writes=[PB[0]])
                            for k in range(8):
                                pe(_I("matmul", PS[1][:, :], lhsT=Wfu[:, k, fs], rhs=hT[:, k, cs], start=(k == 0), stop=(k == 7)), reads=[bFu[k], hTb[c]], writes=[PB[1]])
                            act(_I("activation", t1, PS[0][:, :], AF.Silu), reads=[PB[0]], writes=[bt1])
                            dve(_I("tensor_tensor", aT[:, f, :], t1, PS[1][:, :], ALU.mult), reads=[bt1, PB[1]], writes=[baT])
                        for tl in range(4):
                            tt = cl * 4 + tl
                            for h2 in range(2):
                                ns = slice(h2 * 512, (h2 + 1) * 512)
                                bk = 5 + h2
                                for f in range(nf):
                                    pe(_I("matmul", PS[bk][:, :], lhsT=aT[:, f, tl * 128:(tl + 1) * 128], rhs=Wfd[:, f, ns], start=(f == 0), stop=(f == nf - 1)), reads=[baT, bFd[f]], writes=[PB[bk]])
                                dve(_I("tensor_tensor", t2, PS[bk][:, :], g2bc[:, ns], ALU.mult), reads=[PB[bk], bG], writes=[bt2])
                                dve(_I("tensor_tensor", xacc[:, tt, ns], xacc[:, tt, ns], t2, ALU.add), reads=[bt2, xab[tt]], writes=[xab[tt]])
                for tt in range(8):
                    t = hf * 8 + tt
                    S.dma(out_d[s, t * 128:(t + 1) * 128, :], xacc[:, tt, :], reads=[xab[tt]], writes=[Buf()], is_output=True)
        S.emit()
    return nc


def _prep_common(inp):
    f = lambda a: np.ascontiguousarray(np.asarray(a, dtype=np.float32))
    gn = np.concatenate([inp["g_norm1"][0].reshape(8, 128).T, inp["g_norm2"][0].reshape(8, 128).T], axis=1)
    gvec = np.concatenate([inp[k][0] for k in ("g_q_a", "g_kc_a", "g_ks_a", "g_kw_a", "g_q_b", "g_k_b")])[None, :]
    peT = np.concatenate([inp["pe_ck"][0].T, inp["pe_cv"][0].T], axis=1)
    return {
        "w_ada": f(inp["w_ada"][0]), "b_ada": f(inp["b_ada"]), "gn": f(gn), "w_in": f(inp["w_in"][0]),
        "gvec": f(gvec), "peT": f(peT), "w_ck1": f(inp["w_ck1"][0]), "w_ck2": f(inp["w_ck2"][0]),
        "w_cv1": f(inp["w_cv1"][0]), "w_cv2": f(inp["w_cv2"][0]), "w_o_a": f(inp["w_o_a"][0]),
        "w_o_b": f(inp["w_o_b"][0]), "w_out": f(inp["w_out"][0]), "w_ff_gate": f(inp["w_ff_gate"][0]),
        "w_ff_up": f(inp["w_ff_up"][0]), "w_ff_down": f(inp["w_ff_down"][0]),
    }


def _core_map(common, x, c, i, nseq):
    m = dict(common)
    m["x"] = np.ascontiguousarray(x[i * nseq:(i + 1) * nseq])
    cc = np.zeros((4, D), np.float32)
    cc[:nseq] = c[i * nseq:(i + 1) * nseq]
    m["cT"] = np.ascontiguousarray(cc.T.reshape(8, 128, 4).transpose(1, 0, 2))
    return m


def kernel(**inputs):
    x = np.asarray(inputs["x"], dtype=np.float32)
    c = np.asarray(inputs["c"], dtype=np.float32)
    n = 8
    nseq = x.shape[0] // n
    nc = build_nc(nseq)
    common = _prep_common(inputs)
    in_maps = [_core_map(common, x, c, i, nseq) for i in range(n)]
    res = run_bass_kernel_spmd(nc, in_maps, core_ids=list(range(n)))
    return np.concatenate([r["out"] for r in res.results], axis=0).astype(np.float32)
```

```python
import contextlib
import numpy as np
import concourse.bass as bass
import concourse.mybir as mybir
from concourse.bass_utils import run_bass_kernel_spmd

F32 = mybir.dt.float32
BF16 = mybir.dt.bfloat16
AF = mybir.ActivationFunctionType
ALU = mybir.AluOpType
AX = mybir.AxisListType

S_TOK = 2048
D = 1024
NT = 16
DIN = 4576
DFF = 2816
EPS = 1e-6
NBIS = 16
NEG = -30000.0


class Buf:
    __slots__ = ("name", "w", "r")

    def __init__(self, name=""):
        self.name = name
        self.w = None
        self.r = {}


class Sched:
    ENGS = ("pe", "act", "dve", "pool", "sp")
    NDMA = 24

    def __init__(self, nc):
        self.nc = nc
        self.streams = {e: [] for e in self.ENGS}
        self.count = {e: 0 for e in self.ENGS}
        self.waited = {e: {} for e in self.ENGS}
        self.dma_uses = [0] * self.NDMA
        self.dma_rr = 0
        self.out_events = []

    def _deps(self, eng, reads, writes):
        deps = {}

        def add(ev):
            if ev is None:
                return
            k, v = ev
            if deps.get(k, 0) < v:
                deps[k] = v
        for b in reads:
            add(b.w)
        for b in writes:
            if b.w is not None and b.w[0] != eng:
                add(b.w)
            for k, v in b.r.items():
                if k != eng:
                    add((k, v))
        waits = []
        for k, v in deps.items():
            if k == "pe" and eng == "pe":
                continue
            if self.waited[eng].get(k, 0) < v:
                self.waited[eng][k] = v
                waits.append((k, v))
        return waits

    def _commit(self, ev, reads, writes):
        k, v = ev
        for b in writes:
            b.w = ev
            b.r = {}
        for b in reads:
            if b.r.get(k, 0) < v:
                b.r[k] = v

    def op(self, eng, fn, reads=(), writes=()):
        waits = self._deps(eng, reads, writes)
        self.count[eng] += 1
        ev = (eng, self.count[eng])
        self.streams[eng].append((fn, waits, ev))
        self._commit(ev, reads, writes)
        return ev

    def dma(self, out, in_, reads=(), writes=(), q="sp", is_output=False, **kw):
        i = self.dma_rr
        self.dma_rr = (self.dma_rr + 1) % self.NDMA
        waits = self._deps(q, reads, writes)
        key = "dma%d" % i
        prev = self.dma_uses[i] * 16
        if prev and self.waited[q].get(key, 0) < prev:
            self.waited[q][key] = prev
            waits.append((key, prev))
        self.dma_uses[i] += 1
        ev = (key, self.dma_uses[i] * 16)
        fn = lambda e, out=out, in_=in_, kw=kw: e.dma_start(out=out, in_=in_, **kw)
        self.streams[q].append((fn, waits, ev))
        self._commit(ev, reads, writes)
        if is_output:
            self.out_events.append(ev)
        return ev

    def barrier(self):
        allv = [(e, self.count[e]) for e in self.ENGS if self.count[e]]
        allv += [("dma%d" % i, self.dma_uses[i] * 16) for i in range(self.NDMA) if self.dma_uses[i]]
        for e in self.ENGS:
            waits = []
            for k, v in allv:
                if k == e:
                    continue
                if self.waited[e].get(k, 0) < v:
                    self.waited[e][k] = v
                    waits.append((k, v))
            if waits:
                self.streams[e].append((None, waits, None))

    def emit(self):
        nc = self.nc
        with contextlib.ExitStack() as st:
            sems = {}
            for e in self.ENGS:
                sems[e] = st.enter_context(nc.semaphore("s_" + e))
            for i in range(self.NDMA):
                sems["dma%d" % i] = st.enter_context(nc.semaphore("s_dma%d" % i))
            final = {}
            for k, v in self.out_events:
                final[k] = max(final.get(k, 0), v)
            block = st.enter_context(nc.Block())

            def run(engname, e):
                for fn, waits, ev in self.streams[engname]:
                    for k, v in waits:
                        e.wait_ge(sems[k], v)
                    if fn is None:
                        continue
                    ins = fn(e)
                    k, v = ev
                    ins.then_inc(sems[k], 16 if k.startswith("dma") else 1)
                if engname == "sp":
                    for k, v in final.items():
                        e.wait_ge(sems[k], v)

            @block.tensor
            def _(e):
                run("pe", e)

            @block.scalar
            def _(e):
                run("act", e)

            @block.vector
            def _(e):
                run("dve", e)

            @block.gpsimd
            def _(e):
                run("pool", e)

            @block.sync
            def _(e):
                run("sp", e)


class Arena:
    def __init__(self, ap16):
        self.ap = ap16
        self.off = 0

    def reset(self):
        self.off = 0

    def take(self, ncols, dt=BF16):
        if dt == F32:
            self.off = (self.off + 1) // 2 * 2
            n16 = ncols * 2
        else:
            n16 = ncols
        assert self.off + n16 <= self.ap.shape[1], (self.off, n16, self.ap.shape)
        v = self.ap[:, self.off:self.off + n16]
        self.off += n16
        self.off = (self.off + 1) // 2 * 2
        return v.bitcast(F32) if dt == F32 else v


_REGS = {}


def _I(name, *args, **kw):
    if name == "affine_select":
        def thunk(e):
            a = list(args)
            key = (id(e), float(a[4]))
            if key not in _REGS:
                _REGS[key] = e.to_reg(float(a[4]))
            a[4] = _REGS[key]
            return e.affine_select(*a, **kw)
        return thunk
    return lambda e: getattr(e, name)(*args, **kw)


def build_nc(nseq=4, dbg=False):
    nc = bass.Bass("TRN2", target_bir_lowering=False)
    _REGS.clear()
    S = Sched(nc)

    def din(name, shape):
        return nc.dram_tensor(name, shape, F32, kind="ExternalInput").ap()
    x_d = din("x", [nseq, S_TOK, D])
    cT_d = din("cT", [128, 8, 4])
    wada_d = din("w_ada", [D, 6 * D])
    bada_d = din("b_ada", [1, 6 * D])
    gn_d = din("gn", [128, 16])
    win_d = din("w_in", [D, DIN])
    gv_d = din("gvec", [1, 6 * 64])
    peT_d = din("peT", [64, 64])
    wck1_d = din("w_ck1", [2048, 128])
    wck2_d = din("w_ck2", [128, 64])
    wcv1_d = din("w_cv1", [2048, 128])
    wcv2_d = din("w_cv2", [128, 64])
    woa_d = din("w_o_a", [512, D])
    wob_d = din("w_o_b", [512, D])
    wout_d = din("w_out", [D, D])
    wfg_d = din("w_ff_gate", [D, DFF])
    wfu_d = din("w_ff_up", [D, DFF])
    wfd_d = din("w_ff_down", [DFF, D])
    out_d = nc.dram_tensor("out", [nseq, S_TOK, D], F32, kind="ExternalOutput").ap()
    modrow_d = nc.dram_tensor("modrow", [4, 6 * D], F32, kind="Internal").ap()
    oT_d = nc.dram_tensor("oT_scr", [2, 4, 128, S_TOK], BF16, kind="ExternalOutput" if dbg else "Internal").ap()
    if dbg:
        hT_dbg = nc.dram_tensor("hT_dbg", [128, 8, S_TOK], BF16, kind="ExternalOutput").ap()
        x1_dbg = nc.dram_tensor("x1_dbg", [S_TOK, D], F32, kind="ExternalOutput").ap()
        mod_dbg = nc.dram_tensor("mod_dbg", [4, 6 * D], F32, kind="ExternalOutput").ap()

    st = contextlib.ExitStack()

    def sb(name, shape, dt=F32):
        return st.enter_context(nc.sbuf_tensor(name, shape, dt))

    with st:
        PS = [st.enter_context(nc.psum_tensor("ps%d" % i, [128, 512], F32)) for i in range(8)]
        PB = [Buf("ps%d" % i) for i in range(8)]

        def psb(i):
            return PS[i][:].bitcast(BF16)

        ident_b = sb("ident_b", [128, 128], BF16)
        ident_f = sb("ident_f", [128, 128], F32)
        Cm = sb("Cm", [128, 128], BF16)
        Wm = sb("Wm", [128, 128], BF16)
        cmask = sb("cmask", [128, S_TOK], BF16)
        ET = sb("ET", [68, S_TOK], BF16)
        selA = sb("selA", [128, NT, 32], F32)
        selB = sb("selB", [128, NT, 32], F32)
        aqc = sb("aqc", [128, NT, 8, 4], BF16)
        akc = sb("akc", [128, NT, 4], BF16)
        gbc = sb("gbc", [128, 6 * 64], F32)
        gains = sb("gains", [128, 4, 64], F32)
        gnc = sb("gnc", [128, 16], F32)
        modT = sb("modT", [128, 32, 4], F32)
        cb2 = sb("cb2", [128, 2], F32)
        pow2 = sb("pow2", [128, NBIS + 1], F32)
        hT = sb("hT", [128, 8, S_TOK], BF16)
        Vc = sb("Vc", [128, 2, 97], BF16)
        kc_aug = sb("kc_aug", [128, 2, 68], BF16)
        KcT = sb("KcT", [128, 2, 128], BF16)
        U_t = sb("U", [128, 65400], BF16)
        U = Arena(U_t[:])
        bC = Buf("consts")
        bOT = [Buf(), Buf()]
        bOut = Buf()

        hTb = [Buf("hT%d" % c) for c in range(4)]

        def pool(fn, reads=(), writes=()):
            return S.op("pool", fn, reads, writes)

        def dve(fn, reads=(), writes=()):
            return S.op("dve", fn, reads, writes)

        def act(fn, reads=(), writes=()):
            return S.op("act", fn, reads, writes)

        def pe(fn, reads=(), writes=()):
            return S.op("pe", fn, reads, writes)

        pool(_I("memset", ident_b[:], 1.0), writes=[bC])
        pool(_I("affine_select", ident_b[:], ident_b[:], [[1, 128]], ALU.is_equal, 0.0, base=0, channel_multiplier=-1), reads=[bC], writes=[bC])
        pool(_I("memset", ident_f[:], 1.0), writes=[bC])
        pool(_I("affine_select", ident_f[:], ident_f[:], [[1, 128]], ALU.is_equal, 0.0, base=0, channel_multiplier=-1), reads=[bC], writes=[bC])
        pool(_I("memset", Cm[:], 1.0), writes=[bC])
        pool(_I("affine_select", Cm[:], Cm[:], [[1, 128]], ALU.is_ge, 0.0, base=0, channel_multiplier=-1), reads=[bC], writes=[bC])
        pool(_I("memset", Wm[:], 1.0), writes=[bC])
        pool(_I("affine_select", Wm[:], Wm[:], [[-1, 128]], ALU.is_gt, 0.0, base=0, channel_multiplier=1), reads=[bC], writes=[bC])
        pool(_I("memset", cmask[:], 1.0), writes=[bC])
        pool(_I("affine_select", cmask[:], cmask[:], [[1, S_TOK]], ALU.is_ge, 0.0, base=-31, channel_multiplier=-16), reads=[bC], writes=[bC])
        pool(_I("memset", ET[0:32, :], 1.0), writes=[bC])
        pool(_I("memset", ET[32:64, :], 0.0), writes=[bC])
        pool(_I("memset", ET[64:68, :], 0.0), writes=[bC])
        pool(_I("affine_select", ET[0:32, :], ET[0:32, :], [[1, S_TOK]], ALU.is_ge, 0.0, base=0, channel_multiplier=-64), reads=[bC], writes=[bC])
        pool(_I("affine_select", ET[0:32, :], ET[0:32, :], [[-1, S_TOK]], ALU.is_ge, 0.0, base=63, channel_multiplier=64), reads=[bC], writes=[bC])
        for g in range(2):
            pool(_I("memset", Vc[:, g, 64:97], 1.0), writes=[bC])
            pool(_I("affine_select", Vc[:, g, 65:97], Vc[:, g, 65:97], [[-64, 32]], ALU.is_ge, 0.0, base=31, channel_multiplier=16), reads=[bC], writes=[bC])
            pool(_I("affine_select", Vc[:, g, 65:97], Vc[:, g, 65:97], [[64, 32]], ALU.is_ge, 0.0, base=63, channel_multiplier=-16), reads=[bC], writes=[bC])
        Dt = U.take(NT * 32, F32).rearrange("p (t j) -> p t j", j=32)
        jt = U.take(NT * 32, F32).rearrange("p (t j) -> p t j", j=32)
        f0 = U.take(NT * 32, F32).rearrange("p (t j) -> p t j", j=32)
        for lo_, base in ((0, 0), (64, -1)):
            pool(_I("iota", Dt[lo_:lo_ + 64], [[-2, NT], [1, 32]], base=base, channel_multiplier=0, allow_small_or_imprecise_dtypes=True), writes=[bC])
        pool(_I("iota", jt[:], [[0, NT], [1, 32]], base=0, channel_multiplier=0, allow_small_or_imprecise_dtypes=True), writes=[bC])
        dve(_I("tensor_single_scalar", f0[:], jt[:], 0.0, ALU.is_equal), reads=[bC], writes=[bC])
        dve(_I("tensor_single_scalar", jt[:], Dt[:], 0.0, ALU.is_equal), reads=[bC], writes=[bC])
        dve(_I("tensor_max", f0[:], f0[:], jt[:]), reads=[bC], writes=[bC])
        dve(_I("tensor_single_scalar", jt[:], Dt[:], -1.0, ALU.is_equal), reads=[bC], writes=[bC])
        dve(_I("tensor_max", f0[:], f0[:], jt[:]), reads=[bC], writes=[bC])
        dve(_I("tensor_single_scalar", jt[:], Dt[:], 0.0, ALU.is_le), reads=[bC], writes=[bC])
        dve(_I("tensor_sub", selA[:], jt[:], f0[:]), reads=[bC], writes=[bC])
        dve(_I("tensor_add", selB[:], jt[:], f0[:]), reads=[bC], writes=[bC])
        dve(_I("tensor_scalar", selB[:], selB[:], -1.0, 1e9, ALU.add, ALU.mult), reads=[bC], writes=[bC])
        hi_t = sb("hi_t", [128, NT], F32)
        lo_t = sb("lo_t", [128, 1], F32)
        for lo_, base in ((0, 0), (64, 64)):
            pool(_I("iota", hi_t[lo_:lo_ + 64], [[128, NT]], base=base, channel_multiplier=0, allow_small_or_imprecise_dtypes=True), writes=[bC])
            pool(_I("iota", lo_t[lo_:lo_ + 64], [[0, 1]], base=0, channel_multiplier=1, allow_small_or_imprecise_dtypes=True), writes=[bC])
        for h in range(8):
            sl = 2.0 ** -(h + 1)
            dve(_I("memset", aqc[:, :, h, 0:2], sl), reads=[bC], writes=[bC])
            dve(_I("tensor_scalar", aqc[:, :, h, 2], hi_t[:], -sl, None, ALU.mult), reads=[bC], writes=[bC])
            dve(_I("tensor_scalar", aqc[:, :, h, 3], lo_t[:].to_broadcast([128, NT]), -sl, None, ALU.mult), reads=[bC], writes=[bC])
        dve(_I("memset", akc[:, :, 2:4], 1.0), reads=[bC], writes=[bC])
        dve(_I("tensor_copy", akc[:, :, 0], hi_t[:]), reads=[bC], writes=[bC])
        dve(_I("tensor_copy", akc[:, :, 1], lo_t[:].to_broadcast([128, NT])), reads=[bC], writes=[bC])
        pn = sb("pn", [128, 1], F32)
        pool(_I("iota", pn[:], [[0, 1]], base=0, channel_multiplier=16, allow_small_or_imprecise_dtypes=True), writes=[bC])
        for g in range(2):
            dve(_I("tensor_copy", kc_aug[:, g, 64:65], pn[:]), reads=[bC], writes=[bC])
            dve(_I("memset", kc_aug[:, g, 65:66], 31.0), reads=[bC], writes=[bC])
            dve(_I("memset", kc_aug[:, g, 66:68], 1.0), reads=[bC], writes=[bC])
        for j in range(NBIS + 1):
            dve(_I("memset", pow2[:, j:j + 1], 2.0 ** -j * (1.01 if j == NBIS else 1.0)), reads=[bC], writes=[bC])
        S.dma(gbc[:], gv_d.partition_broadcast(128), writes=[bC])
        S.dma(gnc[:], gn_d, writes=[bC])

        def gsl(i):
            return gbc[:, i * 64:(i + 1) * 64]
        for idx, (gk, gq) in enumerate(((2, 0), (3, 0), (1, 0), (5, 4))):
            dve(_I("scalar_tensor_tensor", gains[:, idx, :], gsl(gk), 0.125, gsl(gq), ALU.mult, ALU.mult), reads=[bC], writes=[bC])

        S.barrier()
        U.reset()
        scT = U.take(32, F32).rearrange("p (k b) -> p k b", b=4)
        wchunk = U.take(8 * 512, F32).rearrange("p (k n) -> p k n", k=8)
        bchunk = U.take(512, F32)
        mchunk = U.take(512, F32)
        bsc, bw, bbc, bm = Buf(), Buf(), Buf(), Buf()
        S.dma(scT, cT_d, writes=[bsc])
        act(_I("activation", scT, scT, AF.Silu), reads=[bsc], writes=[bsc])
        wada_v = wada_d.rearrange("(k p) n -> p k n", p=128)
        LNV = {0: 0, 1: 1, 3: 2, 4: 3}
        for c in range(12):
            S.dma(wchunk, wada_v[:, :, c * 512:(c + 1) * 512], writes=[bw])
            S.dma(bchunk[0:4, :], bada_d[:, c * 512:(c + 1) * 512].partition_broadcast(4), writes=[bbc])
            for k in range(8):
                pe(_I("matmul", PS[0][0:4, :], lhsT=scT[:, k, :], rhs=wchunk[:, k, :], start=(k == 0), stop=(k == 7)), reads=[bsc, bw], writes=[PB[0]])
            dve(_I("tensor_tensor", mchunk[0:4, :], PS[0][0:4, :], bchunk[0:4, :], ALU.add), reads=[PB[0], bbc], writes=[bm])
            S.dma(modrow_d[:, c * 512:(c + 1) * 512], mchunk[0:4, :], reads=[bm], writes=[bC])
            if dbg:
                S.dma(mod_dbg[:, c * 512:(c + 1) * 512], mchunk[0:4, :], reads=[bm], writes=[Buf()], is_output=True)
            vec, half = c // 2, c % 2
            if vec in LNV:
                for i in range(4):
                    col = (LNV[vec] * 8 + half * 4 + i) * 4
                    pe(_I("transpose", PS[1][:, col:col + 4], mchunk[0:4, i * 128:(i + 1) * 128], ident_f[0:4, 0:4]), reads=[bm, bC], writes=[PB[1]])
        dve(_I("tensor_copy", modT[:].rearrange("p a b -> p (a b)"), PS[1][:, 0:128]), reads=[PB[1]], writes=[bC])
        for which, gi in ((1, 0), (3, 1)):
            dve(_I("scalar_tensor_tensor",
                modT[:, which * 8:(which + 1) * 8, :], modT[:, which * 8:(which + 1) * 8, :], 1.0,
                gnc[:, gi * 8:(gi + 1) * 8].unsqueeze(2).to_broadcast([128, 8, 4]), ALU.add, ALU.mult), reads=[bC], writes=[bC])
        S.barrier()

        xt = [sb("xt%d" % i, [128, D], F32) for i in range(2)]
        xtb = [Buf() for _ in range(2)]
        xn = sb("xn", [128, D], BF16)
        xnb = Buf()
        sq = [sb("sq%d" % i, [128, 512], F32) for i in range(2)]
        sqb = [Buf(), Buf()]
        st16 = sb("st16", [128, 16], F32)
        stb = Buf()
        PT = [sb("PT%d" % i, [128, 512], BF16) for i in range(6)]
        PTb = [Buf() for _ in range(6)]
        sn = sb("sn", [128, 16], F32)
        snb = Buf()
        tmpo = sb("tmpo", [128, 4, 64], F32)
        tmpb = Buf()
        oaccs = [sb("oacc%d" % i, [128, 8, 64], F32) for i in range(2)]
        oabs = [Buf(), Buf()]
        mneg = sb("mneg", [128, 32], BF16)
        mnegb = Buf()
        obf = sb("obf", [128, 512], BF16)
        obfb = Buf()
        oTs = sb("oTs", [128, 4, 128], BF16)
        oTsb = Buf()
        sm = sb("sm", [128, 64], F32)
        smb = Buf()
        state = {"pt": 0, "sb": 0}
        vstate = {}

        def layernorm(src_tile, src_buf, tl, which, b, psbank):
            act(_I("activation", sq[0][:, :].bitcast(BF16), src_tile, AF.Square, accum_out=st16[:, 0:1]), reads=[src_buf], writes=[sqb[0], stb])
            act(_I("activation", st16[:, 1:2], st16[:, 0:1], AF.Sqrt, bias=EPS, scale=1.0 / D), reads=[stb], writes=[stb])
            dve(_I("reciprocal", st16[:, 2:3], st16[:, 1:2]), reads=[stb], writes=[stb])
            dve(_I("tensor_scalar", xn[:], src_tile, st16[:, 2:3], None, ALU.mult), reads=[src_buf, stb], writes=[xnb])
            for j in range(8):
                bk = psbank + j // 4
                o0 = ((j % 4) * 2 + tl) * 128
                pe(_I("transpose", psb(bk)[:, o0:o0 + 128], xn[:, j * 128:(j + 1) * 128], ident_b[:]), reads=[xnb, bC], writes=[PB[bk]])

        def ln_evac(c2, which, b, psbank, hbuf):
            for j in range(8):
                bk = psbank + j // 4
                src = psb(bk)[:, (j % 4) * 256:(j % 4) * 256 + 256]
                Gc = modT[:, (2 * which + 1) * 8 + j, b:b + 1]
                Sc = modT[:, (2 * which) * 8 + j, b:b + 1]
                dve(_I("tensor_scalar", hT[:, j, c2 * 256:(c2 + 1) * 256], src, Gc, Sc, ALU.mult, ALU.add),
                    reads=[PB[bk], bC], writes=[hbuf])

        def load_w(dst, src, buf, q="pool"):
            S.dma(dst, src, writes=[buf], q=q)

        win_v = win_d.rearrange("(k p) n -> p k n", p=128)

        def rms_heads(psbank, nh, dst_stats):
            i = state["sb"] = (state["sb"] + 1) % 2
            act(_I("activation", sq[i][:, 0:nh * 64], PS[psbank][:, 0:nh * 64], AF.Square), reads=[PB[psbank]], writes=[sqb[i]])
            dve(_I("tensor_reduce", dst_stats, sq[i][:, 0:nh * 64].rearrange("p (h d) -> p h d", d=64), AX.X, ALU.add), reads=[sqb[i]], writes=[stb])

        def rstd_from(stats):
            act(_I("activation", stats, stats, AF.Sqrt, bias=EPS, scale=1.0 / 64), reads=[stb], writes=[stb])
            dve(_I("reciprocal", stats, stats), reads=[stb], writes=[stb])

        def make_units(QTh, qb, KTk, kb, kind, t, tiles, maskmm, pvb, slot, full_mask=None, banks=(4, 5)):
            ntl = len(tiles)
            return [dict(QTh=QTh, qb=qb, KTk=KTk, kb=kb, kind=kind, t=t, grp=tiles[g0:g0 + 4], g0=g0, ntl=ntl, maskmm=maskmm,
                         pvb=pvb, slot=slot, full_mask=full_mask, banks=banks) for g0 in range(0, ntl, 4)]

        def emit_A(u):
            qs = slice(u["t"] * 128, (u["t"] + 1) * 128)
            banks = u["banks"]
            bk = banks[state["pt"] % len(banks)]
            pi = state["pt"] % (len(PT) - 1)
            state["pt"] += 1
            u["pi"] = pi
            grp = u["grp"]
            for i, (j, mt) in enumerate(grp):
                ks = slice(j * 128, (j + 1) * 128)
                pe(_I("matmul", PS[bk][:, i * 128:(i + 1) * 128], lhsT=u["KTk"][0:68, ks], rhs=u["QTh"][0:68, qs], start=True, stop=(u["maskmm"] is None)),
                   reads=[u["kb"], u["qb"]], writes=[PB[bk]])
                if u["maskmm"] is not None:
                    MTg, mb = u["maskmm"]
                    pe(_I("matmul", PS[bk][:, i * 128:(i + 1) * 128], lhsT=ET[0:68, ks], rhs=MTg[0:68, qs], start=False, stop=True),
                       reads=[mb, bC], writes=[PB[bk]])
            n = len(grp) * 128
            act(_I("activation", PT[pi][:, 0:n], PS[bk][:, 0:n], AF.Exp), reads=[PB[bk]], writes=[PTb[pi]])
            if u["full_mask"] is not None:
                mT, mTb = u["full_mask"]
                j0 = grp[0][0]
                pool(_I("tensor_tensor", PT[pi][:, 0:n], PT[pi][:, 0:n], mT[:, j0:j0 + len(grp), :].rearrange("p j c -> p (j c)"), ALU.mult),
                     reads=[mTb, PTb[pi]], writes=[PTb[pi]])
            for i, (j, mt) in enumerate(grp):
                if mt is not None:
                    mk = Cm if mt == "C" else Wm
                    pool(_I("tensor_tensor", PT[pi][:, i * 128:(i + 1) * 128], PT[pi][:, i * 128:(i + 1) * 128], mk[:], ALU.mult),
                         reads=[bC, PTb[pi]], writes=[PTb[pi]])

        def emit_B(u):
            pi = u["pi"]
            pvb = u["pvb"]
            po = PS[pvb][:, u["slot"] * 65:(u["slot"] + 1) * 65]
            for i, (j, mt) in enumerate(u["grp"]):
                gi = u["g0"] + i
                pe(_I("matmul", po, lhsT=PT[pi][:, i * 128:(i + 1) * 128], rhs=vstate["V"][:, j, u["kind"], :], start=(gi == 0), stop=(gi == u["ntl"] - 1)),
                   reads=[PTb[pi], vstate["bV"]], writes=[PB[pvb]])

        def run_units(units, L, between=None):
            n = len(units)
            for i in range(min(L, n)):
                emit_A(units[i])
            for i in range(n):
                if i + L < n:
                    emit_A(units[i + L])
                emit_B(units[i])
                if units[i].get("post") is not None:
                    units[i]["post"]()
                if between is not None:
                    between(i, n)

        def next_pv():
            state["pv"] = state.get("pv", 0) + 1
            return 6 if state["pv"] % 2 == 0 else 2

        def norm4(pvb, g, gate_view, gate_bufs, first, par):
            oacc, oab = oaccs[par], oabs[par]
            o4 = PS[pvb][:, 0:260].rearrange("p (h c) -> p h c", h=4)
            dve(_I("tensor_scalar", sn[:, 0:4], o4[:, :, 64], 1e-30, None, ALU.max), reads=[PB[pvb]], writes=[snb])
            dve(_I("reciprocal", sn[:, 4:8], sn[:, 0:4]), reads=[snb], writes=[snb])
            if gate_view is not None:
                dve(_I("tensor_tensor", sn[:, 4:8], sn[:, 4:8], gate_view, ALU.mult), reads=[snb] + gate_bufs, writes=[snb])
            wb = sn[:, 4:8].unsqueeze(2).to_broadcast([128, 4, 64])
            if first:
                dve(_I("tensor_tensor", oacc[:, 4 * g:4 * g + 4, :], o4[:, :, 0:64], wb, ALU.mult), reads=[PB[pvb], snb], writes=[oab])
            else:
                dve(_I("tensor_tensor", tmpo[:], o4[:, :, 0:64], wb, ALU.mult), reads=[PB[pvb], snb], writes=[tmpb])
                pool(_I("tensor_tensor", oacc[:, 4 * g:4 * g + 4, :], oacc[:, 4 * g:4 * g + 4, :], tmpo[:], ALU.add), reads=[tmpb, oab], writes=[oab])

        def flush_o(t, mix):
            oacc, oab = oaccs[t % 2], oabs[t % 2]
            dve(_I("tensor_copy", obf[:], oacc[:].rearrange("p h d -> p (h d)")), reads=[oab], writes=[obfb])
            for j in range(4):
                pe(_I("transpose", psb(3)[:, j * 128:(j + 1) * 128], obf[:, j * 128:(j + 1) * 128], ident_b[:]), reads=[obfb, bC], writes=[PB[3]])
            act(_I("activation", oTs[:].rearrange("p j c -> p (j c)"), psb(3)[:, 0:512], AF.Copy), reads=[PB[3]], writes=[oTsb])
            S.dma(oT_d[mix, :, :, t * 128:(t + 1) * 128].rearrange("j p c -> p j c"), oTs[:], reads=[oTsb], writes=[bOT[mix]])

        for s in range(nseq):
            b = s
            S.barrier()

            for c2 in range(8):
                for tl in range(2):
                    t = c2 * 2 + tl
                    i = t % 2
                    S.dma(xt[i][:], x_d[s, t * 128:(t + 1) * 128, :], writes=[xtb[i]])
                    layernorm(xt[i][:], xtb[i], tl, 0, b, 0)
                ln_evac(c2, 0, b, 0, hTb[c2 // 2])

            if dbg and s == 0:
                S.dma(hT_dbg, hT[:], reads=hTb, writes=[Buf()], is_output=True)
            U.reset()
            V_all = U.take(NT * 5 * 65).rearrange("p (t k c) -> p t k c", t=NT, k=5)
            bV = Buf()
            pool(_I("memset", V_all[:, :, :, 64:65], 1.0), writes=[bV])
            vstate["V"] = V_all
            vstate["bV"] = bV
            Wn = U.take(8 * 1304).rearrange("p (k n) -> p k n", k=8)
            QT = U.take(8 * S_TOK).rearrange("p (h n) -> p h n", h=8)
            KT = U.take(5 * S_TOK).rearrange("p (h n) -> p h n", h=5)
            q_aug = U.take(8 * 68).rearrange("p (h d) -> p h d", h=8)
            k_aug = U.take(4 * 68).rearrange("p (h d) -> p h d", h=4)
            kcT = U.take(S_TOK)
            vcT = U.take(S_TOK)
            MT = U.take(2 * S_TOK).rearrange("p (g n) -> p g n", g=2)
            sg = U.take(NT * 24, F32).rearrange("p (t c) -> p t c", c=24)
            bq_aug, bk_aug, bkc, bsg = Buf(), Buf(), Buf(), Buf()
            bWn = [Buf() for _ in range(8)]
            QTb = [Buf() for _ in range(8)]
            KTb = [Buf() for _ in range(5)]
            MTb = [Buf(), Buf()]
            pool(_I("memset", MT[32:64, :, :], 0.0), writes=MTb)
            pool(_I("memset", MT[64:68, :, :], 0.0), writes=MTb)
            for k in range(8):
                load_w(Wn[:, k, :], win_v[:, k, 0:1304], bWn[k])
            for c in range(4):
                for tl in range(4):
                    t = c * 4 + tl
                    ts = slice(t * 128, (t + 1) * 128)
                    pq, pk, pg = (0, 1, 2) if t % 2 == 0 else (4, 5, 6)
                    for k in range(8):
                        pe(_I("matmul", PS[pq][:, :], lhsT=hT[:, k, ts], rhs=Wn[:, k, 0:512], start=(k == 0), stop=(k == 7)), reads=[hTb[c], bWn[k]], writes=[PB[pq]])
                    for k in range(8):
                        pe(_I("matmul", PS[pk][:, :], lhsT=hT[:, k, ts], rhs=Wn[:, k, 768:1280], start=(k == 0), stop=(k == 7)), reads=[hTb[c], bWn[k]], writes=[PB[pk]])
                    for k in range(8):
                        pe(_I("matmul", PS[pg][:, 0:24], lhsT=hT[:, k, ts], rhs=Wn[:, k, 1280:1304], start=(k == 0), stop=(k == 7)), reads=[hTb[c], bWn[k]], writes=[PB[pg]])
                    rms_heads(pq, 8, st16[:, 0:8])
                    rms_heads(pk, 8, st16[:, 8:16])
                    rstd_from(st16[:, 0:16])
                    dve(_I("tensor_tensor", q_aug[:, :, 0:64], PS[pq][:, :].rearrange("p (h d) -> p h d", d=64), st16[:, 0:8].unsqueeze(2).to_broadcast([128, 8, 64]), ALU.mult),
                        reads=[PB[pq], stb], writes=[bq_aug])
                    dve(_I("tensor_copy", q_aug[:, :, 64:68], aqc[:, t, :, :]), reads=[bC], writes=[bq_aug])
                    for (c0, s0, kk, gi) in ((0, 8, 0, 0), (256, 12, 2, 1)):
                        dve(_I("tensor_tensor", sq[0][:, 0:128].rearrange("p (h d) -> p h d", d=64), PS[pk][:, c0:c0 + 128].rearrange("p (h d) -> p h d", d=64),
                                                                   st16[:, s0:s0 + 2].unsqueeze(2).to_broadcast([128, 2, 64]), ALU.mult), reads=[PB[pk], stb], writes=[sqb[0]])
                        dve(_I("tensor_tensor", k_aug[:, kk:kk + 2, 0:64], sq[0][:, 0:128].rearrange("p (h d) -> p h d", d=64),
                                                                   gains[:, gi, :].unsqueeze(1).to_broadcast([128, 2, 64]), ALU.mult), reads=[sqb[0], bC], writes=[bk_aug])
                    dve(_I("tensor_copy", k_aug[:, :, 64:68], akc[:, t, :].unsqueeze(1).to_broadcast([128, 4, 4])), reads=[bC], writes=[bk_aug])
                    act(_I("activation", V_all[:, t, 0:2, 0:64], PS[pk][:, 128:256].rearrange("p (h d) -> p h d", d=64), AF.Copy), reads=[PB[pk]], writes=[bV])
                    act(_I("activation", V_all[:, t, 2:4, 0:64], PS[pk][:, 384:512].rearrange("p (h d) -> p h d", d=64), AF.Copy), reads=[PB[pk]], writes=[bV])
                    act(_I("activation", sg[:, t, :], PS[pg][:, 0:24], AF.Sigmoid), reads=[PB[pg]], writes=[bsg])
                    for h in range(8):
                        pe(_I("transpose", psb(3)[0:68, h * 128:(h + 1) * 128], q_aug[:, h, :], ident_b[:]), reads=[bq_aug, bC], writes=[PB[3]])
                    for kk in range(4):
                        pe(_I("transpose", psb(7)[0:68, kk * 128:(kk + 1) * 128], k_aug[:, kk, :], ident_b[:]), reads=[bk_aug, bC], writes=[PB[7]])
                    act(_I("activation", QT[0:68, :, ts], psb(3)[0:68, :].rearrange("p (h c) -> p h c", h=8), AF.Copy), reads=[PB[3]], writes=QTb)
                    dve(_I("tensor_copy", KT[0:68, 0:4, ts], psb(7)[0:68, 0:512].rearrange("p (h c) -> p h c", h=4)), reads=[PB[7]], writes=KTb[0:4])
                cs = slice(c * 512, (c + 1) * 512)
                for (c0, dst) in ((512, kcT), (640, vcT)):
                    for k in range(8):
                        pe(_I("matmul", PS[0][:, :], lhsT=Wn[:, k, c0:c0 + 128], rhs=hT[:, k, cs], start=(k == 0), stop=(k == 7)), reads=[hTb[c], bWn[k]], writes=[PB[0]])
                    act(_I("activation", dst[:, cs], PS[0][:, :], AF.Copy), reads=[PB[0]], writes=[bkc])

            S.barrier()
            W1 = Wn.rearrange("p k n -> p (k n)")[:, 0:2 * 32 * 128].rearrange("p (a l n) -> p a l n", a=2, l=32)
            W2 = U.take(2 * 64).rearrange("p (a n) -> p a n", a=2)
            peT = U.take(64)
            HT = U.take(128)
            bW1, bH = Buf(), Buf()
            for a, (w1d, w2d) in enumerate(((wck1_d, wck2_d), (wcv1_d, wcv2_d))):
                for half in range(2):
                    load_w(W1[half * 64:half * 64 + 64, a, :, :], w1d.rearrange("(l d) n -> d l n", d=64), bW1)
                load_w(W2[:, a, :], w2d, bW1)
            load_w(peT[0:64, :], peT_d, bW1)
            if s == 0:
                for a in range(2):
                    for l in range(32):
                        pe(_I("matmul", PS[2][:, a:a + 1], lhsT=W1[0:64, a, l, :], rhs=peT[0:64, a * 32 + l:a * 32 + l + 1], start=(l == 0), stop=(l == 31)),
                           reads=[bW1], writes=[PB[2]])
                dve(_I("tensor_copy", cb2[:], PS[2][:, 0:2]), reads=[PB[2]], writes=[bC])
            for a, srcT in enumerate((kcT, vcT)):
                for g in range(2):
                    base = g * 64
                    v3 = srcT[base:base + 64, :].rearrange("p (n s) -> p n s", s=16)
                    for l in range(32):
                        rhs = v3[:, (l // 16):(l // 16) + 127, l % 16]
                        pe(_I("matmul", PS[0][:, 0:127], lhsT=W1[base:base + 64, a, l, :], rhs=rhs, start=(l == 0), stop=(l == 31)),
                           reads=[bW1, bkc], writes=[PB[0]])
                    act(_I("activation", HT[:, 0:127], PS[0][:, 0:127], AF.Silu, bias=cb2[:, a:a + 1]), reads=[PB[0], bC], writes=[bH])
                    pe(_I("matmul", PS[1][0:127, 0:64], lhsT=HT[:, 0:127], rhs=W2[:, a, :], start=True, stop=True), reads=[bH, bW1], writes=[PB[1]])
                    if a == 0:
                        act(_I("activation", sq[0][0:127, 0:64], PS[1][0:127, 0:64], AF.Square, accum_out=st16[0:127, 0:1]), reads=[PB[1]], writes=[sqb[0], stb])
                        rstd_from(st16[0:127, 0:1])
                        dve(_I("tensor_scalar", sq[0][0:127, 0:64], PS[1][0:127, 0:64], st16[0:127, 0:1], None, ALU.mult), reads=[PB[1], stb], writes=[sqb[0]])
                        dve(_I("tensor_tensor", kc_aug[0:127, g, 0:64], sq[0][0:127, 0:64], gains[0:127, 2, :], ALU.mult), reads=[sqb[0], bC], writes=[bC])
                        pe(_I("transpose", psb(3)[0:68, 0:127], kc_aug[0:127, g, :], ident_b[0:127, 0:127]), reads=[bC], writes=[PB[3]])
                        dve(_I("tensor_copy", KcT[0:68, g, 0:127], psb(3)[0:68, 0:127]), reads=[PB[3]], writes=[bC])
                    else:
                        act(_I("activation", Vc[0:127, g, 0:64], PS[1][0:127, 0:64], AF.Copy), reads=[PB[1]], writes=[bC])

            imp = sb("imp_%d" % s, [128, 4, 32], F32) if s == 0 else imp
            impb = Buf()
            MTt = [[Buf() for _ in range(NT)] for _ in range(2)]

            def cmp_items(t):
                qs = slice(t * 128, (t + 1) * 128)
                oacc, oab = oaccs[t % 2], oabs[t % 2]
                items = []
                for g in range(2):
                    o4 = PS[7][:, 0:388].rearrange("p (h c) -> p h c", h=4)

                    def s1(g=g):
                        for hh in range(4):
                            h = 4 * g + hh
                            pe(_I("matmul", PS[4][0:127, hh * 128:(hh + 1) * 128], lhsT=KcT[0:68, g, 0:127], rhs=QT[0:68, h, qs], start=True, stop=True),
                               reads=[bC, QTb[h]], writes=[PB[4]])
                        dve(_I("tensor_scalar", sq[1][0:127, :], PS[4][0:127, :], 60.0, None, ALU.min), reads=[PB[4]], writes=[sqb[1]])
                        act(_I("activation", PT[5][0:127, :], sq[1][0:127, :], AF.Exp), reads=[sqb[1]], writes=[PTb[5]])
                        dve(_I("tensor_tensor", PT[5][0:127, :].rearrange("p (h c) -> p h c", h=4), PT[5][0:127, :].rearrange("p (h c) -> p h c", h=4),
                               cmask[0:127, qs].unsqueeze(1).to_broadcast([127, 4, 128]), ALU.mult), reads=[bC, PTb[5]], writes=[PTb[5]])

                    def s2(g=g, o4=o4):
                        for hh in range(4):
                            pe(_I("matmul", PS[7][:, hh * 97:(hh + 1) * 97], lhsT=PT[5][0:127, hh * 128:(hh + 1) * 128], rhs=Vc[0:127, g, :], start=True, stop=True),
                               reads=[PTb[5], bC], writes=[PB[7]])
                        dve(_I("tensor_scalar", sm[:, 0:4], o4[:, :, 64], 1e-30, None, ALU.max), reads=[PB[7]], writes=[smb])
                        dve(_I("reciprocal", sm[:, 4:8], sm[:, 0:4]), reads=[smb], writes=[smb])
                        dve(_I("tensor_tensor", sm[:, 8:12], sm[:, 4:8], sg[:, t, :].rearrange("p (h r) -> p h r", r=3)[:, 4 * g:4 * g + 4, 0], ALU.mult), reads=[smb, bsg], writes=[smb])
                        dve(_I("tensor_tensor", oacc[:, 4 * g:4 * g + 4, :], o4[:, :, 0:64], sm[:, 8:12].unsqueeze(2).to_broadcast([128, 4, 64]), ALU.mult),
                            reads=[PB[7], smb], writes=[oab])
                        dve(_I("tensor_tensor", imp[:], o4[:, :, 65:97], sm[:, 4:8].unsqueeze(2).to_broadcast([128, 4, 32]), ALU.mult), reads=[PB[7], smb], writes=[impb])
                        dve(_I("tensor_reduce", sm[:, 16:48], imp[:].rearrange("p h j -> p j h"), AX.X, ALU.add), reads=[impb], writes=[smb])
                        dve(_I("tensor_tensor", sm[:, 16:48], sm[:, 16:48], selA[:, t, :], ALU.mult), reads=[smb, bC], writes=[smb])
                        dve(_I("tensor_tensor", sm[:, 16:48], sm[:, 16:48], selB[:, t, :], ALU.add), reads=[smb, bC], writes=[smb])
                        dve(_I("max", out=sm[:, 48:56], in_=sm[:, 16:48]), reads=[smb], writes=[smb])
                        dve(_I("match_replace", out=imp[:, 0, :], in_to_replace=sm[:, 48:56], in_values=sm[:, 16:48], imm_value=-3e38), reads=[smb], writes=[impb])
                        dve(_I("max", out=sm[:, 56:64], in_=imp[:, 0, :]), reads=[impb], writes=[smb])
                        dve(_I("tensor_scalar", sm[:, 16:48], sm[:, 16:48], sm[:, 63:64], None, ALU.is_ge), reads=[smb], writes=[smb])
                        dve(_I("tensor_scalar", mneg[:], sm[:, 16:48], -1.0, -NEG, ALU.add, ALU.mult), reads=[smb], writes=[mnegb])

                    def s3(g=g):
                        pe(_I("transpose", psb(3)[0:32, 0:128], mneg[:], ident_b[:]), reads=[mnegb, bC], writes=[PB[3]])
                        act(_I("activation", MT[0:32, g, qs], psb(3)[0:32, 0:128], AF.Copy), reads=[PB[3]], writes=[MTt[g][t]])
                    items += [s1, s2, s3]
                return items

            for item in cmp_items(0):
                item()
            for t in range(NT):
                sg3 = sg[:, t, :].rearrange("p (h r) -> p h r", r=3)
                par = t % 2
                units = []
                for g in range(2):
                    pvb = next_pv()
                    tiles = [(j, "C" if j == t else None) for j in range(t + 1)]
                    for hh in range(4):
                        h = 4 * g + hh
                        units += make_units(QT[:, h, :], QTb[h], KT[:, g, :], KTb[g], g, t, tiles, (MT[:, g, :], MTt[g][t]), pvb, hh, banks=(4, 5, 0, 1))
                    units[-1]["post"] = (lambda pvb=pvb, g=g, gv=sg3[:, 4 * g:4 * g + 4, 1], par=par: norm4(pvb, g, gv, [bsg], False, par))
                    pvb = next_pv()
                    tiles = [(j, "C" if j == t else ("W" if j == t - 4 else None)) for j in range(max(0, t - 4), t + 1)]
                    for hh in range(4):
                        h = 4 * g + hh
                        units += make_units(QT[:, h, :], QTb[h], KT[:, 2 + g, :], KTb[2 + g], 2 + g, t, tiles, None, pvb, hh, banks=(4, 5, 0, 1))
                    units[-1]["post"] = (lambda pvb=pvb, g=g, gv=sg3[:, 4 * g:4 * g + 4, 2], par=par: norm4(pvb, g, gv, [bsg], False, par))
                W = cmp_items(t + 1) if t + 1 < NT else []
                done = [0]

                def between(i, n, W=W, done=done):
                    target = min(len(W), ((i + 1) * (len(W) + 1)) // n)
                    while done[0] < target:
                        W[done[0]]()
                        done[0] += 1
                run_units(units, 3, between)
                while done[0] < len(W):
                    W[done[0]]()
                    done[0] += 1
                flush_o(t, 0)

            S.barrier()
            U.reset()
            V_all = U.take(NT * 5 * 65).rearrange("p (t k c) -> p t k c", t=NT, k=5)
            bV = Buf()
            pool(_I("memset", V_all[:, :, :, 64:65], 1.0), writes=[bV])
            vstate["V"] = V_all
            vstate["bV"] = bV
            Wd = U.take(8 * 1352).rearrange("p (k n) -> p k n", k=8)
            QT = U.take(8 * S_TOK).rearrange("p (h n) -> p h n", h=8)
            KT = U.take(5 * S_TOK).rearrange("p (h n) -> p h n", h=5)
            q_aug = U.take(8 * 68).rearrange("p (h d) -> p h d", h=8)
            k_aug = U.take(4 * 68).rearrange("p (h d) -> p h d", h=4)
            iqT = U.take(4 * S_TOK).rearrange("p (m n) -> p m n", m=4)
            ikT = U.take(S_TOK)
            iw = U.take(NT * 8, F32).rearrange("p (t c) -> p t c", c=8)
            sc = U.take(S_TOK, F32)
            rl = U.take(512, F32)
            maskq = U.take(S_TOK)
            maskT = U.take(S_TOK).rearrange("p (j c) -> p j c", c=128)
            junk = U.take(S_TOK)
            biq, bik, biw, bsc, brl, bmq, bmT, bjk = (Buf() for _ in range(8))
            bWd = [Buf() for _ in range(8)]
            QTb = [Buf() for _ in range(8)]
            KTb = [Buf() for _ in range(5)]
            for k in range(8):
                load_w(Wd[:, k, 0:1224], win_v[:, k, 1304:2528], bWd[k])
                load_w(Wd[:, k, 1224:1288], win_v[:, k, 2456:2520], bWd[k])
                load_w(Wd[:, k, 1288:1352], win_v[:, k, 2456:2520], bWd[k])
            for c in range(4):
                cs = slice(c * 512, (c + 1) * 512)
                for tl in range(4):
                    t = c * 4 + tl
                    ts = slice(t * 128, (t + 1) * 128)
                    pq, pk, pg = (0, 1, 2) if t % 2 == 0 else (4, 5, 6)
                    for k in range(8):
                        pe(_I("matmul", PS[pq][:, :], lhsT=hT[:, k, ts], rhs=Wd[:, k, 0:512], start=(k == 0), stop=(k == 7)), reads=[hTb[c], bWd[k]], writes=[PB[pq]])
                    for k in range(8):
                        pe(_I("matmul", PS[pk][:, 0:128], lhsT=hT[:, k, ts], rhs=Wd[:, k, 512:640], start=(k == 0), stop=(k == 7)), reads=[hTb[c], bWd[k]], writes=[PB[pk]])
                    for k in range(8):
                        pe(_I("matmul", PS[pg][:, 0:8], lhsT=hT[:, k, ts], rhs=Wd[:, k, 1216:1224], start=(k == 0), stop=(k == 7)), reads=[hTb[c], bWd[k]], writes=[PB[pg]])
                    rms_heads(pq, 8, st16[:, 0:8])
                    rms_heads(pk, 1, st16[:, 8:9])
                    rstd_from(st16[:, 0:9])
                    dve(_I("tensor_tensor", q_aug[:, :, 0:64], PS[pq][:, :].rearrange("p (h d) -> p h d", d=64), st16[:, 0:8].unsqueeze(2).to_broadcast([128, 8, 64]), ALU.mult),
                        reads=[PB[pq], stb], writes=[bq_aug])
                    dve(_I("tensor_copy", q_aug[:, :, 64:68], aqc[:, t, :, :]), reads=[bC], writes=[bq_aug])
                    dve(_I("tensor_scalar", sq[0][:, 0:64], PS[pk][:, 0:64], st16[:, 8:9], None, ALU.mult), reads=[PB[pk], stb], writes=[sqb[0]])
                    dve(_I("tensor_tensor", k_aug[:, 0, 0:64], sq[0][:, 0:64], gains[:, 3, :], ALU.mult), reads=[sqb[0], bC], writes=[bk_aug])
                    dve(_I("tensor_copy", k_aug[:, 0, 64:68], akc[:, t, :]), reads=[bC], writes=[bk_aug])
                    act(_I("activation", V_all[:, t, 4, 0:64], PS[pk][:, 64:128], AF.Copy), reads=[PB[pk]], writes=[bV])
                    act(_I("activation", iw[:, t, :], PS[pg][:, 0:8], AF.Copy, scale=8.0 ** -0.5), reads=[PB[pg]], writes=[biw])
                    for h in range(8):
                        pe(_I("transpose", psb(3)[0:68, h * 128:(h + 1) * 128], q_aug[:, h, :], ident_b[:]), reads=[bq_aug, bC], writes=[PB[3]])
                    pe(_I("transpose", psb(7)[0:68, 0:128], k_aug[:, 0, :], ident_b[:]), reads=[bk_aug, bC], writes=[PB[7]])
                    act(_I("activation", QT[0:68, :, ts], psb(3)[0:68, :].rearrange("p (h c) -> p h c", h=8), AF.Copy), reads=[PB[3]], writes=QTb)
                    dve(_I("tensor_copy", KT[0:68, 4, ts], psb(7)[0:68, 0:128]), reads=[PB[7]], writes=[KTb[4]])
                for m in range(4):
                    for k in range(8):
                        pe(_I("matmul", PS[0][:, :], lhsT=Wd[:, k, 640 + m * 128:640 + (m + 1) * 128], rhs=hT[:, k, cs], start=(k == 0), stop=(k == 7)), reads=[hTb[c], bWd[k]], writes=[PB[0]])
                    act(_I("activation", iqT[:, m, cs], PS[0][:, :], AF.Copy, scale=0.125), reads=[PB[0]], writes=[biq])
                for k in range(8):
                    pe(_I("matmul", PS[1][:, :], lhsT=Wd[:, k, 1224:1352], rhs=hT[:, k, cs], start=(k == 0), stop=(k == 7)), reads=[hTb[c], bWd[k]], writes=[PB[1]])
                act(_I("activation", ikT[:, cs], PS[1][:, :], AF.Copy), reads=[PB[1]], writes=[bik])

            S.barrier()
            Wd_flat = Wd.rearrange("p k n -> p (k n)")
            scs = [sc, Wd_flat[:, 0:4096].bitcast(F32)]
            maskqs = [maskq, Wd_flat[:, 4096:6144]]
            maskTs = [maskT, Wd_flat[:, 6144:8192].rearrange("p (j c) -> p j c", c=128)]
            bscs, bmqs, bmTs = [Buf(), Buf()], [Buf(), Buf()], [Buf(), Buf()]
            smx = [sb("smx%d_%d" % (s, i), [128, 40], F32) for i in range(2)] if s == 0 else smx
            smxb = [Buf(), Buf()]

            rls = [rl, Wd_flat[:, 8192:9216].bitcast(F32)]
            brls = [brl, Buf()]

            def indexer_work(t):
                p = t % 2
                sc_, bsc_, sm_, smb_ = scs[p], bscs[p], smx[p], smxb[p]
                qs = slice(t * 128, (t + 1) * 128)
                nk = (t + 1) * 128
                nch = (nk + 511) // 512
                items = []
                idx = 0
                for h in range(8):
                    base = (h % 2) * 64
                    for cc in range(nch):
                        w = min(512, nk - cc * 512)
                        cs = slice(cc * 512, cc * 512 + w)

                        def piece(h=h, base=base, w=w, cs=cs, k=idx):
                            bk = k % 2
                            rl_, brl_ = rls[k % 2], brls[k % 2]
                            pe(_I("matmul", PS[bk][:, 0:w], lhsT=iqT[base:base + 64, h // 2, qs], rhs=ikT[base:base + 64, cs], start=True, stop=True),
                               reads=[biq, bik], writes=[PB[bk]])
                            act(_I("activation", rl_[:, 0:w], PS[bk][:, 0:w], AF.Relu), reads=[PB[bk]], writes=[brl_])
                            if h == 0:
                                dve(_I("tensor_scalar", sc_[:, cs], rl_[:, 0:w], iw[:, t, 0:1], None, ALU.mult), reads=[brl_, biw], writes=[bsc_])
                            else:
                                dve(_I("scalar_tensor_tensor", sc_[:, cs], rl_[:, 0:w], iw[:, t, h:h + 1], sc_[:, cs], ALU.mult, ALU.add), reads=[brl_, biw, bsc_], writes=[bsc_])
                        items.append(piece)
                        idx += 1

                def post():
                    dve(_I("tensor_reduce", sm_[:, 0:1], sc_[:, 0:nk], AX.X, ALU.max, apply_absolute_value=True), reads=[bsc_], writes=[smb_])
                    pool(_I("affine_select", sc_[:, t * 128:(t + 1) * 128], sc_[:, t * 128:(t + 1) * 128], [[-1, 128]], ALU.is_ge, -3e38, base=0, channel_multiplier=1), reads=[bsc_, smb_], writes=[bsc_])
                    dve(_I("tensor_scalar", sm_[:, 8:8 + NBIS + 1], pow2[:], sm_[:, 0:1], None, ALU.mult), reads=[smb_, bC], writes=[smb_])
                    dve(_I("memset", sm_[:, 1:2], 0.0), reads=[smb_], writes=[smb_])
                items.append(post)
                return items

            def tile_work(t):
                return indexer_work(t) + [(lambda j=j: bisect_iter(t, j)) for j in range(NBIS)] + [lambda: finish_mask(t)]

            def bisect_iter(t, j):
                p = t % 2
                sc_, bsc_, sm_, smb_ = scs[p], bscs[p], smx[p], smxb[p]
                nk = (t + 1) * 128
                dve(_I("tensor_scalar", maskqs[p][:, 0:nk], sc_[:, 0:nk], sm_[:, 1:2], None, ALU.is_ge, ALU.add, accum_out=sm_[:, 2:3]), reads=[bsc_, smb_], writes=[bmqs[p], smb_])
                dve(_I("tensor_scalar", sm_[:, 3:4], sm_[:, 2:3], 255.5, -0.5, ALU.is_ge, ALU.add), reads=[smb_], writes=[smb_])
                dve(_I("scalar_tensor_tensor", sm_[:, 1:2], sm_[:, 3:4], sm_[:, 8 + j:9 + j], sm_[:, 1:2], ALU.mult, ALU.add), reads=[smb_], writes=[smb_])

            def finish_mask(t):
                p = t % 2
                sc_, bsc_, sm_, smb_ = scs[p], bscs[p], smx[p], smxb[p]
                nk = (t + 1) * 128
                dve(_I("tensor_tensor", sm_[:, 1:2], sm_[:, 1:2], sm_[:, 8 + NBIS:9 + NBIS], ALU.subtract), reads=[smb_], writes=[smb_])
                dve(_I("tensor_scalar", maskqs[p][:, 0:nk], sc_[:, 0:nk], sm_[:, 1:2], None, ALU.is_ge), reads=[bsc_, smb_], writes=[bmqs[p]])
                for j in range(t + 1):
                    bk = 3 if (j // 8) % 2 == 0 else 7
                    pe(_I("transpose", psb(bk)[:, (j % 8) * 128:(j % 8 + 1) * 128], maskqs[p][:, j * 128:(j + 1) * 128], ident_b[:]), reads=[bmqs[p], bC], writes=[PB[bk]])
                    if j % 8 == 7 or j == t:
                        j0 = (j // 8) * 8
                        n = j - j0 + 1
                        act(_I("activation", maskTs[p][:, j0:j0 + n, :].rearrange("p j c -> p (j c)"), psb(bk)[:, 0:n * 128], AF.Copy), reads=[PB[bk]], writes=[bmTs[p]])

            for item in tile_work(0):
                item()
            for t in range(NT):
                tiles = [(j, None) for j in range(t + 1)]
                units = []
                for g2 in range(2):
                    pvb = next_pv()
                    for hh in range(4):
                        h = 4 * g2 + hh
                        units += make_units(QT[:, h, :], QTb[h], KT[:, 4, :], KTb[4], 4, t, tiles, None, pvb, hh, full_mask=(maskTs[t % 2], bmTs[t % 2]))
                    units[-1]["post"] = (lambda pvb=pvb, g2=g2, par=t % 2: norm4(pvb, g2, None, [], True, par))
                W = tile_work(t + 1) if t + 1 < NT else []
                done = [0]

                def between(i, n, W=W, done=done):
                    target = min(len(W), ((i + 1) * len(W) * 10) // (n * 9) + 1)
                    while done[0] < target:
                        W[done[0]]()
                        done[0] += 1
                run_units(units, 1, between)
                while done[0] < len(W):
                    W[done[0]]()
                    done[0] += 1
                flush_o(t, 1)

            for hf in range(2):
                S.barrier()
                U.reset()
                Wg = U.take(8 * 2048).rearrange("p (k n) -> p k n", k=8)
                Woa = U.take(4 * D).rearrange("p (k n) -> p k n", k=4)
                Wob = U.take(4 * D).rearrange("p (k n) -> p k n", k=4)
                Wout = U.take(8 * D).rearrange("p (k n) -> p k n", k=8)
                Wff_region = (Wg, Woa, Wob, Wout)
                xacc = U.take(8 * D, F32).rearrange("p (t n) -> p t n", t=8)
                yT = U.take(8 * 512).rearrange("p (k n) -> p k n", k=8)
                oaT = U.take(4 * 512).rearrange("p (k n) -> p k n", k=4)
                obT = U.take(4 * 512).rearrange("p (k n) -> p k n", k=4)
                sga = U.take(512)
                sgb = U.take(512)
                t1 = U.take(512, F32)
                t2 = U.take(512, F32)
                aT = yT
                g1bc = U.take(D, F32)
                g2bc = U.take(D, F32)
                bG = Buf()
                S.dma(g1bc, modrow_d[b:b + 1, 2 * D:3 * D].partition_broadcast(128), writes=[bG])
                S.dma(g2bc, modrow_d[b:b + 1, 5 * D:6 * D].partition_broadcast(128), writes=[bG])
                bya, byT, boa, bob, bsga, bsgb, bt1, bt2, baT = (Buf() for _ in range(9))
                bWg = [Buf() for _ in range(8)]
                bWo = [Buf() for _ in range(8)]
                bWa = [Buf() for _ in range(4)]
                bWb = [Buf() for _ in range(4)]
                xab = [Buf() for _ in range(8)]
                for k in range(8):
                    load_w(Wg[:, k, :], win_v[:, k, 2528:4576], bWg[k])
                    load_w(Wout[:, k, :], wout_d.rearrange("(k p) n -> p k n", p=128)[:, k, :], bWo[k])
                for k in range(4):
                    load_w(Woa[:, k, :], woa_d.rearrange("(k p) n -> p k n", p=128)[:, k, :], bWa[k])
                    load_w(Wob[:, k, :], wob_d.rearrange("(k p) n -> p k n", p=128)[:, k, :], bWb[k])
                for cl in range(2):
                    c = hf * 2 + cl
                    cs = slice(c * 512, (c + 1) * 512)
                    S.dma(oaT, oT_d[0, :, :, cs].rearrange("j p c -> p j c"), reads=[bOT[0]], writes=[boa])
                    S.dma(obT, oT_d[1, :, :, cs].rearrange("j p c -> p j c"), reads=[bOT[1]], writes=[bob])
                    for f in range(8):
                        fs = slice(f * 128, (f + 1) * 128)
                        for k in range(8):
                            pe(_I("matmul", PS[0][:, :], lhsT=Wg[:, k, fs], rhs=hT[:, k, cs], start=(k == 0), stop=(k == 7)), reads=[bWg[k], hTb[c]], writes=[PB[0]])
                        act(_I("activation", sga, PS[0][:, :], AF.Sigmoid), reads=[PB[0]], writes=[bsga])
                        for k in range(8):
                            pe(_I("matmul", PS[1][:, :], lhsT=Wg[:, k, 1024 + f * 128:1024 + (f + 1) * 128], rhs=hT[:, k, cs], start=(k == 0), stop=(k == 7)), reads=[bWg[k], hTb[c]], writes=[PB[1]])
                        act(_I("activation", sgb, PS[1][:, :], AF.Sigmoid), reads=[PB[1]], writes=[bsgb])
                        for k in range(4):
                            pe(_I("matmul", PS[2][:, :], lhsT=Woa[:, k, fs], rhs=oaT[:, k, :], start=(k == 0), stop=(k == 3)), reads=[bWa[k], boa], writes=[PB[2]])
                        for k in range(4):
                            pe(_I("matmul", PS[4][:, :], lhsT=Wob[:, k, fs], rhs=obT[:, k, :], start=(k == 0), stop=(k == 3)), reads=[bWb[k], bob], writes=[PB[4]])
                        dve(_I("tensor_tensor", t1, PS[2][:, :], sga, ALU.mult), reads=[PB[2], bsga], writes=[bt1])
                        dve(_I("tensor_tensor", t2, PS[4][:, :], sgb, ALU.mult), reads=[PB[4], bsgb], writes=[bt2])
                        dve(_I("tensor_tensor", yT[:, f, :], t1, t2, ALU.add), reads=[bt1, bt2], writes=[byT])
                    for tl in range(4):
                        t = c * 4 + tl
                        tt = t - hf * 8
                        i = t % 2
                        S.dma(xt[i][:], x_d[s, t * 128:(t + 1) * 128, :], writes=[xtb[i]])
                        for h2 in range(2):
                            ns = slice(h2 * 512, (h2 + 1) * 512)
                            bk = 5 + h2
                            for k in range(8):
                                pe(_I("matmul", PS[bk][:, :], lhsT=yT[:, k, tl * 128:(tl + 1) * 128], rhs=Wout[:, k, ns], start=(k == 0), stop=(k == 7)), reads=[byT, bWo[k]], writes=[PB[bk]])
                            dve(_I("tensor_tensor", t1, PS[bk][:, :], g1bc[:, ns], ALU.mult), reads=[PB[bk], bG], writes=[bt1])
                            dve(_I("tensor_tensor", xacc[:, tt, ns], t1, xt[i][:, ns], ALU.add), reads=[bt1, xtb[i]], writes=[xab[tt]])
                        if dbg and s == 0:
                            S.dma(x1_dbg[t * 128:(t + 1) * 128, :], xacc[:, tt, :], reads=[xab[tt]], writes=[Buf()], is_output=True)
                        layernorm(xacc[:, tt, :], xab[tt], tl % 2, 1, b, 0)
                        if tl % 2 == 1:
                            ln_evac(t // 2, 1, b, 0, hTb[c])
                S.barrier()
                for (f0_, nf) in ((0, 8), (8, 8), (16, 6)):
                    Wfg = Wg.rearrange("p k n -> p (k n)")[:, 0:8 * 1024].rearrange("p (k n) -> p k n", k=8)
                    Wfu = Wg.rearrange("p k n -> p (k n)")[:, 8 * 1024:16 * 1024].rearrange("p (k n) -> p k n", k=8)
                    Wfd = Wout.rearrange("p k n -> p (k n)")[:, 0:8 * D].rearrange("p (k n) -> p k n", k=8)
                    nfc = nf * 128
                    if f0_ == 0:
                        bFg = [Buf() for _ in range(8)]
                        bFu = [Buf() for _ in range(8)]
                        bFd = [Buf() for _ in range(8)]
                    for k in range(8):
                        load_w(Wfg[:, k, 0:nfc], wfg_d.rearrange("(k p) n -> p k n", p=128)[:, k, f0_ * 128:f0_ * 128 + nfc], bFg[k])
                        load_w(Wfu[:, k, 0:nfc], wfu_d.rearrange("(k p) n -> p k n", p=128)[:, k, f0_ * 128:f0_ * 128 + nfc], bFu[k])
                    for k in range(nf):
                        load_w(Wfd[:, k, :], wfd_d[(f0_ + k) * 128:(f0_ + k + 1) * 128, :], bFd[k])
                    for cl in range(2):
                        c = hf * 2 + cl
                        cs = slice(c * 512, (c + 1) * 512)
                        for f in range(nf):
                            fs = slice(f * 128, (f + 1) * 128)
                            for k in range(8):
                                pe(_I("matmul", PS[0][:, :], lhsT=Wfg[:, k, fs], rhs=hT[:, k, cs], start=(k == 0), stop=(k == 7)), reads=[bFg[k], hTb[c]], writes=[PB[0]])
                            for k in range(8):
                                pe(_I("matmul", PS[1][:, :], lhsT=Wfu[:, k, fs], rhs=hT[:, k, cs], start=(k == 0), stop=(k == 7)), reads=[bFu[k], hTb[c]], writes=[PB[1]])
                            act(_I("activation", t1, PS[0][:, :], AF.Silu), reads=[PB[0]], writes=[bt1])
                            dve(_I("tensor_tensor", aT[:, f, :], t1, PS[1][:, :], ALU.mult), reads=[bt1, PB[1]], writes=[baT])
                        for tl in range(4):
                            tt = cl * 4 + tl
                            for h2 in range(2):
                                ns = slice(h2 * 512, (h2 + 1) * 512)
                                bk = 5 + h2
                                for f in range(nf):
                                    pe(_I("matmul", PS[bk][:, :], lhsT=aT[:, f, tl * 128:(tl + 1) * 128], rhs=Wfd[:, f, ns], start=(f == 0), stop=(f == nf - 1)), reads=[baT, bFd[f]], writes=[PB[bk]])
                                dve(_I("tensor_tensor", t2, PS[bk][:, :], g2bc[:, ns], ALU.mult), reads=[PB[bk], bG], writes=[bt2])
                                dve(_I("tensor_tensor", xacc[:, tt, ns], xacc[:, tt, ns], t2, ALU.add), reads=[bt2, xab[tt]], writes=[xab[tt]])
                for tt in range(8):
                    t = hf * 8 + tt
                    S.dma(out_d[s, t * 128:(t + 1) * 128, :], xacc[:, tt, :], reads=[xab[tt]], writes=[Buf()], is_output=True)
        S.emit()
    return nc


def _prep_common(inp):
    f = lambda a: np.ascontiguousarray(np.asarray(a, dtype=np.float32))
    gn = np.concatenate([inp["g_norm1"][0].reshape(8, 128).T, inp["g_norm2"][0].reshape(8, 128).T], axis=1)
    gvec = np.concatenate([inp[k][0] for k in ("g_q_a", "g_kc_a", "g_ks_a", "g_kw_a", "g_q_b", "g_k_b")])[None, :]
    peT = np.concatenate([inp["pe_ck"][0].T, inp["pe_cv"][0].T], axis=1)
    return {
        "w_ada": f(inp["w_ada"][0]), "b_ada": f(inp["b_ada"]), "gn": f(gn), "w_in": f(inp["w_in"][0]),
        "gvec": f(gvec), "peT": f(peT), "w_ck1": f(inp["w_ck1"][0]), "w_ck2": f(inp["w_ck2"][0]),
        "w_cv1": f(inp["w_cv1"][0]), "w_cv2": f(inp["w_cv2"][0]), "w_o_a": f(inp["w_o_a"][0]),
        "w_o_b": f(inp["w_o_b"][0]), "w_out": f(inp["w_out"][0]), "w_ff_gate": f(inp["w_ff_gate"][0]),
        "w_ff_up": f(inp["w_ff_up"][0]), "w_ff_down": f(inp["w_ff_down"][0]),
    }


def _core_map(common, x, c, i, nseq):
    m = dict(common)
    m["x"] = np.ascontiguousarray(x[i * nseq:(i + 1) * nseq])
    cc = np.zeros((4, D), np.float32)
    cc[:nseq] = c[i * nseq:(i + 1) * nseq]
    m["cT"] = np.ascontiguousarray(cc.T.reshape(8, 128, 4).transpose(1, 0, 2))
    return m


def kernel(**inputs):
    x = np.asarray(inputs["x"], dtype=np.float32)
    c = np.asarray(inputs["c"], dtype=np.float32)
    n = 8
    nseq = x.shape[0] // n
    nc = build_nc(nseq)
    common = _prep_common(inputs)
    in_maps = [_core_map(common, x, c, i, nseq) for i in range(n)]
    res = run_bass_kernel_spmd(nc, in_maps, core_ids=list(range(n)))
    return np.concatenate([r["out"] for r in res.results], axis=0).astype(np.float32)
```

```python
import contextlib
import numpy as np
import concourse.bass as bass
import concourse.mybir as mybir
from concourse.bass_utils import run_bass_kernel_spmd

F32 = mybir.dt.float32
BF16 = mybir.dt.bfloat16
AF = mybir.ActivationFunctionType
ALU = mybir.AluOpType
AX = mybir.AxisListType

S_TOK = 2048
D = 1024
NT = 16
DIN = 4576
DFF = 2816
EPS = 1e-6
NBIS = 14
NEG = -30000.0


class Buf:
    __slots__ = ("name", "w", "r")

    def __init__(self, name=""):
        self.name = name
        self.w = None
        self.r = {}


class Sched:
    ENGS = ("pe", "act", "dve", "pool", "sp")
    NDMA = 24

    def __init__(self, nc):
        self.nc = nc
        self.streams = {e: [] for e in self.ENGS}
        self.count = {e: 0 for e in self.ENGS}
        self.waited = {e: {} for e in self.ENGS}
        self.dma_uses = [0] * self.NDMA
        self.dma_rr = 0
        self.out_events = []

    def _deps(self, eng, reads, writes):
        deps = {}

        def add(ev):
            if ev is None:
                return
            k, v = ev
            if deps.get(k, 0) < v:
                deps[k] = v
        for b in reads:
            add(b.w)
        for b in writes:
            if b.w is not None and b.w[0] != eng:
                add(b.w)
            for k, v in b.r.items():
                if k != eng:
                    add((k, v))
        waits = []
        for k, v in deps.items():
            if k == "pe" and eng == "pe":
                continue
            if self.waited[eng].get(k, 0) < v:
                self.waited[eng][k] = v
                waits.append((k, v))
        return waits

    def _commit(self, ev, reads, writes):
        k, v = ev
        for b in writes:
            b.w = ev
            b.r = {}
        for b in reads:
            if b.r.get(k, 0) < v:
                b.r[k] = v

    def op(self, eng, fn, reads=(), writes=()):
        waits = self._deps(eng, reads, writes)
        self.count[eng] += 1
        ev = (eng, self.count[eng])
        self.streams[eng].append((fn, waits, ev))
        self._commit(ev, reads, writes)
        return ev

    def dma(self, out, in_, reads=(), writes=(), q="sp", is_output=False, **kw):
        i = self.dma_rr
        self.dma_rr = (self.dma_rr + 1) % self.NDMA
        waits = self._deps(q, reads, writes)
        key = "dma%d" % i
        prev = self.dma_uses[i] * 16
        if prev and self.waited[q].get(key, 0) < prev:
            self.waited[q][key] = prev
            waits.append((key, prev))
        self.dma_uses[i] += 1
        ev = (key, self.dma_uses[i] * 16)
        fn = lambda e, out=out, in_=in_, kw=kw: e.dma_start(out=out, in_=in_, **kw)
        self.streams[q].append((fn, waits, ev))
        self._commit(ev, reads, writes)
        if is_output:
            self.out_events.append(ev)
        return ev

    def barrier(self):
        allv = [(e, self.count[e]) for e in self.ENGS if self.count[e]]
        allv += [("dma%d" % i, self.dma_uses[i] * 16) for i in range(self.NDMA) if self.dma_uses[i]]
        for e in self.ENGS:
            waits = []
            for k, v in allv:
                if k == e:
                    continue
                if self.waited[e].get(k, 0) < v:
                    self.waited[e][k] = v
                    waits.append((k, v))
            if waits:
                self.streams[e].append((None, waits, None))

    def emit(self):
        nc = self.nc
        with contextlib.ExitStack() as st:
            sems = {}
            for e in self.ENGS:
                sems[e] = st.enter_context(nc.semaphore("s_" + e))
            for i in range(self.NDMA):
                sems["dma%d" % i] = st.enter_context(nc.semaphore("s_dma%d" % i))
            final = {}
            for k, v in self.out_events:
                final[k] = max(final.get(k, 0), v)
            block = st.enter_context(nc.Block())

            def run(engname, e):
                for fn, waits, ev in self.streams[engname]:
                    for k, v in waits:
                        e.wait_ge(sems[k], v)
                    if fn is None:
                        continue
                    ins = fn(e)
                    k, v = ev
                    ins.then_inc(sems[k], 16 if k.startswith("dma") else 1)
                if engname == "sp":
                    for k, v in final.items():
                        e.wait_ge(sems[k], v)

            @block.tensor
            def _(e):
                run("pe", e)

            @block.scalar
            def _(e):
                run("act", e)

            @block.vector
            def _(e):
                run("dve", e)

            @block.gpsimd
            def _(e):
                run("pool", e)

            @block.sync
            def _(e):
                run("sp", e)


class Arena:
    def __init__(self, ap16):
        self.ap = ap16
        self.off = 0

    def reset(self):
        self.off = 0

    def take(self, ncols, dt=BF16):
        if dt == F32:
            self.off = (self.off + 1) // 2 * 2
            n16 = ncols * 2
        else:
            n16 = ncols
        assert self.off + n16 <= self.ap.shape[1], (self.off, n16, self.ap.shape)
        v = self.ap[:, self.off:self.off + n16]
        self.off += n16
        self.off = (self.off + 1) // 2 * 2
        return v.bitcast(F32) if dt == F32 else v


_REGS = {}


def _I(name, *args, **kw):
    if name == "affine_select":
        def thunk(e):
            a = list(args)
            key = (id(e), float(a[4]))
            if key not in _REGS:
                _REGS[key] = e.to_reg(float(a[4]))
            a[4] = _REGS[key]
            return e.affine_select(*a, **kw)
        return thunk
    return lambda e: getattr(e, name)(*args, **kw)


def build_nc(nseq=4, dbg=False):
    nc = bass.Bass("TRN2", target_bir_lowering=False)
    _REGS.clear()
    S = Sched(nc)

    def din(name, shape):
        return nc.dram_tensor(name, shape, F32, kind="ExternalInput").ap()
    x_d = din("x", [nseq, S_TOK, D])
    cT_d = din("cT", [128, 8, 4])
    wada_d = din("w_ada", [D, 6 * D])
    bada_d = din("b_ada", [1, 6 * D])
    gn_d = din("gn", [128, 16])
    win_d = din("w_in", [D, DIN])
    gv_d = din("gvec", [1, 6 * 64])
    peT_d = din("peT", [64, 64])
    wck1_d = din("w_ck1", [2048, 128])
    wck2_d = din("w_ck2", [128, 64])
    wcv1_d = din("w_cv1", [2048, 128])
    wcv2_d = din("w_cv2", [128, 64])
    woa_d = din("w_o_a", [512, D])
    wob_d = din("w_o_b", [512, D])
    wout_d = din("w_out", [D, D])
    wfg_d = din("w_ff_gate", [D, DFF])
    wfu_d = din("w_ff_up", [D, DFF])
    wfd_d = din("w_ff_down", [DFF, D])
    out_d = nc.dram_tensor("out", [nseq, S_TOK, D], F32, kind="ExternalOutput").ap()
    modrow_d = nc.dram_tensor("modrow", [4, 6 * D], F32, kind="Internal").ap()
    oT_d = nc.dram_tensor("oT_scr", [2, 4, 128, S_TOK], BF16, kind="ExternalOutput" if dbg else "Internal").ap()
    if dbg:
        hT_dbg = nc.dram_tensor("hT_dbg", [128, 8, S_TOK], BF16, kind="ExternalOutput").ap()
        x1_dbg = nc.dram_tensor("x1_dbg", [S_TOK, D], F32, kind="ExternalOutput").ap()
        mod_dbg = nc.dram_tensor("mod_dbg", [4, 6 * D], F32, kind="ExternalOutput").ap()

    st = contextlib.ExitStack()

    def sb(name, shape, dt=F32):
        return st.enter_context(nc.sbuf_tensor(name, shape, dt))

    with st:
        PS = [st.enter_context(nc.psum_tensor("ps%d" % i, [128, 512], F32)) for i in range(8)]
        PB = [Buf("ps%d" % i) for i in range(8)]

        def psb(i):
            return PS[i][:].bitcast(BF16)

        ident_b = sb("ident_b", [128, 128], BF16)
        ident_f = sb("ident_f", [128, 128], F32)
        Cm = sb("Cm", [128, 128], BF16)
        Wm = sb("Wm", [128, 128], BF16)
        cmask = sb("cmask", [128, S_TOK], BF16)
        ET = sb("ET", [68, S_TOK], BF16)
        selA = sb("selA", [128, NT, 32], F32)
        selB = sb("selB", [128, NT, 32], F32)
        aqc = sb("aqc", [128, NT, 8, 4], BF16)
        akc = sb("akc", [128, NT, 4], BF16)
        gbc = sb("gbc", [128, 6 * 64], F32)
        gains = sb("gains", [128, 4, 64], F32)
        gnc = sb("gnc", [128, 16], F32)
        modT = sb("modT", [128, 32, 4], F32)
        cb2 = sb("cb2", [128, 2], F32)
        pow2 = sb("pow2", [128, NBIS + 1], F32)
        hT = sb("hT", [128, 8, S_TOK], BF16)
        Vc = sb("Vc", [128, 2, 97], BF16)
        kc_aug = sb("kc_aug", [128, 2, 68], BF16)
        KcT = sb("KcT", [128, 2, 128], BF16)
        U_t = sb("U", [128, 65400], BF16)
        U = Arena(U_t[:])
        bC = Buf("consts")
        bOT = [Buf(), Buf()]
        bOut = Buf()

        hTb = [Buf("hT%d" % c) for c in range(4)]

        def pool(fn, reads=(), writes=()):
            return S.op("pool", fn, reads, writes)

        def dve(fn, reads=(), writes=()):
            return S.op("dve", fn, reads, writes)

        def act(fn, reads=(), writes=()):
            return S.op("act", fn, reads, writes)

        def pe(fn, reads=(), writes=()):
            return S.op("pe", fn, reads, writes)

        pool(_I("memset", ident_b[:], 1.0), writes=[bC])
        pool(_I("affine_select", ident_b[:], ident_b[:], [[1, 128]], ALU.is_equal, 0.0, base=0, channel_multiplier=-1), reads=[bC], writes=[bC])
        pool(_I("memset", ident_f[:], 1.0), writes=[bC])
        pool(_I("affine_select", ident_f[:], ident_f[:], [[1, 128]], ALU.is_equal, 0.0, base=0, channel_multiplier=-1), reads=[bC], writes=[bC])
        pool(_I("memset", Cm[:], 1.0), writes=[bC])
        pool(_I("affine_select", Cm[:], Cm[:], [[1, 128]], ALU.is_ge, 0.0, base=0, channel_multiplier=-1), reads=[bC], writes=[bC])
        pool(_I("memset", Wm[:], 1.0), writes=[bC])
        pool(_I("affine_select", Wm[:], Wm[:], [[-1, 128]], ALU.is_gt, 0.0, base=0, channel_multiplier=1), reads=[bC], writes=[bC])
        pool(_I("memset", cmask[:], 1.0), writes=[bC])
        pool(_I("affine_select", cmask[:], cmask[:], [[1, S_TOK]], ALU.is_ge, 0.0, base=-31, channel_multiplier=-16), reads=[bC], writes=[bC])
        pool(_I("memset", ET[0:32, :], 1.0), writes=[bC])
        pool(_I("memset", ET[32:64, :], 0.0), writes=[bC])
        pool(_I("memset", ET[64:68, :], 0.0), writes=[bC])
        pool(_I("affine_select", ET[0:32, :], ET[0:32, :], [[1, S_TOK]], ALU.is_ge, 0.0, base=0, channel_multiplier=-64), reads=[bC], writes=[bC])
        pool(_I("affine_select", ET[0:32, :], ET[0:32, :], [[-1, S_TOK]], ALU.is_ge, 0.0, base=63, channel_multiplier=64), reads=[bC], writes=[bC])
        for g in range(2):
            pool(_I("memset", Vc[:, g, 64:97], 1.0), writes=[bC])
            pool(_I("affine_select", Vc[:, g, 65:97], Vc[:, g, 65:97], [[-64, 32]], ALU.is_ge, 0.0, base=31, channel_multiplier=16), reads=[bC], writes=[bC])
            pool(_I("affine_select", Vc[:, g, 65:97], Vc[:, g, 65:97], [[64, 32]], ALU.is_ge, 0.0, base=63, channel_multiplier=-16), reads=[bC], writes=[bC])
        Dt = U.take(NT * 32, F32).rearrange("p (t j) -> p t j", j=32)
        jt = U.take(NT * 32, F32).rearrange("p (t j) -> p t j", j=32)
        f0 = U.take(NT * 32, F32).rearrange("p (t j) -> p t j", j=32)
        for lo_, base in ((0, 0), (64, -1)):
            pool(_I("iota", Dt[lo_:lo_ + 64], [[-2, NT], [1, 32]], base=base, channel_multiplier=0, allow_small_or_imprecise_dtypes=True), writes=[bC])
        pool(_I("iota", jt[:], [[0, NT], [1, 32]], base=0, channel_multiplier=0, allow_small_or_imprecise_dtypes=True), writes=[bC])
        dve(_I("tensor_single_scalar", f0[:], jt[:], 0.0, ALU.is_equal), reads=[bC], writes=[bC])
        dve(_I("tensor_single_scalar", jt[:], Dt[:], 0.0, ALU.is_equal), reads=[bC], writes=[bC])
        dve(_I("tensor_max", f0[:], f0[:], jt[:]), reads=[bC], writes=[bC])
        dve(_I("tensor_single_scalar", jt[:], Dt[:], -1.0, ALU.is_equal), reads=[bC], writes=[bC])
        dve(_I("tensor_max", f0[:], f0[:], jt[:]), reads=[bC], writes=[bC])
        dve(_I("tensor_single_scalar", jt[:], Dt[:], 0.0, ALU.is_le), reads=[bC], writes=[bC])
        dve(_I("tensor_sub", selA[:], jt[:], f0[:]), reads=[bC], writes=[bC])
        dve(_I("tensor_add", selB[:], jt[:], f0[:]), reads=[bC], writes=[bC])
        dve(_I("tensor_scalar", selB[:], selB[:], -1.0, 1e9, ALU.add, ALU.mult), reads=[bC], writes=[bC])
        hi_t = sb("hi_t", [128, NT], F32)
        lo_t = sb("lo_t", [128, 1], F32)
        for lo_, base in ((0, 0), (64, 64)):
            pool(_I("iota", hi_t[lo_:lo_ + 64], [[128, NT]], base=base, channel_multiplier=0, allow_small_or_imprecise_dtypes=True), writes=[bC])
            pool(_I("iota", lo_t[lo_:lo_ + 64], [[0, 1]], base=0, channel_multiplier=1, allow_small_or_imprecise_dtypes=True), writes=[bC])
        for h in range(8):
            sl = 2.0 ** -(h + 1)
            dve(_I("memset", aqc[:, :, h, 0:2], sl), reads=[bC], writes=[bC])
            dve(_I("tensor_scalar", aqc[:, :, h, 2], hi_t[:], -sl, None, ALU.mult), reads=[bC], writes=[bC])
            dve(_I("tensor_scalar", aqc[:, :, h, 3], lo_t[:].to_broadcast([128, NT]), -sl, None, ALU.mult), reads=[bC], writes=[bC])
        dve(_I("memset", akc[:, :, 2:4], 1.0), reads=[bC], writes=[bC])
        dve(_I("tensor_copy", akc[:, :, 0], hi_t[:]), reads=[bC], writes=[bC])
        dve(_I("tensor_copy", akc[:, :, 1], lo_t[:].to_broadcast([128, NT])), reads=[bC], writes=[bC])
        pn = sb("pn", [128, 1], F32)
        pool(_I("iota", pn[:], [[0, 1]], base=0, channel_multiplier=16, allow_small_or_imprecise_dtypes=True), writes=[bC])
        for g in range(2):
            dve(_I("tensor_copy", kc_aug[:, g, 64:65], pn[:]), reads=[bC], writes=[bC])
            dve(_I("memset", kc_aug[:, g, 65:66], 31.0), reads=[bC], writes=[bC])
            dve(_I("memset", kc_aug[:, g, 66:68], 1.0), reads=[bC], writes=[bC])
        for j in range(NBIS + 1):
            dve(_I("memset", pow2[:, j:j + 1], 2.0 ** -j * (1.01 if j == NBIS else 1.0)), reads=[bC], writes=[bC])
        S.dma(gbc[:], gv_d.partition_broadcast(128), writes=[bC])
        S.dma(gnc[:], gn_d, writes=[bC])

        def gsl(i):
            return gbc[:, i * 64:(i + 1) * 64]
        for idx, (gk, gq) in enumerate(((2, 0), (3, 0), (1, 0), (5, 4))):
            dve(_I("scalar_tensor_tensor", gains[:, idx, :], gsl(gk), 0.125, gsl(gq), ALU.mult, ALU.mult), reads=[bC], writes=[bC])

        S.barrier()
        U.reset()
        scT = U.take(32, F32).rearrange("p (k b) -> p k b", b=4)
        wchunk = U.take(8 * 512, F32).rearrange("p (k n) -> p k n", k=8)
        bchunk = U.take(512, F32)
        mchunk = U.take(512, F32)
        bsc, bw, bbc, bm = Buf(), Buf(), Buf(), Buf()
        S.dma(scT, cT_d, writes=[bsc])
        act(_I("activation", scT, scT, AF.Silu), reads=[bsc], writes=[bsc])
        wada_v = wada_d.rearrange("(k p) n -> p k n", p=128)
        LNV = {0: 0, 1: 1, 3: 2, 4: 3}
        for c in range(12):
            S.dma(wchunk, wada_v[:, :, c * 512:(c + 1) * 512], writes=[bw])
            S.dma(bchunk[0:4, :], bada_d[:, c * 512:(c + 1) * 512].partition_broadcast(4), writes=[bbc])
            for k in range(8):
                pe(_I("matmul", PS[0][0:4, :], lhsT=scT[:, k, :], rhs=wchunk[:, k, :], start=(k == 0), stop=(k == 7)), reads=[bsc, bw], writes=[PB[0]])
            dve(_I("tensor_tensor", mchunk[0:4, :], PS[0][0:4, :], bchunk[0:4, :], ALU.add), reads=[PB[0], bbc], writes=[bm])
            S.dma(modrow_d[:, c * 512:(c + 1) * 512], mchunk[0:4, :], reads=[bm], writes=[bC])
            if dbg:
                S.dma(mod_dbg[:, c * 512:(c + 1) * 512], mchunk[0:4, :], reads=[bm], writes=[Buf()], is_output=True)
            vec, half = c // 2, c % 2
            if vec in LNV:
                for i in range(4):
                    col = (LNV[vec] * 8 + half * 4 + i) * 4
                    pe(_I("transpose", PS[1][:, col:col + 4], mchunk[0:4, i * 128:(i + 1) * 128], ident_f[0:4, 0:4]), reads=[bm, bC], writes=[PB[1]])
        dve(_I("tensor_copy", modT[:].rearrange("p a b -> p (a b)"), PS[1][:, 0:128]), reads=[PB[1]], writes=[bC])
        for which, gi in ((1, 0), (3, 1)):
            dve(_I("scalar_tensor_tensor",
                modT[:, which * 8:(which + 1) * 8, :], modT[:, which * 8:(which + 1) * 8, :], 1.0,
                gnc[:, gi * 8:(gi + 1) * 8].unsqueeze(2).to_broadcast([128, 8, 4]), ALU.add, ALU.mult), reads=[bC], writes=[bC])
        S.barrier()

        xt = [sb("xt%d" % i, [128, D], F32) for i in range(2)]
        xtb = [Buf() for _ in range(2)]
        xn = sb("xn", [128, D], BF16)
        xnb = Buf()
        sq = [sb("sq%d" % i, [128, 512], F32) for i in range(2)]
        sqb = [Buf(), Buf()]
        st16 = sb("st16", [128, 16], F32)
        stb = Buf()
        PT = [sb("PT%d" % i, [128, 512], BF16) for i in range(6)]
        PTb = [Buf() for _ in range(6)]
        sn = sb("sn", [128, 16], F32)
        snb = Buf()
        tmpo = sb("tmpo", [128, 4, 64], F32)
        tmpb = Buf()
        oaccs = [sb("oacc%d" % i, [128, 8, 64], F32) for i in range(2)]
        oabs = [Buf(), Buf()]
        mneg = sb("mneg", [128, 32], BF16)
        mnegb = Buf()
        obf = sb("obf", [128, 512], BF16)
        obfb = Buf()
        oTs = sb("oTs", [128, 4, 128], BF16)
        oTsb = Buf()
        sm = sb("sm", [128, 64], F32)
        smb = Buf()
        state = {"pt": 0, "sb": 0}
        vstate = {}

        def layernorm(src_tile, src_buf, tl, which, b, psbank):
            act(_I("activation", sq[0][:, :].bitcast(BF16), src_tile, AF.Square, accum_out=st16[:, 0:1]), reads=[src_buf], writes=[sqb[0], stb])
            act(_I("activation", st16[:, 1:2], st16[:, 0:1], AF.Sqrt, bias=EPS, scale=1.0 / D), reads=[stb], writes=[stb])
            dve(_I("reciprocal", st16[:, 2:3], st16[:, 1:2]), reads=[stb], writes=[stb])
            dve(_I("tensor_scalar", xn[:], src_tile, st16[:, 2:3], None, ALU.mult), reads=[src_buf, stb], writes=[xnb])
            for j in range(8):
                bk = psbank + j // 4
                o0 = ((j % 4) * 2 + tl) * 128
                pe(_I("transpose", psb(bk)[:, o0:o0 + 128], xn[:, j * 128:(j + 1) * 128], ident_b[:]), reads=[xnb, bC], writes=[PB[bk]])

        def ln_evac(c2, which, b, psbank, hbuf):
            for j in range(8):
                bk = psbank + j // 4
                src = psb(bk)[:, (j % 4) * 256:(j % 4) * 256 + 256]
                Gc = modT[:, (2 * which + 1) * 8 + j, b:b + 1]
                Sc = modT[:, (2 * which) * 8 + j, b:b + 1]
                dve(_I("tensor_scalar", hT[:, j, c2 * 256:(c2 + 1) * 256], src, Gc, Sc, ALU.mult, ALU.add),
                    reads=[PB[bk], bC], writes=[hbuf])

        def load_w(dst, src, buf, q="pool"):
            S.dma(dst, src, writes=[buf], q=q)

        win_v = win_d.rearrange("(k p) n -> p k n", p=128)

        def rms_heads(psbank, nh, dst_stats):
            i = state["sb"] = (state["sb"] + 1) % 2
            act(_I("activation", sq[i][:, 0:nh * 64], PS[psbank][:, 0:nh * 64], AF.Square), reads=[PB[psbank]], writes=[sqb[i]])
            dve(_I("tensor_reduce", dst_stats, sq[i][:, 0:nh * 64].rearrange("p (h d) -> p h d", d=64), AX.X, ALU.add), reads=[sqb[i]], writes=[stb])

        def rstd_from(stats):
            act(_I("activation", stats, stats, AF.Sqrt, bias=EPS, scale=1.0 / 64), reads=[stb], writes=[stb])
            dve(_I("reciprocal", stats, stats), reads=[stb], writes=[stb])

        def make_units(QTh, qb, KTk, kb, kind, t, tiles, maskmm, pvb, slot, full_mask=None, banks=(4, 5)):
            ntl = len(tiles)
            return [dict(QTh=QTh, qb=qb, KTk=KTk, kb=kb, kind=kind, t=t, grp=tiles[g0:g0 + 4], g0=g0, ntl=ntl, maskmm=maskmm,
                         pvb=pvb, slot=slot, full_mask=full_mask, banks=banks) for g0 in range(0, ntl, 4)]

        def emit_A(u):
            qs = slice(u["t"] * 128, (u["t"] + 1) * 128)
            banks = u["banks"]
            bk = banks[state["pt"] % len(banks)]
            pi = state["pt"] % (len(PT) - 1)
            state["pt"] += 1
            u["pi"] = pi
            grp = u["grp"]
            for i, (j, mt) in enumerate(grp):
                ks = slice(j * 128, (j + 1) * 128)
                pe(_I("matmul", PS[bk][:, i * 128:(i + 1) * 128], lhsT=u["KTk"][0:68, ks], rhs=u["QTh"][0:68, qs], start=True, stop=(u["maskmm"] is None)),
                   reads=[u["kb"], u["qb"]], writes=[PB[bk]])
                if u["maskmm"] is not None:
                    MTg, mb = u["maskmm"]
                    pe(_I("matmul", PS[bk][:, i * 128:(i + 1) * 128], lhsT=ET[0:68, ks], rhs=MTg[0:68, qs], start=False, stop=True),
                       reads=[mb, bC], writes=[PB[bk]])
            n = len(grp) * 128
            act(_I("activation", PT[pi][:, 0:n], PS[bk][:, 0:n], AF.Exp), reads=[PB[bk]], writes=[PTb[pi]])
            if u["full_mask"] is not None:
                mT, mTb = u["full_mask"]
                j0 = grp[0][0]
                pool(_I("tensor_tensor", PT[pi][:, 0:n], PT[pi][:, 0:n], mT[:, j0:j0 + len(grp), :].rearrange("p j c -> p (j c)"), ALU.mult),
                     reads=[mTb, PTb[pi]], writes=[PTb[pi]])
            for i, (j, mt) in enumerate(grp):
                if mt is not None:
                    mk = Cm if mt == "C" else Wm
                    pool(_I("tensor_tensor", PT[pi][:, i * 128:(i + 1) * 128], PT[pi][:, i * 128:(i + 1) * 128], mk[:], ALU.mult),
                         reads=[bC, PTb[pi]], writes=[PTb[pi]])

        def emit_B(u):
            pi = u["pi"]
            pvb = u["pvb"]
            po = PS[pvb][:, u["slot"] * 65:(u["slot"] + 1) * 65]
            for i, (j, mt) in enumerate(u["grp"]):
                gi = u["g0"] + i
                pe(_I("matmul", po, lhsT=PT[pi][:, i * 128:(i + 1) * 128], rhs=vstate["V"][:, j, u["kind"], :], start=(gi == 0), stop=(gi == u["ntl"] - 1)),
                   reads=[PTb[pi], vstate["bV"]], writes=[PB[pvb]])

        def run_units(units, L, between=None):
            n = len(units)
            for i in range(min(L, n)):
                emit_A(units[i])
            for i in range(n):
                if i + L < n:
                    emit_A(units[i + L])
                emit_B(units[i])
                if units[i].get("post") is not None:
                    units[i]["post"]()
                if between is not None:
                    between(i, n)

        def next_pv():
            state["pv"] = state.get("pv", 0) + 1
            return 6 if state["pv"] % 2 == 0 else 2

        def norm4(pvb, g, gate_view, gate_bufs, first, par):
            oacc, oab = oaccs[par], oabs[par]
            o4 = PS[pvb][:, 0:260].rearrange("p (h c) -> p h c", h=4)
            dve(_I("tensor_scalar", sn[:, 0:4], o4[:, :, 64], 1e-30, None, ALU.max), reads=[PB[pvb]], writes=[snb])
            dve(_I("reciprocal", sn[:, 4:8], sn[:, 0:4]), reads=[snb], writes=[snb])
            if gate_view is not None:
                dve(_I("tensor_tensor", sn[:, 4:8], sn[:, 4:8], gate_view, ALU.mult), reads=[snb] + gate_bufs, writes=[snb])
            wb = sn[:, 4:8].unsqueeze(2).to_broadcast([128, 4, 64])
            if first:
                dve(_I("tensor_tensor", oacc[:, 4 * g:4 * g + 4, :], o4[:, :, 0:64], wb, ALU.mult), reads=[PB[pvb], snb], writes=[oab])
            else:
                dve(_I("tensor_tensor", tmpo[:], o4[:, :, 0:64], wb, ALU.mult), reads=[PB[pvb], snb], writes=[tmpb])
                pool(_I("tensor_tensor", oacc[:, 4 * g:4 * g + 4, :], oacc[:, 4 * g:4 * g + 4, :], tmpo[:], ALU.add), reads=[tmpb, oab], writes=[oab])

        def flush_o(t, mix):
            oacc, oab = oaccs[t % 2], oabs[t % 2]
            dve(_I("tensor_copy", obf[:], oacc[:].rearrange("p h d -> p (h d)")), reads=[oab], writes=[obfb])
            for j in range(4):
                pe(_I("transpose", psb(3)[:, j * 128:(j + 1) * 128], obf[:, j * 128:(j + 1) * 128], ident_b[:]), reads=[obfb, bC], writes=[PB[3]])
            act(_I("activation", oTs[:].rearrange("p j c -> p (j c)"), psb(3)[:, 0:512], AF.Copy), reads=[PB[3]], writes=[oTsb])
            S.dma(oT_d[mix, :, :, t * 128:(t + 1) * 128].rearrange("j p c -> p j c"), oTs[:], reads=[oTsb], writes=[bOT[mix]])

        for s in range(nseq):
            b = s
            S.barrier()

            for c2 in range(8):
                for tl in range(2):
                    t = c2 * 2 + tl
                    i = t % 2
                    S.dma(xt[i][:], x_d[s, t * 128:(t + 1) * 128, :], writes=[xtb[i]])
                    layernorm(xt[i][:], xtb[i], tl, 0, b, 0)
                ln_evac(c2, 0, b, 0, hTb[c2 // 2])

            if dbg and s == 0:
                S.dma(hT_dbg, hT[:], reads=hTb, writes=[Buf()], is_output=True)
            U.reset()
            V_all = U.take(NT * 5 * 65).rearrange("p (t k c) -> p t k c", t=NT, k=5)
            bV = Buf()
            pool(_I("memset", V_all[:, :, :, 64:65], 1.0), writes=[bV])
            vstate["V"] = V_all
            vstate["bV"] = bV
            Wn = U.take(8 * 1304).rearrange("p (k n) -> p k n", k=8)
            QT = U.take(8 * S_TOK).rearrange("p (h n) -> p h n", h=8)
            KT = U.take(5 * S_TOK).rearrange("p (h n) -> p h n", h=5)
            q_aug = U.take(8 * 68).rearrange("p (h d) -> p h d", h=8)
            k_aug = U.take(4 * 68).rearrange("p (h d) -> p h d", h=4)
            kcT = U.take(S_TOK)
            vcT = U.take(S_TOK)
            MT = U.take(2 * S_TOK).rearrange("p (g n) -> p g n", g=2)
            sg = U.take(NT * 24, F32).rearrange("p (t c) -> p t c", c=24)
            bq_aug, bk_aug, bkc, bsg = Buf(), Buf(), Buf(), Buf()
            bWn = [Buf() for _ in range(8)]
            QTb = [Buf() for _ in range(8)]
            KTb = [Buf() for _ in range(5)]
            MTb = [Buf(), Buf()]
            pool(_I("memset", MT[32:64, :, :], 0.0), writes=MTb)
            pool(_I("memset", MT[64:68, :, :], 0.0), writes=MTb)
            for k in range(8):
                load_w(Wn[:, k, :], win_v[:, k, 0:1304], bWn[k])
            for c in range(4):
                for tl in range(4):
                    t = c * 4 + tl
                    ts = slice(t * 128, (t + 1) * 128)
                    pq, pk, pg = (0, 1, 2) if t % 2 == 0 else (4, 5, 6)
                    for k in range(8):
                        pe(_I("matmul", PS[pq][:, :], lhsT=hT[:, k, ts], rhs=Wn[:, k, 0:512], start=(k == 0), stop=(k == 7)), reads=[hTb[c], bWn[k]], writes=[PB[pq]])
                    for k in range(8):
                        pe(_I("matmul", PS[pk][:, :], lhsT=hT[:, k, ts], rhs=Wn[:, k, 768:1280], start=(k == 0), stop=(k == 7)), reads=[hTb[c], bWn[k]], writes=[PB[pk]])
                    for k in range(8):
                        pe(_I("matmul", PS[pg][:, 0:24], lhsT=hT[:, k, ts], rhs=Wn[:, k, 1280:1304], start=(k == 0), stop=(k == 7)), reads=[hTb[c], bWn[k]], writes=[PB[pg]])
                    rms_heads(pq, 8, st16[:, 0:8])
                    rms_heads(pk, 8, st16[:, 8:16])
                    rstd_from(st16[:, 0:16])
                    dve(_I("tensor_tensor", q_aug[:, :, 0:64], PS[pq][:, :].rearrange("p (h d) -> p h d", d=64), st16[:, 0:8].unsqueeze(2).to_broadcast([128, 8, 64]), ALU.mult),
                        reads=[PB[pq], stb], writes=[bq_aug])
                    dve(_I("tensor_copy", q_aug[:, :, 64:68], aqc[:, t, :, :]), reads=[bC], writes=[bq_aug])
                    for (c0, s0, kk, gi) in ((0, 8, 0, 0), (256, 12, 2, 1)):
                        dve(_I("tensor_tensor", sq[0][:, 0:128].rearrange("p (h d) -> p h d", d=64), PS[pk][:, c0:c0 + 128].rearrange("p (h d) -> p h d", d=64),
                                                                   st16[:, s0:s0 + 2].unsqueeze(2).to_broadcast([128, 2, 64]), ALU.mult), reads=[PB[pk], stb], writes=[sqb[0]])
                        dve(_I("tensor_tensor", k_aug[:, kk:kk + 2, 0:64], sq[0][:, 0:128].rearrange("p (h d) -> p h d", d=64),
                                                                   gains[:, gi, :].unsqueeze(1).to_broadcast([128, 2, 64]), ALU.mult), reads=[sqb[0], bC], writes=[bk_aug])
                    dve(_I("tensor_copy", k_aug[:, :, 64:68], akc[:, t, :].unsqueeze(1).to_broadcast([128, 4, 4])), reads=[bC], writes=[bk_aug])
                    act(_I("activation", V_all[:, t, 0:2, 0:64], PS[pk][:, 128:256].rearrange("p (h d) -> p h d", d=64), AF.Copy), reads=[PB[pk]], writes=[bV])
                    act(_I("activation", V_all[:, t, 2:4, 0:64], PS[pk][:, 384:512].rearrange("p (h d) -> p h d", d=64), AF.Copy), reads=[PB[pk]], writes=[bV])
                    act(_I("activation", sg[:, t, :], PS[pg][:, 0:24], AF.Sigmoid), reads=[PB[pg]], writes=[bsg])
                    for h in range(8):
                        pe(_I("transpose", psb(3)[0:68, h * 128:(h + 1) * 128], q_aug[:, h, :], ident_b[:]), reads=[bq_aug, bC], writes=[PB[3]])
                    for kk in range(4):
                        pe(_I("transpose", psb(7)[0:68, kk * 128:(kk + 1) * 128], k_aug[:, kk, :], ident_b[:]), reads=[bk_aug, bC], writes=[PB[7]])
                    act(_I("activation", QT[0:68, :, ts], psb(3)[0:68, :].rearrange("p (h c) -> p h c", h=8), AF.Copy), reads=[PB[3]], writes=QTb)
                    dve(_I("tensor_copy", KT[0:68, 0:4, ts], psb(7)[0:68, 0:512].rearrange("p (h c) -> p h c", h=4)), reads=[PB[7]], writes=KTb[0:4])
                cs = slice(c * 512, (c + 1) * 512)
                for (c0, dst) in ((512, kcT), (640, vcT)):
                    for k in range(8):
                        pe(_I("matmul", PS[0][:, :], lhsT=Wn[:, k, c0:c0 + 128], rhs=hT[:, k, cs], start=(k == 0), stop=(k == 7)), reads=[hTb[c], bWn[k]], writes=[PB[0]])
                    act(_I("activation", dst[:, cs], PS[0][:, :], AF.Copy), reads=[PB[0]], writes=[bkc])

            S.barrier()
            W1 = Wn.rearrange("p k n -> p (k n)")[:, 0:2 * 32 * 128].rearrange("p (a l n) -> p a l n", a=2, l=32)
            W2 = U.take(2 * 64).rearrange("p (a n) -> p a n", a=2)
            peT = U.take(64)
            HT = U.take(128)
            bW1, bH = Buf(), Buf()
            for a, (w1d, w2d) in enumerate(((wck1_d, wck2_d), (wcv1_d, wcv2_d))):
                for half in range(2):
                    load_w(W1[half * 64:half * 64 + 64, a, :, :], w1d.rearrange("(l d) n -> d l n", d=64), bW1)
                load_w(W2[:, a, :], w2d, bW1)
            load_w(peT[0:64, :], peT_d, bW1)
            if s == 0:
                for a in range(2):
                    for l in range(32):
                        pe(_I("matmul", PS[2][:, a:a + 1], lhsT=W1[0:64, a, l, :], rhs=peT[0:64, a * 32 + l:a * 32 + l + 1], start=(l == 0), stop=(l == 31)),
                           reads=[bW1], writes=[PB[2]])
                dve(_I("tensor_copy", cb2[:], PS[2][:, 0:2]), reads=[PB[2]], writes=[bC])
            for a, srcT in enumerate((kcT, vcT)):
                for g in range(2):
                    base = g * 64
                    v3 = srcT[base:base + 64, :].rearrange("p (n s) -> p n s", s=16)
                    for l in range(32):
                        rhs = v3[:, (l // 16):(l // 16) + 127, l % 16]
                        pe(_I("matmul", PS[0][:, 0:127], lhsT=W1[base:base + 64, a, l, :], rhs=rhs, start=(l == 0), stop=(l == 31)),
                           reads=[bW1, bkc], writes=[PB[0]])
                    act(_I("activation", HT[:, 0:127], PS[0][:, 0:127], AF.Silu, bias=cb2[:, a:a + 1]), reads=[PB[0], bC], writes=[bH])
                    pe(_I("matmul", PS[1][0:127, 0:64], lhsT=HT[:, 0:127], rhs=W2[:, a, :], start=True, stop=True), reads=[bH, bW1], writes=[PB[1]])
                    if a == 0:
                        act(_I("activation", sq[0][0:127, 0:64], PS[1][0:127, 0:64], AF.Square, accum_out=st16[0:127, 0:1]), reads=[PB[1]], writes=[sqb[0], stb])
                        rstd_from(st16[0:127, 0:1])
                        dve(_I("tensor_scalar", sq[0][0:127, 0:64], PS[1][0:127, 0:64], st16[0:127, 0:1], None, ALU.mult), reads=[PB[1], stb], writes=[sqb[0]])
                        dve(_I("tensor_tensor", kc_aug[0:127, g, 0:64], sq[0][0:127, 0:64], gains[0:127, 2, :], ALU.mult), reads=[sqb[0], bC], writes=[bC])
                        pe(_I("transpose", psb(3)[0:68, 0:127], kc_aug[0:127, g, :], ident_b[0:127, 0:127]), reads=[bC], writes=[PB[3]])
                        dve(_I("tensor_copy", KcT[0:68, g, 0:127], psb(3)[0:68, 0:127]), reads=[PB[3]], writes=[bC])
                    else:
                        act(_I("activation", Vc[0:127, g, 0:64], PS[1][0:127, 0:64], AF.Copy), reads=[PB[1]], writes=[bC])

            imp = sb("imp_%d" % s, [128, 4, 32], F32) if s == 0 else imp
            impb = Buf()
            MTt = [[Buf() for _ in range(NT)] for _ in range(2)]

            def cmp_items(t):
                qs = slice(t * 128, (t + 1) * 128)
                oacc, oab = oaccs[t % 2], oabs[t % 2]
                items = []
                for g in range(2):
                    o4 = PS[7][:, 0:388].rearrange("p (h c) -> p h c", h=4)

                    def s1(g=g):
                        for hh in range(4):
                            h = 4 * g + hh
                            pe(_I("matmul", PS[4][0:127, hh * 128:(hh + 1) * 128], lhsT=KcT[0:68, g, 0:127], rhs=QT[0:68, h, qs], start=True, stop=True),
                               reads=[bC, QTb[h]], writes=[PB[4]])
                        dve(_I("tensor_scalar", sq[1][0:127, :], PS[4][0:127, :], 60.0, None, ALU.min), reads=[PB[4]], writes=[sqb[1]])
                        act(_I("activation", PT[5][0:127, :], sq[1][0:127, :], AF.Exp), reads=[sqb[1]], writes=[PTb[5]])
                        dve(_I("tensor_tensor", PT[5][0:127, :].rearrange("p (h c) -> p h c", h=4), PT[5][0:127, :].rearrange("p (h c) -> p h c", h=4),
                               cmask[0:127, qs].unsqueeze(1).to_broadcast([127, 4, 128]), ALU.mult), reads=[bC, PTb[5]], writes=[PTb[5]])

                    def s2(g=g, o4=o4):
                        for hh in range(4):
                            pe(_I("matmul", PS[7][:, hh * 97:(hh + 1) * 97], lhsT=PT[5][0:127, hh * 128:(hh + 1) * 128], rhs=Vc[0:127, g, :], start=True, stop=True),
                               reads=[PTb[5], bC], writes=[PB[7]])
                        dve(_I("tensor_scalar", sm[:, 0:4], o4[:, :, 64], 1e-30, None, ALU.max), reads=[PB[7]], writes=[smb])
                        dve(_I("reciprocal", sm[:, 4:8], sm[:, 0:4]), reads=[smb], writes=[smb])
                        dve(_I("tensor_tensor", sm[:, 8:12], sm[:, 4:8], sg[:, t, :].rearrange("p (h r) -> p h r", r=3)[:, 4 * g:4 * g + 4, 0], ALU.mult), reads=[smb, bsg], writes=[smb])
                        dve(_I("tensor_tensor", oacc[:, 4 * g:4 * g + 4, :], o4[:, :, 0:64], sm[:, 8:12].unsqueeze(2).to_broadcast([128, 4, 64]), ALU.mult),
                            reads=[PB[7], smb], writes=[oab])
                        dve(_I("tensor_tensor", imp[:], o4[:, :, 65:97], sm[:, 4:8].unsqueeze(2).to_broadcast([128, 4, 32]), ALU.mult), reads=[PB[7], smb], writes=[impb])
                        dve(_I("tensor_reduce", sm[:, 16:48], imp[:].rearrange("p h j -> p j h"), AX.X, ALU.add), reads=[impb], writes=[smb])
                        dve(_I("tensor_tensor", sm[:, 16:48], sm[:, 16:48], selA[:, t, :], ALU.mult), reads=[smb, bC], writes=[smb])
                        dve(_I("tensor_tensor", sm[:, 16:48], sm[:, 16:48], selB[:, t, :], ALU.add), reads=[smb, bC], writes=[smb])
                        dve(_I("max", out=sm[:, 48:56], in_=sm[:, 16:48]), reads=[smb], writes=[smb])
                        dve(_I("match_replace", out=imp[:, 0, :], in_to_replace=sm[:, 48:56], in_values=sm[:, 16:48], imm_value=-3e38), reads=[smb], writes=[impb])
                        dve(_I("max", out=sm[:, 56:64], in_=imp[:, 0, :]), reads=[impb], writes=[smb])
                        dve(_I("tensor_scalar", sm[:, 16:48], sm[:, 16:48], sm[:, 63:64], None, ALU.is_ge), reads=[smb], writes=[smb])
                        dve(_I("tensor_scalar", mneg[:], sm[:, 16:48], -1.0, -NEG, ALU.add, ALU.mult), reads=[smb], writes=[mnegb])

                    def s3(g=g):
                        pe(_I("transpose", psb(3)[0:32, 0:128], mneg[:], ident_b[:]), reads=[mnegb, bC], writes=[PB[3]])
                        act(_I("activation", MT[0:32, g, qs], psb(3)[0:32, 0:128], AF.Copy), reads=[PB[3]], writes=[MTt[g][t]])
                    items += [s1, s2, s3]
                return items

            for item in cmp_items(0):
                item()
            for t in range(NT):
                sg3 = sg[:, t, :].rearrange("p (h r) -> p h r", r=3)
                par = t % 2
                units = []
                for g in range(2):
                    pvb = next_pv()
                    tiles = [(j, "C" if j == t else None) for j in range(t + 1)]
                    for hh in range(4):
                        h = 4 * g + hh
                        units += make_units(QT[:, h, :], QTb[h], KT[:, g, :], KTb[g], g, t, tiles, (MT[:, g, :], MTt[g][t]), pvb, hh, banks=(4, 5, 0, 1))
                    units[-1]["post"] = (lambda pvb=pvb, g=g, gv=sg3[:, 4 * g:4 * g + 4, 1], par=par: norm4(pvb, g, gv, [bsg], False, par))
                    pvb = next_pv()
                    tiles = [(j, "C" if j == t else ("W" if j == t - 4 else None)) for j in range(max(0, t - 4), t + 1)]
                    for hh in range(4):
                        h = 4 * g + hh
                        units += make_units(QT[:, h, :], QTb[h], KT[:, 2 + g, :], KTb[2 + g], 2 + g, t, tiles, None, pvb, hh, banks=(4, 5, 0, 1))
                    units[-1]["post"] = (lambda pvb=pvb, g=g, gv=sg3[:, 4 * g:4 * g + 4, 2], par=par: norm4(pvb, g, gv, [bsg], False, par))
                W = cmp_items(t + 1) if t + 1 < NT else []
                done = [0]

                def between(i, n, W=W, done=done):
                    target = min(len(W), ((i + 1) * (len(W) + 1)) // n)
                    while done[0] < target:
                        W[done[0]]()
                        done[0] += 1
                run_units(units, 3, between)
                while done[0] < len(W):
                    W[done[0]]()
                    done[0] += 1
                flush_o(t, 0)

            S.barrier()
            U.reset()
            V_all = U.take(NT * 5 * 65).rearrange("p (t k c) -> p t k c", t=NT, k=5)
            bV = Buf()
            pool(_I("memset", V_all[:, :, :, 64:65], 1.0), writes=[bV])
            vstate["V"] = V_all
            vstate["bV"] = bV
            Wd = U.take(8 * 1352).rearrange("p (k n) -> p k n", k=8)
            QT = U.take(8 * S_TOK).rearrange("p (h n) -> p h n", h=8)
            KT = U.take(5 * S_TOK).rearrange("p (h n) -> p h n", h=5)
            q_aug = U.take(8 * 68).rearrange("p (h d) -> p h d", h=8)
            k_aug = U.take(4 * 68).rearrange("p (h d) -> p h d", h=4)
            iqT = U.take(4 * S_TOK).rearrange("p (m n) -> p m n", m=4)
            ikT = U.take(S_TOK)
            iw = U.take(NT * 8, F32).rearrange("p (t c) -> p t c", c=8)
            sc = U.take(S_TOK, F32)
            rl = U.take(512, F32)
            maskq = U.take(S_TOK)
            maskT = U.take(S_TOK).rearrange("p (j c) -> p j c", c=128)
            junk = U.take(S_TOK)
            biq, bik, biw, bsc, brl, bmq, bmT, bjk = (Buf() for _ in range(8))
            bWd = [Buf() for _ in range(8)]
            QTb = [Buf() for _ in range(8)]
            KTb = [Buf() for _ in range(5)]
            for k in range(8):
                load_w(Wd[:, k, 0:1224], win_v[:, k, 1304:2528], bWd[k])
                load_w(Wd[:, k, 1224:1288], win_v[:, k, 2456:2520], bWd[k])
                load_w(Wd[:, k, 1288:1352], win_v[:, k, 2456:2520], bWd[k])
            for c in range(4):
                cs = slice(c * 512, (c + 1) * 512)
                for tl in range(4):
                    t = c * 4 + tl
                    ts = slice(t * 128, (t + 1) * 128)
                    pq, pk, pg = (0, 1, 2) if t % 2 == 0 else (4, 5, 6)
                    for k in range(8):
                        pe(_I("matmul", PS[pq][:, :], lhsT=hT[:, k, ts], rhs=Wd[:, k, 0:512], start=(k == 0), stop=(k == 7)), reads=[hTb[c], bWd[k]], writes=[PB[pq]])
                    for k in range(8):
                        pe(_I("matmul", PS[pk][:, 0:128], lhsT=hT[:, k, ts], rhs=Wd[:, k, 512:640], start=(k == 0), stop=(k == 7)), reads=[hTb[c], bWd[k]], writes=[PB[pk]])
                    for k in range(8):
                        pe(_I("matmul", PS[pg][:, 0:8], lhsT=hT[:, k, ts], rhs=Wd[:, k, 1216:1224], start=(k == 0), stop=(k == 7)), reads=[hTb[c], bWd[k]], writes=[PB[pg]])
                    rms_heads(pq, 8, st16[:, 0:8])
                    rms_heads(pk, 1, st16[:, 8:9])
                    rstd_from(st16[:, 0:9])
                    dve(_I("tensor_tensor", q_aug[:, :, 0:64], PS[pq][:, :].rearrange("p (h d) -> p h d", d=64), st16[:, 0:8].unsqueeze(2).to_broadcast([128, 8, 64]), ALU.mult),
                        reads=[PB[pq], stb], writes=[bq_aug])
                    dve(_I("tensor_copy", q_aug[:, :, 64:68], aqc[:, t, :, :]), reads=[bC], writes=[bq_aug])
                    dve(_I("tensor_scalar", sq[0][:, 0:64], PS[pk][:, 0:64], st16[:, 8:9], None, ALU.mult), reads=[PB[pk], stb], writes=[sqb[0]])
                    dve(_I("tensor_tensor", k_aug[:, 0, 0:64], sq[0][:, 0:64], gains[:, 3, :], ALU.mult), reads=[sqb[0], bC], writes=[bk_aug])
                    dve(_I("tensor_copy", k_aug[:, 0, 64:68], akc[:, t, :]), reads=[bC], writes=[bk_aug])
                    act(_I("activation", V_all[:, t, 4, 0:64], PS[pk][:, 64:128], AF.Copy), reads=[PB[pk]], writes=[bV])
                    act(_I("activation", iw[:, t, :], PS[pg][:, 0:8], AF.Copy, scale=8.0 ** -0.5), reads=[PB[pg]], writes=[biw])
                    for h in range(8):
                        pe(_I("transpose", psb(3)[0:68, h * 128:(h + 1) * 128], q_aug[:, h, :], ident_b[:]), reads=[bq_aug, bC], writes=[PB[3]])
                    pe(_I("transpose", psb(7)[0:68, 0:128], k_aug[:, 0, :], ident_b[:]), reads=[bk_aug, bC], writes=[PB[7]])
                    act(_I("activation", QT[0:68, :, ts], psb(3)[0:68, :].rearrange("p (h c) -> p h c", h=8), AF.Copy), reads=[PB[3]], writes=QTb)
                    dve(_I("tensor_copy", KT[0:68, 4, ts], psb(7)[0:68, 0:128]), reads=[PB[7]], writes=[KTb[4]])
                for m in range(4):
                    for k in range(8):
                        pe(_I("matmul", PS[0][:, :], lhsT=Wd[:, k, 640 + m * 128:640 + (m + 1) * 128], rhs=hT[:, k, cs], start=(k == 0), stop=(k == 7)), reads=[hTb[c], bWd[k]], writes=[PB[0]])
                    act(_I("activation", iqT[:, m, cs], PS[0][:, :], AF.Copy, scale=0.125), reads=[PB[0]], writes=[biq])
                for k in range(8):
                    pe(_I("matmul", PS[1][:, :], lhsT=Wd[:, k, 1224:1352], rhs=hT[:, k, cs], start=(k == 0), stop=(k == 7)), reads=[hTb[c], bWd[k]], writes=[PB[1]])
                act(_I("activation", ikT[:, cs], PS[1][:, :], AF.Copy), reads=[PB[1]], writes=[bik])

            S.barrier()
            Wd_flat = Wd.rearrange("p k n -> p (k n)")
            scs = [sc, Wd_flat[:, 0:4096].bitcast(F32)]
            maskqs = [maskq, Wd_flat[:, 4096:6144]]
            maskTs = [maskT, Wd_flat[:, 6144:8192].rearrange("p (j c) -> p j c", c=128)]
            bscs, bmqs, bmTs = [Buf(), Buf()], [Buf(), Buf()], [Buf(), Buf()]
            smx = [sb("smx%d_%d" % (s, i), [128, 40], F32) for i in range(2)] if s == 0 else smx
            smxb = [Buf(), Buf()]

            rls = [rl, Wd_flat[:, 8192:9216].bitcast(F32)]
            brls = [brl, Buf()]

            def indexer_work(t):
                p = t % 2
                sc_, bsc_, sm_, smb_ = scs[p], bscs[p], smx[p], smxb[p]
                qs = slice(t * 128, (t + 1) * 128)
                nk = (t + 1) * 128
                nch = (nk + 511) // 512
                items = []
                idx = 0
                for h in range(8):
                    base = (h % 2) * 64
                    for cc in range(nch):
                        w = min(512, nk - cc * 512)
                        cs = slice(cc * 512, cc * 512 + w)

                        def piece(h=h, base=base, w=w, cs=cs, k=idx):
                            bk = k % 2
                            rl_, brl_ = rls[k % 2], brls[k % 2]
                            pe(_I("matmul", PS[bk][:, 0:w], lhsT=iqT[base:base + 64, h // 2, qs], rhs=ikT[base:base + 64, cs], start=True, stop=True),
                               reads=[biq, bik], writes=[PB[bk]])
                            act(_I("activation", rl_[:, 0:w], PS[bk][:, 0:w], AF.Relu), reads=[PB[bk]], writes=[brl_])
                            if h == 0:
                                dve(_I("tensor_scalar", sc_[:, cs], rl_[:, 0:w], iw[:, t, 0:1], None, ALU.mult), reads=[brl_, biw], writes=[bsc_])
                            else:
                                dve(_I("scalar_tensor_tensor", sc_[:, cs], rl_[:, 0:w], iw[:, t, h:h + 1], sc_[:, cs], ALU.mult, ALU.add), reads=[brl_, biw, bsc_], writes=[bsc_])
                        items.append(piece)
                        idx += 1

                def post():
                    dve(_I("tensor_reduce", sm_[:, 0:1], sc_[:, 0:nk], AX.X, ALU.max, apply_absolute_value=True), reads=[bsc_], writes=[smb_])
                    pool(_I("affine_select", sc_[:, t * 128:(t + 1) * 128], sc_[:, t * 128:(t + 1) * 128], [[-1, 128]], ALU.is_ge, -3e38, base=0, channel_multiplier=1), reads=[bsc_, smb_], writes=[bsc_])
                    dve(_I("tensor_scalar", sm_[:, 8:8 + NBIS + 1], pow2[:], sm_[:, 0:1], None, ALU.mult), reads=[smb_, bC], writes=[smb_])
                    dve(_I("memset", sm_[:, 1:2], 0.0), reads=[smb_], writes=[smb_])
                items.append(post)
                return items

            def tile_work(t):
                return indexer_work(t) + [(lambda j=j: bisect_iter(t, j)) for j in range(NBIS)] + [lambda: finish_mask(t)]

            def bisect_iter(t, j):
                p = t % 2
                sc_, bsc_, sm_, smb_ = scs[p], bscs[p], smx[p], smxb[p]
                nk = (t + 1) * 128
                dve(_I("tensor_scalar", maskqs[p][:, 0:nk], sc_[:, 0:nk], sm_[:, 1:2], None, ALU.is_ge, ALU.add, accum_out=sm_[:, 2:3]), reads=[bsc_, smb_], writes=[bmqs[p], smb_])
                dve(_I("tensor_scalar", sm_[:, 3:4], sm_[:, 2:3], 255.5, -0.5, ALU.is_ge, ALU.add), reads=[smb_], writes=[smb_])
                dve(_I("scalar_tensor_tensor", sm_[:, 1:2], sm_[:, 3:4], sm_[:, 8 + j:9 + j], sm_[:, 1:2], ALU.mult, ALU.add), reads=[smb_], writes=[smb_])

            def finish_mask(t):
                p = t % 2
                sc_, bsc_, sm_, smb_ = scs[p], bscs[p], smx[p], smxb[p]
                nk = (t + 1) * 128
                dve(_I("tensor_tensor", sm_[:, 1:2], sm_[:, 1:2], sm_[:, 8 + NBIS:9 + NBIS], ALU.subtract), reads=[smb_], writes=[smb_])
                dve(_I("tensor_scalar", maskqs[p][:, 0:nk], sc_[:, 0:nk], sm_[:, 1:2], None, ALU.is_ge), reads=[bsc_, smb_], writes=[bmqs[p]])
                for j in range(t + 1):
                    bk = 3 if (j // 8) % 2 == 0 else 7
                    pe(_I("transpose", psb(bk)[:, (j % 8) * 128:(j % 8 + 1) * 128], maskqs[p][:, j * 128:(j + 1) * 128], ident_b[:]), reads=[bmqs[p], bC], writes=[PB[bk]])
                    if j % 8 == 7 or j == t:
                        j0 = (j // 8) * 8
                        n = j - j0 + 1
                        act(_I("activation", maskTs[p][:, j0:j0 + n, :].rearrange("p j c -> p (j c)"), psb(bk)[:, 0:n * 128], AF.Copy), reads=[PB[bk]], writes=[bmTs[p]])

            for item in tile_work(0):
                item()
            for t in range(NT):
                tiles = [(j, None) for j in range(t + 1)]
                units = []
                for g2 in range(2):
                    pvb = next_pv()
                    for hh in range(4):
                        h = 4 * g2 + hh
                        units += make_units(QT[:, h, :], QTb[h], KT[:, 4, :], KTb[4], 4, t, tiles, None, pvb, hh, full_mask=(maskTs[t % 2], bmTs[t % 2]))
                    units[-1]["post"] = (lambda pvb=pvb, g2=g2, par=t % 2: norm4(pvb, g2, None, [], True, par))
                W = tile_work(t + 1) if t + 1 < NT else []
                done = [0]

                def between(i, n, W=W, done=done):
                    target = min(len(W), ((i + 1) * len(W) * 10) // (n * 9) + 1)
                    while done[0] < target:
                        W[done[0]]()
                        done[0] += 1
                run_units(units, 1, between)
                while done[0] < len(W):
                    W[done[0]]()
                    done[0] += 1
                flush_o(t, 1)

            for hf in range(2):
                S.barrier()
                U.reset()
                Wg = U.take(8 * 2048).rearrange("p (k n) -> p k n", k=8)
                Woa = U.take(4 * D).rearrange("p (k n) -> p k n", k=4)
                Wob = U.take(4 * D).rearrange("p (k n) -> p k n", k=4)
                Wout = U.take(8 * D).rearrange("p (k n) -> p k n", k=8)
                Wff_region = (Wg, Woa, Wob, Wout)
                xacc = U.take(8 * D, F32).rearrange("p (t n) -> p t n", t=8)
                yT = U.take(8 * 512).rearrange("p (k n) -> p k n", k=8)
                oaT = U.take(4 * 512).rearrange("p (k n) -> p k n", k=4)
                obT = U.take(4 * 512).rearrange("p (k n) -> p k n", k=4)
                sga = U.take(512)
                sgb = U.take(512)
                t1 = U.take(512, F32)
                t2 = U.take(512, F32)
                aT = yT
                g1bc = U.take(D, F32)
                g2bc = U.take(D, F32)
                bG = Buf()
                S.dma(g1bc, modrow_d[b:b + 1, 2 * D:3 * D].partition_broadcast(128), writes=[bG])
                S.dma(g2bc, modrow_d[b:b + 1, 5 * D:6 * D].partition_broadcast(128), writes=[bG])
                bya, byT, boa, bob, bsga, bsgb, bt1, bt2, baT = (Buf() for _ in range(9))
                bWg = [Buf() for _ in range(8)]
                bWo = [Buf() for _ in range(8)]
                bWa = [Buf() for _ in range(4)]
                bWb = [Buf() for _ in range(4)]
                xab = [Buf() for _ in range(8)]
                for k in range(8):
                    load_w(Wg[:, k, :], win_v[:, k, 2528:4576], bWg[k])
                    load_w(Wout[:, k, :], wout_d.rearrange("(k p) n -> p k n", p=128)[:, k, :], bWo[k])
                for k in range(4):
                    load_w(Woa[:, k, :], woa_d.rearrange("(k p) n -> p k n", p=128)[:, k, :], bWa[k])
                    load_w(Wob[:, k, :], wob_d.rearrange("(k p) n -> p k n", p=128)[:, k, :], bWb[k])
                for cl in range(2):
                    c = hf * 2 + cl
                    cs = slice(c * 512, (c + 1) * 512)
                    S.dma(oaT, oT_d[0, :, :, cs].rearrange("j p c -> p j c"), reads=[bOT[0]], writes=[boa])
                    S.dma(obT, oT_d[1, :, :, cs].rearrange("j p c -> p j c"), reads=[bOT[1]], writes=[bob])
                    for f in range(8):
                        fs = slice(f * 128, (f + 1) * 128)
                        for k in range(8):
                            pe(_I("matmul", PS[0][:, :], lhsT=Wg[:, k, fs], rhs=hT[:, k, cs], start=(k == 0), stop=(k == 7)), reads=[bWg[k], hTb[c]], writes=[PB[0]])
                        act(_I("activation", sga, PS[0][:, :], AF.Sigmoid), reads=[PB[0]], writes=[bsga])
                        for k in range(8):
                            pe(_I("matmul", PS[1][:, :], lhsT=Wg[:, k, 1024 + f * 128:1024 + (f + 1) * 128], rhs=hT[:, k, cs], start=(k == 0), stop=(k == 7)), reads=[bWg[k], hTb[c]], writes=[PB[1]])
                        act(_I("activation", sgb, PS[1][:, :], AF.Sigmoid), reads=[PB[1]], writes=[bsgb])
                        for k in range(4):
                            pe(_I("matmul", PS[2][:, :], lhsT=Woa[:, k, fs], rhs=oaT[:, k, :], start=(k == 0), stop=(k == 3)), reads=[bWa[k], boa], writes=[PB[2]])
                        for k in range(4):
                            pe(_I("matmul", PS[4][:, :], lhsT=Wob[:, k, fs], rhs=obT[:, k, :], start=(k == 0), stop=(k == 3)), reads=[bWb[k], bob], writes=[PB[4]])
                        dve(_I("tensor_tensor", t1, PS[2][:, :], sga, ALU.mult), reads=[PB[2], bsga], writes=[bt1])
                        dve(_I("tensor_tensor", t2, PS[4][:, :], sgb, ALU.mult), reads=[PB[4], bsgb], writes=[bt2])
                        dve(_I("tensor_tensor", yT[:, f, :], t1, t2, ALU.add), reads=[bt1, bt2], writes=[byT])
                    for tl in range(4):
                        t = c * 4 + tl
                        tt = t - hf * 8
                        i = t % 2
                        S.dma(xt[i][:], x_d[s, t * 128:(t + 1) * 128, :], writes=[xtb[i]])
                        for h2 in range(2):
                            ns = slice(h2 * 512, (h2 + 1) * 512)
                            bk = 5 + h2
                            for k in range(8):
                                pe(_I("matmul", PS[bk][:, :], lhsT=yT[:, k, tl * 128:(tl + 1) * 128], rhs=Wout[:, k, ns], start=(k == 0), stop=(k == 7)), reads=[byT, bWo[k]], writes=[PB[bk]])
                            dve(_I("tensor_tensor", t1, PS[bk][:, :], g1bc[:, ns], ALU.mult), reads=[PB[bk], bG], writes=[bt1])
                            dve(_I("tensor_tensor", xacc[:, tt, ns], t1, xt[i][:, ns], ALU.add), reads=[bt1, xtb[i]], writes=[xab[tt]])
                        if dbg and s == 0:
                            S.dma(x1_dbg[t * 128:(t + 1) * 128, :], xacc[:, tt, :], reads=[xab[tt]], writes=[Buf()], is_output=True)
                        layernorm(xacc[:, tt, :], xab[tt], tl % 2, 1, b, 0)
                        if tl % 2 == 1:
                            ln_evac(t // 2, 1, b, 0, hTb[c])
                S.barrier()
                for (f0_, nf) in ((0, 8), (8, 8), (16, 6)):
                    Wfg = Wg.rearrange("p k n -> p (k n)")[:, 0:8 * 1024].rearrange("p (k n) -> p k n", k=8)
                    Wfu = Wg.rearrange("p k n -> p (k n)")[:, 8 * 1024:16 * 1024].rearrange("p (k n) -> p k n", k=8)
                    Wfd = Wout.rearrange("p k n -> p (k n)")[:, 0:8 * D].rearrange("p (k n) -> p k n", k=8)
                    nfc = nf * 128
                    if f0_ == 0:
                        bFg = [Buf() for _ in range(8)]
                        bFu = [Buf() for _ in range(8)]
                        bFd = [Buf() for _ in range(8)]
                    for k in range(8):
                        load_w(Wfg[:, k, 0:nfc], wfg_d.rearrange("(k p) n -> p k n", p=128)[:, k, f0_ * 128:f0_ * 128 + nfc], bFg[k])
                        load_w(Wfu[:, k, 0:nfc], wfu_d.rearrange("(k p) n -> p k n", p=128)[:, k, f0_ * 128:f0_ * 128 + nfc], bFu[k])
                    for k in range(nf):
                        load_w(Wfd[:, k, :], wfd_d[(f0_ + k) * 128:(f0_ + k + 1) * 128, :], bFd[k])
                    for cl in range(2):
                        c = hf * 2 + cl
                        cs = slice(c * 512, (c + 1) * 512)
                        for f in range(nf):
                            fs = slice(f * 128, (f + 1) * 128)
                            for k in range(8):
                                pe(_I("matmul", PS[0][:, :], lhsT=Wfg[:, k, fs], rhs=hT[:, k, cs], start=(k == 0), stop=(k == 7)), reads=[bFg[k], hTb[c]], writes=[PB[0]])
                            for k in range(8):
                                pe(_I("matmul", PS[1][:, :], lhsT=Wfu[:, k, fs], rhs=hT[:, k, cs], start=(k == 0), stop=(k == 7)), reads=[bFu[k], hTb[c]], writes=[PB[1]])
                            act(_I("activation", t1, PS[0][:, :], AF.Silu), reads=[PB[0]], writes=[bt1])
                            dve(_I("tensor_tensor", aT[:, f, :], t1, PS[1][:, :], ALU.mult), reads=[bt1, PB[1]], writes=[baT])
                        for tl in range(4):
                            tt = cl * 4 + tl
                            for h2 in range(2):
                                ns = slice(h2 * 512, (h2 + 1) * 512)
                                bk = 5 + h2
                                for f in range(nf):
                                    pe(_I("matmul", PS[bk][:, :], lhsT=aT[:, f, tl * 128:(tl + 1) * 128], rhs=Wfd[:, f, ns], start=(f == 0), stop=(f == nf - 1)), reads=[baT, bFd[f]], writes=[PB[bk]])
                                dve(_I("tensor_tensor", t2, PS[bk][:, :], g2bc[:, ns], ALU.mult), reads=[PB[bk], bG], writes=[bt2])
                                dve(_I("tensor_tensor", xacc[:, tt, ns], xacc[:, tt, ns], t2, ALU.add), reads=[bt2, xab[tt]], writes=[xab[tt]])
                for tt in range(8):
                    t = hf * 8 + tt
                    S.dma(out_d[s, t * 128:(t + 1) * 128, :], xacc[:, tt, :], reads=[xab[tt]], writes=[Buf()], is_output=True)
        S.emit()
    return nc


def _prep_common(inp):
    f = lambda a: np.ascontiguousarray(np.asarray(a, dtype=np.float32))
    gn = np.concatenate([inp["g_norm1"][0].reshape(8, 128).T, inp["g_norm2"][0].reshape(8, 128).T], axis=1)
    gvec = np.concatenate([inp[k][0] for k in ("g_q_a", "g_kc_a", "g_ks_a", "g_kw_a", "g_q_b", "g_k_b")])[None, :]
    peT = np.concatenate([inp["pe_ck"][0].T, inp["pe_cv"][0].T], axis=1)
    return {
        "w_ada": f(inp["w_ada"][0]), "b_ada": f(inp["b_ada"]), "gn": f(gn), "w_in": f(inp["w_in"][0]),
        "gvec": f(gvec), "peT": f(peT), "w_ck1": f(inp["w_ck1"][0]), "w_ck2": f(inp["w_ck2"][0]),
        "w_cv1": f(inp["w_cv1"][0]), "w_cv2": f(inp["w_cv2"][0]), "w_o_a": f(inp["w_o_a"][0]),
        "w_o_b": f(inp["w_o_b"][0]), "w_out": f(inp["w_out"][0]), "w_ff_gate": f(inp["w_ff_gate"][0]),
        "w_ff_up": f(inp["w_ff_up"][0]), "w_ff_down": f(inp["w_ff_down"][0]),
    }


def _core_map(common, x, c, i, nseq):
    m = dict(common)
    m["x"] = np.ascontiguousarray(x[i * nseq:(i + 1) * nseq])
    cc = np.zeros((4, D), np.float32)
    cc[:nseq] = c[i * nseq:(i + 1) * nseq]
    m["cT"] = np.ascontiguousarray(cc.T.reshape(8, 128, 4).transpose(1, 0, 2))
    return m


def kernel(**inputs):
    x = np.asarray(inputs["x"], dtype=np.float32)
    c = np.asarray(inputs["c"], dtype=np.float32)
    n = 8
    nseq = x.shape[0] // n
    nc = build_nc(nseq)
    common = _prep_common(inputs)
    in_maps = [_core_map(common, x, c, i, nseq) for i in range(n)]
    res = run_bass_kernel_spmd(nc, in_maps, core_ids=list(range(n)))
    return np.concatenate([r["out"] for r in res.results], axis=0).astype(np.float32)
```
